# Optimizing a Trainium2 kernel written in Bass

```python
import math
import jax, jax.numpy as jnp
from jax import lax
import numpy as np

D_MODEL = 1024
BATCH = 4
SEQ = 4096
DEPTH = 1

MIX_WIDTH = D_MODEL
DIFF_WIDTH = MIX_WIDTH // 2
MLA_WIDTH = MIX_WIDTH - DIFF_WIDTH
DIFF_HEADS = 4
DIFF_HEAD_DIM = DIFF_WIDTH // DIFF_HEADS // 2
DIFF_V_DIM = 2 * DIFF_HEAD_DIM
MLA_HEADS = 4
MLA_V_DIM = MLA_WIDTH // MLA_HEADS
MLA_NOPE_DIM = 128
MLA_ROPE_DIM = 64
MLA_QK_DIM = MLA_NOPE_DIM + MLA_ROPE_DIM
MLA_Q_RANK = 256
MLA_KV_RANK = 128
ROPE_THETA = 10000.0

DIFF_Q_COLS = DIFF_HEADS * 2 * DIFF_HEAD_DIM
DIFF_K_COLS = DIFF_HEADS * 2 * DIFF_HEAD_DIM
DIFF_V_COLS = DIFF_HEADS * DIFF_V_DIM
IN_SPLITS = (DIFF_Q_COLS, DIFF_K_COLS, DIFF_V_COLS, MLA_Q_RANK, MLA_KV_RANK, MLA_ROPE_DIM)
IN_WIDTH = sum(IN_SPLITS)
IN_OFFSETS = tuple(int(o) for o in np.cumsum(IN_SPLITS)[:-1])

N_EXPERTS = 32
TOP_K = 4
D_FF_EXPERT = D_MODEL
SWIGLU_LIMIT = 7.0
SWIGLU_ALPHA = 1.702
MOE_BLOCK = 128

Q_BLOCK = 128
LN_EPS = 1e-5
SUBLN_EPS = 1e-5
MLA_RMS_EPS = 1e-6
DN_ALPHA = (2.0 * DEPTH) ** 0.25
DN_BETA = (8.0 * DEPTH) ** -0.25

kernel_name = "hybrid_diffattn_mla_moe_deepnorm"


def lambda_init_fn(layer_idx):
    return 0.8 - 0.6 * math.exp(-0.3 * layer_idx)


def layer_norm(x, g, b):
    xf = x.astype(jnp.float32)
    mu = jnp.mean(xf, axis=-1, keepdims=True)
    var = jnp.mean(jnp.square(xf - mu), axis=-1, keepdims=True)
    y = (xf - mu) * lax.rsqrt(var + LN_EPS) * g.astype(jnp.float32) + b.astype(jnp.float32)
    return y.astype(x.dtype)


def rms_norm(x, g, eps):
    xf = x.astype(jnp.float32)
    y = xf * lax.rsqrt(jnp.mean(jnp.square(xf), axis=-1, keepdims=True) + eps) * g.astype(jnp.float32)
    return y.astype(x.dtype)


def rope(x, positions):
    d = x.shape[-1]
    inv_freq = 1.0 / (ROPE_THETA ** (jnp.arange(0, d, 2, dtype=jnp.float32) / d))
    ang = positions.astype(jnp.float32)[..., None] * inv_freq
    cos = jnp.cos(ang)[:, :, None, :]
    sin = jnp.sin(ang)[:, :, None, :]
    xf = x.astype(jnp.float32)
    x1, x2 = xf[..., : d // 2], xf[..., d // 2:]
    return jnp.concatenate([x1 * cos - x2 * sin, x2 * cos + x1 * sin], axis=-1).astype(x.dtype)


def causal_mask(block_idx, seq):
    qpos = block_idx * Q_BLOCK + jnp.arange(Q_BLOCK)
    return jnp.arange(seq)[None, :] <= qpos[:, None]


def diff_attention(q, k, v, lam):
    B, S, H, _, dh = q.shape
    nb = S // Q_BLOCK
    scale = dh ** -0.5
    qb = q.reshape(B, nb, Q_BLOCK, H, 2, dh).transpose(1, 0, 2, 3, 4, 5)

    def block(args):
        qi, i = args
        s = jnp.einsum('bqhcd,bkhcd->bhcqk', qi, k, preferred_element_type=jnp.float32) * scale
        s = jnp.where(causal_mask(i, S), s, -jnp.inf)
        p = jax.nn.softmax(s, axis=-1)
        w = p[:, :, 0] - lam * p[:, :, 1]
        return jnp.einsum('bhqk,bkhd->bqhd', w.astype(v.dtype), v)

    out = lax.map(block, (qb, jnp.arange(nb)))
    return out.transpose(1, 0, 2, 3, 4).reshape(B, S, H, v.shape[-1])


def causal_attention(q, k, v, scale):
    B, S, H, dk = q.shape
    nb = S // Q_BLOCK
    qb = q.reshape(B, nb, Q_BLOCK, H, dk).transpose(1, 0, 2, 3, 4)

    def block(args):
        qi, i = args
        s = jnp.einsum('bqhd,bkhd->bhqk', qi, k, preferred_element_type=jnp.float32) * scale
        p = jax.nn.softmax(jnp.where(causal_mask(i, S), s, -jnp.inf), axis=-1)
        return jnp.einsum('bhqk,bkhd->bqhd', p.astype(v.dtype), v)

    out = lax.map(block, (qb, jnp.arange(nb)))
    return out.transpose(1, 0, 2, 3, 4).reshape(B, S, H, v.shape[-1])


def moe_ffn(x, w_router, b_router, w_gate_up, b_gate_up, w_down, b_down):
    B, S, D = x.shape
    T = B * S
    xt = x.reshape(T, D)
    logits = (xt @ w_router + b_router).astype(jnp.float32)
    top_logit, top_idx = lax.top_k(logits, TOP_K)
    gates = jax.nn.softmax(top_logit, axis=-1)

    A = T * TOP_K
    flat_e = top_idx.reshape(A)
    flat_tok = jnp.repeat(jnp.arange(T, dtype=jnp.int32), TOP_K)
    flat_g = gates.reshape(A)
    order = jnp.argsort(flat_e, stable=True)
    sorted_e = flat_e[order]
    counts = jnp.bincount(flat_e, length=N_EXPERTS)
    padded = (counts + MOE_BLOCK - 1) // MOE_BLOCK * MOE_BLOCK
    start = jnp.cumsum(counts) - counts
    pad_end = jnp.cumsum(padded)
    pad_start = pad_end - padded
    dest = pad_start[sorted_e] + jnp.arange(A) - start[sorted_e]
    n_blocks = -(-A // MOE_BLOCK) + N_EXPERTS
    cap = n_blocks * MOE_BLOCK
    slot_tok = jnp.zeros((cap,), jnp.int32).at[dest].set(flat_tok[order])
    slot_g = jnp.zeros((cap,), jnp.float32).at[dest].set(flat_g[order])
    block_e = jnp.minimum(
        jnp.searchsorted(pad_end, jnp.arange(n_blocks) * MOE_BLOCK, side='right'), N_EXPERTS - 1)

    def expert_block(args):
        tok, g, e = args
        xb = xt[tok]
        h = xb @ w_gate_up[e] + b_gate_up[e]
        gate = jnp.minimum(h[:, :D_FF_EXPERT], SWIGLU_LIMIT)
        up = jnp.clip(h[:, D_FF_EXPERT:], -SWIGLU_LIMIT, SWIGLU_LIMIT)
        act = (up + 1.0) * (gate * jax.nn.sigmoid(gate * SWIGLU_ALPHA))
        y = act @ w_down[e] + b_down[e]
        return y * g[:, None].astype(y.dtype)

    ys = lax.map(expert_block, (slot_tok.reshape(n_blocks, MOE_BLOCK),
                                slot_g.reshape(n_blocks, MOE_BLOCK), block_e))
    out = jnp.zeros((T, D), x.dtype).at[slot_tok].add(ys.reshape(cap, D).astype(x.dtype))
    return out.reshape(B, S, D)


def setup_inputs(seed: int = 0) -> dict:
    key = jax.random.key(seed)
    ks = jax.random.split(key, 24)
    f32 = jnp.float32
    L, D, E, F = DEPTH, D_MODEL, N_EXPERTS, D_FF_EXPERT

    def nrm(k, shape, scale):
        return jax.random.normal(k, shape, f32) * scale

    return {
        "x": nrm(ks[0], (BATCH, SEQ, D), 1.0),
        "positions": jnp.broadcast_to(jnp.arange(SEQ, dtype=jnp.int32), (BATCH, SEQ)),
        "w_in": nrm(ks[1], (L, D, IN_WIDTH), D ** -0.5),
        "lambda_q1": nrm(ks[2], (L, DIFF_HEAD_DIM), 0.1),
        "lambda_k1": nrm(ks[3], (L, DIFF_HEAD_DIM), 0.1),
        "lambda_q2": nrm(ks[4], (L, DIFF_HEAD_DIM), 0.1),
        "lambda_k2": nrm(ks[5], (L, DIFF_HEAD_DIM), 0.1),
        "subln_g": 1.0 + nrm(ks[6], (L, DIFF_V_DIM), 0.02),
        "mla_q_norm_g": 1.0 + nrm(ks[7], (L, MLA_Q_RANK), 0.02),
        "w_uq": nrm(ks[8], (L, MLA_Q_RANK, MLA_HEADS * MLA_QK_DIM), MLA_Q_RANK ** -0.5),
        "mla_kv_norm_g": 1.0 + nrm(ks[9], (L, MLA_KV_RANK), 0.02),
        "w_ukv": nrm(ks[10], (L, MLA_KV_RANK, MLA_HEADS * (MLA_NOPE_DIM + MLA_V_DIM)), MLA_KV_RANK ** -0.5),
        "w_o": nrm(ks[11], (L, MIX_WIDTH, D), MIX_WIDTH ** -0.5 * DN_BETA),
        "ln1_g": 1.0 + nrm(ks[12], (L, D), 0.02),
        "ln1_b": nrm(ks[13], (L, D), 0.02),
        "w_router": nrm(ks[14], (L, D, E), D ** -0.5),
        "b_router": nrm(ks[15], (L, E), 0.01),
        "w_gate_up": nrm(ks[16], (L, E, D, 2 * F), D ** -0.5),
        "b_gate_up": nrm(ks[17], (L, E, 2 * F), 0.01),
        "w_down": nrm(ks[18], (L, E, F, D), F ** -0.5 * DN_BETA),
        "b_down": nrm(ks[19], (L, E, D), 0.01),
        "ln2_g": 1.0 + nrm(ks[20], (L, D), 0.02),
        "ln2_b": nrm(ks[21], (L, D), 0.02),
    }


def reference(x, positions, w_in, lambda_q1, lambda_k1, lambda_q2, lambda_k2, subln_g,
              mla_q_norm_g, w_uq, mla_kv_norm_g, w_ukv, w_o, ln1_g, ln1_b,
              w_router, b_router, w_gate_up, b_gate_up, w_down, b_down, ln2_g, ln2_b):
    B, S, D = x.shape
    for l in range(DEPTH):
        lam_init = lambda_init_fn(l)
        h = x @ w_in[l]
        dq, dk, dv, c_q, c_kv, k_r = jnp.split(h, IN_OFFSETS, axis=-1)

        dq = rope(dq.reshape(B, S, 2 * DIFF_HEADS, DIFF_HEAD_DIM), positions)
        dk = rope(dk.reshape(B, S, 2 * DIFF_HEADS, DIFF_HEAD_DIM), positions)
        dq = dq.reshape(B, S, DIFF_HEADS, 2, DIFF_HEAD_DIM)
        dk = dk.reshape(B, S, DIFF_HEADS, 2, DIFF_HEAD_DIM)
        dv = dv.reshape(B, S, DIFF_HEADS, DIFF_V_DIM)
        lam = (jnp.exp(jnp.sum(lambda_q1[l].astype(jnp.float32) * lambda_k1[l].astype(jnp.float32)))
               - jnp.exp(jnp.sum(lambda_q2[l].astype(jnp.float32) * lambda_k2[l].astype(jnp.float32)))
               + lam_init)
        o_a = diff_attention(dq, dk, dv, lam)
        o_a = rms_norm(o_a, subln_g[l], SUBLN_EPS) * (1.0 - lam_init)

        c_q = rms_norm(c_q, mla_q_norm_g[l], MLA_RMS_EPS)
        q = (c_q @ w_uq[l]).reshape(B, S, MLA_HEADS, MLA_QK_DIM)
        q = jnp.concatenate([q[..., :MLA_NOPE_DIM], rope(q[..., MLA_NOPE_DIM:], positions)], axis=-1)
        c_kv = rms_norm(c_kv, mla_kv_norm_g[l], MLA_RMS_EPS)
        kv = (c_kv @ w_ukv[l]).reshape(B, S, MLA_HEADS, MLA_NOPE_DIM + MLA_V_DIM)
        k_nope, v_m = kv[..., :MLA_NOPE_DIM], kv[..., MLA_NOPE_DIM:]
        k_pe = jnp.broadcast_to(rope(k_r[:, :, None, :], positions), (B, S, MLA_HEADS, MLA_ROPE_DIM))
        k = jnp.concatenate([k_nope, k_pe], axis=-1)
        o_b = causal_attention(q, k, v_m, MLA_QK_DIM ** -0.5)

        mixed = jnp.concatenate([o_a.reshape(B, S, DIFF_WIDTH), o_b.reshape(B, S, MLA_WIDTH)], axis=-1) @ w_o[l]
        x = layer_norm(DN_ALPHA * x + mixed, ln1_g[l], ln1_b[l])

        y = moe_ffn(x, w_router[l], b_router[l], w_gate_up[l], b_gate_up[l], w_down[l], b_down[l])
        x = layer_norm(DN_ALPHA * x + y, ln2_g[l], ln2_b[l])
    return x
```

```python
import math
import contextlib
import numpy as np
import concourse.bass as bass
import concourse.mybir as mybir
from concourse.bass_utils import run_bass_kernel_spmd

F32 = mybir.dt.float32
BF16 = mybir.dt.bfloat16
I32 = mybir.dt.int32
ALU = mybir.AluOpType
AF = mybir.ActivationFunctionType
AX = mybir.AxisListType

NCORES = 8
D = 1024
S = 4096
TQ = 2048
NE = 32
LAM_INIT = 0.8 - 0.6 * math.exp(0.0)
DN_ALPHA = 2.0 ** 0.25
TWO_PI = 2.0 * math.pi
PI_S = math.pi * (1.0 - 1e-6)
NDMA_SEM = 8


_FENCE = {}


class Buf:
    __slots__ = ("w", "r", "excl")

    def __init__(self, excl=False):
        self.w = None
        self.r = dict(_FENCE)
        self.excl = excl


class Eng:
    def __init__(self, name, h, sem, dma_sems):
        self.name = name
        self.h = h
        self.sem = sem
        self.count = 0
        self.seen = {}
        self.dma_sems = dma_sems
        self.dma_val = [0] * len(dma_sems)
        self.rr = 0


class FW:
    def __init__(self, nc, es):
        self.nc = nc
        self.es = es
        mk = lambda n: es.enter_context(nc.semaphore(n))
        self.pe = Eng("pe", nc.tensor, mk("s_pe"), [])
        self.act = Eng("act", nc.scalar, mk("s_act"), [])
        self.dve = Eng("dve", nc.vector, mk("s_dve"), [])
        self.pool = Eng("pool", nc.gpsimd, mk("s_pool"), [mk(f"d_pool{i}") for i in range(NDMA_SEM)])
        self.sp = Eng("sp", nc.sync, mk("s_sp"), [mk(f"d_sp{i}") for i in range(NDMA_SEM)])
        self.nwait = 0

    def _wait(self, E, tok):
        sem, val = tok
        if sem is E.sem and (E is self.pe or val > E.count):
            return
        k = id(sem)
        if E.seen.get(k, 0) >= val:
            return
        E.h.wait_ge(sem, val)
        E.seen[k] = val
        self.nwait += 1

    def _deps(self, E, reads, writes):
        for b in reads:
            if b.w is not None:
                self._wait(E, b.w)
            if b.excl:
                for tok in b.r.values():
                    self._wait(E, tok)
        for b in writes:
            if b.w is not None:
                self._wait(E, b.w)
            for tok in b.r.values():
                self._wait(E, tok)

    def _mark(self, tok, reads, writes):
        for b in reads:
            b.r[id(tok[0])] = tok
        for b in writes:
            b.w = tok
            b.r = {}

    def op(self, E, reads, writes, build, inc=True):
        self._deps(E, reads, writes)
        ins = build()
        tok = (E.sem, E.count + 1)
        if inc:
            ins.then_inc(E.sem, 1)
            E.count += 1
        self._mark(tok, reads, writes)
        return ins

    def dma(self, Q, out, in_, reads, writes, **kw):
        i = Q.rr % len(Q.dma_sems)
        Q.rr += 1
        sem = Q.dma_sems[i]
        if Q.dma_val[i] > 0:
            self._wait(Q, (sem, Q.dma_val[i]))
        self._deps(Q, reads, writes)
        Q.h.dma_start(out=out, in_=in_, **kw).then_inc(sem, 16)
        Q.dma_val[i] += 16
        tok = (sem, Q.dma_val[i])
        self._mark(tok, reads, writes)
        return tok

    def fence(self):
        _FENCE.clear()
        for Q in (self.sp, self.pool):
            for sem, v in zip(Q.dma_sems, Q.dma_val):
                if v > 0:
                    _FENCE[id(sem)] = (sem, v)
        for X in (self.pe, self.act, self.dve, self.pool, self.sp):
            if X.count > 0:
                _FENCE[id(X.sem)] = (X.sem, X.count)

    def finish(self, E):
        for Q in (self.sp, self.pool):
            for sem, v in zip(Q.dma_sems, Q.dma_val):
                if v > 0:
                    self._wait(E, (sem, v))
        for X in (self.pe, self.act, self.dve, self.pool, self.sp):
            if X is not E and X.count > 0:
                self._wait(E, (X.sem, X.count))


class Rot:
    def __init__(self, items):
        self.items = items
        self.i = 0

    def get(self):
        it = self.items[self.i % len(self.items)]
        self.i += 1
        return it


def build_program(stage="full"):
    nc = bass.Bass("TRN2", target_bir_lowering=False)
    dt_in = lambda name, shape, dt=F32: nc.dram_tensor(name, shape, dt, kind="ExternalInput").ap()
    xT_d = dt_in("xT", [D, S])
    xq_d = dt_in("xq", [TQ, D])
    pos_d = dt_in("pos", [1, S], I32)
    cst_d = dt_in("cst", [128, 640])
    w_in_d = dt_in("w_in", [D, 1984])
    lam_d = dt_in("lamv", [4, 64])
    subg_d = dt_in("subln_g", [128, 1])
    gq_d = dt_in("gq", [128, 2])
    gkv_d = dt_in("gkv", [128, 1])
    w_uq_d = dt_in("w_uq", [256, 768])
    w_ukv_d = dt_in("w_ukv", [128, 1024])
    w_o_d = dt_in("w_o", [D, D])
    lnv_d = dt_in("lnv", [4, D])
    if stage == "full":
        w_r_d = dt_in("w_router", [D, NE])
        b_r_d = dt_in("b_router", [1, NE])
        wgu_d = dt_in("wgu_t", [NE, 8, 128, 2048])
        bgu_d = dt_in("bguT", [128, NE * 16])
        wd_d = dt_in("w_down", [NE, D, D])
        bd_d = dt_in("b_down", [NE, D])
    out_d = nc.dram_tensor("out", [TQ, D], F32, kind="ExternalOutput").ap()

    _FENCE.clear()
    with contextlib.ExitStack() as es:
        fw = FW(nc, es)
        PE, ACT, DVE, POOL, SP = fw.pe, fw.act, fw.dve, fw.pool, fw.sp

        def sb(name, shape, dt, st=es):
            return st.enter_context(nc.sbuf_tensor("sb_" + name, shape, dt))

        psb = [es.enter_context(nc.psum_tensor(f"ps{i}", [128, 512], F32)) for i in range(8)]
        pb = [Buf(excl=True) for _ in range(8)]

        cst = sb("cst", [128, 640], F32); bcst = Buf()
        fw.dma(SP, cst[:], cst_d[:, :], [], [bcst])
        ident = cst[:, 0:128]
        ropec = cst[:, 512:516]
        cbf = sb("cbf", [128, 512], BF16); bcbf = Buf()
        fw.op(DVE, [bcst], [bcbf], lambda: nc.vector.tensor_copy(cbf[:, 0:384], cst[:, 128:512]))
        fw.op(DVE, [], [bcbf], lambda: nc.vector.memset(cbf[:, 384:512], 1.0))
        perm_bf = cbf[:, 0:128]
        masks_bf = [cbf[:, 128:256], cbf[:, 256:384]]
        ones_bf = cbf[:, 384:512]
        ones32 = sb("ones32", [128, 128], F32); bones = Buf()
        fw.op(POOL, [], [bones], lambda: nc.gpsimd.memset(ones32[:], 1.0))
        small = sb("small", [128, 64], F32); bsmall = Buf()
        fw.dma(SP, small[:, 0:1], subg_d[:, :], [], [bsmall])
        fw.dma(SP, small[:, 3:5], gq_d[:, :], [], [bsmall])
        fw.dma(SP, small[:, 5:6], gkv_d[:, :], [], [bsmall])
        fw.op(DVE, [], [bsmall], lambda: nc.vector.memset(small[:, 8:9], 1e-6))
        fw.op(DVE, [], [bsmall], lambda: nc.vector.memset(small[:, 9:10], 1e-5))
        EPS6 = small[:, 8:9]
        EPS5 = small[:, 9:10]
        lamt = sb("lamt", [128, 256], F32); blam = Buf()
        fw.dma(SP, lamt[:].rearrange("p (a b) -> p a b", a=4), lam_d.partition_broadcast(128), [], [blam])
        fw.op(DVE, [blam], [blam], lambda: nc.vector.tensor_tensor(lamt[:, 0:64], lamt[:, 0:64], lamt[:, 64:128], op=ALU.mult))
        fw.op(DVE, [blam], [blam], lambda: nc.vector.tensor_tensor(lamt[:, 128:192], lamt[:, 128:192], lamt[:, 192:256], op=ALU.mult))
        fw.op(DVE, [blam], [bsmall], lambda: nc.vector.reduce_sum(small[:, 6:7], lamt[:, 0:64], axis=AX.X))
        fw.op(DVE, [blam], [bsmall], lambda: nc.vector.reduce_sum(small[:, 7:8], lamt[:, 128:192], axis=AX.X))
        fw.op(ACT, [bsmall], [bsmall], lambda: nc.scalar.activation(small[:, 6:8], small[:, 6:8], AF.Exp))
        fw.op(DVE, [bsmall], [bsmall], lambda: nc.vector.tensor_tensor(small[:, 2:3], small[:, 7:8], small[:, 6:7], op=ALU.subtract))
        fw.op(DVE, [bsmall], [bsmall], lambda: nc.vector.tensor_scalar(small[:, 2:3], small[:, 2:3], -LAM_INIT, None, op0=ALU.add))
        fw.op(DVE, [bsmall], [bsmall], lambda: nc.vector.tensor_scalar(small[:, 1:2], small[:, 0:1], 1.0 - LAM_INIT, None, op0=ALU.mult))

        otx = sb("otx", [128, 8, TQ], BF16)
        botx = [Buf() for _ in range(16)]

        with contextlib.ExitStack() as sa:
            cosT = sb("cosT", [128, S], F32, sa)
            sinS = sb("sinS", [128, S], F32, sa)
            btab = [Buf() for _ in range(8)]
            ckvn = sb("ckvn", [128, S], BF16, sa); bckvn = [Buf() for _ in range(8)]
            cqn = sb("cqn", [128, 2, TQ], BF16, sa); bcqn = [Buf() for _ in range(4)]
            KR = sb("KR", [128, S], BF16, sa); bKR = [Buf() for _ in range(8)]
            fw.op(POOL, [], bKR, lambda: nc.gpsimd.memset(KR[64:128, :], 0.0))
            s32 = Rot([(sb(f"s32_{i}", [128, 512], F32, sa), Buf()) for i in range(8)])
            s16 = Rot([(sb(f"s16_{i}", [128, 512], BF16, sa), Buf()) for i in range(4)])
            si32 = sb("si32", [128, 512], I32, sa); bsi32 = Buf()
            tmpb = Rot([(psb[i], pb[i]) for i in (6, 7, 0, 1)])

            def mm_acc(out_ap, pairs, reads, writes):
                n = len(pairs)
                for i, (l, r) in enumerate(pairs):
                    fw.op(PE, reads, writes,
                          lambda: nc.tensor.matmul(out_ap, l, r, start=(i == 0), stop=(i == n - 1)),
                          inc=(i == n - 1))

            def rope(src_ps, bsrc, rows, tc, dst_ap, bdst):
                cols = slice(tc * 512, (tc + 1) * 512)
                import os as _os
                _cut = int(_os.environ.get("ROPE_CUT", "9")) if rows == 128 else 9
                if _cut < 1:
                    return
                hb, bhb = s16.get()
                fw.op(ACT, [bsrc], [bhb], lambda: nc.scalar.copy(hb[0:rows, :], src_ps))
                if _cut < 2:
                    return
                sw, bsw = tmpb.get()
                fw.op(PE, [bhb, bcbf], [bsw], lambda: nc.tensor.matmul(sw[0:rows, :], perm_bf[0:rows, 0:rows], hb[0:rows, :], start=True, stop=True))
                if _cut < 3:
                    return
                t1, bt1 = s32.get()
                fw.op(DVE, [bsrc, btab[tc]], [bt1], lambda: nc.vector.tensor_tensor(t1[0:rows, :], src_ps, cosT[0:rows, cols], op=ALU.mult))
                if _cut < 4:
                    return
                t2, bt2 = s32.get()
                fw.op(DVE, [bsw, btab[tc]], [bt2], lambda: nc.vector.tensor_tensor(t2[0:rows, :], sw[0:rows, :], sinS[0:rows, cols], op=ALU.mult))
                if _cut < 5:
                    return
                fw.op(POOL, [bt1, bt2], [bdst], lambda: nc.gpsimd.tensor_tensor(dst_ap, t1[0:rows, :], t2[0:rows, :], op=ALU.add))

            def rms_scale(ps_list, bps_list, n_feat, eps, gcols, dst_aps, bdst):
                sqs = []
                for ps_ap, bps in zip(ps_list, bps_list):
                    sq, bsq = s32.get()
                    fw.op(ACT, [bps], [bsq], lambda: nc.scalar.activation(sq[:], ps_ap, AF.Square))
                    sqs.append((sq, bsq))
                ss, bss = tmpb.get()
                for i, (sq, bsq) in enumerate(sqs):
                    fw.op(PE, [bsq, bones], [bss],
                          lambda: nc.tensor.matmul(ss[:], ones32[:], sq[:], start=(i == 0), stop=(i == len(sqs) - 1)),
                          inc=(i == len(sqs) - 1))
                rstd, brstd = s32.get()
                fw.op(ACT, [bss, bsmall], [brstd], lambda: nc.scalar.activation(rstd[:], ss[:], AF.Sqrt, bias=eps, scale=1.0 / n_feat))
                fw.op(DVE, [brstd], [brstd], lambda: nc.vector.reciprocal(rstd[:], rstd[:]))
                for ps_ap, bps, gc, dst in zip(ps_list, bps_list, gcols, dst_aps):
                    fw.op(DVE, [bps, brstd, bsmall], [bdst],
                          lambda: nc.vector.scalar_tensor_tensor(dst, in0=ps_ap, scalar=gc, in1=rstd[:], op0=ALU.mult, op1=ALU.mult))

            def attention(nsub, s_emit, s_reads, Vt, bV, scale, finalize):
                S_B = [(psb[0], pb[0]), (psb[1], pb[1])]
                O_B = [(psb[2], pb[2]), (psb[3], pb[3])]
                L_B = [(psb[4], pb[4]), (psb[5], pb[5])]
                for g in range(4):
                    units = []
                    nkb = 4 * g + 4
                    for half in (0, 1):
                        for kl in range(nkb):
                            i = kl - 4 * g
                            col0 = 0 if i < 0 else i * 128
                            mt = None if i < 0 else half
                            for c in range(nsub):
                                units.append((c, half * 16 + kl, col0, mt))
                    nun = len(units)
                    first = [True] * nsub
                    last_idx = {}
                    for ui, u in enumerate(units):
                        last_idx[u[0]] = ui
                    pts = {}

                    def emit_s(ui):
                        c, kb, col0, mt = units[ui]
                        sbk, bsbk = S_B[ui % 2]
                        s_emit(c, kb, g, col0, sbk, bsbk)
                        pT, bpT = s16.get()
                        fw.op(ACT, [bsbk], [bpT], lambda: nc.scalar.activation(pT[:, col0:512], sbk[:, col0:512], AF.Exp, scale=scale))
                        if mt is not None:
                            fw.op(POOL, [bpT, bcbf], [bpT], lambda: nc.gpsimd.tensor_tensor(pT[:, col0:col0 + 128], pT[:, col0:col0 + 128], masks_bf[mt], op=ALU.mult))
                        pts[ui] = (pT, bpT)

                    def emit_pv(ui):
                        c, kb, col0, mt = units[ui]
                        pT, bpT = pts.pop(ui)
                        o, bo = O_B[c]
                        l, bl = L_B[c]
                        st = first[c]
                        first[c] = False
                        sp_ = (last_idx[c] == ui)
                        fw.op(PE, [bpT, bV[kb // 4]], [bo], lambda: nc.tensor.matmul(o[:, col0:512], Vt[:, kb, :], pT[:, col0:512], start=st, stop=sp_), inc=False)
                        fw.op(PE, [bpT, bcbf], [bl], lambda: nc.tensor.matmul(l[:, col0:512], ones_bf, pT[:, col0:512], start=st, stop=sp_))

                    LOOK = 2
                    for ui in range(min(LOOK, nun)):
                        emit_s(ui)
                    for ui in range(nun):
                        emit_pv(ui)
                        if ui + LOOK < nun:
                            emit_s(ui + LOOK)
                    finalize(g, O_B, L_B)

            with contextlib.ExitStack() as sx:
                xTb = sb("xTb", [128, 8, S], BF16, sx); bxT = [Buf() for _ in range(8)]
                xT_v = xT_d.rearrange("(dc p) t -> p dc t", p=128)
                for tc in range(8):
                    fw.dma(POOL, xTb[:, :, tc * 512:(tc + 1) * 512], xT_v[:, :, tc * 512:(tc + 1) * 512], [], [bxT[tc]])
                w_in_v = w_in_d.rearrange("(dc p) c -> p dc c", p=128)

                for tc in range(8):
                    cols = slice(tc * 512, (tc + 1) * 512)
                    fw.dma(SP, si32[:], pos_d[0:1, cols].partition_broadcast(128), [], [bsi32])
                    ang, bang = s32.get()
                    fw.op(DVE, [bsi32], [bang], lambda: nc.vector.tensor_copy(ang[:], si32[:]))
                    fw.op(DVE, [bang, bcst], [bang], lambda: nc.vector.tensor_scalar(ang[:], ang[:], ropec[:, 0:1], None, op0=ALU.mult))
                    for which in (0, 1):
                        shift = 0.5 if which == 0 else 0.75
                        u, bu = s32.get()
                        fw.op(DVE, [bang], [bu], lambda: nc.vector.tensor_scalar(u[:], ang[:], 1.0 / TWO_PI, shift, op0=ALU.mult, op1=ALU.add))
                        ki, bki = s32.get()
                        kiv = ki[:].bitcast(I32)
                        fw.op(DVE, [bu], [bki], lambda: nc.vector.tensor_copy(kiv, u[:]))
                        kf, bkf = s32.get()
                        fw.op(POOL, [bki], [bkf], lambda: nc.gpsimd.tensor_copy(kf[:], kiv))
                        fw.op(POOL, [bkf, bu], [bu], lambda: nc.gpsimd.tensor_tensor(u[:], u[:], kf[:], op=ALU.subtract))
                        fw.op(DVE, [bu], [bkf], lambda: nc.vector.scalar_tensor_tensor(kf[:], in0=u[:], scalar=0.0, in1=u[:], op0=ALU.is_lt, op1=ALU.add))
                        if which == 0:
                            fw.op(ACT, [bkf, bcst], [btab[tc]], lambda: nc.scalar.activation(sinS[:, cols], kf[:], AF.Sin, bias=ropec[:, 2:3], scale=ropec[:, 1:2]))
                        else:
                            fw.op(ACT, [bkf, bcst], [btab[tc]], lambda: nc.scalar.activation(cosT[:, cols], kf[:], AF.Sin, bias=ropec[:, 3:4], scale=TWO_PI * (1.0 - 1e-6)))

                if stage.startswith("tabtt"):
                    import os as _os
                    r0, r1 = [int(v) for v in _os.environ.get("TT_ROWS", "0,128").split(",")]
                    mode = _os.environ.get("TT_MODE", "psum_cos")
                    t1, bt1 = s32.get()
                    kps, bkps = tmpb.get()
                    fw.op(PE, [bcbf], [bkps], lambda: nc.tensor.matmul(kps[:], perm_bf, cbf[:, 0:512], start=True, stop=True))
                    if mode == "psum_cos":
                        fw.op(DVE, [bkps, btab[0]], [bt1], lambda: nc.vector.tensor_tensor(t1[r0:r1, :], kps[r0:r1, :], cosT[r0:r1, 0:512], op=ALU.mult))
                    elif mode == "sb_cos":
                        t2, bt2 = s32.get()
                        fw.op(DVE, [], [bt2], lambda: nc.vector.memset(t2[:], 1.0))
                        fw.op(DVE, [bt2, btab[0]], [bt1], lambda: nc.vector.tensor_tensor(t1[r0:r1, :], t2[r0:r1, :], cosT[r0:r1, 0:512], op=ALU.mult))
                    elif mode == "psum_sb":
                        t2, bt2 = s32.get()
                        fw.op(DVE, [], [bt2], lambda: nc.vector.memset(t2[:], 1.0))
                        fw.op(DVE, [bkps, bt2], [bt1], lambda: nc.vector.tensor_tensor(t1[r0:r1, :], kps[r0:r1, :], t2[r0:r1, :], op=ALU.mult))
                    fw.dma(SP, out_d[0:128, 0:512], t1[:], [bt1], [])
                    fw.dma(SP, out_d[128:256, 0:512], cosT[:, 0:512], [btab[0]], [])
                    fw.dma(SP, out_d[256:384, 0:512], sinS[:, 0:512], [btab[0]], [])
                    fw.finish(SP)
                    return nc
                if stage == "tab":
                    fw.finish(SP)
                    return nc
                with contextlib.ExitStack() as sm0:
                    WC = sb("WC", [128, 8, 448], BF16, sm0); bWC = Buf()
                    fw.dma(POOL, WC[:], w_in_v[:, :, 1536:1984], [], [bWC])
                    for tc in range(8):
                        cols = slice(tc * 512, (tc + 1) * 512)
                        ckv, bckv = tmpb.get()
                        mm_acc(ckv[:], [(WC[:, dc, 256:384], xTb[:, dc, cols]) for dc in range(8)], [bWC, bxT[tc]], [bckv])
                        rms_scale([ckv[:]], [bckv], 128.0, EPS6, [small[:, 5:6]], [ckvn[:, cols]], bckvn[tc])
                        kr, bkr = tmpb.get()
                        mm_acc(kr[0:64, :], [(WC[:, dc, 384:448], xTb[:, dc, cols]) for dc in range(8)], [bWC, bxT[tc]], [bkr])
                        rope(kr[0:64, :], bkr, 64, tc, KR[0:64, cols], bKR[tc])
                        if tc < 4:
                            cq0, bcq0 = tmpb.get()
                            mm_acc(cq0[:], [(WC[:, dc, 0:128], xTb[:, dc, cols]) for dc in range(8)], [bWC, bxT[tc]], [bcq0])
                            cq1, bcq1 = tmpb.get()
                            mm_acc(cq1[:], [(WC[:, dc, 128:256], xTb[:, dc, cols]) for dc in range(8)], [bWC, bxT[tc]], [bcq1])
                            rms_scale([cq0[:], cq1[:]], [bcq0, bcq1], 256.0, EPS6, [small[:, 3:4], small[:, 4:5]],
                                      [cqn[:, 0, cols], cqn[:, 1, cols]], bcqn[tc])

                if stage == "m0":
                    fw.finish(SP)
                    return nc
                fw.fence()
                with contextlib.ExitStack() as sd:
                    WQ = sb("WQ", [128, 8, 128], BF16, sd); WK = sb("WK", [128, 8, 128], BF16, sd); WV = sb("WV", [128, 8, 128], BF16, sd)
                    bW = Buf()
                    KT = sb("KT", [128, S], BF16, sd); bKT = [Buf() for _ in range(8)]
                    QT = sb("QT", [128, TQ], BF16, sd); bQT = [Buf() for _ in range(4)]
                    Vt = sb("Vt", [128, 32, 128], BF16, sd); bV = [Buf() for _ in range(8)]
                    for h in range(4):
                        fw.dma(POOL, WQ[:], w_in_v[:, :, 128 * h:128 * h + 128], [], [bW])
                        fw.dma(POOL, WK[:], w_in_v[:, :, 512 + 128 * h:512 + 128 * h + 128], [], [bW])
                        fw.dma(POOL, WV[:], w_in_v[:, :, 1024 + 128 * h:1024 + 128 * h + 128], [], [bW])
                        if stage == "dprojD":
                            fw.finish(SP)
                            return nc
                        import os as _os
                        _ntc = int(_os.environ.get("DPROJ_NTC", "8"))
                        _noq = _os.environ.get("DPROJ_NOQ", "0") == "1"
                        for tc in range(_ntc):
                            cols = slice(tc * 512, (tc + 1) * 512)
                            if stage != "dprojV":
                                kps, bkps = tmpb.get()
                                mm_acc(kps[:], [(WK[:, dc, :], xTb[:, dc, cols]) for dc in range(8)], [bW, bxT[tc]], [bkps])
                                rope(kps[:], bkps, 128, tc, KT[:, cols], bKT[tc])
                            if tc < 4 and stage != "dprojV" and not _noq:
                                qps, bqps = tmpb.get()
                                mm_acc(qps[:], [(WQ[:, dc, :], xTb[:, dc, cols]) for dc in range(8)], [bW, bxT[tc]], [bqps])
                                rope(qps[:], bqps, 128, tc, QT[:, cols], bQT[tc])
                            if stage == "dprojK":
                                continue
                            vps, bvps = tmpb.get()
                            for i in range(4):
                                mm_acc(vps[:, i * 128:(i + 1) * 128],
                                       [(xTb[:, dc, tc * 512 + i * 128: tc * 512 + (i + 1) * 128], WV[:, dc, :]) for dc in range(8)],
                                       [bW, bxT[tc]], [bvps])
                            fw.op(ACT, [bvps], [bV[tc]], lambda: nc.scalar.copy(Vt[:, tc * 4:(tc + 1) * 4, :], vps[:].rearrange("p (a b) -> p a b", a=4)))

                        if stage in ("dproj", "dprojK", "dprojV"):
                            fw.finish(SP)
                            return nc
                        def s_emit(c, kb, g, col0, sbk, bsbk):
                            fw.op(PE, [bKT[kb // 4], bQT[g]], [bsbk],
                                  lambda: nc.tensor.matmul(sbk[:, col0:512], KT[64 * c:64 * c + 64, kb * 128:(kb + 1) * 128],
                                                           QT[64 * c:64 * c + 64, g * 512 + col0:(g + 1) * 512], start=True, stop=True))

                        def fin_diff(g, O_B, L_B, h=h):
                            ds = []
                            for c in range(2):
                                rl, brl = s32.get()
                                fw.op(DVE, [L_B[c][1]], [brl], lambda: nc.vector.reciprocal(rl[:], L_B[c][0][:]))
                                fw.op(DVE, [O_B[c][1], brl], [brl], lambda: nc.vector.tensor_tensor(rl[:], O_B[c][0][:], rl[:], op=ALU.mult))
                                ds.append((rl, brl))
                            dd, bdd = s32.get()
                            fw.op(DVE, [ds[0][1], ds[1][1], bsmall], [bdd],
                                  lambda: nc.vector.scalar_tensor_tensor(dd[:], in0=ds[1][0][:], scalar=small[:, 2:3], in1=ds[0][0][:], op0=ALU.mult, op1=ALU.add))
                            sq, bsq = s32.get()
                            fw.op(ACT, [bdd], [bsq], lambda: nc.scalar.activation(sq[:], dd[:], AF.Square))
                            ss, bss = tmpb.get()
                            fw.op(PE, [bsq, bones], [bss], lambda: nc.tensor.matmul(ss[:], ones32[:], sq[:], start=True, stop=True))
                            rstd, brstd = s32.get()
                            fw.op(ACT, [bss, bsmall], [brstd], lambda: nc.scalar.activation(rstd[:], ss[:], AF.Sqrt, bias=EPS5, scale=1.0 / 128.0))
                            fw.op(DVE, [brstd], [brstd], lambda: nc.vector.reciprocal(rstd[:], rstd[:]))
                            wr = [botx[4 * g + i] for i in range(4)]
                            fw.op(DVE, [bdd, brstd, bsmall], wr,
                                  lambda: nc.vector.scalar_tensor_tensor(otx[:, h, g * 512:(g + 1) * 512], in0=dd[:], scalar=small[:, 1:2], in1=rstd[:], op0=ALU.mult, op1=ALU.mult))

                        attention(2, s_emit, None, Vt, bV, 64.0 ** -0.5, fin_diff)
                        if stage == "datt":
                            fw.finish(SP)
                            return nc
            fw.fence()
            with contextlib.ExitStack() as sm:
                wuq = sb("wuq", [128, 2, 768], BF16, sm); bwuq = Buf()
                wukv = sb("wukv", [128, 1024], BF16, sm); bwukv = Buf()
                fw.dma(POOL, wuq[:], w_uq_d.rearrange("(rc p) c -> p rc c", p=128), [], [bwuq])
                fw.dma(POOL, wukv[:], w_ukv_d[:, :], [], [bwukv])
                KTm = sb("KTm", [128, S], BF16, sm); bKTm = [Buf() for _ in range(8)]
                Vm = sb("Vm", [128, 32, 128], BF16, sm); bVm = [Buf() for _ in range(8)]
                QTn = sb("QTn", [128, TQ], BF16, sm); bQTn = [Buf() for _ in range(4)]
                QTr = sb("QTr", [128, TQ], BF16, sm); bQTr = [Buf() for _ in range(4)]
                fw.op(POOL, [], bQTr, lambda: nc.gpsimd.memset(QTr[64:128, :], 0.0))
                for h in range(4):
                    for tc in range(8):
                        cols = slice(tc * 512, (tc + 1) * 512)
                        kn, bkn = tmpb.get()
                        fw.op(PE, [bwukv, bckvn[tc]], [bkn], lambda: nc.tensor.matmul(kn[:], wukv[:, h * 256:h * 256 + 128], ckvn[:, cols], start=True, stop=True))
                        fw.op(ACT, [bkn], [bKTm[tc]], lambda: nc.scalar.copy(KTm[:, cols], kn[:]))
                        vps, bvps = tmpb.get()
                        for i in range(4):
                            fw.op(PE, [bwukv, bckvn[tc]], [bvps],
                                  lambda: nc.tensor.matmul(vps[:, i * 128:(i + 1) * 128], ckvn[:, tc * 512 + i * 128: tc * 512 + (i + 1) * 128],
                                                           wukv[:, h * 256 + 128:h * 256 + 256], start=True, stop=True), inc=(i == 3))
                        fw.op(DVE, [bvps], [bVm[tc]], lambda: nc.vector.tensor_copy(Vm[:, tc * 4:(tc + 1) * 4, :], vps[:].rearrange("p (a b) -> p a b", a=4)))
                        if tc < 4:
                            qn, bqn = tmpb.get()
                            mm_acc(qn[:], [(wuq[:, rc, h * 192:h * 192 + 128], cqn[:, rc, cols]) for rc in range(2)], [bwuq, bcqn[tc]], [bqn])
                            fw.op(ACT, [bqn], [bQTn[tc]], lambda: nc.scalar.copy(QTn[:, cols], qn[:]))
                            qr, bqr = tmpb.get()
                            mm_acc(qr[0:64, :], [(wuq[:, rc, h * 192 + 128:h * 192 + 192], cqn[:, rc, cols]) for rc in range(2)], [bwuq, bcqn[tc]], [bqr])
                            rope(qr[0:64, :], bqr, 64, tc, QTr[0:64, cols], bQTr[tc])

                    if stage == "mproj":
                        fw.finish(SP)
                        return nc
                    def s_emit_m(c, kb, g, col0, sbk, bsbk):
                        fw.op(PE, [bKTm[kb // 4], bQTn[g]], [bsbk],
                              lambda: nc.tensor.matmul(sbk[:, col0:512], KTm[:, kb * 128:(kb + 1) * 128], QTn[:, g * 512 + col0:(g + 1) * 512], start=True, stop=False), inc=False)
                        fw.op(PE, [bKR[kb // 4], bQTr[g]], [bsbk],
                              lambda: nc.tensor.matmul(sbk[:, col0:512], KR[:, kb * 128:(kb + 1) * 128], QTr[:, g * 512 + col0:(g + 1) * 512], start=False, stop=True))

                    def fin_mla(g, O_B, L_B, h=h):
                        rl, brl = s32.get()
                        fw.op(DVE, [L_B[0][1]], [brl], lambda: nc.vector.reciprocal(rl[:], L_B[0][0][:]))
                        wr = [botx[4 * g + i] for i in range(4)]
                        fw.op(DVE, [O_B[0][1], brl], wr, lambda: nc.vector.tensor_tensor(otx[:, 4 + h, g * 512:(g + 1) * 512], O_B[0][0][:], rl[:], op=ALU.mult))

                    attention(1, s_emit_m, None, Vm, bVm, 192.0 ** -0.5, fin_mla)
                    if stage == "matt":
                        fw.finish(SP)
                        return nc

        fw.fence()
        ACC = sb("ACC", [128, 16, D], F32); bACC = [Buf() for _ in range(16)]
        sm2 = sb("sm2", [128, 16, 8], F32); bsm2 = [Buf() for _ in range(16)]

        def layer_norm(z, bz, tb, lnbc, blnbc, dst, bdst, junk, bjunk):
            sc = sm2[:, tb, :]
            bs = bsm2[tb]
            fw.op(DVE, [bz], [bs], lambda: nc.vector.reduce_sum(sc[:, 0:1], z, axis=AX.X))
            fw.op(DVE, [bs], [bs], lambda: nc.vector.tensor_scalar(sc[:, 1:2], sc[:, 0:1], -1.0 / D, None, op0=ALU.mult))
            fw.op(ACT, [bz, bs], [bz], lambda: nc.scalar.activation(z, z, AF.Identity, bias=sc[:, 1:2]))
            fw.op(ACT, [bz], [bjunk], lambda: nc.scalar.activation(junk, z, AF.Square))
            fw.op(DVE, [bjunk], [bs], lambda: nc.vector.reduce_sum(sc[:, 2:3], junk, axis=AX.X))
            fw.op(ACT, [bs, bsmall], [bs], lambda: nc.scalar.activation(sc[:, 3:4], sc[:, 2:3], AF.Sqrt, bias=EPS5, scale=1.0 / D))
            fw.op(DVE, [bs], [bs], lambda: nc.vector.reciprocal(sc[:, 3:4], sc[:, 3:4]))
            fw.op(DVE, [bz, bs, blnbc], [bz], lambda: nc.vector.scalar_tensor_tensor(z, in0=z, scalar=sc[:, 3:4], in1=lnbc[:, 0, :], op0=ALU.mult, op1=ALU.mult))
            fw.op(POOL, [bz, blnbc], [bdst], lambda: nc.gpsimd.tensor_tensor(dst, z, lnbc[:, 1, :], op=ALU.add))

        with contextlib.ExitStack() as so:
            ln1bc = sb("ln1bc", [128, 2, D], F32, so); bln1 = Buf()
            fw.dma(SP, ln1bc[:], lnv_d[0:2, :].partition_broadcast(128), [], [bln1])
            wo = sb("wo", [128, 8, D], BF16, so); bwo = Buf()
            fw.dma(POOL, wo[:], w_o_d.rearrange("(hh p) o -> p hh o", p=128), [], [bwo])
            xqt = Rot([(sb(f"xqt{i}", [128, D], F32, so), Buf()) for i in range(2)])
            zt = Rot([(sb(f"zt{i}", [128, D], F32, so), Buf()) for i in range(2)])
            jk = Rot([(sb(f"jk{i}", [128, D], F32, so), Buf()) for i in range(2)])
            mixb = Rot([((psb[0], pb[0]), (psb[1], pb[1])), ((psb[2], pb[2]), (psb[3], pb[3]))])
            for tb in range(16):
                xt_, bxt_ = xqt.get()
                fw.dma(SP, xt_[:], xq_d[tb * 128:(tb + 1) * 128, :], [], [bxt_])
                banks = mixb.get()
                z, bz = zt.get()
                for half in range(2):
                    mps, bmps = banks[half]
                    for hh in range(8):
                        fw.op(PE, [botx[tb], bwo], [bmps],
                              lambda: nc.tensor.matmul(mps[:], otx[:, hh, tb * 128:(tb + 1) * 128], wo[:, hh, half * 512:(half + 1) * 512], start=(hh == 0), stop=(hh == 7)),
                              inc=(hh == 7))
                    fw.op(DVE, [bmps, bxt_], [bz],
                          lambda: nc.vector.scalar_tensor_tensor(z[:, half * 512:(half + 1) * 512], in0=xt_[:, half * 512:(half + 1) * 512], scalar=DN_ALPHA, in1=mps[:], op0=ALU.mult, op1=ALU.add))
                j_, bj_ = jk.get()
                layer_norm(z[:], bz, tb, ln1bc, bln1, ACC[:, tb, :], bACC[tb], j_[:], bj_)

        fw.fence()
        if stage == "ln1":
            for tb in range(16):
                fw.dma(SP, out_d[tb * 128:(tb + 1) * 128, :], ACC[:, tb, :], [bACC[tb]], [])
            fw.finish(SP)
            return nc

        G = sb("G", [128, 16, NE], F32); bG = [Buf() for _ in range(16)]
        bguT = sb("bguT", [128, NE * 16], F32); bbgu = Buf()
        fw.dma(SP, bguT[:], bgu_d[:, :], [], [bbgu])
        with contextlib.ExitStack() as sr:
            wr32 = sb("wr32", [128, 8, NE], F32, sr); bwr = Buf()
            fw.dma(SP, wr32[:], w_r_d.rearrange("(dc p) e -> p dc e", p=128), [], [bwr])
            brbc = sb("brbc", [128, NE], F32, sr); bbr = Buf()
            fw.dma(SP, brbc[:], b_r_d.partition_broadcast(128), [], [bbr])
            bd32 = sb("bd32", [NE, D], F32, sr); bbd = Buf()
            fw.dma(SP, bd32[:], bd_d[:, :], [], [bbd])
            GT = sb("GT", [NE, TQ], F32, sr); bGT = [Buf() for _ in range(16)]
            x1T32 = Rot([(sb(f"x1T32_{i}", [128, 8, 128], F32, sr), Buf()) for i in range(2)])
            rt = Rot([(sb(f"rt{i}", [128, 128], F32, sr), Buf()) for i in range(2)])
            tpb = Rot([((psb[0], pb[0]), (psb[1], pb[1])), ((psb[2], pb[2]), (psb[3], pb[3]))])
            tmp2 = Rot([(psb[i], pb[i]) for i in (4, 5, 6, 7)])
            for tb in range(16):
                banks = tpb.get()
                xT32, bxT32 = x1T32.get()
                for hb_ in range(2):
                    tp, btp = banks[hb_]
                    for q in range(4):
                        dc = hb_ * 4 + q
                        fw.op(PE, [bACC[tb], bcst], [btp], lambda: nc.tensor.transpose(tp[:, q * 128:(q + 1) * 128], ACC[:, tb, dc * 128:(dc + 1) * 128], ident), inc=(q == 3))
                    fw.op(ACT, [btp], [bxT32], lambda: nc.scalar.copy(xT32[:, hb_ * 4:(hb_ + 1) * 4, :], tp[:].rearrange("p (a b) -> p a b", a=4)))
                    fw.op(DVE, [btp], [botx[tb]], lambda: nc.vector.tensor_copy(otx[:, hb_ * 4:(hb_ + 1) * 4, tb * 128:(tb + 1) * 128], tp[:].rearrange("p (a b) -> p a b", a=4)))
                lgp, blgp = tmp2.get()
                for dc in range(8):
                    fw.op(PE, [bxT32, bwr], [blgp], lambda: nc.tensor.matmul(lgp[:, 0:NE], xT32[:, dc, :], wr32[:, dc, :], start=(dc == 0), stop=(dc == 7)), inc=(dc == 7))
                r_, br_ = rt.get()
                lg = r_[:, 0:32]; m8 = r_[:, 32:40]; ex = r_[:, 40:72]; mk = r_[:, 72:104]; misc = r_[:, 104:112]
                fw.op(DVE, [blgp, bbr], [br_], lambda: nc.vector.tensor_tensor(lg, lgp[:, 0:NE], brbc[:], op=ALU.add))
                fw.op(DVE, [br_], [br_], lambda: nc.vector.max(out=m8, in_=lg))
                fw.op(DVE, [br_], [br_], lambda: nc.vector.tensor_scalar(misc[:, 0:1], m8[:, 0:1], -1.0, None, op0=ALU.mult))
                fw.op(ACT, [br_], [br_], lambda: nc.scalar.activation(ex, lg, AF.Exp, bias=misc[:, 0:1]))
                fw.op(DVE, [br_], [br_], lambda: nc.vector.tensor_scalar(mk, lg, m8[:, 3:4], None, op0=ALU.is_ge))
                fw.op(DVE, [br_], [br_], lambda: nc.vector.tensor_tensor(ex, ex, mk, op=ALU.mult))
                fw.op(DVE, [br_], [br_], lambda: nc.vector.reduce_sum(misc[:, 1:2], ex, axis=AX.X))
                fw.op(DVE, [br_], [br_], lambda: nc.vector.reciprocal(misc[:, 2:3], misc[:, 1:2]))
                fw.op(DVE, [br_], [bG[tb]], lambda: nc.vector.tensor_scalar(G[:, tb, :], ex, misc[:, 2:3], None, op0=ALU.mult))
                gtp, bgtp = tmp2.get()
                fw.op(PE, [bG[tb], bcst], [bgtp], lambda: nc.tensor.transpose(gtp[0:NE, 0:128], G[:, tb, :], ident))
                fw.op(ACT, [bgtp], [bGT[tb]], lambda: nc.scalar.copy(GT[:, tb * 128:(tb + 1) * 128], gtp[0:NE, 0:128]))
                for half in range(2):
                    bdp, bbdp = tmp2.get()
                    fw.op(PE, [bGT[tb], bbd], [bbdp], lambda: nc.tensor.matmul(bdp[:], GT[:, tb * 128:(tb + 1) * 128], bd32[:, half * 512:(half + 1) * 512], start=True, stop=True))
                    fw.op(DVE, [bbdp, bACC[tb]], [bACC[tb]],
                          lambda: nc.vector.scalar_tensor_tensor(ACC[:, tb, half * 512:(half + 1) * 512], in0=ACC[:, tb, half * 512:(half + 1) * 512], scalar=DN_ALPHA, in1=bdp[:], op0=ALU.mult, op1=ALU.add))

        fw.fence()
        with contextlib.ExitStack() as se:
            stg = Rot([(sb(f"stg{i}", [128, 2048], F32, se), Buf()) for i in range(3)])
            wgr = Rot([(sb(f"wg{i}", [128, 8, 2, 128], BF16, se), Buf()) for i in range(3)])
            Wd = sb("Wd", [128, 8, D], BF16, se); bWd = [Buf() for _ in range(4)]
            ACTT = sb("ACTT", [128, 8, TQ], BF16, se); bACTT = [[Buf() for _ in range(4)] for _ in range(8)]
            sgc = Rot([(sb(f"sgc{i}", [128, 512], F32, se), Buf()) for i in range(2)])
            ssg = Rot([(sb(f"ssg{i}", [128, 512], F32, se), Buf()) for i in range(2)])
            suc = Rot([(sb(f"suc{i}", [128, 512], F32, se), Buf()) for i in range(2)])
            gub = Rot([((psb[0], pb[0]), (psb[1], pb[1])), ((psb[2], pb[2]), (psb[3], pb[3]))])
            yb = Rot([(psb[i], pb[i]) for i in (4, 5, 6, 7)])
            wd_v = wd_d.rearrange("e (fc p) o -> e p fc o", p=128)

            def load_wg(e, fc):
                st_, bst_ = stg.get()
                fw.dma(SP, st_[:], wgu_d[e, fc], [], [bst_])
                wg_, bwg_ = wgr.get()
                fw.op(ACT, [bst_], [bwg_], lambda: nc.scalar.copy(wg_[:].rearrange("p a b c -> p (a b c)"), st_[:]))
                return wg_, bwg_

            def load_wd(e, pc):
                st_, bst_ = stg.get()
                fw.dma(SP, st_[:].rearrange("p (a b) -> p a b", a=2), wd_v[e, :, 2 * pc:2 * pc + 2, :], [], [bst_])
                fw.op(ACT, [bst_], [bWd[pc]], lambda: nc.scalar.copy(Wd[:, 2 * pc:2 * pc + 2, :], st_[:].rearrange("p (a b) -> p a b", a=2)))

            slices = [(e, fc) for e in range(NE) for fc in range(8)]
            PRE = 2
            WD_SCHED = {1: 0, 3: 1, 5: 2, 6: 3}
            loaded = {}
            for k in range(min(PRE, len(slices))):
                loaded[k] = load_wg(*slices[k])
            for k, (e, fc) in enumerate(slices):
                if k + PRE < len(slices):
                    loaded[k + PRE] = load_wg(*slices[k + PRE])
                wg_, bwg_ = loaded.pop(k)
                if fc in WD_SCHED:
                    load_wd(e, WD_SCHED[fc])
                for tg in range(4):
                    tcols = slice(tg * 512, (tg + 1) * 512)
                    (gps, bgps), (ups, bups) = gub.get()
                    rd = [bwg_] + [botx[tg * 4 + i] for i in range(4)]
                    for dc in range(8):
                        fw.op(PE, rd, [bgps], lambda: nc.tensor.matmul(gps[:], wg_[:, dc, 0, :], otx[:, dc, tcols], start=(dc == 0), stop=(dc == 7)), inc=(dc == 7))
                    for dc in range(8):
                        fw.op(PE, rd, [bups], lambda: nc.tensor.matmul(ups[:], wg_[:, dc, 1, :], otx[:, dc, tcols], start=(dc == 0), stop=(dc == 7)), inc=(dc == 7))
                    gc_, bgc_ = sgc.get()
                    sg_, bsg_ = ssg.get()
                    uc_, buc_ = suc.get()
                    bg_col = bguT[:, e * 16 + fc:e * 16 + fc + 1]
                    bu_col = bguT[:, e * 16 + 8 + fc:e * 16 + 8 + fc + 1]
                    fw.op(DVE, [bgps, bbgu], [bgc_], lambda: nc.vector.tensor_scalar(gc_[:], gps[:], bg_col, 7.0, op0=ALU.add, op1=ALU.min))
                    fw.op(ACT, [bgc_], [bsg_], lambda: nc.scalar.activation(sg_[:], gc_[:], AF.Sigmoid, scale=1.702))
                    fw.op(DVE, [bups, bbgu], [buc_], lambda: nc.vector.tensor_scalar(uc_[:], ups[:], bu_col, 7.0, op0=ALU.add, op1=ALU.min))
                    fw.op(POOL, [buc_], [buc_], lambda: nc.gpsimd.tensor_scalar(uc_[:], uc_[:], -7.0, 1.0, op0=ALU.max, op1=ALU.add))
                    fw.op(POOL, [bgc_, bsg_], [bsg_], lambda: nc.gpsimd.tensor_tensor(sg_[:], sg_[:], gc_[:], op=ALU.mult))
                    fw.op(POOL, [bsg_, buc_], [bACTT[fc][tg]], lambda: nc.gpsimd.tensor_tensor(ACTT[:, fc, tcols], sg_[:], uc_[:], op=ALU.mult))
                if fc == 7:
                    for tb in range(16):
                        for half in range(2):
                            yp, byp = yb.get()
                            rd = [bACTT[f][tb // 4] for f in range(8)] + bWd
                            for f in range(8):
                                fw.op(PE, rd, [byp], lambda: nc.tensor.matmul(yp[:], ACTT[:, f, tb * 128:(tb + 1) * 128], Wd[:, f, half * 512:(half + 1) * 512], start=(f == 0), stop=(f == 7)), inc=(f == 7))
                            fw.op(DVE, [byp, bG[tb], bACC[tb]], [bACC[tb]],
                                  lambda: nc.vector.scalar_tensor_tensor(ACC[:, tb, half * 512:(half + 1) * 512], in0=yp[:], scalar=G[:, tb, e:e + 1], in1=ACC[:, tb, half * 512:(half + 1) * 512], op0=ALU.mult, op1=ALU.add))

        fw.fence()
        with contextlib.ExitStack() as sf:
            ln2bc = sb("ln2bc", [128, 2, D], F32, sf); bln2 = Buf()
            fw.dma(SP, ln2bc[:], lnv_d[2:4, :].partition_broadcast(128), [], [bln2])
            ot = Rot([(sb(f"ot{i}", [128, D], F32, sf), Buf()) for i in range(2)])
            jk2 = Rot([(sb(f"jk2{i}", [128, D], F32, sf), Buf()) for i in range(2)])
            for tb in range(16):
                o_, bo_ = ot.get()
                j_, bj_ = jk2.get()
                layer_norm(ACC[:, tb, :], bACC[tb], tb, ln2bc, bln2, o_[:], bo_, j_[:], bj_)
                fw.dma(SP, out_d[tb * 128:(tb + 1) * 128, :], o_[:], [bo_], [])
        fw.finish(SP)
    return nc


_PROG = {}


def _consts(r):
    c = np.zeros((128, 640), np.float32)
    c[:, 0:128] = np.eye(128, dtype=np.float32)
    m = np.arange(128)
    partner = np.where(m % 64 < 32, m + 32, m - 32)
    c[partner, 128 + m] = 1.0
    k = np.arange(128)[:, None]
    q = np.arange(128)[None, :]
    c[:, 256:384] = (k <= q).astype(np.float32)
    c[:, 384:512] = 1.0 if r == 1 else 0.0
    invf = 1.0 / (10000.0 ** ((np.arange(128) % 32) * 2.0 / 64.0))
    first = (np.arange(128) % 64) < 32
    c[:, 512] = invf
    sc = TWO_PI * (1.0 - 1e-6)
    c[:, 513] = np.where(first, -sc, sc)
    c[:, 514] = np.where(first, PI_S, -PI_S)
    c[:, 515] = -PI_S
    return c


def _prep_inputs(inp):
    f32 = lambda a: np.ascontiguousarray(np.asarray(a), dtype=np.float32)
    x = f32(inp["x"])
    positions = np.ascontiguousarray(np.asarray(inp["positions"]), dtype=np.int32)
    wgu = f32(inp["w_gate_up"])[0]
    wgu_t = np.ascontiguousarray(
        wgu.reshape(NE, 8, 128, 2, 8, 128).transpose(0, 4, 2, 1, 3, 5)).reshape(NE, 8, 128, 2048)
    bgu = f32(inp["b_gate_up"])[0]
    bguT = np.ascontiguousarray(bgu.reshape(NE, 16, 128).transpose(2, 0, 1)).reshape(128, NE * 16)
    shared = {
        "w_in": f32(inp["w_in"])[0],
        "lamv": np.ascontiguousarray(np.stack([f32(inp["lambda_q1"])[0], f32(inp["lambda_k1"])[0],
                                               f32(inp["lambda_q2"])[0], f32(inp["lambda_k2"])[0]], 0)),
        "subln_g": f32(inp["subln_g"])[0].reshape(128, 1),
        "gq": np.ascontiguousarray(f32(inp["mla_q_norm_g"])[0].reshape(2, 128).T),
        "gkv": f32(inp["mla_kv_norm_g"])[0].reshape(128, 1),
        "w_uq": f32(inp["w_uq"])[0],
        "w_ukv": f32(inp["w_ukv"])[0],
        "w_o": f32(inp["w_o"])[0],
        "lnv": np.ascontiguousarray(np.stack([f32(inp["ln1_g"])[0], f32(inp["ln1_b"])[0],
                                              f32(inp["ln2_g"])[0], f32(inp["ln2_b"])[0]], 0)),
        "w_router": f32(inp["w_router"])[0],
        "b_router": f32(inp["b_router"])[0].reshape(1, NE),
        "wgu_t": wgu_t,
        "bguT": bguT,
        "w_down": f32(inp["w_down"])[0],
        "b_down": f32(inp["b_down"])[0],
    }
    in_maps = []
    toks = []
    for c in range(NCORES):
        b, r = c // 2, c % 2
        own = [2 * j + r for j in range(16)]
        oth = [2 * j + (1 - r) for j in range(16)]
        tok = np.concatenate([np.arange(g * 128, (g + 1) * 128) for g in own + oth])
        toks.append((b, tok[:TQ]))
        xb = x[b][tok]
        m = dict(shared)
        m["xT"] = np.ascontiguousarray(xb.T)
        m["xq"] = np.ascontiguousarray(xb[:TQ])
        m["pos"] = np.ascontiguousarray(positions[b][tok].reshape(1, S))
        m["cst"] = _consts(r)
        in_maps.append(m)
    return in_maps, toks


def kernel(**inputs):
    stage = inputs.pop("_stage", "full")
    if stage not in _PROG:
        _PROG[stage] = build_program(stage)
    nc = _PROG[stage]
    in_maps, toks = _prep_inputs(inputs)
    if stage != "full":
        moe = ("w_router", "b_router", "wgu_t", "bguT", "w_down", "b_down")
        in_maps = [{k: v for k, v in m.items() if k not in moe} for m in in_maps]
    res = run_bass_kernel_spmd(nc, in_maps, core_ids=list(range(NCORES)))
    out = np.zeros((4, S, D), np.float32)
    for c in range(NCORES):
        b, tok = toks[c]
        out[b, tok] = res.results[c]["out"]
    return out
```

```python
import math
import contextlib
import numpy as np
import concourse.bass as bass
import concourse.mybir as mybir
from concourse.bass_utils import run_bass_kernel_spmd

F32 = mybir.dt.float32
BF16 = mybir.dt.bfloat16
I32 = mybir.dt.int32
ALU = mybir.AluOpType
AF = mybir.ActivationFunctionType
AX = mybir.AxisListType

NCORES = 8
D = 1024
S = 4096
TQ = 2048
NE = 32
LAM_INIT = 0.8 - 0.6 * math.exp(0.0)
DN_ALPHA = 2.0 ** 0.25
TWO_PI = 2.0 * math.pi
PI_S = math.pi * (1.0 - 1e-6)
NDMA_SEM = 8


_FENCE = {}


class Buf:
    __slots__ = ("w", "r", "excl")

    def __init__(self, excl=False):
        self.w = None
        self.r = dict(_FENCE)
        self.excl = excl


class Eng:
    def __init__(self, name, h, sem, dma_sems):
        self.name = name
        self.h = h
        self.sem = sem
        self.count = 0
        self.seen = {}
        self.dma_sems = dma_sems
        self.dma_val = [0] * len(dma_sems)
        self.rr = 0


class FW:
    def __init__(self, nc, es):
        self.nc = nc
        self.es = es
        mk = lambda n: es.enter_context(nc.semaphore(n))
        self.pe = Eng("pe", nc.tensor, mk("s_pe"), [])
        self.act = Eng("act", nc.scalar, mk("s_act"), [])
        self.dve = Eng("dve", nc.vector, mk("s_dve"), [])
        self.pool = Eng("pool", nc.gpsimd, mk("s_pool"), [mk(f"d_pool{i}") for i in range(NDMA_SEM)])
        self.sp = Eng("sp", nc.sync, mk("s_sp"), [mk(f"d_sp{i}") for i in range(NDMA_SEM)])
        self.nwait = 0

    def _wait(self, E, tok):
        sem, val = tok
        if sem is E.sem and (E is self.pe or val > E.count):
            return
        k = id(sem)
        if E.seen.get(k, 0) >= val:
            return
        E.h.wait_ge(sem, val)
        E.seen[k] = val
        self.nwait += 1

    def _deps(self, E, reads, writes):
        for b in reads:
            if b.w is not None:
                self._wait(E, b.w)
            if b.excl:
                for tok in b.r.values():
                    self._wait(E, tok)
        for b in writes:
            if b.w is not None:
                self._wait(E, b.w)
            for tok in b.r.values():
                self._wait(E, tok)

    def _mark(self, tok, reads, writes):
        for b in reads:
            b.r[id(tok[0])] = tok
        for b in writes:
            b.w = tok
            b.r = {}

    def op(self, E, reads, writes, build, inc=True):
        self._deps(E, reads, writes)
        ins = build()
        tok = (E.sem, E.count + 1)
        if inc:
            ins.then_inc(E.sem, 1)
            E.count += 1
        self._mark(tok, reads, writes)
        return ins

    def dma(self, Q, out, in_, reads, writes, **kw):
        i = Q.rr % len(Q.dma_sems)
        Q.rr += 1
        sem = Q.dma_sems[i]
        if Q.dma_val[i] > 0:
            self._wait(Q, (sem, Q.dma_val[i]))
        self._deps(Q, reads, writes)
        Q.h.dma_start(out=out, in_=in_, **kw).then_inc(sem, 16)
        Q.dma_val[i] += 16
        tok = (sem, Q.dma_val[i])
        self._mark(tok, reads, writes)
        return tok

    def fence(self):
        _FENCE.clear()
        for Q in (self.sp, self.pool):
            for sem, v in zip(Q.dma_sems, Q.dma_val):
                if v > 0:
                    _FENCE[id(sem)] = (sem, v)
        for X in (self.pe, self.act, self.dve, self.pool, self.sp):
            if X.count > 0:
                _FENCE[id(X.sem)] = (X.sem, X.count)

    def finish(self, E):
        for Q in (self.sp, self.pool):
            for sem, v in zip(Q.dma_sems, Q.dma_val):
                if v > 0:
                    self._wait(E, (sem, v))
        for X in (self.pe, self.act, self.dve, self.pool, self.sp):
            if X is not E and X.count > 0:
                self._wait(E, (X.sem, X.count))


class Rot:
    def __init__(self, items):
        self.items = items
        self.i = 0

    def get(self):
        it = self.items[self.i % len(self.items)]
        self.i += 1
        return it


def build_program(stage="full"):
    nc = bass.Bass("TRN2", target_bir_lowering=False)
    dt_in = lambda name, shape, dt=F32: nc.dram_tensor(name, shape, dt, kind="ExternalInput").ap()
    xT_d = dt_in("xT", [D, S])
    xq_d = dt_in("xq", [TQ, D])
    pos_d = dt_in("pos", [1, S], I32)
    cst_d = dt_in("cst", [128, 640])
    w_in_d = dt_in("w_in", [D, 1984])
    lam_d = dt_in("lamv", [4, 64])
    subg_d = dt_in("subln_g", [128, 1])
    gq_d = dt_in("gq", [128, 2])
    gkv_d = dt_in("gkv", [128, 1])
    w_uq_d = dt_in("w_uq", [256, 768])
    w_ukv_d = dt_in("w_ukv", [128, 1024])
    w_o_d = dt_in("w_o", [D, D])
    lnv_d = dt_in("lnv", [4, D])
    if stage == "full":
        w_r_d = dt_in("w_router", [D, NE])
        b_r_d = dt_in("b_router", [1, NE])
        wgu_d = dt_in("wgu_t", [NE, 8, 128, 2048])
        bgu_d = dt_in("bguT", [128, NE * 16])
        wd_d = dt_in("w_down", [NE, D, D])
        bd_d = dt_in("b_down", [NE, D])
    out_d = nc.dram_tensor("out", [TQ, D], F32, kind="ExternalOutput").ap()

    _FENCE.clear()
    with contextlib.ExitStack() as es:
        fw = FW(nc, es)
        PE, ACT, DVE, POOL, SP = fw.pe, fw.act, fw.dve, fw.pool, fw.sp

        def sb(name, shape, dt, st=es):
            return st.enter_context(nc.sbuf_tensor("sb_" + name, shape, dt))

        psb = [es.enter_context(nc.psum_tensor(f"ps{i}", [128, 512], F32)) for i in range(8)]
        pb = [Buf(excl=True) for _ in range(8)]

        cst = sb("cst", [128, 640], F32); bcst = Buf()
        fw.dma(SP, cst[:], cst_d[:, :], [], [bcst])
        ident = cst[:, 0:128]
        ropec = cst[:, 512:516]
        cbf = sb("cbf", [128, 512], BF16); bcbf = Buf()
        fw.op(DVE, [bcst], [bcbf], lambda: nc.vector.tensor_copy(cbf[:, 0:384], cst[:, 128:512]))
        fw.op(DVE, [], [bcbf], lambda: nc.vector.memset(cbf[:, 384:512], 1.0))
        perm_bf = cbf[:, 0:128]
        masks_bf = [cbf[:, 128:256], cbf[:, 256:384]]
        ones_bf = cbf[:, 384:512]
        ones32 = sb("ones32", [128, 128], F32); bones = Buf()
        fw.op(POOL, [], [bones], lambda: nc.gpsimd.memset(ones32[:], 1.0))
        small = sb("small", [128, 64], F32); bsmall = Buf()
        fw.dma(SP, small[:, 0:1], subg_d[:, :], [], [bsmall])
        fw.dma(SP, small[:, 3:5], gq_d[:, :], [], [bsmall])
        fw.dma(SP, small[:, 5:6], gkv_d[:, :], [], [bsmall])
        fw.op(DVE, [], [bsmall], lambda: nc.vector.memset(small[:, 8:9], 1e-6))
        fw.op(DVE, [], [bsmall], lambda: nc.vector.memset(small[:, 9:10], 1e-5))
        EPS6 = small[:, 8:9]
        EPS5 = small[:, 9:10]
        lamt = sb("lamt", [128, 256], F32); blam = Buf()
        fw.dma(SP, lamt[:].rearrange("p (a b) -> p a b", a=4), lam_d.partition_broadcast(128), [], [blam])
        fw.op(DVE, [blam], [blam], lambda: nc.vector.tensor_tensor(lamt[:, 0:64], lamt[:, 0:64], lamt[:, 64:128], op=ALU.mult))
        fw.op(DVE, [blam], [blam], lambda: nc.vector.tensor_tensor(lamt[:, 128:192], lamt[:, 128:192], lamt[:, 192:256], op=ALU.mult))
        fw.op(DVE, [blam], [bsmall], lambda: nc.vector.reduce_sum(small[:, 6:7], lamt[:, 0:64], axis=AX.X))
        fw.op(DVE, [blam], [bsmall], lambda: nc.vector.reduce_sum(small[:, 7:8], lamt[:, 128:192], axis=AX.X))
        fw.op(ACT, [bsmall], [bsmall], lambda: nc.scalar.activation(small[:, 6:8], small[:, 6:8], AF.Exp))
        fw.op(DVE, [bsmall], [bsmall], lambda: nc.vector.tensor_tensor(small[:, 2:3], small[:, 7:8], small[:, 6:7], op=ALU.subtract))
        fw.op(DVE, [bsmall], [bsmall], lambda: nc.vector.tensor_scalar(small[:, 2:3], small[:, 2:3], -LAM_INIT, None, op0=ALU.add))
        fw.op(DVE, [bsmall], [bsmall], lambda: nc.vector.tensor_scalar(small[:, 1:2], small[:, 0:1], 1.0 - LAM_INIT, None, op0=ALU.mult))

        otx = sb("otx", [128, 8, TQ], BF16)
        botx = [Buf() for _ in range(16)]

        with contextlib.ExitStack() as sa:
            cosT = sb("cosT", [128, S], F32, sa)
            sinS = sb("sinS", [128, S], F32, sa)
            btab = [Buf() for _ in range(8)]
            ckvn = sb("ckvn", [128, S], BF16, sa); bckvn = [Buf() for _ in range(8)]
            cqn = sb("cqn", [128, 2, TQ], BF16, sa); bcqn = [Buf() for _ in range(4)]
            KR = sb("KR", [128, S], BF16, sa); bKR = [Buf() for _ in range(8)]
            fw.op(POOL, [], bKR, lambda: nc.gpsimd.memset(KR[64:128, :], 0.0))
            s32 = Rot([(sb(f"s32_{i}", [128, 512], F32, sa), Buf()) for i in range(8)])
            s16 = Rot([(sb(f"s16_{i}", [128, 512], BF16, sa), Buf()) for i in range(4)])
            si32 = sb("si32", [128, 512], I32, sa); bsi32 = Buf()
            tmpb = Rot([(psb[i], pb[i]) for i in (6, 7, 0, 1)])

            def mm_acc(out_ap, pairs, reads, writes):
                n = len(pairs)
                for i, (l, r) in enumerate(pairs):
                    fw.op(PE, reads, writes,
                          lambda: nc.tensor.matmul(out_ap, l, r, start=(i == 0), stop=(i == n - 1)),
                          inc=(i == n - 1))

            def rope(src_ps, bsrc, rows, tc, dst_ap, bdst):
                cols = slice(tc * 512, (tc + 1) * 512)
                import os as _os
                _cut = int(_os.environ.get("ROPE_CUT", "9")) if rows == 128 else 9
                if _cut < 1:
                    return
                hb, bhb = s16.get()
                fw.op(ACT, [bsrc], [bhb], lambda: nc.scalar.copy(hb[0:rows, :], src_ps))
                if _cut < 2:
                    return
                sw, bsw = tmpb.get()
                fw.op(PE, [bhb, bcbf], [bsw], lambda: nc.tensor.matmul(sw[0:rows, :], perm_bf[0:rows, 0:rows], hb[0:rows, :], start=True, stop=True))
                if _cut < 3:
                    return
                t1, bt1 = s32.get()
                fw.op(DVE, [bsrc, btab[tc]], [bt1], lambda: nc.vector.tensor_tensor(t1[0:rows, :], src_ps, cosT[0:rows, cols], op=ALU.mult))
                if _cut < 4:
                    return
                t2, bt2 = s32.get()
                fw.op(DVE, [bsw, btab[tc]], [bt2], lambda: nc.vector.tensor_tensor(t2[0:rows, :], sw[0:rows, :], sinS[0:rows, cols], op=ALU.mult))
                if _cut < 5:
                    return
                fw.op(POOL, [bt1, bt2], [bdst], lambda: nc.gpsimd.tensor_tensor(dst_ap, t1[0:rows, :], t2[0:rows, :], op=ALU.add))

            def rms_scale(ps_list, bps_list, n_feat, eps, gcols, dst_aps, bdst):
                sqs = []
                for ps_ap, bps in zip(ps_list, bps_list):
                    sq, bsq = s32.get()
                    fw.op(ACT, [bps], [bsq], lambda: nc.scalar.activation(sq[:], ps_ap, AF.Square))
                    sqs.append((sq, bsq))
                ss, bss = tmpb.get()
                for i, (sq, bsq) in enumerate(sqs):
                    fw.op(PE, [bsq, bones], [bss],
                          lambda: nc.tensor.matmul(ss[:], ones32[:], sq[:], start=(i == 0), stop=(i == len(sqs) - 1)),
                          inc=(i == len(sqs) - 1))
                rstd, brstd = s32.get()
                fw.op(ACT, [bss, bsmall], [brstd], lambda: nc.scalar.activation(rstd[:], ss[:], AF.Sqrt, bias=eps, scale=1.0 / n_feat))
                fw.op(DVE, [brstd], [brstd], lambda: nc.vector.reciprocal(rstd[:], rstd[:]))
                for ps_ap, bps, gc, dst in zip(ps_list, bps_list, gcols, dst_aps):
                    fw.op(DVE, [bps, brstd, bsmall], [bdst],
                          lambda: nc.vector.scalar_tensor_tensor(dst, in0=ps_ap, scalar=gc, in1=rstd[:], op0=ALU.mult, op1=ALU.mult))

            def attention(nsub, s_emit, s_reads, Vt, bV, scale, finalize):
                S_B = [(psb[0], pb[0]), (psb[1], pb[1])]
                O_B = [(psb[2], pb[2]), (psb[3], pb[3])]
                L_B = [(psb[4], pb[4]), (psb[5], pb[5])]
                for g in range(4):
                    units = []
                    nkb = 4 * g + 4
                    for half in (0, 1):
                        for kl in range(nkb):
                            i = kl - 4 * g
                            col0 = 0 if i < 0 else i * 128
                            mt = None if i < 0 else half
                            for c in range(nsub):
                                units.append((c, half * 16 + kl, col0, mt))
                    nun = len(units)
                    first = [True] * nsub
                    last_idx = {}
                    for ui, u in enumerate(units):
                        last_idx[u[0]] = ui
                    pts = {}

                    def emit_s(ui):
                        c, kb, col0, mt = units[ui]
                        sbk, bsbk = S_B[ui % 2]
                        s_emit(c, kb, g, col0, sbk, bsbk)
                        pT, bpT = s16.get()
                        fw.op(ACT, [bsbk], [bpT], lambda: nc.scalar.activation(pT[:, col0:512], sbk[:, col0:512], AF.Exp, scale=scale))
                        if mt is not None:
                            fw.op(POOL, [bpT, bcbf], [bpT], lambda: nc.gpsimd.tensor_tensor(pT[:, col0:col0 + 128], pT[:, col0:col0 + 128], masks_bf[mt], op=ALU.mult))
                        pts[ui] = (pT, bpT)

                    def emit_pv(ui):
                        c, kb, col0, mt = units[ui]
                        pT, bpT = pts.pop(ui)
                        o, bo = O_B[c]
                        l, bl = L_B[c]
                        st = first[c]
                        first[c] = False
                        sp_ = (last_idx[c] == ui)
                        fw.op(PE, [bpT, bV[kb // 4]], [bo], lambda: nc.tensor.matmul(o[:, col0:512], Vt[:, kb, :], pT[:, col0:512], start=st, stop=sp_), inc=False)
                        fw.op(PE, [bpT, bcbf], [bl], lambda: nc.tensor.matmul(l[:, col0:512], ones_bf, pT[:, col0:512], start=st, stop=sp_))

                    LOOK = 2
                    for ui in range(min(LOOK, nun)):
                        emit_s(ui)
                    for ui in range(nun):
                        emit_pv(ui)
                        if ui + LOOK < nun:
                            emit_s(ui + LOOK)
                    finalize(g, O_B, L_B)

            with contextlib.ExitStack() as sx:
                xTb = sb("xTb", [128, 8, S], BF16, sx); bxT = [Buf() for _ in range(8)]
                xT_v = xT_d.rearrange("(dc p) t -> p dc t", p=128)
                for tc in range(8):
                    fw.dma(POOL, xTb[:, :, tc * 512:(tc + 1) * 512], xT_v[:, :, tc * 512:(tc + 1) * 512], [], [bxT[tc]])
                w_in_v = w_in_d.rearrange("(dc p) c -> p dc c", p=128)

                for tc in range(8):
                    cols = slice(tc * 512, (tc + 1) * 512)
                    fw.dma(SP, si32[:], pos_d[0:1, cols].partition_broadcast(128), [], [bsi32])
                    ang, bang = s32.get()
                    fw.op(DVE, [bsi32], [bang], lambda: nc.vector.tensor_copy(ang[:], si32[:]))
                    fw.op(DVE, [bang, bcst], [bang], lambda: nc.vector.tensor_scalar(ang[:], ang[:], ropec[:, 0:1], None, op0=ALU.mult))
                    for which in (0, 1):
                        shift = 0.5 if which == 0 else 0.75
                        u, bu = s32.get()
                        fw.op(DVE, [bang], [bu], lambda: nc.vector.tensor_scalar(u[:], ang[:], 1.0 / TWO_PI, shift, op0=ALU.mult, op1=ALU.add))
                        ki, bki = s32.get()
                        kiv = ki[:].bitcast(I32)
                        fw.op(DVE, [bu], [bki], lambda: nc.vector.tensor_copy(kiv, u[:]))
                        kf, bkf = s32.get()
                        fw.op(POOL, [bki], [bkf], lambda: nc.gpsimd.tensor_copy(kf[:], kiv))
                        fw.op(POOL, [bkf, bu], [bu], lambda: nc.gpsimd.tensor_tensor(u[:], u[:], kf[:], op=ALU.subtract))
                        fw.op(DVE, [bu], [bkf], lambda: nc.vector.scalar_tensor_tensor(kf[:], in0=u[:], scalar=0.0, in1=u[:], op0=ALU.is_lt, op1=ALU.add))
                        if which == 0:
                            fw.op(ACT, [bkf, bcst], [btab[tc]], lambda: nc.scalar.activation(sinS[:, cols], kf[:], AF.Sin, bias=ropec[:, 2:3], scale=ropec[:, 1:2]))
                        else:
                            fw.op(ACT, [bkf, bcst], [btab[tc]], lambda: nc.scalar.activation(cosT[:, cols], kf[:], AF.Sin, bias=ropec[:, 3:4], scale=TWO_PI * (1.0 - 1e-6)))

                if stage.startswith("tabtt"):
                    import os as _os
                    r0, r1 = [int(v) for v in _os.environ.get("TT_ROWS", "0,128").split(",")]
                    mode = _os.environ.get("TT_MODE", "psum_cos")
                    t1, bt1 = s32.get()
                    kps, bkps = tmpb.get()
                    fw.op(PE, [bcbf], [bkps], lambda: nc.tensor.matmul(kps[:], perm_bf, cbf[:, 0:512], start=True, stop=True))
                    if mode == "psum_cos":
                        fw.op(DVE, [bkps, btab[0]], [bt1], lambda: nc.vector.tensor_tensor(t1[r0:r1, :], kps[r0:r1, :], cosT[r0:r1, 0:512], op=ALU.mult))
                    elif mode == "sb_cos":
                        t2, bt2 = s32.get()
                        fw.op(DVE, [], [bt2], lambda: nc.vector.memset(t2[:], 1.0))
                        fw.op(DVE, [bt2, btab[0]], [bt1], lambda: nc.vector.tensor_tensor(t1[r0:r1, :], t2[r0:r1, :], cosT[r0:r1, 0:512], op=ALU.mult))
                    elif mode == "psum_sb":
                        t2, bt2 = s32.get()
                        fw.op(DVE, [], [bt2], lambda: nc.vector.memset(t2[:], 1.0))
                        fw.op(DVE, [bkps, bt2], [bt1], lambda: nc.vector.tensor_tensor(t1[r0:r1, :], kps[r0:r1, :], t2[r0:r1, :], op=ALU.mult))
                    fw.dma(SP, out_d[0:128, 0:512], t1[:], [bt1], [])
                    fw.dma(SP, out_d[128:256, 0:512], cosT[:, 0:512], [btab[0]], [])
                    fw.dma(SP, out_d[256:384, 0:512], sinS[:, 0:512], [btab[0]], [])
                    fw.finish(SP)
                    return nc
                if stage == "tab":
                    fw.finish(SP)
                    return nc
                with contextlib.ExitStack() as sm0:
                    WC = sb("WC", [128, 8, 448], BF16, sm0); bWC = Buf()
                    fw.dma(POOL, WC[:], w_in_v[:, :, 1536:1984], [], [bWC])
                    for tc in range(8):
                        cols = slice(tc * 512, (tc + 1) * 512)
                        ckv, bckv = tmpb.get()
                        mm_acc(ckv[:], [(WC[:, dc, 256:384], xTb[:, dc, cols]) for dc in range(8)], [bWC, bxT[tc]], [bckv])
                        rms_scale([ckv[:]], [bckv], 128.0, EPS6, [small[:, 5:6]], [ckvn[:, cols]], bckvn[tc])
                        kr, bkr = tmpb.get()
                        mm_acc(kr[0:64, :], [(WC[:, dc, 384:448], xTb[:, dc, cols]) for dc in range(8)], [bWC, bxT[tc]], [bkr])
                        rope(kr[0:64, :], bkr, 64, tc, KR[0:64, cols], bKR[tc])
                        if tc < 4:
                            cq0, bcq0 = tmpb.get()
                            mm_acc(cq0[:], [(WC[:, dc, 0:128], xTb[:, dc, cols]) for dc in range(8)], [bWC, bxT[tc]], [bcq0])
                            cq1, bcq1 = tmpb.get()
                            mm_acc(cq1[:], [(WC[:, dc, 128:256], xTb[:, dc, cols]) for dc in range(8)], [bWC, bxT[tc]], [bcq1])
                            rms_scale([cq0[:], cq1[:]], [bcq0, bcq1], 256.0, EPS6, [small[:, 3:4], small[:, 4:5]],
                                      [cqn[:, 0, cols], cqn[:, 1, cols]], bcqn[tc])

                if stage == "m0":
                    fw.finish(SP)
                    return nc
                fw.fence()
                with contextlib.ExitStack() as sd:
                    WQ = sb("WQ", [128, 8, 128], BF16, sd); WK = sb("WK", [128, 8, 128], BF16, sd); WV = sb("WV", [128, 8, 128], BF16, sd)
                    bW = Buf()
                    KT = sb("KT", [128, S], BF16, sd); bKT = [Buf() for _ in range(8)]
                    QT = sb("QT", [128, TQ], BF16, sd); bQT = [Buf() for _ in range(4)]
                    Vt = sb("Vt", [128, 32, 128], BF16, sd); bV = [Buf() for _ in range(8)]
                    for h in range(4):
                        fw.dma(POOL, WQ[:], w_in_v[:, :, 128 * h:128 * h + 128], [], [bW])
                        fw.dma(POOL, WK[:], w_in_v[:, :, 512 + 128 * h:512 + 128 * h + 128], [], [bW])
                        fw.dma(POOL, WV[:], w_in_v[:, :, 1024 + 128 * h:1024 + 128 * h + 128], [], [bW])
                        if stage == "dprojD":
                            fw.finish(SP)
                            return nc
                        import os as _os
                        _ntc = int(_os.environ.get("DPROJ_NTC", "8"))
                        _noq = _os.environ.get("DPROJ_NOQ", "0") == "1"
                        for tc in range(_ntc):
                            cols = slice(tc * 512, (tc + 1) * 512)
                            if stage != "dprojV":
                                kps, bkps = tmpb.get()
                                mm_acc(kps[:], [(WK[:, dc, :], xTb[:, dc, cols]) for dc in range(8)], [bW, bxT[tc]], [bkps])
                                rope(kps[:], bkps, 128, tc, KT[:, cols], bKT[tc])
                            if tc < 4 and stage != "dprojV" and not _noq:
                                qps, bqps = tmpb.get()
                                mm_acc(qps[:], [(WQ[:, dc, :], xTb[:, dc, cols]) for dc in range(8)], [bW, bxT[tc]], [bqps])
                                rope(qps[:], bqps, 128, tc, QT[:, cols], bQT[tc])
                            if stage == "dprojK":
                                continue
                            vps, bvps = tmpb.get()
                            for i in range(4):
                                mm_acc(vps[:, i * 128:(i + 1) * 128],
                                       [(xTb[:, dc, tc * 512 + i * 128: tc * 512 + (i + 1) * 128], WV[:, dc, :]) for dc in range(8)],
                                       [bW, bxT[tc]], [bvps])
                            fw.op(ACT, [bvps], [bV[tc]], lambda: nc.scalar.copy(Vt[:, tc * 4:(tc + 1) * 4, :], vps[:].rearrange("p (a b) -> p a b", a=4)))

                        if stage in ("dproj", "dprojK", "dprojV"):
                            fw.finish(SP)
                            return nc
                        def s_emit(c, kb, g, col0, sbk, bsbk):
                            fw.op(PE, [bKT[kb // 4], bQT[g]], [bsbk],
                                  lambda: nc.tensor.matmul(sbk[:, col0:512], KT[64 * c:64 * c + 64, kb * 128:(kb + 1) * 128],
                                                           QT[64 * c:64 * c + 64, g * 512 + col0:(g + 1) * 512], start=True, stop=True))

                        def fin_diff(g, O_B, L_B, h=h):
                            ds = []
                            for c in range(2):
                                rl, brl = s32.get()
                                fw.op(DVE, [L_B[c][1]], [brl], lambda: nc.vector.reciprocal(rl[:], L_B[c][0][:]))
                                fw.op(DVE, [O_B[c][1], brl], [brl], lambda: nc.vector.tensor_tensor(rl[:], O_B[c][0][:], rl[:], op=ALU.mult))
                                ds.append((rl, brl))
                            dd, bdd = s32.get()
                            fw.op(DVE, [ds[0][1], ds[1][1], bsmall], [bdd],
                                  lambda: nc.vector.scalar_tensor_tensor(dd[:], in0=ds[1][0][:], scalar=small[:, 2:3], in1=ds[0][0][:], op0=ALU.mult, op1=ALU.add))
                            sq, bsq = s32.get()
                            fw.op(ACT, [bdd], [bsq], lambda: nc.scalar.activation(sq[:], dd[:], AF.Square))
                            ss, bss = tmpb.get()
                            fw.op(PE, [bsq, bones], [bss], lambda: nc.tensor.matmul(ss[:], ones32[:], sq[:], start=True, stop=True))
                            rstd, brstd = s32.get()
                            fw.op(ACT, [bss, bsmall], [brstd], lambda: nc.scalar.activation(rstd[:], ss[:], AF.Sqrt, bias=EPS5, scale=1.0 / 128.0))
                            fw.op(DVE, [brstd], [brstd], lambda: nc.vector.reciprocal(rstd[:], rstd[:]))
                            wr = [botx[4 * g + i] for i in range(4)]
                            fw.op(DVE, [bdd, brstd, bsmall], wr,
                                  lambda: nc.vector.scalar_tensor_tensor(otx[:, h, g * 512:(g + 1) * 512], in0=dd[:], scalar=small[:, 1:2], in1=rstd[:], op0=ALU.mult, op1=ALU.mult))

                        attention(2, s_emit, None, Vt, bV, 64.0 ** -0.5, fin_diff)
                        if stage == "datt":
                            fw.finish(SP)
                            return nc
            fw.fence()
            with contextlib.ExitStack() as sm:
                wuq = sb("wuq", [128, 2, 768], BF16, sm); bwuq = Buf()
                wukv = sb("wukv", [128, 1024], BF16, sm); bwukv = Buf()
                fw.dma(POOL, wuq[:], w_uq_d.rearrange("(rc p) c -> p rc c", p=128), [], [bwuq])
                fw.dma(POOL, wukv[:], w_ukv_d[:, :], [], [bwukv])
                KTm = sb("KTm", [128, S], BF16, sm); bKTm = [Buf() for _ in range(8)]
                Vm = sb("Vm", [128, 32, 128], BF16, sm); bVm = [Buf() for _ in range(8)]
                QTn = sb("QTn", [128, TQ], BF16, sm); bQTn = [Buf() for _ in range(4)]
                QTr = sb("QTr", [128, TQ], BF16, sm); bQTr = [Buf() for _ in range(4)]
                fw.op(POOL, [], bQTr, lambda: nc.gpsimd.memset(QTr[64:128, :], 0.0))
                for h in range(4):
                    for tc in range(8):
                        cols = slice(tc * 512, (tc + 1) * 512)
                        kn, bkn = tmpb.get()
                        fw.op(PE, [bwukv, bckvn[tc]], [bkn], lambda: nc.tensor.matmul(kn[:], wukv[:, h * 256:h * 256 + 128], ckvn[:, cols], start=True, stop=True))
                        fw.op(ACT, [bkn], [bKTm[tc]], lambda: nc.scalar.copy(KTm[:, cols], kn[:]))
                        vps, bvps = tmpb.get()
                        for i in range(4):
                            fw.op(PE, [bwukv, bckvn[tc]], [bvps],
                                  lambda: nc.tensor.matmul(vps[:, i * 128:(i + 1) * 128], ckvn[:, tc * 512 + i * 128: tc * 512 + (i + 1) * 128],
                                                           wukv[:, h * 256 + 128:h * 256 + 256], start=True, stop=True), inc=(i == 3))
                        fw.op(DVE, [bvps], [bVm[tc]], lambda: nc.vector.tensor_copy(Vm[:, tc * 4:(tc + 1) * 4, :], vps[:].rearrange("p (a b) -> p a b", a=4)))
                        if tc < 4:
                            qn, bqn = tmpb.get()
                            mm_acc(qn[:], [(wuq[:, rc, h * 192:h * 192 + 128], cqn[:, rc, cols]) for rc in range(2)], [bwuq, bcqn[tc]], [bqn])
                            fw.op(ACT, [bqn], [bQTn[tc]], lambda: nc.scalar.copy(QTn[:, cols], qn[:]))
                            qr, bqr = tmpb.get()
                            mm_acc(qr[0:64, :], [(wuq[:, rc, h * 192 + 128:h * 192 + 192], cqn[:, rc, cols]) for rc in range(2)], [bwuq, bcqn[tc]], [bqr])
                            rope(qr[0:64, :], bqr, 64, tc, QTr[0:64, cols], bQTr[tc])

                    if stage == "mproj":
                        fw.finish(SP)
                        return nc
                    def s_emit_m(c, kb, g, col0, sbk, bsbk):
                        fw.op(PE, [bKTm[kb // 4], bQTn[g]], [bsbk],
                              lambda: nc.tensor.matmul(sbk[:, col0:512], KTm[:, kb * 128:(kb + 1) * 128], QTn[:, g * 512 + col0:(g + 1) * 512], start=True, stop=False), inc=False)
                        fw.op(PE, [bKR[kb // 4], bQTr[g]], [bsbk],
                              lambda: nc.tensor.matmul(sbk[:, col0:512], KR[:, kb * 128:(kb + 1) * 128], QTr[:, g * 512 + col0:(g + 1) * 512], start=False, stop=True))

                    def fin_mla(g, O_B, L_B, h=h):
                        rl, brl = s32.get()
                        fw.op(DVE, [L_B[0][1]], [brl], lambda: nc.vector.reciprocal(rl[:], L_B[0][0][:]))
                        wr = [botx[4 * g + i] for i in range(4)]
                        fw.op(DVE, [O_B[0][1], brl], wr, lambda: nc.vector.tensor_tensor(otx[:, 4 + h, g * 512:(g + 1) * 512], O_B[0][0][:], rl[:], op=ALU.mult))

                    attention(1, s_emit_m, None, Vm, bVm, 192.0 ** -0.5, fin_mla)
                    if stage == "matt":
                        fw.finish(SP)
                        return nc

        fw.fence()
        ACC = sb("ACC", [128, 16, D], F32); bACC = [Buf() for _ in range(16)]
        sm2 = sb("sm2", [128, 16, 8], F32); bsm2 = [Buf() for _ in range(16)]

        def layer_norm(z, bz, tb, lnbc, blnbc, dst, bdst, junk, bjunk):
            sc = sm2[:, tb, :]
            bs = bsm2[tb]
            fw.op(DVE, [bz], [bs], lambda: nc.vector.reduce_sum(sc[:, 0:1], z, axis=AX.X))
            fw.op(DVE, [bs], [bs], lambda: nc.vector.tensor_scalar(sc[:, 1:2], sc[:, 0:1], -1.0 / D, None, op0=ALU.mult))
            fw.op(ACT, [bz, bs], [bz], lambda: nc.scalar.activation(z, z, AF.Identity, bias=sc[:, 1:2]))
            fw.op(ACT, [bz], [bjunk], lambda: nc.scalar.activation(junk, z, AF.Square))
            fw.op(DVE, [bjunk], [bs], lambda: nc.vector.reduce_sum(sc[:, 2:3], junk, axis=AX.X))
            fw.op(ACT, [bs, bsmall], [bs], lambda: nc.scalar.activation(sc[:, 3:4], sc[:, 2:3], AF.Sqrt, bias=EPS5, scale=1.0 / D))
            fw.op(DVE, [bs], [bs], lambda: nc.vector.reciprocal(sc[:, 3:4], sc[:, 3:4]))
            fw.op(DVE, [bz, bs, blnbc], [bz], lambda: nc.vector.scalar_tensor_tensor(z, in0=z, scalar=sc[:, 3:4], in1=lnbc[:, 0, :], op0=ALU.mult, op1=ALU.mult))
            fw.op(POOL, [bz, blnbc], [bdst], lambda: nc.gpsimd.tensor_tensor(dst, z, lnbc[:, 1, :], op=ALU.add))

        with contextlib.ExitStack() as so:
            ln1bc = sb("ln1bc", [128, 2, D], F32, so); bln1 = Buf()
            fw.dma(SP, ln1bc[:], lnv_d[0:2, :].partition_broadcast(128), [], [bln1])
            wo = sb("wo", [128, 8, D], BF16, so); bwo = Buf()
            fw.dma(POOL, wo[:], w_o_d.rearrange("(hh p) o -> p hh o", p=128), [], [bwo])
            xqt = Rot([(sb(f"xqt{i}", [128, D], F32, so), Buf()) for i in range(2)])
            zt = Rot([(sb(f"zt{i}", [128, D], F32, so), Buf()) for i in range(2)])
            jk = Rot([(sb(f"jk{i}", [128, D], F32, so), Buf()) for i in range(2)])
            mixb = Rot([((psb[0], pb[0]), (psb[1], pb[1])), ((psb[2], pb[2]), (psb[3], pb[3]))])
            for tb in range(16):
                xt_, bxt_ = xqt.get()
                fw.dma(SP, xt_[:], xq_d[tb * 128:(tb + 1) * 128, :], [], [bxt_])
                banks = mixb.get()
                z, bz = zt.get()
                for half in range(2):
                    mps, bmps = banks[half]
                    for hh in range(8):
                        fw.op(PE, [botx[tb], bwo], [bmps],
                              lambda: nc.tensor.matmul(mps[:], otx[:, hh, tb * 128:(tb + 1) * 128], wo[:, hh, half * 512:(half + 1) * 512], start=(hh == 0), stop=(hh == 7)),
                              inc=(hh == 7))
                    fw.op(DVE, [bmps, bxt_], [bz],
                          lambda: nc.vector.scalar_tensor_tensor(z[:, half * 512:(half + 1) * 512], in0=xt_[:, half * 512:(half + 1) * 512], scalar=DN_ALPHA, in1=mps[:], op0=ALU.mult, op1=ALU.add))
                j_, bj_ = jk.get()
                layer_norm(z[:], bz, tb, ln1bc, bln1, ACC[:, tb, :], bACC[tb], j_[:], bj_)

        fw.fence()
        if stage == "ln1":
            for tb in range(16):
                fw.dma(SP, out_d[tb * 128:(tb + 1) * 128, :], ACC[:, tb, :], [bACC[tb]], [])
            fw.finish(SP)
            return nc

        G = sb("G", [128, 16, NE], F32); bG = [Buf() for _ in range(16)]
        bguT = sb("bguT", [128, NE * 16], F32); bbgu = Buf()
        fw.dma(SP, bguT[:], bgu_d[:, :], [], [bbgu])
        with contextlib.ExitStack() as sr:
            wr32 = sb("wr32", [128, 8, NE], F32, sr); bwr = Buf()
            fw.dma(SP, wr32[:], w_r_d.rearrange("(dc p) e -> p dc e", p=128), [], [bwr])
            brbc = sb("brbc", [128, NE], F32, sr); bbr = Buf()
            fw.dma(SP, brbc[:], b_r_d.partition_broadcast(128), [], [bbr])
            bd32 = sb("bd32", [NE, D], F32, sr); bbd = Buf()
            fw.dma(SP, bd32[:], bd_d[:, :], [], [bbd])
            GT = sb("GT", [NE, TQ], F32, sr); bGT = [Buf() for _ in range(16)]
            x1T32 = Rot([(sb(f"x1T32_{i}", [128, 8, 128], F32, sr), Buf()) for i in range(2)])
            rt = Rot([(sb(f"rt{i}", [128, 128], F32, sr), Buf()) for i in range(2)])
            tpb = Rot([((psb[0], pb[0]), (psb[1], pb[1])), ((psb[2], pb[2]), (psb[3], pb[3]))])
            tmp2 = Rot([(psb[i], pb[i]) for i in (4, 5, 6, 7)])
            for tb in range(16):
                banks = tpb.get()
                xT32, bxT32 = x1T32.get()
                for hb_ in range(2):
                    tp, btp = banks[hb_]
                    for q in range(4):
                        dc = hb_ * 4 + q
                        fw.op(PE, [bACC[tb], bcst], [btp], lambda: nc.tensor.transpose(tp[:, q * 128:(q + 1) * 128], ACC[:, tb, dc * 128:(dc + 1) * 128], ident), inc=(q == 3))
                    fw.op(ACT, [btp], [bxT32], lambda: nc.scalar.copy(xT32[:, hb_ * 4:(hb_ + 1) * 4, :], tp[:].rearrange("p (a b) -> p a b", a=4)))
                    fw.op(DVE, [btp], [botx[tb]], lambda: nc.vector.tensor_copy(otx[:, hb_ * 4:(hb_ + 1) * 4, tb * 128:(tb + 1) * 128], tp[:].rearrange("p (a b) -> p a b", a=4)))
                lgp, blgp = tmp2.get()
                for dc in range(8):
                    fw.op(PE, [bxT32, bwr], [blgp], lambda: nc.tensor.matmul(lgp[:, 0:NE], xT32[:, dc, :], wr32[:, dc, :], start=(dc == 0), stop=(dc == 7)), inc=(dc == 7))
                r_, br_ = rt.get()
                lg = r_[:, 0:32]; m8 = r_[:, 32:40]; ex = r_[:, 40:72]; mk = r_[:, 72:104]; misc = r_[:, 104:112]
                fw.op(DVE, [blgp, bbr], [br_], lambda: nc.vector.tensor_tensor(lg, lgp[:, 0:NE], brbc[:], op=ALU.add))
                fw.op(DVE, [br_], [br_], lambda: nc.vector.max(out=m8, in_=lg))
                fw.op(DVE, [br_], [br_], lambda: nc.vector.tensor_scalar(misc[:, 0:1], m8[:, 0:1], -1.0, None, op0=ALU.mult))
                fw.op(ACT, [br_], [br_], lambda: nc.scalar.activation(ex, lg, AF.Exp, bias=misc[:, 0:1]))
                fw.op(DVE, [br_], [br_], lambda: nc.vector.tensor_scalar(mk, lg, m8[:, 3:4], None, op0=ALU.is_ge))
                fw.op(DVE, [br_], [br_], lambda: nc.vector.tensor_tensor(ex, ex, mk, op=ALU.mult))
                fw.op(DVE, [br_], [br_], lambda: nc.vector.reduce_sum(misc[:, 1:2], ex, axis=AX.X))
                fw.op(DVE, [br_], [br_], lambda: nc.vector.reciprocal(misc[:, 2:3], misc[:, 1:2]))
                fw.op(DVE, [br_], [bG[tb]], lambda: nc.vector.tensor_scalar(G[:, tb, :], ex, misc[:, 2:3], None, op0=ALU.mult))
                gtp, bgtp = tmp2.get()
                fw.op(PE, [bG[tb], bcst], [bgtp], lambda: nc.tensor.transpose(gtp[0:NE, 0:128], G[:, tb, :], ident))
                fw.op(ACT, [bgtp], [bGT[tb]], lambda: nc.scalar.copy(GT[:, tb * 128:(tb + 1) * 128], gtp[0:NE, 0:128]))
                for half in range(2):
                    bdp, bbdp = tmp2.get()
                    fw.op(PE, [bGT[tb], bbd], [bbdp], lambda: nc.tensor.matmul(bdp[:], GT[:, tb * 128:(tb + 1) * 128], bd32[:, half * 512:(half + 1) * 512], start=True, stop=True))
                    fw.op(DVE, [bbdp, bACC[tb]], [bACC[tb]],
                          lambda: nc.vector.scalar_tensor_tensor(ACC[:, tb, half * 512:(half + 1) * 512], in0=ACC[:, tb, half * 512:(half + 1) * 512], scalar=DN_ALPHA, in1=bdp[:], op0=ALU.mult, op1=ALU.add))

        fw.fence()
        with contextlib.ExitStack() as se:
            stg = Rot([(sb(f"stg{i}", [128, 2048], F32, se), Buf()) for i in range(3)])
            wgr = Rot([(sb(f"wg{i}", [128, 8, 2, 128], BF16, se), Buf()) for i in range(3)])
            Wd = sb("Wd", [128, 8, D], BF16, se); bWd = [Buf() for _ in range(4)]
            ACTT = sb("ACTT", [128, 8, TQ], BF16, se); bACTT = [[Buf() for _ in range(4)] for _ in range(8)]
            sgc = Rot([(sb(f"sgc{i}", [128, 512], F32, se), Buf()) for i in range(2)])
            ssg = Rot([(sb(f"ssg{i}", [128, 512], F32, se), Buf()) for i in range(2)])
            suc = Rot([(sb(f"suc{i}", [128, 512], F32, se), Buf()) for i in range(2)])
            gub = Rot([((psb[0], pb[0]), (psb[1], pb[1])), ((psb[2], pb[2]), (psb[3], pb[3]))])
            yb = Rot([(psb[i], pb[i]) for i in (4, 5, 6, 7)])
            wd_v = wd_d.rearrange("e (fc p) o -> e p fc o", p=128)

            def load_wg(e, fc):
                st_, bst_ = stg.get()
                fw.dma(SP, st_[:], wgu_d[e, fc], [], [bst_])
                wg_, bwg_ = wgr.get()
                fw.op(ACT, [bst_], [bwg_], lambda: nc.scalar.copy(wg_[:].rearrange("p a b c -> p (a b c)"), st_[:]))
                return wg_, bwg_

            def load_wd(e, pc):
                st_, bst_ = stg.get()
                fw.dma(SP, st_[:].rearrange("p (a b) -> p a b", a=2), wd_v[e, :, 2 * pc:2 * pc + 2, :], [], [bst_])
                fw.op(ACT, [bst_], [bWd[pc]], lambda: nc.scalar.copy(Wd[:, 2 * pc:2 * pc + 2, :], st_[:].rearrange("p (a b) -> p a b", a=2)))

            slices = [(e, fc) for e in range(NE) for fc in range(8)]
            PRE = 2
            WD_SCHED = {1: 0, 3: 1, 5: 2, 6: 3}
            loaded = {}
            for k in range(min(PRE, len(slices))):
                loaded[k] = load_wg(*slices[k])
            for k, (e, fc) in enumerate(slices):
                if k + PRE < len(slices):
                    loaded[k + PRE] = load_wg(*slices[k + PRE])
                wg_, bwg_ = loaded.pop(k)
                if fc in WD_SCHED:
                    load_wd(e, WD_SCHED[fc])
                for tg in range(4):
                    tcols = slice(tg * 512, (tg + 1) * 512)
                    (gps, bgps), (ups, bups) = gub.get()
                    rd = [bwg_] + [botx[tg * 4 + i] for i in range(4)]
                    for dc in range(8):
                        fw.op(PE, rd, [bgps], lambda: nc.tensor.matmul(gps[:], wg_[:, dc, 0, :], otx[:, dc, tcols], start=(dc == 0), stop=(dc == 7)), inc=(dc == 7))
                    for dc in range(8):
                        fw.op(PE, rd, [bups], lambda: nc.tensor.matmul(ups[:], wg_[:, dc, 1, :], otx[:, dc, tcols], start=(dc == 0), stop=(dc == 7)), inc=(dc == 7))
                    gc_, bgc_ = sgc.get()
                    sg_, bsg_ = ssg.get()
                    uc_, buc_ = suc.get()
                    bg_col = bguT[:, e * 16 + fc:e * 16 + fc + 1]
                    bu_col = bguT[:, e * 16 + 8 + fc:e * 16 + 8 + fc + 1]
                    fw.op(DVE, [bgps, bbgu], [bgc_], lambda: nc.vector.tensor_scalar(gc_[:], gps[:], bg_col, 7.0, op0=ALU.add, op1=ALU.min))
                    fw.op(ACT, [bups, bbgu], [buc_], lambda: nc.scalar.activation(uc_[:], ups[:], AF.Identity, bias=bu_col))
                    fw.op(ACT, [bgc_], [bsg_], lambda: nc.scalar.activation(sg_[:], gc_[:], AF.Sigmoid, scale=1.702))
                    fw.op(DVE, [buc_], [buc_], lambda: nc.vector.tensor_scalar(uc_[:], uc_[:], 7.0, -7.0, op0=ALU.min, op1=ALU.max))
                    fw.op(POOL, [bgc_, bsg_], [bsg_], lambda: nc.gpsimd.tensor_tensor(sg_[:], sg_[:], gc_[:], op=ALU.mult))
                    fw.op(DVE, [bsg_, buc_], [bACTT[fc][tg]],
                          lambda: nc.vector.scalar_tensor_tensor(ACTT[:, fc, tcols], in0=uc_[:], scalar=1.0, in1=sg_[:], op0=ALU.add, op1=ALU.mult))
                if fc == 7:
                    for tb in range(16):
                        for half in range(2):
                            yp, byp = yb.get()
                            rd = [bACTT[f][tb // 4] for f in range(8)] + bWd
                            for f in range(8):
                                fw.op(PE, rd, [byp], lambda: nc.tensor.matmul(yp[:], ACTT[:, f, tb * 128:(tb + 1) * 128], Wd[:, f, half * 512:(half + 1) * 512], start=(f == 0), stop=(f == 7)), inc=(f == 7))
                            fw.op(DVE, [byp, bG[tb], bACC[tb]], [bACC[tb]],
                                  lambda: nc.vector.scalar_tensor_tensor(ACC[:, tb, half * 512:(half + 1) * 512], in0=yp[:], scalar=G[:, tb, e:e + 1], in1=ACC[:, tb, half * 512:(half + 1) * 512], op0=ALU.mult, op1=ALU.add))

        fw.fence()
        with contextlib.ExitStack() as sf:
            ln2bc = sb("ln2bc", [128, 2, D], F32, sf); bln2 = Buf()
            fw.dma(SP, ln2bc[:], lnv_d[2:4, :].partition_broadcast(128), [], [bln2])
            ot = Rot([(sb(f"ot{i}", [128, D], F32, sf), Buf()) for i in range(2)])
            jk2 = Rot([(sb(f"jk2{i}", [128, D], F32, sf), Buf()) for i in range(2)])
            for tb in range(16):
                o_, bo_ = ot.get()
                j_, bj_ = jk2.get()
                layer_norm(ACC[:, tb, :], bACC[tb], tb, ln2bc, bln2, o_[:], bo_, j_[:], bj_)
                fw.dma(SP, out_d[tb * 128:(tb + 1) * 128, :], o_[:], [bo_], [])
        fw.finish(SP)
    return nc


_PROG = {}


def _consts(r):
    c = np.zeros((128, 640), np.float32)
    c[:, 0:128] = np.eye(128, dtype=np.float32)
    m = np.arange(128)
    partner = np.where(m % 64 < 32, m + 32, m - 32)
    c[partner, 128 + m] = 1.0
    k = np.arange(128)[:, None]
    q = np.arange(128)[None, :]
    c[:, 256:384] = (k <= q).astype(np.float32)
    c[:, 384:512] = 1.0 if r == 1 else 0.0
    invf = 1.0 / (10000.0 ** ((np.arange(128) % 32) * 2.0 / 64.0))
    first = (np.arange(128) % 64) < 32
    c[:, 512] = invf
    sc = TWO_PI * (1.0 - 1e-6)
    c[:, 513] = np.where(first, -sc, sc)
    c[:, 514] = np.where(first, PI_S, -PI_S)
    c[:, 515] = -PI_S
    return c


def _prep_inputs(inp):
    f32 = lambda a: np.ascontiguousarray(np.asarray(a), dtype=np.float32)
    x = f32(inp["x"])
    positions = np.ascontiguousarray(np.asarray(inp["positions"]), dtype=np.int32)
    wgu = f32(inp["w_gate_up"])[0]
    wgu_t = np.ascontiguousarray(
        wgu.reshape(NE, 8, 128, 2, 8, 128).transpose(0, 4, 2, 1, 3, 5)).reshape(NE, 8, 128, 2048)
    bgu = f32(inp["b_gate_up"])[0]
    bguT = np.ascontiguousarray(bgu.reshape(NE, 16, 128).transpose(2, 0, 1)).reshape(128, NE * 16)
    shared = {
        "w_in": f32(inp["w_in"])[0],
        "lamv": np.ascontiguousarray(np.stack([f32(inp["lambda_q1"])[0], f32(inp["lambda_k1"])[0],
                                               f32(inp["lambda_q2"])[0], f32(inp["lambda_k2"])[0]], 0)),
        "subln_g": f32(inp["subln_g"])[0].reshape(128, 1),
        "gq": np.ascontiguousarray(f32(inp["mla_q_norm_g"])[0].reshape(2, 128).T),
        "gkv": f32(inp["mla_kv_norm_g"])[0].reshape(128, 1),
        "w_uq": f32(inp["w_uq"])[0],
        "w_ukv": f32(inp["w_ukv"])[0],
        "w_o": f32(inp["w_o"])[0],
        "lnv": np.ascontiguousarray(np.stack([f32(inp["ln1_g"])[0], f32(inp["ln1_b"])[0],
                                              f32(inp["ln2_g"])[0], f32(inp["ln2_b"])[0]], 0)),
        "w_router": f32(inp["w_router"])[0],
        "b_router": f32(inp["b_router"])[0].reshape(1, NE),
        "wgu_t": wgu_t,
        "bguT": bguT,
        "w_down": f32(inp["w_down"])[0],
        "b_down": f32(inp["b_down"])[0],
    }
    in_maps = []
    toks = []
    for c in range(NCORES):
        b, r = c // 2, c % 2
        own = [2 * j + r for j in range(16)]
        oth = [2 * j + (1 - r) for j in range(16)]
        tok = np.concatenate([np.arange(g * 128, (g + 1) * 128) for g in own + oth])
        toks.append((b, tok[:TQ]))
        xb = x[b][tok]
        m = dict(shared)
        m["xT"] = np.ascontiguousarray(xb.T)
        m["xq"] = np.ascontiguousarray(xb[:TQ])
        m["pos"] = np.ascontiguousarray(positions[b][tok].reshape(1, S))
        m["cst"] = _consts(r)
        in_maps.append(m)
    return in_maps, toks


def kernel(**inputs):
    stage = inputs.pop("_stage", "full")
    if stage not in _PROG:
        _PROG[stage] = build_program(stage)
    nc = _PROG[stage]
    in_maps, toks = _prep_inputs(inputs)
    if stage != "full":
        moe = ("w_router", "b_router", "wgu_t", "bguT", "w_down", "b_down")
        in_maps = [{k: v for k, v in m.items() if k not in moe} for m in in_maps]
    res = run_bass_kernel_spmd(nc, in_maps, core_ids=list(range(NCORES)))
    out = np.zeros((4, S, D), np.float32)
    for c in range(NCORES):
        b, tok = toks[c]
        out[b, tok] = res.results[c]["out"]
    return out
```

```python
import math
import contextlib
import numpy as np
import concourse.bass as bass
import concourse.mybir as mybir
from concourse.bass_utils import run_bass_kernel_spmd

F32 = mybir.dt.float32
BF16 = mybir.dt.bfloat16
I32 = mybir.dt.int32
ALU = mybir.AluOpType
AF = mybir.ActivationFunctionType
AX = mybir.AxisListType

NCORES = 8
D = 1024
S = 4096
TQ = 2048
NE = 32
LAM_INIT = 0.8 - 0.6 * math.exp(0.0)
DN_ALPHA = 2.0 ** 0.25
TWO_PI = 2.0 * math.pi
PI_S = math.pi * (1.0 - 1e-6)
NDMA_SEM = 8
CAP = 384


_FENCE = {}


class Buf:
    __slots__ = ("w", "r", "excl")

    def __init__(self, excl=False):
        self.w = None
        self.r = dict(_FENCE)
        self.excl = excl


class Eng:
    def __init__(self, name, h, sem, dma_sems):
        self.name = name
        self.h = h
        self.sem = sem
        self.count = 0
        self.seen = {}
        self.dma_sems = dma_sems
        self.dma_val = [0] * len(dma_sems)
        self.rr = 0


class FW:
    def __init__(self, nc, es):
        self.nc = nc
        self.es = es
        mk = lambda n: es.enter_context(nc.semaphore(n))
        self.pe = Eng("pe", nc.tensor, mk("s_pe"), [])
        self.act = Eng("act", nc.scalar, mk("s_act"), [])
        self.dve = Eng("dve", nc.vector, mk("s_dve"), [])
        self.pool = Eng("pool", nc.gpsimd, mk("s_pool"), [mk(f"d_pool{i}") for i in range(NDMA_SEM)])
        self.sp = Eng("sp", nc.sync, mk("s_sp"), [mk(f"d_sp{i}") for i in range(NDMA_SEM)])
        self.nwait = 0

    def _wait(self, E, tok):
        sem, val = tok
        if sem is E.sem and (E is self.pe or val > E.count):
            return
        k = id(sem)
        if E.seen.get(k, 0) >= val:
            return
        E.h.wait_ge(sem, val)
        E.seen[k] = val
        self.nwait += 1

    def _deps(self, E, reads, writes):
        for b in reads:
            if b.w is not None:
                self._wait(E, b.w)
            if b.excl:
                for tok in b.r.values():
                    self._wait(E, tok)
        for b in writes:
            if b.w is not None:
                self._wait(E, b.w)
            for tok in b.r.values():
                self._wait(E, tok)

    def _mark(self, tok, reads, writes):
        for b in reads:
            b.r[id(tok[0])] = tok
        for b in writes:
            b.w = tok
            b.r = {}

    def op(self, E, reads, writes, build, inc=True):
        self._deps(E, reads, writes)
        ins = build()
        tok = (E.sem, E.count + 1)
        if inc:
            ins.then_inc(E.sem, 1)
            E.count += 1
        self._mark(tok, reads, writes)
        return ins

    def dma(self, Q, out, in_, reads, writes, **kw):
        i = Q.rr % len(Q.dma_sems)
        Q.rr += 1
        sem = Q.dma_sems[i]
        if Q.dma_val[i] > 0:
            self._wait(Q, (sem, Q.dma_val[i]))
        self._deps(Q, reads, writes)
        Q.h.dma_start(out=out, in_=in_, **kw).then_inc(sem, 16)
        Q.dma_val[i] += 16
        tok = (sem, Q.dma_val[i])
        self._mark(tok, reads, writes)
        return tok

    def fence(self):
        _FENCE.clear()
        for Q in (self.sp, self.pool):
            for sem, v in zip(Q.dma_sems, Q.dma_val):
                if v > 0:
                    _FENCE[id(sem)] = (sem, v)
        for X in (self.pe, self.act, self.dve, self.pool, self.sp):
            if X.count > 0:
                _FENCE[id(X.sem)] = (X.sem, X.count)

    def finish(self, E):
        for Q in (self.sp, self.pool):
            for sem, v in zip(Q.dma_sems, Q.dma_val):
                if v > 0:
                    self._wait(E, (sem, v))
        for X in (self.pe, self.act, self.dve, self.pool, self.sp):
            if X is not E and X.count > 0:
                self._wait(E, (X.sem, X.count))


class Rot:
    def __init__(self, items):
        self.items = items
        self.i = 0

    def get(self):
        it = self.items[self.i % len(self.items)]
        self.i += 1
        return it


def build_program(stage="full"):
    nc = bass.Bass("TRN2", target_bir_lowering=False)
    dt_in = lambda name, shape, dt=F32: nc.dram_tensor(name, shape, dt, kind="ExternalInput").ap()
    xT_d = dt_in("xT", [D, S])
    xq_d = dt_in("xq", [TQ, D])
    pos_d = dt_in("pos", [1, S], I32)
    cst_d = dt_in("cst", [128, 1280])
    w_in_d = dt_in("w_in", [D, 1984])
    lam_d = dt_in("lamv", [4, 64])
    subg_d = dt_in("subln_g", [128, 1])
    gq_d = dt_in("gq", [128, 2])
    gkv_d = dt_in("gkv", [128, 1])
    w_uq_d = dt_in("w_uq", [256, 768])
    w_ukv_d = dt_in("w_ukv", [128, 1024])
    w_o_d = dt_in("w_o", [D, D])
    lnv_d = dt_in("lnv", [4, D])
    if stage == "full":
        w_r_d = dt_in("w_router", [D, NE])
        b_r_d = dt_in("b_router", [1, NE])
        wgu_d = dt_in("wgu_t", [NE, 8, 128, 2048])
        bgu_d = dt_in("bguT", [128, NE * 16])
        wd_d = dt_in("w_down", [NE, D, D])
        bd_d = dt_in("b_down", [NE, D])
    out_d = nc.dram_tensor("out", [TQ, D], F32, kind="ExternalOutput").ap()

    _FENCE.clear()
    with contextlib.ExitStack() as es:
        fw = FW(nc, es)
        PE, ACT, DVE, POOL, SP = fw.pe, fw.act, fw.dve, fw.pool, fw.sp

        def sb(name, shape, dt, st=es):
            return st.enter_context(nc.sbuf_tensor("sb_" + name, shape, dt))

        psb = [es.enter_context(nc.psum_tensor(f"ps{i}", [128, 512], F32)) for i in range(8)]
        pb = [Buf(excl=True) for _ in range(8)]

        cst = sb("cst", [128, 1280], F32); bcst = Buf()
        fw.dma(SP, cst[:], cst_d[:, :], [], [bcst])
        ident = cst[:, 0:128]
        ropec = cst[:, 512:516]
        cbf = sb("cbf", [128, 512], BF16); bcbf = Buf()
        fw.op(DVE, [bcst], [bcbf], lambda: nc.vector.tensor_copy(cbf[:, 0:384], cst[:, 128:512]))
        fw.op(DVE, [], [bcbf], lambda: nc.vector.memset(cbf[:, 384:512], 1.0))
        perm_bf = cbf[:, 0:128]
        masks_bf = [cbf[:, 128:256], cbf[:, 256:384]]
        ones_bf = cbf[:, 384:512]
        ones32 = sb("ones32", [128, 128], F32); bones = Buf()
        fw.op(POOL, [], [bones], lambda: nc.gpsimd.memset(ones32[:], 1.0))
        small = sb("small", [128, 64], F32); bsmall = Buf()
        fw.dma(SP, small[:, 0:1], subg_d[:, :], [], [bsmall])
        fw.dma(SP, small[:, 3:5], gq_d[:, :], [], [bsmall])
        fw.dma(SP, small[:, 5:6], gkv_d[:, :], [], [bsmall])
        fw.op(DVE, [], [bsmall], lambda: nc.vector.memset(small[:, 8:9], 1e-6))
        fw.op(DVE, [], [bsmall], lambda: nc.vector.memset(small[:, 9:10], 1e-5))
        EPS6 = small[:, 8:9]
        EPS5 = small[:, 9:10]
        lamt = sb("lamt", [128, 256], F32); blam = Buf()
        fw.dma(SP, lamt[:].rearrange("p (a b) -> p a b", a=4), lam_d.partition_broadcast(128), [], [blam])
        fw.op(DVE, [blam], [blam], lambda: nc.vector.tensor_tensor(lamt[:, 0:64], lamt[:, 0:64], lamt[:, 64:128], op=ALU.mult))
        fw.op(DVE, [blam], [blam], lambda: nc.vector.tensor_tensor(lamt[:, 128:192], lamt[:, 128:192], lamt[:, 192:256], op=ALU.mult))
        fw.op(DVE, [blam], [bsmall], lambda: nc.vector.reduce_sum(small[:, 6:7], lamt[:, 0:64], axis=AX.X))
        fw.op(DVE, [blam], [bsmall], lambda: nc.vector.reduce_sum(small[:, 7:8], lamt[:, 128:192], axis=AX.X))
        fw.op(ACT, [bsmall], [bsmall], lambda: nc.scalar.activation(small[:, 6:8], small[:, 6:8], AF.Exp))
        fw.op(DVE, [bsmall], [bsmall], lambda: nc.vector.tensor_tensor(small[:, 2:3], small[:, 7:8], small[:, 6:7], op=ALU.subtract))
        fw.op(DVE, [bsmall], [bsmall], lambda: nc.vector.tensor_scalar(small[:, 2:3], small[:, 2:3], -LAM_INIT, None, op0=ALU.add))
        fw.op(DVE, [bsmall], [bsmall], lambda: nc.vector.tensor_scalar(small[:, 1:2], small[:, 0:1], 1.0 - LAM_INIT, None, op0=ALU.mult))

        otx = sb("otx", [128, 8, TQ], BF16)
        botx = [Buf() for _ in range(16)]

        with contextlib.ExitStack() as sa:
            cosT = sb("cosT", [128, S], F32, sa)
            sinS = sb("sinS", [128, S], F32, sa)
            btab = [Buf() for _ in range(8)]
            ckvn = sb("ckvn", [128, S], BF16, sa); bckvn = [Buf() for _ in range(8)]
            cqn = sb("cqn", [128, 2, TQ], BF16, sa); bcqn = [Buf() for _ in range(4)]
            KR = sb("KR", [128, S], BF16, sa); bKR = [Buf() for _ in range(8)]
            fw.op(POOL, [], bKR, lambda: nc.gpsimd.memset(KR[64:128, :], 0.0))
            s32 = Rot([(sb(f"s32_{i}", [128, 512], F32, sa), Buf()) for i in range(8)])
            s16 = Rot([(sb(f"s16_{i}", [128, 512], BF16, sa), Buf()) for i in range(4)])
            si32 = sb("si32", [128, 512], I32, sa); bsi32 = Buf()
            tmpb = Rot([(psb[i], pb[i]) for i in (6, 7, 0, 1)])

            def mm_acc(out_ap, pairs, reads, writes):
                n = len(pairs)
                for i, (l, r) in enumerate(pairs):
                    fw.op(PE, reads, writes,
                          lambda: nc.tensor.matmul(out_ap, l, r, start=(i == 0), stop=(i == n - 1)),
                          inc=(i == n - 1))

            def rope(src_ps, bsrc, rows, tc, dst_ap, bdst):
                cols = slice(tc * 512, (tc + 1) * 512)
                import os as _os
                _cut = int(_os.environ.get("ROPE_CUT", "9")) if rows == 128 else 9
                if _cut < 1:
                    return
                hb, bhb = s16.get()
                fw.op(ACT, [bsrc], [bhb], lambda: nc.scalar.copy(hb[0:rows, :], src_ps))
                if _cut < 2:
                    return
                sw, bsw = tmpb.get()
                fw.op(PE, [bhb, bcbf], [bsw], lambda: nc.tensor.matmul(sw[0:rows, :], perm_bf[0:rows, 0:rows], hb[0:rows, :], start=True, stop=True))
                if _cut < 3:
                    return
                t1, bt1 = s32.get()
                fw.op(DVE, [bsrc, btab[tc]], [bt1], lambda: nc.vector.tensor_tensor(t1[0:rows, :], src_ps, cosT[0:rows, cols], op=ALU.mult))
                if _cut < 4:
                    return
                t2, bt2 = s32.get()
                fw.op(DVE, [bsw, btab[tc]], [bt2], lambda: nc.vector.tensor_tensor(t2[0:rows, :], sw[0:rows, :], sinS[0:rows, cols], op=ALU.mult))
                if _cut < 5:
                    return
                fw.op(POOL, [bt1, bt2], [bdst], lambda: nc.gpsimd.tensor_tensor(dst_ap, t1[0:rows, :], t2[0:rows, :], op=ALU.add))

            def rms_scale(ps_list, bps_list, n_feat, eps, gcols, dst_aps, bdst):
                sqs = []
                for ps_ap, bps in zip(ps_list, bps_list):
                    sq, bsq = s32.get()
                    fw.op(ACT, [bps], [bsq], lambda: nc.scalar.activation(sq[:], ps_ap, AF.Square))
                    sqs.append((sq, bsq))
                ss, bss = tmpb.get()
                for i, (sq, bsq) in enumerate(sqs):
                    fw.op(PE, [bsq, bones], [bss],
                          lambda: nc.tensor.matmul(ss[:], ones32[:], sq[:], start=(i == 0), stop=(i == len(sqs) - 1)),
                          inc=(i == len(sqs) - 1))
                rstd, brstd = s32.get()
                fw.op(ACT, [bss, bsmall], [brstd], lambda: nc.scalar.activation(rstd[:], ss[:], AF.Sqrt, bias=eps, scale=1.0 / n_feat))
                fw.op(DVE, [brstd], [brstd], lambda: nc.vector.reciprocal(rstd[:], rstd[:]))
                for ps_ap, bps, gc, dst in zip(ps_list, bps_list, gcols, dst_aps):
                    fw.op(DVE, [bps, brstd, bsmall], [bdst],
                          lambda: nc.vector.scalar_tensor_tensor(dst, in0=ps_ap, scalar=gc, in1=rstd[:], op0=ALU.mult, op1=ALU.mult))

            def attention(nsub, s_emit, s_reads, Vt, bV, scale, finalize):
                S_B = [(psb[0], pb[0]), (psb[1], pb[1])]
                O_B = [(psb[2], pb[2]), (psb[3], pb[3])]
                L_B = [(psb[4], pb[4]), (psb[5], pb[5])]
                for g in range(4):
                    units = []
                    nkb = 4 * g + 4
                    for half in (0, 1):
                        for kl in range(nkb):
                            i = kl - 4 * g
                            col0 = 0 if i < 0 else i * 128
                            mt = None if i < 0 else half
                            for c in range(nsub):
                                units.append((c, half * 16 + kl, col0, mt))
                    nun = len(units)
                    first = [True] * nsub
                    last_idx = {}
                    for ui, u in enumerate(units):
                        last_idx[u[0]] = ui
                    pts = {}

                    def emit_s(ui):
                        c, kb, col0, mt = units[ui]
                        sbk, bsbk = S_B[ui % 2]
                        s_emit(c, kb, g, col0, sbk, bsbk)
                        pT, bpT = s16.get()
                        fw.op(ACT, [bsbk], [bpT], lambda: nc.scalar.activation(pT[:, col0:512], sbk[:, col0:512], AF.Exp, scale=scale))
                        if mt is not None:
                            fw.op(POOL, [bpT, bcbf], [bpT], lambda: nc.gpsimd.tensor_tensor(pT[:, col0:col0 + 128], pT[:, col0:col0 + 128], masks_bf[mt], op=ALU.mult))
                        pts[ui] = (pT, bpT)

                    def emit_pv(ui):
                        c, kb, col0, mt = units[ui]
                        pT, bpT = pts.pop(ui)
                        o, bo = O_B[c]
                        l, bl = L_B[c]
                        st = first[c]
                        first[c] = False
                        sp_ = (last_idx[c] == ui)
                        fw.op(PE, [bpT, bV[kb // 4]], [bo], lambda: nc.tensor.matmul(o[:, col0:512], Vt[:, kb, :], pT[:, col0:512], start=st, stop=sp_), inc=False)
                        fw.op(PE, [bpT, bcbf], [bl], lambda: nc.tensor.matmul(l[:, col0:512], ones_bf, pT[:, col0:512], start=st, stop=sp_))

                    LOOK = 2
                    for ui in range(min(LOOK, nun)):
                        emit_s(ui)
                    for ui in range(nun):
                        emit_pv(ui)
                        if ui + LOOK < nun:
                            emit_s(ui + LOOK)
                    finalize(g, O_B, L_B)

            with contextlib.ExitStack() as sx:
                xTb = sb("xTb", [128, 8, S], BF16, sx); bxT = [Buf() for _ in range(8)]
                xT_v = xT_d.rearrange("(dc p) t -> p dc t", p=128)
                for tc in range(8):
                    fw.dma(POOL, xTb[:, :, tc * 512:(tc + 1) * 512], xT_v[:, :, tc * 512:(tc + 1) * 512], [], [bxT[tc]])
                w_in_v = w_in_d.rearrange("(dc p) c -> p dc c", p=128)

                for tc in range(8):
                    cols = slice(tc * 512, (tc + 1) * 512)
                    fw.dma(SP, si32[:], pos_d[0:1, cols].partition_broadcast(128), [], [bsi32])
                    ang, bang = s32.get()
                    fw.op(DVE, [bsi32], [bang], lambda: nc.vector.tensor_copy(ang[:], si32[:]))
                    fw.op(DVE, [bang, bcst], [bang], lambda: nc.vector.tensor_scalar(ang[:], ang[:], ropec[:, 0:1], None, op0=ALU.mult))
                    for which in (0, 1):
                        shift = 0.5 if which == 0 else 0.75
                        u, bu = s32.get()
                        fw.op(DVE, [bang], [bu], lambda: nc.vector.tensor_scalar(u[:], ang[:], 1.0 / TWO_PI, shift, op0=ALU.mult, op1=ALU.add))
                        ki, bki = s32.get()
                        kiv = ki[:].bitcast(I32)
                        fw.op(DVE, [bu], [bki], lambda: nc.vector.tensor_copy(kiv, u[:]))
                        kf, bkf = s32.get()
                        fw.op(POOL, [bki], [bkf], lambda: nc.gpsimd.tensor_copy(kf[:], kiv))
                        fw.op(POOL, [bkf, bu], [bu], lambda: nc.gpsimd.tensor_tensor(u[:], u[:], kf[:], op=ALU.subtract))
                        fw.op(DVE, [bu], [bkf], lambda: nc.vector.scalar_tensor_tensor(kf[:], in0=u[:], scalar=0.0, in1=u[:], op0=ALU.is_lt, op1=ALU.add))
                        if which == 0:
                            fw.op(ACT, [bkf, bcst], [btab[tc]], lambda: nc.scalar.activation(sinS[:, cols], kf[:], AF.Sin, bias=ropec[:, 2:3], scale=ropec[:, 1:2]))
                        else:
                            fw.op(ACT, [bkf, bcst], [btab[tc]], lambda: nc.scalar.activation(cosT[:, cols], kf[:], AF.Sin, bias=ropec[:, 3:4], scale=TWO_PI * (1.0 - 1e-6)))

                if stage.startswith("tabtt"):
                    import os as _os
                    r0, r1 = [int(v) for v in _os.environ.get("TT_ROWS", "0,128").split(",")]
                    mode = _os.environ.get("TT_MODE", "psum_cos")
                    t1, bt1 = s32.get()
                    kps, bkps = tmpb.get()
                    fw.op(PE, [bcbf], [bkps], lambda: nc.tensor.matmul(kps[:], perm_bf, cbf[:, 0:512], start=True, stop=True))
                    if mode == "psum_cos":
                        fw.op(DVE, [bkps, btab[0]], [bt1], lambda: nc.vector.tensor_tensor(t1[r0:r1, :], kps[r0:r1, :], cosT[r0:r1, 0:512], op=ALU.mult))
                    elif mode == "sb_cos":
                        t2, bt2 = s32.get()
                        fw.op(DVE, [], [bt2], lambda: nc.vector.memset(t2[:], 1.0))
                        fw.op(DVE, [bt2, btab[0]], [bt1], lambda: nc.vector.tensor_tensor(t1[r0:r1, :], t2[r0:r1, :], cosT[r0:r1, 0:512], op=ALU.mult))
                    elif mode == "psum_sb":
                        t2, bt2 = s32.get()
                        fw.op(DVE, [], [bt2], lambda: nc.vector.memset(t2[:], 1.0))
                        fw.op(DVE, [bkps, bt2], [bt1], lambda: nc.vector.tensor_tensor(t1[r0:r1, :], kps[r0:r1, :], t2[r0:r1, :], op=ALU.mult))
                    fw.dma(SP, out_d[0:128, 0:512], t1[:], [bt1], [])
                    fw.dma(SP, out_d[128:256, 0:512], cosT[:, 0:512], [btab[0]], [])
                    fw.dma(SP, out_d[256:384, 0:512], sinS[:, 0:512], [btab[0]], [])
                    fw.finish(SP)
                    return nc
                if stage == "tab":
                    fw.finish(SP)
                    return nc
                with contextlib.ExitStack() as sm0:
                    WC = sb("WC", [128, 8, 448], BF16, sm0); bWC = Buf()
                    fw.dma(POOL, WC[:], w_in_v[:, :, 1536:1984], [], [bWC])
                    for tc in range(8):
                        cols = slice(tc * 512, (tc + 1) * 512)
                        ckv, bckv = tmpb.get()
                        mm_acc(ckv[:], [(WC[:, dc, 256:384], xTb[:, dc, cols]) for dc in range(8)], [bWC, bxT[tc]], [bckv])
                        rms_scale([ckv[:]], [bckv], 128.0, EPS6, [small[:, 5:6]], [ckvn[:, cols]], bckvn[tc])
                        kr, bkr = tmpb.get()
                        mm_acc(kr[0:64, :], [(WC[:, dc, 384:448], xTb[:, dc, cols]) for dc in range(8)], [bWC, bxT[tc]], [bkr])
                        rope(kr[0:64, :], bkr, 64, tc, KR[0:64, cols], bKR[tc])
                        if tc < 4:
                            cq0, bcq0 = tmpb.get()
                            mm_acc(cq0[:], [(WC[:, dc, 0:128], xTb[:, dc, cols]) for dc in range(8)], [bWC, bxT[tc]], [bcq0])
                            cq1, bcq1 = tmpb.get()
                            mm_acc(cq1[:], [(WC[:, dc, 128:256], xTb[:, dc, cols]) for dc in range(8)], [bWC, bxT[tc]], [bcq1])
                            rms_scale([cq0[:], cq1[:]], [bcq0, bcq1], 256.0, EPS6, [small[:, 3:4], small[:, 4:5]],
                                      [cqn[:, 0, cols], cqn[:, 1, cols]], bcqn[tc])

                if stage == "m0":
                    fw.finish(SP)
                    return nc
                fw.fence()
                with contextlib.ExitStack() as sd:
                    WQ = sb("WQ", [128, 8, 128], BF16, sd); WK = sb("WK", [128, 8, 128], BF16, sd); WV = sb("WV", [128, 8, 128], BF16, sd)
                    bW = Buf()
                    KT = sb("KT", [128, S], BF16, sd); bKT = [Buf() for _ in range(8)]
                    QT = sb("QT", [128, TQ], BF16, sd); bQT = [Buf() for _ in range(4)]
                    Vt = sb("Vt", [128, 32, 128], BF16, sd); bV = [Buf() for _ in range(8)]
                    for h in range(4):
                        fw.dma(POOL, WQ[:], w_in_v[:, :, 128 * h:128 * h + 128], [], [bW])
                        fw.dma(POOL, WK[:], w_in_v[:, :, 512 + 128 * h:512 + 128 * h + 128], [], [bW])
                        fw.dma(POOL, WV[:], w_in_v[:, :, 1024 + 128 * h:1024 + 128 * h + 128], [], [bW])
                        if stage == "dprojD":
                            fw.finish(SP)
                            return nc
                        import os as _os
                        _ntc = int(_os.environ.get("DPROJ_NTC", "8"))
                        _noq = _os.environ.get("DPROJ_NOQ", "0") == "1"
                        for tc in range(_ntc):
                            cols = slice(tc * 512, (tc + 1) * 512)
                            if stage != "dprojV":
                                kps, bkps = tmpb.get()
                                mm_acc(kps[:], [(WK[:, dc, :], xTb[:, dc, cols]) for dc in range(8)], [bW, bxT[tc]], [bkps])
                                rope(kps[:], bkps, 128, tc, KT[:, cols], bKT[tc])
                            if tc < 4 and stage != "dprojV" and not _noq:
                                qps, bqps = tmpb.get()
                                mm_acc(qps[:], [(WQ[:, dc, :], xTb[:, dc, cols]) for dc in range(8)], [bW, bxT[tc]], [bqps])
                                rope(qps[:], bqps, 128, tc, QT[:, cols], bQT[tc])
                            if stage == "dprojK":
                                continue
                            vps, bvps = tmpb.get()
                            for i in range(4):
                                mm_acc(vps[:, i * 128:(i + 1) * 128],
                                       [(xTb[:, dc, tc * 512 + i * 128: tc * 512 + (i + 1) * 128], WV[:, dc, :]) for dc in range(8)],
                                       [bW, bxT[tc]], [bvps])
                            fw.op(ACT, [bvps], [bV[tc]], lambda: nc.scalar.copy(Vt[:, tc * 4:(tc + 1) * 4, :], vps[:].rearrange("p (a b) -> p a b", a=4)))

                        if stage in ("dproj", "dprojK", "dprojV"):
                            fw.finish(SP)
                            return nc
                        def s_emit(c, kb, g, col0, sbk, bsbk):
                            fw.op(PE, [bKT[kb // 4], bQT[g]], [bsbk],
                                  lambda: nc.tensor.matmul(sbk[:, col0:512], KT[64 * c:64 * c + 64, kb * 128:(kb + 1) * 128],
                                                           QT[64 * c:64 * c + 64, g * 512 + col0:(g + 1) * 512], start=True, stop=True))

                        def fin_diff(g, O_B, L_B, h=h):
                            ds = []
                            for c in range(2):
                                rl, brl = s32.get()
                                fw.op(DVE, [L_B[c][1]], [brl], lambda: nc.vector.reciprocal(rl[:], L_B[c][0][:]))
                                fw.op(DVE, [O_B[c][1], brl], [brl], lambda: nc.vector.tensor_tensor(rl[:], O_B[c][0][:], rl[:], op=ALU.mult))
                                ds.append((rl, brl))
                            dd, bdd = s32.get()
                            fw.op(DVE, [ds[0][1], ds[1][1], bsmall], [bdd],
                                  lambda: nc.vector.scalar_tensor_tensor(dd[:], in0=ds[1][0][:], scalar=small[:, 2:3], in1=ds[0][0][:], op0=ALU.mult, op1=ALU.add))
                            sq, bsq = s32.get()
                            fw.op(ACT, [bdd], [bsq], lambda: nc.scalar.activation(sq[:], dd[:], AF.Square))
                            ss, bss = tmpb.get()
                            fw.op(PE, [bsq, bones], [bss], lambda: nc.tensor.matmul(ss[:], ones32[:], sq[:], start=True, stop=True))
                            rstd, brstd = s32.get()
                            fw.op(ACT, [bss, bsmall], [brstd], lambda: nc.scalar.activation(rstd[:], ss[:], AF.Sqrt, bias=EPS5, scale=1.0 / 128.0))
                            fw.op(DVE, [brstd], [brstd], lambda: nc.vector.reciprocal(rstd[:], rstd[:]))
                            wr = [botx[4 * g + i] for i in range(4)]
                            fw.op(DVE, [bdd, brstd, bsmall], wr,
                                  lambda: nc.vector.scalar_tensor_tensor(otx[:, h, g * 512:(g + 1) * 512], in0=dd[:], scalar=small[:, 1:2], in1=rstd[:], op0=ALU.mult, op1=ALU.mult))

                        attention(2, s_emit, None, Vt, bV, 64.0 ** -0.5, fin_diff)
                        if stage == "datt":
                            fw.finish(SP)
                            return nc
            fw.fence()
            with contextlib.ExitStack() as sm:
                wuq = sb("wuq", [128, 2, 768], BF16, sm); bwuq = Buf()
                wukv = sb("wukv", [128, 1024], BF16, sm); bwukv = Buf()
                fw.dma(POOL, wuq[:], w_uq_d.rearrange("(rc p) c -> p rc c", p=128), [], [bwuq])
                fw.dma(POOL, wukv[:], w_ukv_d[:, :], [], [bwukv])
                KTm = sb("KTm", [128, S], BF16, sm); bKTm = [Buf() for _ in range(8)]
                Vm = sb("Vm", [128, 32, 128], BF16, sm); bVm = [Buf() for _ in range(8)]
                QTn = sb("QTn", [128, TQ], BF16, sm); bQTn = [Buf() for _ in range(4)]
                QTr = sb("QTr", [128, TQ], BF16, sm); bQTr = [Buf() for _ in range(4)]
                fw.op(POOL, [], bQTr, lambda: nc.gpsimd.memset(QTr[64:128, :], 0.0))
                for h in range(4):
                    for tc in range(8):
                        cols = slice(tc * 512, (tc + 1) * 512)
                        kn, bkn = tmpb.get()
                        fw.op(PE, [bwukv, bckvn[tc]], [bkn], lambda: nc.tensor.matmul(kn[:], wukv[:, h * 256:h * 256 + 128], ckvn[:, cols], start=True, stop=True))
                        fw.op(ACT, [bkn], [bKTm[tc]], lambda: nc.scalar.copy(KTm[:, cols], kn[:]))
                        vps, bvps = tmpb.get()
                        for i in range(4):
                            fw.op(PE, [bwukv, bckvn[tc]], [bvps],
                                  lambda: nc.tensor.matmul(vps[:, i * 128:(i + 1) * 128], ckvn[:, tc * 512 + i * 128: tc * 512 + (i + 1) * 128],
                                                           wukv[:, h * 256 + 128:h * 256 + 256], start=True, stop=True), inc=(i == 3))
                        fw.op(DVE, [bvps], [bVm[tc]], lambda: nc.vector.tensor_copy(Vm[:, tc * 4:(tc + 1) * 4, :], vps[:].rearrange("p (a b) -> p a b", a=4)))
                        if tc < 4:
                            qn, bqn = tmpb.get()
                            mm_acc(qn[:], [(wuq[:, rc, h * 192:h * 192 + 128], cqn[:, rc, cols]) for rc in range(2)], [bwuq, bcqn[tc]], [bqn])
                            fw.op(ACT, [bqn], [bQTn[tc]], lambda: nc.scalar.copy(QTn[:, cols], qn[:]))
                            qr, bqr = tmpb.get()
                            mm_acc(qr[0:64, :], [(wuq[:, rc, h * 192 + 128:h * 192 + 192], cqn[:, rc, cols]) for rc in range(2)], [bwuq, bcqn[tc]], [bqr])
                            rope(qr[0:64, :], bqr, 64, tc, QTr[0:64, cols], bQTr[tc])

                    if stage == "mproj":
                        fw.finish(SP)
                        return nc
                    def s_emit_m(c, kb, g, col0, sbk, bsbk):
                        fw.op(PE, [bKTm[kb // 4], bQTn[g]], [bsbk],
                              lambda: nc.tensor.matmul(sbk[:, col0:512], KTm[:, kb * 128:(kb + 1) * 128], QTn[:, g * 512 + col0:(g + 1) * 512], start=True, stop=False), inc=False)
                        fw.op(PE, [bKR[kb // 4], bQTr[g]], [bsbk],
                              lambda: nc.tensor.matmul(sbk[:, col0:512], KR[:, kb * 128:(kb + 1) * 128], QTr[:, g * 512 + col0:(g + 1) * 512], start=False, stop=True))

                    def fin_mla(g, O_B, L_B, h=h):
                        rl, brl = s32.get()
                        fw.op(DVE, [L_B[0][1]], [brl], lambda: nc.vector.reciprocal(rl[:], L_B[0][0][:]))
                        wr = [botx[4 * g + i] for i in range(4)]
                        fw.op(DVE, [O_B[0][1], brl], wr, lambda: nc.vector.tensor_tensor(otx[:, 4 + h, g * 512:(g + 1) * 512], O_B[0][0][:], rl[:], op=ALU.mult))

                    attention(1, s_emit_m, None, Vm, bVm, 192.0 ** -0.5, fin_mla)
                    if stage == "matt":
                        fw.finish(SP)
                        return nc

        fw.fence()
        ACC = sb("ACC", [128, 16, D], F32); bACC = [Buf() for _ in range(16)]
        sm2 = sb("sm2", [128, 16, 8], F32); bsm2 = [Buf() for _ in range(16)]

        def layer_norm(z, bz, tb, lnbc, blnbc, dst, bdst, junk, bjunk):
            sc = sm2[:, tb, :]
            bs = bsm2[tb]
            fw.op(DVE, [bz], [bs], lambda: nc.vector.reduce_sum(sc[:, 0:1], z, axis=AX.X))
            fw.op(DVE, [bs], [bs], lambda: nc.vector.tensor_scalar(sc[:, 1:2], sc[:, 0:1], -1.0 / D, None, op0=ALU.mult))
            fw.op(ACT, [bz, bs], [bz], lambda: nc.scalar.activation(z, z, AF.Identity, bias=sc[:, 1:2]))
            fw.op(ACT, [bz], [bjunk], lambda: nc.scalar.activation(junk, z, AF.Square))
            fw.op(DVE, [bjunk], [bs], lambda: nc.vector.reduce_sum(sc[:, 2:3], junk, axis=AX.X))
            fw.op(ACT, [bs, bsmall], [bs], lambda: nc.scalar.activation(sc[:, 3:4], sc[:, 2:3], AF.Sqrt, bias=EPS5, scale=1.0 / D))
            fw.op(DVE, [bs], [bs], lambda: nc.vector.reciprocal(sc[:, 3:4], sc[:, 3:4]))
            fw.op(DVE, [bz, bs, blnbc], [bz], lambda: nc.vector.scalar_tensor_tensor(z, in0=z, scalar=sc[:, 3:4], in1=lnbc[:, 0, :], op0=ALU.mult, op1=ALU.mult))
            fw.op(POOL, [bz, blnbc], [bdst], lambda: nc.gpsimd.tensor_tensor(dst, z, lnbc[:, 1, :], op=ALU.add))

        with contextlib.ExitStack() as so:
            ln1bc = sb("ln1bc", [128, 2, D], F32, so); bln1 = Buf()
            fw.dma(SP, ln1bc[:], lnv_d[0:2, :].partition_broadcast(128), [], [bln1])
            wo = sb("wo", [128, 8, D], BF16, so); bwo = Buf()
            fw.dma(POOL, wo[:], w_o_d.rearrange("(hh p) o -> p hh o", p=128), [], [bwo])
            xqt = Rot([(sb(f"xqt{i}", [128, D], F32, so), Buf()) for i in range(2)])
            zt = Rot([(sb(f"zt{i}", [128, D], F32, so), Buf()) for i in range(2)])
            jk = Rot([(sb(f"jk{i}", [128, D], F32, so), Buf()) for i in range(2)])
            mixb = Rot([((psb[0], pb[0]), (psb[1], pb[1])), ((psb[2], pb[2]), (psb[3], pb[3]))])
            for tb in range(16):
                xt_, bxt_ = xqt.get()
                fw.dma(SP, xt_[:], xq_d[tb * 128:(tb + 1) * 128, :], [], [bxt_])
                banks = mixb.get()
                z, bz = zt.get()
                for half in range(2):
                    mps, bmps = banks[half]
                    for hh in range(8):
                        fw.op(PE, [botx[tb], bwo], [bmps],
                              lambda: nc.tensor.matmul(mps[:], otx[:, hh, tb * 128:(tb + 1) * 128], wo[:, hh, half * 512:(half + 1) * 512], start=(hh == 0), stop=(hh == 7)),
                              inc=(hh == 7))
                    fw.op(DVE, [bmps, bxt_], [bz],
                          lambda: nc.vector.scalar_tensor_tensor(z[:, half * 512:(half + 1) * 512], in0=xt_[:, half * 512:(half + 1) * 512], scalar=DN_ALPHA, in1=mps[:], op0=ALU.mult, op1=ALU.add))
                j_, bj_ = jk.get()
                layer_norm(z[:], bz, tb, ln1bc, bln1, ACC[:, tb, :], bACC[tb], j_[:], bj_)

        fw.fence()
        if stage == "ln1":
            for tb in range(16):
                fw.dma(SP, out_d[tb * 128:(tb + 1) * 128, :], ACC[:, tb, :], [bACC[tb]], [])
            fw.finish(SP)
            return nc

        G = sb("G", [128, 16, NE], F32); bG = [Buf() for _ in range(16)]
        MK = sb("MK", [128, 16, NE], F32); bMK = [Buf() for _ in range(16)]
        posm = sb("posm", [128, 16, NE], F32); bposm = [Buf() for _ in range(16)]
        posmT = sb("posmT", [NE, TQ], F32); bposmT = [Buf() for _ in range(4)]
        X1B = otx[:].rearrange("p a b -> p (a b)").rearrange("p (t d) -> p t d", t=16)
        bguT = sb("bguT", [128, NE * 16], F32); bbgu = Buf()
        fw.dma(SP, bguT[:], bgu_d[:, :], [], [bbgu])
        with contextlib.ExitStack() as sr:
            wr32 = sb("wr32", [128, 8, NE], F32, sr); bwr = Buf()
            fw.dma(SP, wr32[:], w_r_d.rearrange("(dc p) e -> p dc e", p=128), [], [bwr])
            brbc = sb("brbc", [128, NE], F32, sr); bbr = Buf()
            fw.dma(SP, brbc[:], b_r_d.partition_broadcast(128), [], [bbr])
            bd32 = sb("bd32", [NE, D], F32, sr); bbd = Buf()
            fw.dma(SP, bd32[:], bd_d[:, :], [], [bbd])
            GT = sb("GT", [NE, TQ], F32, sr); bGT = [Buf() for _ in range(16)]
            x1T32 = Rot([(sb(f"x1T32_{i}", [128, 8, 128], F32, sr), Buf()) for i in range(2)])
            rt = Rot([(sb(f"rt{i}", [128, 128], F32, sr), Buf()) for i in range(2)])
            tpb = Rot([((psb[0], pb[0]), (psb[1], pb[1])), ((psb[2], pb[2]), (psb[3], pb[3]))])
            tmp2 = Rot([(psb[i], pb[i]) for i in (4, 5, 6, 7)])
            for tb in range(16):
                fw.op(DVE, [bACC[tb]], [botx[tb]], lambda: nc.vector.tensor_copy(X1B[:, tb, :], ACC[:, tb, :]))
                banks = tpb.get()
                xT32, bxT32 = x1T32.get()
                for hb_ in range(2):
                    tp, btp = banks[hb_]
                    for q in range(4):
                        dc = hb_ * 4 + q
                        fw.op(PE, [bACC[tb], bcst], [btp], lambda: nc.tensor.transpose(tp[:, q * 128:(q + 1) * 128], ACC[:, tb, dc * 128:(dc + 1) * 128], ident), inc=(q == 3))
                    fw.op(ACT, [btp], [bxT32], lambda: nc.scalar.copy(xT32[:, hb_ * 4:(hb_ + 1) * 4, :], tp[:].rearrange("p (a b) -> p a b", a=4)))
                lgp, blgp = tmp2.get()
                for dc in range(8):
                    fw.op(PE, [bxT32, bwr], [blgp], lambda: nc.tensor.matmul(lgp[:, 0:NE], xT32[:, dc, :], wr32[:, dc, :], start=(dc == 0), stop=(dc == 7)), inc=(dc == 7))
                r_, br_ = rt.get()
                lg = r_[:, 0:32]; m8 = r_[:, 32:40]; ex = r_[:, 40:72]; mk = r_[:, 72:104]; misc = r_[:, 104:112]
                fw.op(DVE, [blgp, bbr], [br_], lambda: nc.vector.tensor_tensor(lg, lgp[:, 0:NE], brbc[:], op=ALU.add))
                fw.op(DVE, [br_], [br_], lambda: nc.vector.max(out=m8, in_=lg))
                fw.op(DVE, [br_], [br_], lambda: nc.vector.tensor_scalar(misc[:, 0:1], m8[:, 0:1], -1.0, None, op0=ALU.mult))
                fw.op(ACT, [br_], [br_], lambda: nc.scalar.activation(ex, lg, AF.Exp, bias=misc[:, 0:1]))
                fw.op(DVE, [br_], [bMK[tb]], lambda: nc.vector.tensor_scalar(MK[:, tb, :], lg, m8[:, 3:4], None, op0=ALU.is_ge))
                fw.op(DVE, [br_, bMK[tb]], [br_], lambda: nc.vector.tensor_tensor(ex, ex, MK[:, tb, :], op=ALU.mult))
                fw.op(DVE, [br_], [br_], lambda: nc.vector.reduce_sum(misc[:, 1:2], ex, axis=AX.X))
                fw.op(DVE, [br_], [br_], lambda: nc.vector.reciprocal(misc[:, 2:3], misc[:, 1:2]))
                fw.op(DVE, [br_], [bG[tb]], lambda: nc.vector.tensor_scalar(G[:, tb, :], ex, misc[:, 2:3], None, op0=ALU.mult))
                gtp, bgtp = tmp2.get()
                fw.op(PE, [bG[tb], bcst], [bgtp], lambda: nc.tensor.transpose(gtp[0:NE, 0:128], G[:, tb, :], ident))
                fw.op(ACT, [bgtp], [bGT[tb]], lambda: nc.scalar.copy(GT[:, tb * 128:(tb + 1) * 128], gtp[0:NE, 0:128]))
                for half in range(2):
                    bdp, bbdp = tmp2.get()
                    fw.op(PE, [bGT[tb], bbd], [bbdp], lambda: nc.tensor.matmul(bdp[:], GT[:, tb * 128:(tb + 1) * 128], bd32[:, half * 512:(half + 1) * 512], start=True, stop=True))
                    fw.op(DVE, [bbdp, bACC[tb]], [bACC[tb]],
                          lambda: nc.vector.scalar_tensor_tensor(ACC[:, tb, half * 512:(half + 1) * 512], in0=ACC[:, tb, half * 512:(half + 1) * 512], scalar=DN_ALPHA, in1=bdp[:], op0=ALU.mult, op1=ALU.add))

            triu = cst[:, 1024:1152]
            for tb in range(16):
                pp, bpp = tmp2.get()
                fw.op(PE, [bMK[tb], bcst], [bpp], lambda: nc.tensor.matmul(pp[:, 0:NE], triu, MK[:, tb, :], start=True, stop=(tb == 0)), inc=(tb == 0))
                for t2_ in range(tb):
                    fw.op(PE, [bMK[t2_], bones], [bpp], lambda: nc.tensor.matmul(pp[:, 0:NE], ones32[:], MK[:, t2_, :], start=False, stop=(t2_ == tb - 1)), inc=(t2_ == tb - 1))
                fw.op(DVE, [bpp, bMK[tb]], [bposm[tb]], lambda: nc.vector.scalar_tensor_tensor(posm[:, tb, :], in0=pp[:, 0:NE], scalar=1.0, in1=MK[:, tb, :], op0=ALU.add, op1=ALU.mult))
                fw.op(DVE, [bposm[tb]], [bposm[tb]], lambda: nc.vector.tensor_scalar(posm[:, tb, :], posm[:, tb, :], -1.0, None, op0=ALU.add))
                ptp, bptp = tmp2.get()
                fw.op(PE, [bposm[tb], bcst], [bptp], lambda: nc.tensor.transpose(ptp[0:NE, 0:128], posm[:, tb, :], ident))
                fw.op(ACT, [bptp], [bposmT[tb // 4]], lambda: nc.scalar.copy(posmT[:, tb * 128:(tb + 1) * 128], ptp[0:NE, 0:128]))
        fw.fence()
        with contextlib.ExitStack() as se:
            NSB = CAP // 128
            stg = Rot([(sb(f"stg{i}", [128, 2048], F32, se), Buf()) for i in range(2)])
            wgr = Rot([(sb(f"wg{i}", [128, 8, 2, 128], BF16, se), Buf()) for i in range(3)])
            Wd = sb("Wd", [128, 8, D], BF16, se); bWd = [Buf() for _ in range(4)]
            xgT = sb("xgT", [128, 8, CAP], BF16, se); bxg = [Buf() for _ in range(8)]
            ACTT = sb("ACTT", [128, 8, CAP], BF16, se); bACTT = [Buf() for _ in range(8)]
            Sel = sb("Sel", [128, 16, CAP], BF16, se); bSel = [Buf() for _ in range(16)]
            SelT = Rot([(sb(f"SelT{i}", [128, NSB, 512], BF16, se), Buf()) for i in range(2)])
            yb = sb("yb", [128, NSB, D], BF16, se); byb = [Buf() for _ in range(NSB)]
            gc_ = sb("gc", [128, CAP], F32, se); bgc_ = Buf()
            sg_ = sb("sg", [128, CAP], F32, se); bsg_ = Buf()
            uc_ = sb("uc", [128, CAP], F32, se); buc_ = Buf()
            le = sb("le", [NE, 128], F32, se); ble = Buf()
            gab = Rot([(psb[i], pb[i]) for i in (0, 1)])
            gub = Rot([((psb[2], pb[2]), (psb[3], pb[3])), ((psb[4], pb[4]), (psb[5], pb[5]))])
            yb_b = Rot([(psb[i], pb[i]) for i in (6, 7)])
            wd_v = wd_d.rearrange("e (fc p) o -> e p fc o", p=128)
            iotaC = cst[:, 640:640 + CAP]
            kiota = cst[0:NE, 1152:1280]

            def load_wg(e, fc):
                st_, bst_ = stg.get()
                fw.dma(SP, st_[:], wgu_d[e, fc], [], [bst_])
                wg_, bwg_ = wgr.get()
                fw.op(ACT, [bst_], [bwg_], lambda: nc.scalar.copy(wg_[:].rearrange("p a b c -> p (a b c)"), st_[:]))
                return wg_, bwg_

            def load_wd(e, pc):
                st_, bst_ = stg.get()
                fw.dma(SP, st_[:].rearrange("p (a b) -> p a b", a=2), wd_v[e, :, 2 * pc:2 * pc + 2, :], [], [bst_])
                fw.op(ACT, [bst_], [bWd[pc]], lambda: nc.scalar.copy(Wd[:, 2 * pc:2 * pc + 2, :], st_[:].rearrange("p (a b) -> p a b", a=2)))

            slices = [(e, fc) for e in range(NE) for fc in range(8)]
            PRE = 2
            WD_SCHED = {1: 0, 3: 1, 5: 2, 6: 3}
            loaded = {}
            for k in range(min(PRE, len(slices))):
                loaded[k] = load_wg(*slices[k])
            for k, (e, fc) in enumerate(slices):
                if fc == 0:
                    for tb in range(16):
                        fw.op(DVE, [bposm[tb], bcst], [bSel[tb]], lambda: nc.vector.tensor_scalar(Sel[:, tb, :], iotaC, posm[:, tb, e:e + 1], None, op0=ALU.is_equal))
                    for dc in range(8):
                        gp, bgp = gab.get()
                        for tb in range(16):
                            fw.op(PE, [botx[tb], bSel[tb]], [bgp], lambda: nc.tensor.matmul(gp[:, 0:CAP], X1B[:, tb, dc * 128:(dc + 1) * 128], Sel[:, tb, :], start=(tb == 0), stop=(tb == 15)), inc=(tb == 15))
                        fw.op(ACT, [bgp], [bxg[dc]], lambda: nc.scalar.copy(xgT[:, dc, :], gp[:, 0:CAP]))
                if k + PRE < len(slices):
                    loaded[k + PRE] = load_wg(*slices[k + PRE])
                wg_, bwg_ = loaded.pop(k)
                if fc in WD_SCHED:
                    load_wd(e, WD_SCHED[fc])
                (gps, bgps), (ups, bups) = gub.get()
                rd = [bwg_] + bxg
                for dc in range(8):
                    fw.op(PE, rd, [bgps], lambda: nc.tensor.matmul(gps[:, 0:CAP], wg_[:, dc, 0, :], xgT[:, dc, :], start=(dc == 0), stop=(dc == 7)), inc=(dc == 7))
                for dc in range(8):
                    fw.op(PE, rd, [bups], lambda: nc.tensor.matmul(ups[:, 0:CAP], wg_[:, dc, 1, :], xgT[:, dc, :], start=(dc == 0), stop=(dc == 7)), inc=(dc == 7))
                bg_col = bguT[:, e * 16 + fc:e * 16 + fc + 1]
                bu_col = bguT[:, e * 16 + 8 + fc:e * 16 + 8 + fc + 1]
                fw.op(DVE, [bgps, bbgu], [bgc_], lambda: nc.vector.tensor_scalar(gc_[:], gps[:, 0:CAP], bg_col, 7.0, op0=ALU.add, op1=ALU.min))
                fw.op(ACT, [bups, bbgu], [buc_], lambda: nc.scalar.activation(uc_[:], ups[:, 0:CAP], AF.Identity, bias=bu_col))
                fw.op(ACT, [bgc_], [bsg_], lambda: nc.scalar.activation(sg_[:], gc_[:], AF.Sigmoid, scale=1.702))
                fw.op(DVE, [buc_], [buc_], lambda: nc.vector.tensor_scalar(uc_[:], uc_[:], 7.0, -7.0, op0=ALU.min, op1=ALU.max))
                fw.op(POOL, [bgc_, bsg_], [bsg_], lambda: nc.gpsimd.tensor_tensor(sg_[:], sg_[:], gc_[:], op=ALU.mult))
                fw.op(DVE, [bsg_, buc_], [bACTT[fc]],
                      lambda: nc.vector.scalar_tensor_tensor(ACTT[:, fc, :], in0=uc_[:], scalar=1.0, in1=sg_[:], op0=ALU.add, op1=ALU.mult))
                if fc == 7:
                    for sbk in range(NSB):
                        for half in range(2):
                            yp, byp = yb_b.get()
                            rd2 = bACTT + bWd
                            for f in range(8):
                                fw.op(PE, rd2, [byp], lambda: nc.tensor.matmul(yp[:], ACTT[:, f, sbk * 128:(sbk + 1) * 128], Wd[:, f, half * 512:(half + 1) * 512], start=(f == 0), stop=(f == 7)), inc=(f == 7))
                            fw.op(ACT, [byp], [byb[sbk]], lambda: nc.scalar.copy(yb[:, sbk, half * 512:(half + 1) * 512], yp[:]))
                    fw.op(DVE, [bcst], [ble], lambda: nc.vector.tensor_scalar(le[:], kiota, float(e), None, op0=ALU.is_equal))
                    for ch in range(4):
                        bc, bbc = gab.get()
                        fw.op(PE, [ble, bposmT[ch]], [bbc], lambda: nc.tensor.matmul(bc[:], le[:], posmT[:, ch * 512:(ch + 1) * 512], start=True, stop=True))
                        st_, bst2 = SelT.get()
                        for sbk in range(NSB):
                            fw.op(DVE, [bbc, bcst], [bst2], lambda: nc.vector.tensor_scalar(st_[:, sbk, :], bc[:], cst[:, 516 + sbk:517 + sbk], None, op0=ALU.is_equal))
                        for tb4 in range(4):
                            tb = ch * 4 + tb4
                            for half in range(2):
                                yp, byp = yb_b.get()
                                for sbk in range(NSB):
                                    fw.op(PE, [bst2] + byb, [byp], lambda: nc.tensor.matmul(yp[:], st_[:, sbk, tb4 * 128:(tb4 + 1) * 128], yb[:, sbk, half * 512:(half + 1) * 512], start=(sbk == 0), stop=(sbk == NSB - 1)), inc=(sbk == NSB - 1))
                                fw.op(DVE, [byp, bG[tb], bACC[tb]], [bACC[tb]],
                                      lambda: nc.vector.scalar_tensor_tensor(ACC[:, tb, half * 512:(half + 1) * 512], in0=yp[:], scalar=G[:, tb, e:e + 1], in1=ACC[:, tb, half * 512:(half + 1) * 512], op0=ALU.mult, op1=ALU.add))

        fw.fence()
        with contextlib.ExitStack() as sf:
            ln2bc = sb("ln2bc", [128, 2, D], F32, sf); bln2 = Buf()
            fw.dma(SP, ln2bc[:], lnv_d[2:4, :].partition_broadcast(128), [], [bln2])
            ot = Rot([(sb(f"ot{i}", [128, D], F32, sf), Buf()) for i in range(2)])
            jk2 = Rot([(sb(f"jk2{i}", [128, D], F32, sf), Buf()) for i in range(2)])
            for tb in range(16):
                o_, bo_ = ot.get()
                j_, bj_ = jk2.get()
                layer_norm(ACC[:, tb, :], bACC[tb], tb, ln2bc, bln2, o_[:], bo_, j_[:], bj_)
                fw.dma(SP, out_d[tb * 128:(tb + 1) * 128, :], o_[:], [bo_], [])
        fw.finish(SP)
    return nc


_PROG = {}


def _consts(r):
    c = np.zeros((128, 1280), np.float32)
    c[:, 0:128] = np.eye(128, dtype=np.float32)
    m = np.arange(128)
    partner = np.where(m % 64 < 32, m + 32, m - 32)
    c[partner, 128 + m] = 1.0
    k = np.arange(128)[:, None]
    q = np.arange(128)[None, :]
    c[:, 256:384] = (k <= q).astype(np.float32)
    c[:, 384:512] = 1.0 if r == 1 else 0.0
    invf = 1.0 / (10000.0 ** ((np.arange(128) % 32) * 2.0 / 64.0))
    first = (np.arange(128) % 64) < 32
    c[:, 512] = invf
    sc = TWO_PI * (1.0 - 1e-6)
    c[:, 513] = np.where(first, -sc, sc)
    c[:, 514] = np.where(first, PI_S, -PI_S)
    c[:, 515] = -PI_S
    p = np.arange(128, dtype=np.float32)
    c[:, 516] = p
    c[:, 517] = p + 128.0
    c[:, 518] = p + 256.0
    c[:, 640:1024] = np.arange(CAP, dtype=np.float32)[None, :]
    c[:, 1024:1152] = (p[:, None] < p[None, :]).astype(np.float32)
    c[:, 1152:1280] = p[:, None]
    return c


def _prep_inputs(inp):
    f32 = lambda a: np.ascontiguousarray(np.asarray(a), dtype=np.float32)
    x = f32(inp["x"])
    positions = np.ascontiguousarray(np.asarray(inp["positions"]), dtype=np.int32)
    wgu = f32(inp["w_gate_up"])[0]
    wgu_t = np.ascontiguousarray(
        wgu.reshape(NE, 8, 128, 2, 8, 128).transpose(0, 4, 2, 1, 3, 5)).reshape(NE, 8, 128, 2048)
    bgu = f32(inp["b_gate_up"])[0]
    bguT = np.ascontiguousarray(bgu.reshape(NE, 16, 128).transpose(2, 0, 1)).reshape(128, NE * 16)
    shared = {
        "w_in": f32(inp["w_in"])[0],
        "lamv": np.ascontiguousarray(np.stack([f32(inp["lambda_q1"])[0], f32(inp["lambda_k1"])[0],
                                               f32(inp["lambda_q2"])[0], f32(inp["lambda_k2"])[0]], 0)),
        "subln_g": f32(inp["subln_g"])[0].reshape(128, 1),
        "gq": np.ascontiguousarray(f32(inp["mla_q_norm_g"])[0].reshape(2, 128).T),
        "gkv": f32(inp["mla_kv_norm_g"])[0].reshape(128, 1),
        "w_uq": f32(inp["w_uq"])[0],
        "w_ukv": f32(inp["w_ukv"])[0],
        "w_o": f32(inp["w_o"])[0],
        "lnv": np.ascontiguousarray(np.stack([f32(inp["ln1_g"])[0], f32(inp["ln1_b"])[0],
                                              f32(inp["ln2_g"])[0], f32(inp["ln2_b"])[0]], 0)),
        "w_router": f32(inp["w_router"])[0],
        "b_router": f32(inp["b_router"])[0].reshape(1, NE),
        "wgu_t": wgu_t,
        "bguT": bguT,
        "w_down": f32(inp["w_down"])[0],
        "b_down": f32(inp["b_down"])[0],
    }
    in_maps = []
    toks = []
    for c in range(NCORES):
        b, r = c // 2, c % 2
        own = [2 * j + r for j in range(16)]
        oth = [2 * j + (1 - r) for j in range(16)]
        tok = np.concatenate([np.arange(g * 128, (g + 1) * 128) for g in own + oth])
        toks.append((b, tok[:TQ]))
        xb = x[b][tok]
        m = dict(shared)
        m["xT"] = np.ascontiguousarray(xb.T)
        m["xq"] = np.ascontiguousarray(xb[:TQ])
        m["pos"] = np.ascontiguousarray(positions[b][tok].reshape(1, S))
        m["cst"] = _consts(r)
        in_maps.append(m)
    return in_maps, toks


def kernel(**inputs):
    stage = inputs.pop("_stage", "full")
    if stage not in _PROG:
        _PROG[stage] = build_program(stage)
    nc = _PROG[stage]
    in_maps, toks = _prep_inputs(inputs)
    if stage != "full":
        moe = ("w_router", "b_router", "wgu_t", "bguT", "w_down", "b_down")
        in_maps = [{k: v for k, v in m.items() if k not in moe} for m in in_maps]
    res = run_bass_kernel_spmd(nc, in_maps, core_ids=list(range(NCORES)))
    out = np.zeros((4, S, D), np.float32)
    for c in range(NCORES):
        b, tok = toks[c]
        out[b, tok] = res.results[c]["out"]
    return out
```

```python
import math
import contextlib
import numpy as np
import concourse.bass as bass
import concourse.mybir as mybir
from concourse.bass_utils import run_bass_kernel_spmd

F32 = mybir.dt.float32
BF16 = mybir.dt.bfloat16
I32 = mybir.dt.int32
ALU = mybir.AluOpType
AF = mybir.ActivationFunctionType
AX = mybir.AxisListType

NCORES = 8
D = 1024
S = 4096
TQ = 2048
NE = 32
LAM_INIT = 0.8 - 0.6 * math.exp(0.0)
DN_ALPHA = 2.0 ** 0.25
TWO_PI = 2.0 * math.pi
PI_S = math.pi * (1.0 - 1e-6)
NDMA_SEM = 8
CAP = 384


_FENCE = {}


class Buf:
    __slots__ = ("w", "r", "excl")

    def __init__(self, excl=False):
        self.w = None
        self.r = dict(_FENCE)
        self.excl = excl


class Eng:
    def __init__(self, name, h, sem, dma_sems):
        self.name = name
        self.h = h
        self.sem = sem
        self.count = 0
        self.seen = {}
        self.dma_sems = dma_sems
        self.dma_val = [0] * len(dma_sems)
        self.rr = 0


class FW:
    def __init__(self, nc, es):
        self.nc = nc
        self.es = es
        mk = lambda n: es.enter_context(nc.semaphore(n))
        self.pe = Eng("pe", nc.tensor, mk("s_pe"), [])
        self.act = Eng("act", nc.scalar, mk("s_act"), [])
        self.dve = Eng("dve", nc.vector, mk("s_dve"), [])
        self.pool = Eng("pool", nc.gpsimd, mk("s_pool"), [mk(f"d_pool{i}") for i in range(NDMA_SEM)])
        self.sp = Eng("sp", nc.sync, mk("s_sp"), [mk(f"d_sp{i}") for i in range(NDMA_SEM)])
        self.nwait = 0

    def _wait(self, E, tok):
        sem, val = tok
        if sem is E.sem and (E is self.pe or val > E.count):
            return
        k = id(sem)
        if E.seen.get(k, 0) >= val:
            return
        E.h.wait_ge(sem, val)
        E.seen[k] = val
        self.nwait += 1

    def _deps(self, E, reads, writes):
        for b in reads:
            if b.w is not None:
                self._wait(E, b.w)
            if b.excl:
                for tok in b.r.values():
                    self._wait(E, tok)
        for b in writes:
            if b.w is not None:
                self._wait(E, b.w)
            for tok in b.r.values():
                self._wait(E, tok)

    def _mark(self, tok, reads, writes):
        for b in reads:
            b.r[id(tok[0])] = tok
        for b in writes:
            b.w = tok
            b.r = {}

    def op(self, E, reads, writes, build, inc=True):
        self._deps(E, reads, writes)
        ins = build()
        tok = (E.sem, E.count + 1)
        if inc:
            ins.then_inc(E.sem, 1)
            E.count += 1
        self._mark(tok, reads, writes)
        return ins

    def dma(self, Q, out, in_, reads, writes, **kw):
        i = Q.rr % len(Q.dma_sems)
        Q.rr += 1
        sem = Q.dma_sems[i]
        if Q.dma_val[i] > 0:
            self._wait(Q, (sem, Q.dma_val[i]))
        self._deps(Q, reads, writes)
        Q.h.dma_start(out=out, in_=in_, **kw).then_inc(sem, 16)
        Q.dma_val[i] += 16
        tok = (sem, Q.dma_val[i])
        self._mark(tok, reads, writes)
        return tok

    def fence(self):
        _FENCE.clear()
        for Q in (self.sp, self.pool):
            for sem, v in zip(Q.dma_sems, Q.dma_val):
                if v > 0:
                    _FENCE[id(sem)] = (sem, v)
        for X in (self.pe, self.act, self.dve, self.pool, self.sp):
            if X.count > 0:
                _FENCE[id(X.sem)] = (X.sem, X.count)

    def finish(self, E):
        for Q in (self.sp, self.pool):
            for sem, v in zip(Q.dma_sems, Q.dma_val):
                if v > 0:
                    self._wait(E, (sem, v))
        for X in (self.pe, self.act, self.dve, self.pool, self.sp):
            if X is not E and X.count > 0:
                self._wait(E, (X.sem, X.count))


class Rot:
    def __init__(self, items):
        self.items = items
        self.i = 0

    def get(self):
        it = self.items[self.i % len(self.items)]
        self.i += 1
        return it


def build_program(stage="full"):
    nc = bass.Bass("TRN2", target_bir_lowering=False)
    dt_in = lambda name, shape, dt=F32: nc.dram_tensor(name, shape, dt, kind="ExternalInput").ap()
    xT_d = dt_in("xT", [D, S])
    xq_d = dt_in("xq", [TQ, D])
    pos_d = dt_in("pos", [1, S], I32)
    cst_d = dt_in("cst", [128, 1280])
    w_in_d = dt_in("w_in", [D, 1984])
    lam_d = dt_in("lamv", [4, 64])
    subg_d = dt_in("subln_g", [128, 1])
    gq_d = dt_in("gq", [128, 2])
    gkv_d = dt_in("gkv", [128, 1])
    w_uq_d = dt_in("w_uq", [256, 768])
    w_ukv_d = dt_in("w_ukv", [128, 1024])
    w_o_d = dt_in("w_o", [D, D])
    lnv_d = dt_in("lnv", [4, D])
    if stage == "full":
        w_r_d = dt_in("w_router", [D, NE])
        b_r_d = dt_in("b_router", [1, NE])
        wgu_d = dt_in("wgu_t", [NE, 8, 128, 2048])
        bgu_d = dt_in("bguT", [128, NE * 16])
        wd_d = dt_in("w_down", [NE, D, D])
        bd_d = dt_in("b_down", [NE, D])
    out_d = nc.dram_tensor("out", [TQ, D], F32, kind="ExternalOutput").ap()

    _FENCE.clear()
    with contextlib.ExitStack() as es:
        fw = FW(nc, es)
        PE, ACT, DVE, POOL, SP = fw.pe, fw.act, fw.dve, fw.pool, fw.sp

        def sb(name, shape, dt, st=es):
            return st.enter_context(nc.sbuf_tensor("sb_" + name, shape, dt))

        psb = [es.enter_context(nc.psum_tensor(f"ps{i}", [128, 512], F32)) for i in range(8)]
        pb = [Buf(excl=True) for _ in range(8)]

        cst = sb("cst", [128, 1280], F32); bcst = Buf()
        fw.dma(SP, cst[:], cst_d[:, :], [], [bcst])
        ident = cst[:, 0:128]
        ropec = cst[:, 512:516]
        cbf = sb("cbf", [128, 512], BF16); bcbf = Buf()
        fw.op(DVE, [bcst], [bcbf], lambda: nc.vector.tensor_copy(cbf[:, 0:384], cst[:, 128:512]))
        fw.op(DVE, [], [bcbf], lambda: nc.vector.memset(cbf[:, 384:512], 1.0))
        perm_bf = cbf[:, 0:128]
        masks_bf = [cbf[:, 128:256], cbf[:, 256:384]]
        ones_bf = cbf[:, 384:512]
        ones32 = sb("ones32", [128, 128], F32); bones = Buf()
        fw.op(POOL, [], [bones], lambda: nc.gpsimd.memset(ones32[:], 1.0))
        small = sb("small", [128, 64], F32); bsmall = Buf()
        fw.dma(SP, small[:, 0:1], subg_d[:, :], [], [bsmall])
        fw.dma(SP, small[:, 3:5], gq_d[:, :], [], [bsmall])
        fw.dma(SP, small[:, 5:6], gkv_d[:, :], [], [bsmall])
        fw.op(DVE, [], [bsmall], lambda: nc.vector.memset(small[:, 8:9], 1e-6))
        fw.op(DVE, [], [bsmall], lambda: nc.vector.memset(small[:, 9:10], 1e-5))
        EPS6 = small[:, 8:9]
        EPS5 = small[:, 9:10]
        lamt = sb("lamt", [128, 256], F32); blam = Buf()
        fw.dma(SP, lamt[:].rearrange("p (a b) -> p a b", a=4), lam_d.partition_broadcast(128), [], [blam])
        fw.op(DVE, [blam], [blam], lambda: nc.vector.tensor_tensor(lamt[:, 0:64], lamt[:, 0:64], lamt[:, 64:128], op=ALU.mult))
        fw.op(DVE, [blam], [blam], lambda: nc.vector.tensor_tensor(lamt[:, 128:192], lamt[:, 128:192], lamt[:, 192:256], op=ALU.mult))
        fw.op(DVE, [blam], [bsmall], lambda: nc.vector.reduce_sum(small[:, 6:7], lamt[:, 0:64], axis=AX.X))
        fw.op(DVE, [blam], [bsmall], lambda: nc.vector.reduce_sum(small[:, 7:8], lamt[:, 128:192], axis=AX.X))
        fw.op(ACT, [bsmall], [bsmall], lambda: nc.scalar.activation(small[:, 6:8], small[:, 6:8], AF.Exp))
        fw.op(DVE, [bsmall], [bsmall], lambda: nc.vector.tensor_tensor(small[:, 2:3], small[:, 7:8], small[:, 6:7], op=ALU.subtract))
        fw.op(DVE, [bsmall], [bsmall], lambda: nc.vector.tensor_scalar(small[:, 2:3], small[:, 2:3], -LAM_INIT, None, op0=ALU.add))
        fw.op(DVE, [bsmall], [bsmall], lambda: nc.vector.tensor_scalar(small[:, 1:2], small[:, 0:1], 1.0 - LAM_INIT, None, op0=ALU.mult))

        otx = sb("otx", [128, 8, TQ], BF16)
        botx = [Buf() for _ in range(16)]

        with contextlib.ExitStack() as sa:
            cosT = sb("cosT", [128, S], F32, sa)
            sinS = sb("sinS", [128, S], F32, sa)
            btab = [Buf() for _ in range(8)]
            ckvn = sb("ckvn", [128, S], BF16, sa); bckvn = [Buf() for _ in range(8)]
            cqn = sb("cqn", [128, 2, TQ], BF16, sa); bcqn = [Buf() for _ in range(4)]
            KR = sb("KR", [128, S], BF16, sa); bKR = [Buf() for _ in range(8)]
            fw.op(POOL, [], bKR, lambda: nc.gpsimd.memset(KR[64:128, :], 0.0))
            s32 = Rot([(sb(f"s32_{i}", [128, 512], F32, sa), Buf()) for i in range(8)])
            s16 = Rot([(sb(f"s16_{i}", [128, 512], BF16, sa), Buf()) for i in range(4)])
            si32 = sb("si32", [128, 512], I32, sa); bsi32 = Buf()
            tmpb = Rot([(psb[i], pb[i]) for i in (6, 7, 0, 1)])

            def mm_acc(out_ap, pairs, reads, writes):
                n = len(pairs)
                for i, (l, r) in enumerate(pairs):
                    fw.op(PE, reads, writes,
                          lambda: nc.tensor.matmul(out_ap, l, r, start=(i == 0), stop=(i == n - 1)),
                          inc=(i == n - 1))

            def rope(src_ps, bsrc, rows, tc, dst_ap, bdst):
                cols = slice(tc * 512, (tc + 1) * 512)
                import os as _os
                _cut = int(_os.environ.get("ROPE_CUT", "9")) if rows == 128 else 9
                if _cut < 1:
                    return
                hb, bhb = s16.get()
                fw.op(ACT, [bsrc], [bhb], lambda: nc.scalar.copy(hb[0:rows, :], src_ps))
                if _cut < 2:
                    return
                sw, bsw = tmpb.get()
                fw.op(PE, [bhb, bcbf], [bsw], lambda: nc.tensor.matmul(sw[0:rows, :], perm_bf[0:rows, 0:rows], hb[0:rows, :], start=True, stop=True))
                if _cut < 3:
                    return
                t1, bt1 = s32.get()
                fw.op(DVE, [bsrc, btab[tc]], [bt1], lambda: nc.vector.tensor_tensor(t1[0:rows, :], src_ps, cosT[0:rows, cols], op=ALU.mult))
                if _cut < 4:
                    return
                t2, bt2 = s32.get()
                fw.op(DVE, [bsw, btab[tc]], [bt2], lambda: nc.vector.tensor_tensor(t2[0:rows, :], sw[0:rows, :], sinS[0:rows, cols], op=ALU.mult))
                if _cut < 5:
                    return
                fw.op(POOL, [bt1, bt2], [bdst], lambda: nc.gpsimd.tensor_tensor(dst_ap, t1[0:rows, :], t2[0:rows, :], op=ALU.add))

            def rms_scale(ps_list, bps_list, n_feat, eps, gcols, dst_aps, bdst):
                sqs = []
                for ps_ap, bps in zip(ps_list, bps_list):
                    sq, bsq = s32.get()
                    fw.op(ACT, [bps], [bsq], lambda: nc.scalar.activation(sq[:], ps_ap, AF.Square))
                    sqs.append((sq, bsq))
                ss, bss = tmpb.get()
                for i, (sq, bsq) in enumerate(sqs):
                    fw.op(PE, [bsq, bones], [bss],
                          lambda: nc.tensor.matmul(ss[:], ones32[:], sq[:], start=(i == 0), stop=(i == len(sqs) - 1)),
                          inc=(i == len(sqs) - 1))
                rstd, brstd = s32.get()
                fw.op(ACT, [bss, bsmall], [brstd], lambda: nc.scalar.activation(rstd[:], ss[:], AF.Sqrt, bias=eps, scale=1.0 / n_feat))
                fw.op(DVE, [brstd], [brstd], lambda: nc.vector.reciprocal(rstd[:], rstd[:]))
                for ps_ap, bps, gc, dst in zip(ps_list, bps_list, gcols, dst_aps):
                    fw.op(DVE, [bps, brstd, bsmall], [bdst],
                          lambda: nc.vector.scalar_tensor_tensor(dst, in0=ps_ap, scalar=gc, in1=rstd[:], op0=ALU.mult, op1=ALU.mult))

            def attention(nsub, s_emit, s_reads, Vt, bV, scale, finalize):
                S_B = [(psb[0], pb[0]), (psb[1], pb[1])]
                O_B = [(psb[2], pb[2]), (psb[3], pb[3])]
                L_B = [(psb[4], pb[4]), (psb[5], pb[5])]
                for g in range(4):
                    units = []
                    nkb = 4 * g + 4
                    for half in (0, 1):
                        for kl in range(nkb):
                            i = kl - 4 * g
                            col0 = 0 if i < 0 else i * 128
                            mt = None if i < 0 else half
                            for c in range(nsub):
                                units.append((c, half * 16 + kl, col0, mt))
                    nun = len(units)
                    first = [True] * nsub
                    last_idx = {}
                    for ui, u in enumerate(units):
                        last_idx[u[0]] = ui
                    pts = {}

                    def emit_s(ui):
                        c, kb, col0, mt = units[ui]
                        sbk, bsbk = S_B[ui % 2]
                        s_emit(c, kb, g, col0, sbk, bsbk)
                        pT, bpT = s16.get()
                        fw.op(ACT, [bsbk], [bpT], lambda: nc.scalar.activation(pT[:, col0:512], sbk[:, col0:512], AF.Exp, scale=scale))
                        if mt is not None:
                            fw.op(POOL, [bpT, bcbf], [bpT], lambda: nc.gpsimd.tensor_tensor(pT[:, col0:col0 + 128], pT[:, col0:col0 + 128], masks_bf[mt], op=ALU.mult))
                        pts[ui] = (pT, bpT)

                    def emit_pv(ui):
                        c, kb, col0, mt = units[ui]
                        pT, bpT = pts.pop(ui)
                        o, bo = O_B[c]
                        l, bl = L_B[c]
                        st = first[c]
                        first[c] = False
                        sp_ = (last_idx[c] == ui)
                        fw.op(PE, [bpT, bV[kb // 4]], [bo], lambda: nc.tensor.matmul(o[:, col0:512], Vt[:, kb, :], pT[:, col0:512], start=st, stop=sp_), inc=False)
                        fw.op(PE, [bpT, bcbf], [bl], lambda: nc.tensor.matmul(l[:, col0:512], ones_bf, pT[:, col0:512], start=st, stop=sp_))

                    LOOK = 2
                    for ui in range(min(LOOK, nun)):
                        emit_s(ui)
                    for ui in range(nun):
                        emit_pv(ui)
                        if ui + LOOK < nun:
                            emit_s(ui + LOOK)
                    finalize(g, O_B, L_B)

            with contextlib.ExitStack() as sx:
                xTb = sb("xTb", [128, 8, S], BF16, sx); bxT = [Buf() for _ in range(8)]
                xT_v = xT_d.rearrange("(dc p) t -> p dc t", p=128)
                for tc in range(8):
                    fw.dma(POOL, xTb[:, :, tc * 512:(tc + 1) * 512], xT_v[:, :, tc * 512:(tc + 1) * 512], [], [bxT[tc]])
                w_in_v = w_in_d.rearrange("(dc p) c -> p dc c", p=128)

                for tc in range(8):
                    cols = slice(tc * 512, (tc + 1) * 512)
                    fw.dma(SP, si32[:], pos_d[0:1, cols].partition_broadcast(128), [], [bsi32])
                    ang, bang = s32.get()
                    fw.op(DVE, [bsi32], [bang], lambda: nc.vector.tensor_copy(ang[:], si32[:]))
                    fw.op(DVE, [bang, bcst], [bang], lambda: nc.vector.tensor_scalar(ang[:], ang[:], ropec[:, 0:1], None, op0=ALU.mult))
                    for which in (0, 1):
                        shift = 0.5 if which == 0 else 0.75
                        u, bu = s32.get()
                        fw.op(DVE, [bang], [bu], lambda: nc.vector.tensor_scalar(u[:], ang[:], 1.0 / TWO_PI, shift, op0=ALU.mult, op1=ALU.add))
                        ki, bki = s32.get()
                        kiv = ki[:].bitcast(I32)
                        fw.op(DVE, [bu], [bki], lambda: nc.vector.tensor_copy(kiv, u[:]))
                        kf, bkf = s32.get()
                        fw.op(POOL, [bki], [bkf], lambda: nc.gpsimd.tensor_copy(kf[:], kiv))
                        fw.op(POOL, [bkf, bu], [bu], lambda: nc.gpsimd.tensor_tensor(u[:], u[:], kf[:], op=ALU.subtract))
                        fw.op(DVE, [bu], [bkf], lambda: nc.vector.scalar_tensor_tensor(kf[:], in0=u[:], scalar=0.0, in1=u[:], op0=ALU.is_lt, op1=ALU.add))
                        if which == 0:
                            fw.op(ACT, [bkf, bcst], [btab[tc]], lambda: nc.scalar.activation(sinS[:, cols], kf[:], AF.Sin, bias=ropec[:, 2:3], scale=ropec[:, 1:2]))
                        else:
                            fw.op(ACT, [bkf, bcst], [btab[tc]], lambda: nc.scalar.activation(cosT[:, cols], kf[:], AF.Sin, bias=ropec[:, 3:4], scale=TWO_PI * (1.0 - 1e-6)))

                if stage.startswith("tabtt"):
                    import os as _os
                    r0, r1 = [int(v) for v in _os.environ.get("TT_ROWS", "0,128").split(",")]
                    mode = _os.environ.get("TT_MODE", "psum_cos")
                    t1, bt1 = s32.get()
                    kps, bkps = tmpb.get()
                    fw.op(PE, [bcbf], [bkps], lambda: nc.tensor.matmul(kps[:], perm_bf, cbf[:, 0:512], start=True, stop=True))
                    if mode == "psum_cos":
                        fw.op(DVE, [bkps, btab[0]], [bt1], lambda: nc.vector.tensor_tensor(t1[r0:r1, :], kps[r0:r1, :], cosT[r0:r1, 0:512], op=ALU.mult))
                    elif mode == "sb_cos":
                        t2, bt2 = s32.get()
                        fw.op(DVE, [], [bt2], lambda: nc.vector.memset(t2[:], 1.0))
                        fw.op(DVE, [bt2, btab[0]], [bt1], lambda: nc.vector.tensor_tensor(t1[r0:r1, :], t2[r0:r1, :], cosT[r0:r1, 0:512], op=ALU.mult))
                    elif mode == "psum_sb":
                        t2, bt2 = s32.get()
                        fw.op(DVE, [], [bt2], lambda: nc.vector.memset(t2[:], 1.0))
                        fw.op(DVE, [bkps, bt2], [bt1], lambda: nc.vector.tensor_tensor(t1[r0:r1, :], kps[r0:r1, :], t2[r0:r1, :], op=ALU.mult))
                    fw.dma(SP, out_d[0:128, 0:512], t1[:], [bt1], [])
                    fw.dma(SP, out_d[128:256, 0:512], cosT[:, 0:512], [btab[0]], [])
                    fw.dma(SP, out_d[256:384, 0:512], sinS[:, 0:512], [btab[0]], [])
                    fw.finish(SP)
                    return nc
                if stage == "tab":
                    fw.finish(SP)
                    return nc
                with contextlib.ExitStack() as sm0:
                    WC = sb("WC", [128, 8, 448], BF16, sm0); bWC = Buf()
                    fw.dma(POOL, WC[:], w_in_v[:, :, 1536:1984], [], [bWC])
                    for tc in range(8):
                        cols = slice(tc * 512, (tc + 1) * 512)
                        ckv, bckv = tmpb.get()
                        mm_acc(ckv[:], [(WC[:, dc, 256:384], xTb[:, dc, cols]) for dc in range(8)], [bWC, bxT[tc]], [bckv])
                        rms_scale([ckv[:]], [bckv], 128.0, EPS6, [small[:, 5:6]], [ckvn[:, cols]], bckvn[tc])
                        kr, bkr = tmpb.get()
                        mm_acc(kr[0:64, :], [(WC[:, dc, 384:448], xTb[:, dc, cols]) for dc in range(8)], [bWC, bxT[tc]], [bkr])
                        rope(kr[0:64, :], bkr, 64, tc, KR[0:64, cols], bKR[tc])
                        if tc < 4:
                            cq0, bcq0 = tmpb.get()
                            mm_acc(cq0[:], [(WC[:, dc, 0:128], xTb[:, dc, cols]) for dc in range(8)], [bWC, bxT[tc]], [bcq0])
                            cq1, bcq1 = tmpb.get()
                            mm_acc(cq1[:], [(WC[:, dc, 128:256], xTb[:, dc, cols]) for dc in range(8)], [bWC, bxT[tc]], [bcq1])
                            rms_scale([cq0[:], cq1[:]], [bcq0, bcq1], 256.0, EPS6, [small[:, 3:4], small[:, 4:5]],
                                      [cqn[:, 0, cols], cqn[:, 1, cols]], bcqn[tc])

                if stage == "m0":
                    fw.finish(SP)
                    return nc
                fw.fence()
                with contextlib.ExitStack() as sd:
                    WQ = sb("WQ", [128, 8, 128], BF16, sd); WK = sb("WK", [128, 8, 128], BF16, sd); WV = sb("WV", [128, 8, 128], BF16, sd)
                    bW = Buf()
                    KT = sb("KT", [128, S], BF16, sd); bKT = [Buf() for _ in range(8)]
                    QT = sb("QT", [128, TQ], BF16, sd); bQT = [Buf() for _ in range(4)]
                    Vt = sb("Vt", [128, 32, 128], BF16, sd); bV = [Buf() for _ in range(8)]
                    for h in range(4):
                        fw.dma(POOL, WQ[:], w_in_v[:, :, 128 * h:128 * h + 128], [], [bW])
                        fw.dma(POOL, WK[:], w_in_v[:, :, 512 + 128 * h:512 + 128 * h + 128], [], [bW])
                        fw.dma(POOL, WV[:], w_in_v[:, :, 1024 + 128 * h:1024 + 128 * h + 128], [], [bW])
                        if stage == "dprojD":
                            fw.finish(SP)
                            return nc
                        import os as _os
                        _ntc = int(_os.environ.get("DPROJ_NTC", "8"))
                        _noq = _os.environ.get("DPROJ_NOQ", "0") == "1"
                        for tc in range(_ntc):
                            cols = slice(tc * 512, (tc + 1) * 512)
                            if stage != "dprojV":
                                kps, bkps = tmpb.get()
                                mm_acc(kps[:], [(WK[:, dc, :], xTb[:, dc, cols]) for dc in range(8)], [bW, bxT[tc]], [bkps])
                                rope(kps[:], bkps, 128, tc, KT[:, cols], bKT[tc])
                            if tc < 4 and stage != "dprojV" and not _noq:
                                qps, bqps = tmpb.get()
                                mm_acc(qps[:], [(WQ[:, dc, :], xTb[:, dc, cols]) for dc in range(8)], [bW, bxT[tc]], [bqps])
                                rope(qps[:], bqps, 128, tc, QT[:, cols], bQT[tc])
                            if stage == "dprojK":
                                continue
                            vps, bvps = tmpb.get()
                            for i in range(4):
                                mm_acc(vps[:, i * 128:(i + 1) * 128],
                                       [(xTb[:, dc, tc * 512 + i * 128: tc * 512 + (i + 1) * 128], WV[:, dc, :]) for dc in range(8)],
                                       [bW, bxT[tc]], [bvps])
                            fw.op(ACT, [bvps], [bV[tc]], lambda: nc.scalar.copy(Vt[:, tc * 4:(tc + 1) * 4, :], vps[:].rearrange("p (a b) -> p a b", a=4)))

                        if stage in ("dproj", "dprojK", "dprojV"):
                            fw.finish(SP)
                            return nc
                        def s_emit(c, kb, g, col0, sbk, bsbk):
                            fw.op(PE, [bKT[kb // 4], bQT[g]], [bsbk],
                                  lambda: nc.tensor.matmul(sbk[:, col0:512], KT[64 * c:64 * c + 64, kb * 128:(kb + 1) * 128],
                                                           QT[64 * c:64 * c + 64, g * 512 + col0:(g + 1) * 512], start=True, stop=True))

                        def fin_diff(g, O_B, L_B, h=h):
                            ds = []
                            for c in range(2):
                                rl, brl = s32.get()
                                fw.op(DVE, [L_B[c][1]], [brl], lambda: nc.vector.reciprocal(rl[:], L_B[c][0][:]))
                                fw.op(DVE, [O_B[c][1], brl], [brl], lambda: nc.vector.tensor_tensor(rl[:], O_B[c][0][:], rl[:], op=ALU.mult))
                                ds.append((rl, brl))
                            dd, bdd = s32.get()
                            fw.op(DVE, [ds[0][1], ds[1][1], bsmall], [bdd],
                                  lambda: nc.vector.scalar_tensor_tensor(dd[:], in0=ds[1][0][:], scalar=small[:, 2:3], in1=ds[0][0][:], op0=ALU.mult, op1=ALU.add))
                            sq, bsq = s32.get()
                            fw.op(ACT, [bdd], [bsq], lambda: nc.scalar.activation(sq[:], dd[:], AF.Square))
                            ss, bss = tmpb.get()
                            fw.op(PE, [bsq, bones], [bss], lambda: nc.tensor.matmul(ss[:], ones32[:], sq[:], start=True, stop=True))
                            rstd, brstd = s32.get()
                            fw.op(ACT, [bss, bsmall], [brstd], lambda: nc.scalar.activation(rstd[:], ss[:], AF.Sqrt, bias=EPS5, scale=1.0 / 128.0))
                            fw.op(DVE, [brstd], [brstd], lambda: nc.vector.reciprocal(rstd[:], rstd[:]))
                            wr = [botx[4 * g + i] for i in range(4)]
                            fw.op(DVE, [bdd, brstd, bsmall], wr,
                                  lambda: nc.vector.scalar_tensor_tensor(otx[:, h, g * 512:(g + 1) * 512], in0=dd[:], scalar=small[:, 1:2], in1=rstd[:], op0=ALU.mult, op1=ALU.mult))

                        attention(2, s_emit, None, Vt, bV, 64.0 ** -0.5, fin_diff)
                        if stage == "datt":
                            fw.finish(SP)
                            return nc
            fw.fence()
            with contextlib.ExitStack() as sm:
                wuq = sb("wuq", [128, 2, 768], BF16, sm); bwuq = Buf()
                wukv = sb("wukv", [128, 1024], BF16, sm); bwukv = Buf()
                fw.dma(POOL, wuq[:], w_uq_d.rearrange("(rc p) c -> p rc c", p=128), [], [bwuq])
                fw.dma(POOL, wukv[:], w_ukv_d[:, :], [], [bwukv])
                KTm = sb("KTm", [128, S], BF16, sm); bKTm = [Buf() for _ in range(8)]
                Vm = sb("Vm", [128, 32, 128], BF16, sm); bVm = [Buf() for _ in range(8)]
                QTn = sb("QTn", [128, TQ], BF16, sm); bQTn = [Buf() for _ in range(4)]
                QTr = sb("QTr", [128, TQ], BF16, sm); bQTr = [Buf() for _ in range(4)]
                fw.op(POOL, [], bQTr, lambda: nc.gpsimd.memset(QTr[64:128, :], 0.0))
                for h in range(4):
                    for tc in range(8):
                        cols = slice(tc * 512, (tc + 1) * 512)
                        kn, bkn = tmpb.get()
                        fw.op(PE, [bwukv, bckvn[tc]], [bkn], lambda: nc.tensor.matmul(kn[:], wukv[:, h * 256:h * 256 + 128], ckvn[:, cols], start=True, stop=True))
                        fw.op(ACT, [bkn], [bKTm[tc]], lambda: nc.scalar.copy(KTm[:, cols], kn[:]))
                        vps, bvps = tmpb.get()
                        for i in range(4):
                            fw.op(PE, [bwukv, bckvn[tc]], [bvps],
                                  lambda: nc.tensor.matmul(vps[:, i * 128:(i + 1) * 128], ckvn[:, tc * 512 + i * 128: tc * 512 + (i + 1) * 128],
                                                           wukv[:, h * 256 + 128:h * 256 + 256], start=True, stop=True), inc=(i == 3))
                        fw.op(DVE, [bvps], [bVm[tc]], lambda: nc.vector.tensor_copy(Vm[:, tc * 4:(tc + 1) * 4, :], vps[:].rearrange("p (a b) -> p a b", a=4)))
                        if tc < 4:
                            qn, bqn = tmpb.get()
                            mm_acc(qn[:], [(wuq[:, rc, h * 192:h * 192 + 128], cqn[:, rc, cols]) for rc in range(2)], [bwuq, bcqn[tc]], [bqn])
                            fw.op(ACT, [bqn], [bQTn[tc]], lambda: nc.scalar.copy(QTn[:, cols], qn[:]))
                            qr, bqr = tmpb.get()
                            mm_acc(qr[0:64, :], [(wuq[:, rc, h * 192 + 128:h * 192 + 192], cqn[:, rc, cols]) for rc in range(2)], [bwuq, bcqn[tc]], [bqr])
                            rope(qr[0:64, :], bqr, 64, tc, QTr[0:64, cols], bQTr[tc])

                    if stage == "mproj":
                        fw.finish(SP)
                        return nc
                    def s_emit_m(c, kb, g, col0, sbk, bsbk):
                        fw.op(PE, [bKTm[kb // 4], bQTn[g]], [bsbk],
                              lambda: nc.tensor.matmul(sbk[:, col0:512], KTm[:, kb * 128:(kb + 1) * 128], QTn[:, g * 512 + col0:(g + 1) * 512], start=True, stop=False), inc=False)
                        fw.op(PE, [bKR[kb // 4], bQTr[g]], [bsbk],
                              lambda: nc.tensor.matmul(sbk[:, col0:512], KR[:, kb * 128:(kb + 1) * 128], QTr[:, g * 512 + col0:(g + 1) * 512], start=False, stop=True))

                    def fin_mla(g, O_B, L_B, h=h):
                        rl, brl = s32.get()
                        fw.op(DVE, [L_B[0][1]], [brl], lambda: nc.vector.reciprocal(rl[:], L_B[0][0][:]))
                        wr = [botx[4 * g + i] for i in range(4)]
                        fw.op(DVE, [O_B[0][1], brl], wr, lambda: nc.vector.tensor_tensor(otx[:, 4 + h, g * 512:(g + 1) * 512], O_B[0][0][:], rl[:], op=ALU.mult))

                    attention(1, s_emit_m, None, Vm, bVm, 192.0 ** -0.5, fin_mla)
                    if stage == "matt":
                        fw.finish(SP)
                        return nc

        fw.fence()
        ACC = sb("ACC", [128, 16, D], F32); bACC = [Buf() for _ in range(16)]
        sm2 = sb("sm2", [128, 16, 8], F32); bsm2 = [Buf() for _ in range(16)]

        def layer_norm(z, bz, tb, lnbc, blnbc, dst, bdst, junk, bjunk):
            sc = sm2[:, tb, :]
            bs = bsm2[tb]
            fw.op(DVE, [bz], [bs], lambda: nc.vector.reduce_sum(sc[:, 0:1], z, axis=AX.X))
            fw.op(DVE, [bs], [bs], lambda: nc.vector.tensor_scalar(sc[:, 1:2], sc[:, 0:1], -1.0 / D, None, op0=ALU.mult))
            fw.op(ACT, [bz, bs], [bz], lambda: nc.scalar.activation(z, z, AF.Identity, bias=sc[:, 1:2]))
            fw.op(ACT, [bz], [bjunk], lambda: nc.scalar.activation(junk, z, AF.Square))
            fw.op(DVE, [bjunk], [bs], lambda: nc.vector.reduce_sum(sc[:, 2:3], junk, axis=AX.X))
            fw.op(ACT, [bs, bsmall], [bs], lambda: nc.scalar.activation(sc[:, 3:4], sc[:, 2:3], AF.Sqrt, bias=EPS5, scale=1.0 / D))
            fw.op(DVE, [bs], [bs], lambda: nc.vector.reciprocal(sc[:, 3:4], sc[:, 3:4]))
            fw.op(DVE, [bz, bs, blnbc], [bz], lambda: nc.vector.scalar_tensor_tensor(z, in0=z, scalar=sc[:, 3:4], in1=lnbc[:, 0, :], op0=ALU.mult, op1=ALU.mult))
            fw.op(POOL, [bz, blnbc], [bdst], lambda: nc.gpsimd.tensor_tensor(dst, z, lnbc[:, 1, :], op=ALU.add))

        with contextlib.ExitStack() as so:
            ln1bc = sb("ln1bc", [128, 2, D], F32, so); bln1 = Buf()
            fw.dma(SP, ln1bc[:], lnv_d[0:2, :].partition_broadcast(128), [], [bln1])
            wo = sb("wo", [128, 8, D], BF16, so); bwo = Buf()
            fw.dma(POOL, wo[:], w_o_d.rearrange("(hh p) o -> p hh o", p=128), [], [bwo])
            xqt = Rot([(sb(f"xqt{i}", [128, D], F32, so), Buf()) for i in range(2)])
            zt = Rot([(sb(f"zt{i}", [128, D], F32, so), Buf()) for i in range(2)])
            jk = Rot([(sb(f"jk{i}", [128, D], F32, so), Buf()) for i in range(2)])
            mixb = Rot([((psb[0], pb[0]), (psb[1], pb[1])), ((psb[2], pb[2]), (psb[3], pb[3]))])
            for tb in range(16):
                xt_, bxt_ = xqt.get()
                fw.dma(SP, xt_[:], xq_d[tb * 128:(tb + 1) * 128, :], [], [bxt_])
                banks = mixb.get()
                z, bz = zt.get()
                for half in range(2):
                    mps, bmps = banks[half]
                    for hh in range(8):
                        fw.op(PE, [botx[tb], bwo], [bmps],
                              lambda: nc.tensor.matmul(mps[:], otx[:, hh, tb * 128:(tb + 1) * 128], wo[:, hh, half * 512:(half + 1) * 512], start=(hh == 0), stop=(hh == 7)),
                              inc=(hh == 7))
                    fw.op(DVE, [bmps, bxt_], [bz],
                          lambda: nc.vector.scalar_tensor_tensor(z[:, half * 512:(half + 1) * 512], in0=xt_[:, half * 512:(half + 1) * 512], scalar=DN_ALPHA, in1=mps[:], op0=ALU.mult, op1=ALU.add))
                j_, bj_ = jk.get()
                layer_norm(z[:], bz, tb, ln1bc, bln1, ACC[:, tb, :], bACC[tb], j_[:], bj_)

        fw.fence()
        if stage == "ln1":
            for tb in range(16):
                fw.dma(SP, out_d[tb * 128:(tb + 1) * 128, :], ACC[:, tb, :], [bACC[tb]], [])
            fw.finish(SP)
            return nc

        G = sb("G", [128, 16, NE], F32); bG = [Buf() for _ in range(16)]
        MK = sb("MK", [128, 16, NE], F32); bMK = [Buf() for _ in range(16)]
        posm = sb("posm", [128, 16, NE], F32); bposm = [Buf() for _ in range(16)]
        posmT = sb("posmT", [NE, TQ], F32); bposmT = [Buf() for _ in range(4)]
        X1B = otx[:].rearrange("p a b -> p (a b)").rearrange("p (t d) -> p t d", t=16)
        bguT = sb("bguT", [128, NE * 16], F32); bbgu = Buf()
        fw.dma(SP, bguT[:], bgu_d[:, :], [], [bbgu])
        with contextlib.ExitStack() as sr:
            wr32 = sb("wr32", [128, 8, NE], F32, sr); bwr = Buf()
            fw.dma(SP, wr32[:], w_r_d.rearrange("(dc p) e -> p dc e", p=128), [], [bwr])
            brbc = sb("brbc", [128, NE], F32, sr); bbr = Buf()
            fw.dma(SP, brbc[:], b_r_d.partition_broadcast(128), [], [bbr])
            bd32 = sb("bd32", [NE, D], F32, sr); bbd = Buf()
            fw.dma(SP, bd32[:], bd_d[:, :], [], [bbd])
            GT = sb("GT", [NE, TQ], F32, sr); bGT = [Buf() for _ in range(16)]
            x1T32 = Rot([(sb(f"x1T32_{i}", [128, 8, 128], F32, sr), Buf()) for i in range(2)])
            rt = Rot([(sb(f"rt{i}", [128, 128], F32, sr), Buf()) for i in range(2)])
            tpb = Rot([((psb[0], pb[0]), (psb[1], pb[1])), ((psb[2], pb[2]), (psb[3], pb[3]))])
            tmp2 = Rot([(psb[i], pb[i]) for i in (4, 5, 6, 7)])
            for tb in range(16):
                fw.op(DVE, [bACC[tb]], [botx[tb]], lambda: nc.vector.tensor_copy(X1B[:, tb, :], ACC[:, tb, :]))
                banks = tpb.get()
                xT32, bxT32 = x1T32.get()
                for hb_ in range(2):
                    tp, btp = banks[hb_]
                    for q in range(4):
                        dc = hb_ * 4 + q
                        fw.op(PE, [bACC[tb], bcst], [btp], lambda: nc.tensor.transpose(tp[:, q * 128:(q + 1) * 128], ACC[:, tb, dc * 128:(dc + 1) * 128], ident), inc=(q == 3))
                    fw.op(ACT, [btp], [bxT32], lambda: nc.scalar.copy(xT32[:, hb_ * 4:(hb_ + 1) * 4, :], tp[:].rearrange("p (a b) -> p a b", a=4)))
                lgp, blgp = tmp2.get()
                for dc in range(8):
                    fw.op(PE, [bxT32, bwr], [blgp], lambda: nc.tensor.matmul(lgp[:, 0:NE], xT32[:, dc, :], wr32[:, dc, :], start=(dc == 0), stop=(dc == 7)), inc=(dc == 7))
                r_, br_ = rt.get()
                lg = r_[:, 0:32]; m8 = r_[:, 32:40]; ex = r_[:, 40:72]; mk = r_[:, 72:104]; misc = r_[:, 104:112]
                fw.op(DVE, [blgp, bbr], [br_], lambda: nc.vector.tensor_tensor(lg, lgp[:, 0:NE], brbc[:], op=ALU.add))
                fw.op(DVE, [br_], [br_], lambda: nc.vector.max(out=m8, in_=lg))
                fw.op(DVE, [br_], [br_], lambda: nc.vector.tensor_scalar(misc[:, 0:1], m8[:, 0:1], -1.0, None, op0=ALU.mult))
                fw.op(ACT, [br_], [br_], lambda: nc.scalar.activation(ex, lg, AF.Exp, bias=misc[:, 0:1]))
                fw.op(DVE, [br_], [bMK[tb]], lambda: nc.vector.tensor_scalar(MK[:, tb, :], lg, m8[:, 3:4], None, op0=ALU.is_ge))
                fw.op(DVE, [br_, bMK[tb]], [br_], lambda: nc.vector.tensor_tensor(ex, ex, MK[:, tb, :], op=ALU.mult))
                fw.op(DVE, [br_], [br_], lambda: nc.vector.reduce_sum(misc[:, 1:2], ex, axis=AX.X))
                fw.op(DVE, [br_], [br_], lambda: nc.vector.reciprocal(misc[:, 2:3], misc[:, 1:2]))
                fw.op(DVE, [br_], [bG[tb]], lambda: nc.vector.tensor_scalar(G[:, tb, :], ex, misc[:, 2:3], None, op0=ALU.mult))
                gtp, bgtp = tmp2.get()
                fw.op(PE, [bG[tb], bcst], [bgtp], lambda: nc.tensor.transpose(gtp[0:NE, 0:128], G[:, tb, :], ident))
                fw.op(ACT, [bgtp], [bGT[tb]], lambda: nc.scalar.copy(GT[:, tb * 128:(tb + 1) * 128], gtp[0:NE, 0:128]))
                for half in range(2):
                    bdp, bbdp = tmp2.get()
                    fw.op(PE, [bGT[tb], bbd], [bbdp], lambda: nc.tensor.matmul(bdp[:], GT[:, tb * 128:(tb + 1) * 128], bd32[:, half * 512:(half + 1) * 512], start=True, stop=True))
                    fw.op(DVE, [bbdp, bACC[tb]], [bACC[tb]],
                          lambda: nc.vector.scalar_tensor_tensor(ACC[:, tb, half * 512:(half + 1) * 512], in0=ACC[:, tb, half * 512:(half + 1) * 512], scalar=DN_ALPHA, in1=bdp[:], op0=ALU.mult, op1=ALU.add))

            triu = cst[:, 1024:1152]
            for tb in range(16):
                pp, bpp = tmp2.get()
                fw.op(PE, [bMK[tb], bcst], [bpp], lambda: nc.tensor.matmul(pp[:, 0:NE], triu, MK[:, tb, :], start=True, stop=(tb == 0)), inc=(tb == 0))
                for t2_ in range(tb):
                    fw.op(PE, [bMK[t2_], bones], [bpp], lambda: nc.tensor.matmul(pp[:, 0:NE], ones32[:], MK[:, t2_, :], start=False, stop=(t2_ == tb - 1)), inc=(t2_ == tb - 1))
                fw.op(DVE, [bpp, bMK[tb]], [bposm[tb]], lambda: nc.vector.scalar_tensor_tensor(posm[:, tb, :], in0=pp[:, 0:NE], scalar=1.0, in1=MK[:, tb, :], op0=ALU.add, op1=ALU.mult))
                fw.op(DVE, [bposm[tb]], [bposm[tb]], lambda: nc.vector.tensor_scalar(posm[:, tb, :], posm[:, tb, :], -1.0, None, op0=ALU.add))
                ptp, bptp = tmp2.get()
                fw.op(PE, [bposm[tb], bcst], [bptp], lambda: nc.tensor.transpose(ptp[0:NE, 0:128], posm[:, tb, :], ident))
                fw.op(ACT, [bptp], [bposmT[tb // 4]], lambda: nc.scalar.copy(posmT[:, tb * 128:(tb + 1) * 128], ptp[0:NE, 0:128]))
        fw.fence()
        with contextlib.ExitStack() as se:
            NSB = CAP // 128
            stg = Rot([(sb(f"stg{i}", [128, 2048], F32, se), Buf()) for i in range(2)])
            wgr = Rot([(sb(f"wg{i}", [128, 8, 2, 128], BF16, se), Buf()) for i in range(3)])
            Wd = sb("Wd", [128, 8, D], BF16, se); bWd = [Buf() for _ in range(4)]
            xgT = sb("xgT", [128, 8, CAP], BF16, se); bxg = [Buf() for _ in range(8)]
            ACTT = sb("ACTT", [128, 8, CAP], BF16, se); bACTT = [Buf() for _ in range(8)]
            Sel = sb("Sel", [128, 16, CAP], BF16, se); bSel = [Buf() for _ in range(16)]
            SelT = Rot([(sb(f"SelT{i}", [128, NSB, 512], BF16, se), Buf()) for i in range(2)])
            yb = sb("yb", [128, NSB, D], BF16, se); byb = [Buf() for _ in range(NSB)]
            gc_ = sb("gc", [128, CAP], F32, se); bgc_ = Buf()
            sg_ = sb("sg", [128, CAP], F32, se); bsg_ = Buf()
            uc_ = sb("uc", [128, CAP], F32, se); buc_ = Buf()
            le = sb("le", [NE, 128], F32, se); ble = Buf()
            busT = sb("busT", [128, NE * 8], F32, se); bbus = Buf()
            fw.op(DVE, [bbgu], [bbus], lambda: nc.vector.tensor_scalar(busT[:].rearrange("p (e c) -> p e c", c=8), bguT[:].rearrange("p (e c) -> p e c", c=16)[:, :, 8:16], 1.0 / 1.702, None, op0=ALU.mult))
            gab = Rot([(psb[i], pb[i]) for i in (0, 1)])
            gub = Rot([((psb[2], pb[2]), (psb[3], pb[3])), ((psb[4], pb[4]), (psb[5], pb[5]))])
            yb_b = Rot([(psb[i], pb[i]) for i in (6, 7)])
            wd_v = wd_d.rearrange("e (fc p) o -> e p fc o", p=128)
            iotaC = cst[:, 640:640 + CAP]
            kiota = cst[0:NE, 1152:1280]

            def load_wg(e, fc):
                st_, bst_ = stg.get()
                fw.dma(SP, st_[:], wgu_d[e, fc], [], [bst_])
                wg_, bwg_ = wgr.get()
                fw.op(ACT, [bst_], [bwg_], lambda: nc.scalar.copy(wg_[:].rearrange("p a b c -> p (a b c)"), st_[:]))
                return wg_, bwg_

            def load_wd(e, pc):
                st_, bst_ = stg.get()
                fw.dma(SP, st_[:].rearrange("p (a b) -> p a b", a=2), wd_v[e, :, 2 * pc:2 * pc + 2, :], [], [bst_])
                fw.op(ACT, [bst_], [bWd[pc]], lambda: nc.scalar.copy(Wd[:, 2 * pc:2 * pc + 2, :], st_[:].rearrange("p (a b) -> p a b", a=2)))

            def build_sel(e):
                for tb in range(16):
                    fw.op(DVE, [bposm[tb], bcst], [bSel[tb]], lambda: nc.vector.tensor_scalar(Sel[:, tb, :], iotaC, posm[:, tb, e:e + 1], None, op0=ALU.is_equal))

            def gather(e):
                for dc in range(8):
                    gp, bgp = gab.get()
                    for tb in range(16):
                        fw.op(PE, [botx[tb], bSel[tb]], [bgp], lambda: nc.tensor.matmul(gp[:, 0:CAP], X1B[:, tb, dc * 128:(dc + 1) * 128], Sel[:, tb, :], start=(tb == 0), stop=(tb == 15)), inc=(tb == 15))
                    fw.op(ACT, [bgp], [bxg[dc]], lambda: nc.scalar.copy(xgT[:, dc, :], gp[:, 0:CAP]))

            slices = [(e, fc) for e in range(NE) for fc in range(8)]
            PRE = 2
            WD_SCHED = {1: 0, 3: 1, 5: 2, 6: 3}
            loaded = {}
            for k in range(min(PRE, len(slices))):
                loaded[k] = load_wg(*slices[k])
            for k, (e, fc) in enumerate(slices):
                if k == 0:
                    build_sel(0)
                    gather(0)
                    build_sel(1)
                if k + PRE < len(slices):
                    loaded[k + PRE] = load_wg(*slices[k + PRE])
                wg_, bwg_ = loaded.pop(k)
                if fc in WD_SCHED:
                    load_wd(e, WD_SCHED[fc])
                (gps, bgps), (ups, bups) = gub.get()
                rd = [bwg_] + bxg
                for dc in range(8):
                    fw.op(PE, rd, [bgps], lambda: nc.tensor.matmul(gps[:, 0:CAP], wg_[:, dc, 0, :], xgT[:, dc, :], start=(dc == 0), stop=(dc == 7)), inc=(dc == 7))
                for dc in range(8):
                    fw.op(PE, rd, [bups], lambda: nc.tensor.matmul(ups[:, 0:CAP], wg_[:, dc, 1, :], xgT[:, dc, :], start=(dc == 0), stop=(dc == 7)), inc=(dc == 7))
                bg_col = bguT[:, e * 16 + fc:e * 16 + fc + 1]
                bu_col = bguT[:, e * 16 + 8 + fc:e * 16 + 8 + fc + 1]
                bus_col = busT[:, e * 8 + fc:e * 8 + fc + 1]
                fw.op(DVE, [bgps, bbgu], [bgc_], lambda: nc.vector.tensor_scalar(gc_[:], gps[:, 0:CAP], bg_col, 7.0, op0=ALU.add, op1=ALU.min))
                fw.op(ACT, [bups, bbus], [buc_], lambda: nc.scalar.activation(uc_[:], ups[:, 0:CAP], AF.Identity, bias=bus_col, scale=1.0 / 1.702))
                fw.op(ACT, [bgc_], [bsg_], lambda: nc.scalar.activation(sg_[:], gc_[:], AF.Silu, scale=1.702))
                fw.op(DVE, [buc_], [buc_], lambda: nc.vector.tensor_scalar(uc_[:], uc_[:], 7.0 / 1.702, -7.0 / 1.702, op0=ALU.min, op1=ALU.max))
                fw.op(DVE, [bsg_, buc_], [bACTT[fc]],
                      lambda: nc.vector.scalar_tensor_tensor(ACTT[:, fc, :], in0=uc_[:], scalar=1.0 / 1.702, in1=sg_[:], op0=ALU.add, op1=ALU.mult))
                if fc == 7:
                    if e + 1 < NE:
                        gather(e + 1)
                        if e + 2 < NE:
                            build_sel(e + 2)
                    for sbk in range(NSB):
                        for half in range(2):
                            yp, byp = yb_b.get()
                            rd2 = bACTT + bWd
                            for f in range(8):
                                fw.op(PE, rd2, [byp], lambda: nc.tensor.matmul(yp[:], ACTT[:, f, sbk * 128:(sbk + 1) * 128], Wd[:, f, half * 512:(half + 1) * 512], start=(f == 0), stop=(f == 7)), inc=(f == 7))
                            fw.op(ACT, [byp], [byb[sbk]], lambda: nc.scalar.copy(yb[:, sbk, half * 512:(half + 1) * 512], yp[:]))
                    fw.op(DVE, [bcst], [ble], lambda: nc.vector.tensor_scalar(le[:], kiota, float(e), None, op0=ALU.is_equal))
                    for ch in range(4):
                        bc, bbc = gab.get()
                        fw.op(PE, [ble, bposmT[ch]], [bbc], lambda: nc.tensor.matmul(bc[:], le[:], posmT[:, ch * 512:(ch + 1) * 512], start=True, stop=True))
                        st_, bst2 = SelT.get()
                        for sbk in range(NSB):
                            fw.op(DVE, [bbc, bcst], [bst2], lambda: nc.vector.tensor_scalar(st_[:, sbk, :], bc[:], cst[:, 516 + sbk:517 + sbk], None, op0=ALU.is_equal))
                        for tb4 in range(4):
                            tb = ch * 4 + tb4
                            for half in range(2):
                                yp, byp = yb_b.get()
                                for sbk in range(NSB):
                                    fw.op(PE, [bst2] + byb, [byp], lambda: nc.tensor.matmul(yp[:], st_[:, sbk, tb4 * 128:(tb4 + 1) * 128], yb[:, sbk, half * 512:(half + 1) * 512], start=(sbk == 0), stop=(sbk == NSB - 1)), inc=(sbk == NSB - 1))
                                fw.op(DVE, [byp, bG[tb], bACC[tb]], [bACC[tb]],
                                      lambda: nc.vector.scalar_tensor_tensor(ACC[:, tb, half * 512:(half + 1) * 512], in0=yp[:], scalar=G[:, tb, e:e + 1], in1=ACC[:, tb, half * 512:(half + 1) * 512], op0=ALU.mult, op1=ALU.add))

        fw.fence()
        with contextlib.ExitStack() as sf:
            ln2bc = sb("ln2bc", [128, 2, D], F32, sf); bln2 = Buf()
            fw.dma(SP, ln2bc[:], lnv_d[2:4, :].partition_broadcast(128), [], [bln2])
            ot = Rot([(sb(f"ot{i}", [128, D], F32, sf), Buf()) for i in range(2)])
            jk2 = Rot([(sb(f"jk2{i}", [128, D], F32, sf), Buf()) for i in range(2)])
            for tb in range(16):
                o_, bo_ = ot.get()
                j_, bj_ = jk2.get()
                layer_norm(ACC[:, tb, :], bACC[tb], tb, ln2bc, bln2, o_[:], bo_, j_[:], bj_)
                fw.dma(SP, out_d[tb * 128:(tb + 1) * 128, :], o_[:], [bo_], [])
        fw.finish(SP)
    return nc


_PROG = {}


def _consts(r):
    c = np.zeros((128, 1280), np.float32)
    c[:, 0:128] = np.eye(128, dtype=np.float32)
    m = np.arange(128)
    partner = np.where(m % 64 < 32, m + 32, m - 32)
    c[partner, 128 + m] = 1.0
    k = np.arange(128)[:, None]
    q = np.arange(128)[None, :]
    c[:, 256:384] = (k <= q).astype(np.float32)
    c[:, 384:512] = 1.0 if r == 1 else 0.0
    invf = 1.0 / (10000.0 ** ((np.arange(128) % 32) * 2.0 / 64.0))
    first = (np.arange(128) % 64) < 32
    c[:, 512] = invf
    sc = TWO_PI * (1.0 - 1e-6)
    c[:, 513] = np.where(first, -sc, sc)
    c[:, 514] = np.where(first, PI_S, -PI_S)
    c[:, 515] = -PI_S
    p = np.arange(128, dtype=np.float32)
    c[:, 516] = p
    c[:, 517] = p + 128.0
    c[:, 518] = p + 256.0
    c[:, 640:1024] = np.arange(CAP, dtype=np.float32)[None, :]
    c[:, 1024:1152] = (p[:, None] < p[None, :]).astype(np.float32)
    c[:, 1152:1280] = p[:, None]
    return c


def _prep_inputs(inp):
    f32 = lambda a: np.ascontiguousarray(np.asarray(a), dtype=np.float32)
    x = f32(inp["x"])
    positions = np.ascontiguousarray(np.asarray(inp["positions"]), dtype=np.int32)
    wgu = f32(inp["w_gate_up"])[0]
    wgu_t = np.ascontiguousarray(
        wgu.reshape(NE, 8, 128, 2, 8, 128).transpose(0, 4, 2, 1, 3, 5)).reshape(NE, 8, 128, 2048)
    bgu = f32(inp["b_gate_up"])[0]
    bguT = np.ascontiguousarray(bgu.reshape(NE, 16, 128).transpose(2, 0, 1)).reshape(128, NE * 16)
    shared = {
        "w_in": f32(inp["w_in"])[0],
        "lamv": np.ascontiguousarray(np.stack([f32(inp["lambda_q1"])[0], f32(inp["lambda_k1"])[0],
                                               f32(inp["lambda_q2"])[0], f32(inp["lambda_k2"])[0]], 0)),
        "subln_g": f32(inp["subln_g"])[0].reshape(128, 1),
        "gq": np.ascontiguousarray(f32(inp["mla_q_norm_g"])[0].reshape(2, 128).T),
        "gkv": f32(inp["mla_kv_norm_g"])[0].reshape(128, 1),
        "w_uq": f32(inp["w_uq"])[0],
        "w_ukv": f32(inp["w_ukv"])[0],
        "w_o": f32(inp["w_o"])[0],
        "lnv": np.ascontiguousarray(np.stack([f32(inp["ln1_g"])[0], f32(inp["ln1_b"])[0],
                                              f32(inp["ln2_g"])[0], f32(inp["ln2_b"])[0]], 0)),
        "w_router": f32(inp["w_router"])[0],
        "b_router": f32(inp["b_router"])[0].reshape(1, NE),
        "wgu_t": wgu_t,
        "bguT": bguT,
        "w_down": f32(inp["w_down"])[0],
        "b_down": f32(inp["b_down"])[0],
    }
    in_maps = []
    toks = []
    for c in range(NCORES):
        b, r = c // 2, c % 2
        own = [2 * j + r for j in range(16)]
        oth = [2 * j + (1 - r) for j in range(16)]
        tok = np.concatenate([np.arange(g * 128, (g + 1) * 128) for g in own + oth])
        toks.append((b, tok[:TQ]))
        xb = x[b][tok]
        m = dict(shared)
        m["xT"] = np.ascontiguousarray(xb.T)
        m["xq"] = np.ascontiguousarray(xb[:TQ])
        m["pos"] = np.ascontiguousarray(positions[b][tok].reshape(1, S))
        m["cst"] = _consts(r)
        in_maps.append(m)
    return in_maps, toks


def kernel(**inputs):
    stage = inputs.pop("_stage", "full")
    if stage not in _PROG:
        _PROG[stage] = build_program(stage)
    nc = _PROG[stage]
    in_maps, toks = _prep_inputs(inputs)
    if stage != "full":
        moe = ("w_router", "b_router", "wgu_t", "bguT", "w_down", "b_down")
        in_maps = [{k: v for k, v in m.items() if k not in moe} for m in in_maps]
    res = run_bass_kernel_spmd(nc, in_maps, core_ids=list(range(NCORES)))
    out = np.zeros((4, S, D), np.float32)
    for c in range(NCORES):
        b, tok = toks[c]
        out[b, tok] = res.results[c]["out"]
    return out
```

```python
import math
import contextlib
import numpy as np
import concourse.bass as bass
import concourse.mybir as mybir
from concourse.bass_utils import run_bass_kernel_spmd

F32 = mybir.dt.float32
BF16 = mybir.dt.bfloat16
I32 = mybir.dt.int32
ALU = mybir.AluOpType
AF = mybir.ActivationFunctionType
AX = mybir.AxisListType

NCORES = 8
D = 1024
S = 4096
TQ = 2048
NE = 32
LAM_INIT = 0.8 - 0.6 * math.exp(0.0)
DN_ALPHA = 2.0 ** 0.25
TWO_PI = 2.0 * math.pi
PI_S = math.pi * (1.0 - 1e-6)
NDMA_SEM = 8
CAP = 384


_FENCE = {}


class Buf:
    __slots__ = ("w", "r", "excl")

    def __init__(self, excl=False):
        self.w = None
        self.r = dict(_FENCE)
        self.excl = excl


class Eng:
    def __init__(self, name, h, sem, dma_sems):
        self.name = name
        self.h = h
        self.sem = sem
        self.count = 0
        self.seen = {}
        self.dma_sems = dma_sems
        self.dma_val = [0] * len(dma_sems)
        self.rr = 0


class FW:
    def __init__(self, nc, es):
        self.nc = nc
        self.es = es
        mk = lambda n: es.enter_context(nc.semaphore(n))
        self.pe = Eng("pe", nc.tensor, mk("s_pe"), [])
        self.act = Eng("act", nc.scalar, mk("s_act"), [])
        self.dve = Eng("dve", nc.vector, mk("s_dve"), [])
        self.pool = Eng("pool", nc.gpsimd, mk("s_pool"), [mk(f"d_pool{i}") for i in range(NDMA_SEM)])
        self.sp = Eng("sp", nc.sync, mk("s_sp"), [mk(f"d_sp{i}") for i in range(NDMA_SEM)])
        self.nwait = 0

    def _wait(self, E, tok):
        sem, val = tok
        if sem is E.sem and (E is self.pe or val > E.count):
            return
        k = id(sem)
        if E.seen.get(k, 0) >= val:
            return
        E.h.wait_ge(sem, val)
        E.seen[k] = val
        self.nwait += 1

    def _deps(self, E, reads, writes):
        for b in reads:
            if b.w is not None:
                self._wait(E, b.w)
            if b.excl:
                for tok in b.r.values():
                    self._wait(E, tok)
        for b in writes:
            if b.w is not None:
                self._wait(E, b.w)
            for tok in b.r.values():
                self._wait(E, tok)

    def _mark(self, tok, reads, writes):
        for b in reads:
            b.r[id(tok[0])] = tok
        for b in writes:
            b.w = tok
            b.r = {}

    def op(self, E, reads, writes, build, inc=True):
        self._deps(E, reads, writes)
        ins = build()
        tok = (E.sem, E.count + 1)
        if inc:
            ins.then_inc(E.sem, 1)
            E.count += 1
        self._mark(tok, reads, writes)
        return ins

    def dma(self, Q, out, in_, reads, writes, **kw):
        i = Q.rr % len(Q.dma_sems)
        Q.rr += 1
        sem = Q.dma_sems[i]
        if Q.dma_val[i] > 0:
            self._wait(Q, (sem, Q.dma_val[i]))
        self._deps(Q, reads, writes)
        Q.h.dma_start(out=out, in_=in_, **kw).then_inc(sem, 16)
        Q.dma_val[i] += 16
        tok = (sem, Q.dma_val[i])
        self._mark(tok, reads, writes)
        return tok

    def fence(self):
        _FENCE.clear()
        for Q in (self.sp, self.pool):
            for sem, v in zip(Q.dma_sems, Q.dma_val):
                if v > 0:
                    _FENCE[id(sem)] = (sem, v)
        for X in (self.pe, self.act, self.dve, self.pool, self.sp):
            if X.count > 0:
                _FENCE[id(X.sem)] = (X.sem, X.count)

    def finish(self, E):
        for Q in (self.sp, self.pool):
            for sem, v in zip(Q.dma_sems, Q.dma_val):
                if v > 0:
                    self._wait(E, (sem, v))
        for X in (self.pe, self.act, self.dve, self.pool, self.sp):
            if X is not E and X.count > 0:
                self._wait(E, (X.sem, X.count))


class Rot:
    def __init__(self, items):
        self.items = items
        self.i = 0

    def get(self):
        it = self.items[self.i % len(self.items)]
        self.i += 1
        return it


def build_program(stage="full"):
    nc = bass.Bass("TRN2", target_bir_lowering=False)
    dt_in = lambda name, shape, dt=F32: nc.dram_tensor(name, shape, dt, kind="ExternalInput").ap()
    xT_d = dt_in("xT", [D, S])
    xq_d = dt_in("xq", [TQ, D])
    pos_d = dt_in("pos", [1, S], I32)
    cst_d = dt_in("cst", [128, 1280])
    w_in_d = dt_in("w_in", [D, 1984])
    lam_d = dt_in("lamv", [4, 64])
    subg_d = dt_in("subln_g", [128, 1])
    gq_d = dt_in("gq", [128, 2])
    gkv_d = dt_in("gkv", [128, 1])
    w_uq_d = dt_in("w_uq", [256, 768])
    w_ukv_d = dt_in("w_ukv", [128, 1024])
    w_o_d = dt_in("w_o", [D, D])
    lnv_d = dt_in("lnv", [4, D])
    if stage == "full":
        w_r_d = dt_in("w_router", [D, NE])
        b_r_d = dt_in("b_router", [1, NE])
        wgu_d = dt_in("wgu_t", [NE, 8, 128, 2048])
        bgu_d = dt_in("bguT", [128, NE * 16])
        wd_d = dt_in("w_down", [NE, D, D])
        bd_d = dt_in("b_down", [NE, D])
    out_d = nc.dram_tensor("out", [TQ, D], F32, kind="ExternalOutput").ap()

    _FENCE.clear()
    with contextlib.ExitStack() as es:
        fw = FW(nc, es)
        PE, ACT, DVE, POOL, SP = fw.pe, fw.act, fw.dve, fw.pool, fw.sp

        def sb(name, shape, dt, st=es):
            return st.enter_context(nc.sbuf_tensor("sb_" + name, shape, dt))

        psb = [es.enter_context(nc.psum_tensor(f"ps{i}", [128, 512], F32)) for i in range(8)]
        pb = [Buf(excl=True) for _ in range(8)]

        cst = sb("cst", [128, 1280], F32); bcst = Buf()
        fw.dma(SP, cst[:], cst_d[:, :], [], [bcst])
        ident = cst[:, 0:128]
        ropec = cst[:, 512:516]
        cbf = sb("cbf", [128, 512], BF16); bcbf = Buf()
        fw.op(DVE, [bcst], [bcbf], lambda: nc.vector.tensor_copy(cbf[:, 0:384], cst[:, 128:512]))
        fw.op(DVE, [], [bcbf], lambda: nc.vector.memset(cbf[:, 384:512], 1.0))
        perm_bf = cbf[:, 0:128]
        masks_bf = [cbf[:, 128:256], cbf[:, 256:384]]
        ones_bf = cbf[:, 384:512]
        ones32 = sb("ones32", [128, 128], F32); bones = Buf()
        fw.op(POOL, [], [bones], lambda: nc.gpsimd.memset(ones32[:], 1.0))
        small = sb("small", [128, 64], F32); bsmall = Buf()
        fw.dma(SP, small[:, 0:1], subg_d[:, :], [], [bsmall])
        fw.dma(SP, small[:, 3:5], gq_d[:, :], [], [bsmall])
        fw.dma(SP, small[:, 5:6], gkv_d[:, :], [], [bsmall])
        fw.op(DVE, [], [bsmall], lambda: nc.vector.memset(small[:, 8:9], 1e-6))
        fw.op(DVE, [], [bsmall], lambda: nc.vector.memset(small[:, 9:10], 1e-5))
        EPS6 = small[:, 8:9]
        EPS5 = small[:, 9:10]
        lamt = sb("lamt", [128, 256], F32); blam = Buf()
        fw.dma(SP, lamt[:].rearrange("p (a b) -> p a b", a=4), lam_d.partition_broadcast(128), [], [blam])
        fw.op(DVE, [blam], [blam], lambda: nc.vector.tensor_tensor(lamt[:, 0:64], lamt[:, 0:64], lamt[:, 64:128], op=ALU.mult))
        fw.op(DVE, [blam], [blam], lambda: nc.vector.tensor_tensor(lamt[:, 128:192], lamt[:, 128:192], lamt[:, 192:256], op=ALU.mult))
        fw.op(DVE, [blam], [bsmall], lambda: nc.vector.reduce_sum(small[:, 6:7], lamt[:, 0:64], axis=AX.X))
        fw.op(DVE, [blam], [bsmall], lambda: nc.vector.reduce_sum(small[:, 7:8], lamt[:, 128:192], axis=AX.X))
        fw.op(ACT, [bsmall], [bsmall], lambda: nc.scalar.activation(small[:, 6:8], small[:, 6:8], AF.Exp))
        fw.op(DVE, [bsmall], [bsmall], lambda: nc.vector.tensor_tensor(small[:, 2:3], small[:, 7:8], small[:, 6:7], op=ALU.subtract))
        fw.op(DVE, [bsmall], [bsmall], lambda: nc.vector.tensor_scalar(small[:, 2:3], small[:, 2:3], -LAM_INIT, None, op0=ALU.add))
        fw.op(DVE, [bsmall], [bsmall], lambda: nc.vector.tensor_scalar(small[:, 1:2], small[:, 0:1], 1.0 - LAM_INIT, None, op0=ALU.mult))

        otx = sb("otx", [128, 8, TQ], BF16)
        botx = [Buf() for _ in range(16)]

        with contextlib.ExitStack() as sa:
            cosT = sb("cosT", [128, S], F32, sa)
            sinS = sb("sinS", [128, S], F32, sa)
            btab = [Buf() for _ in range(8)]
            ckvn = sb("ckvn", [128, S], BF16, sa); bckvn = [Buf() for _ in range(8)]
            cqn = sb("cqn", [128, 2, TQ], BF16, sa); bcqn = [Buf() for _ in range(4)]
            KR = sb("KR", [128, S], BF16, sa); bKR = [Buf() for _ in range(8)]
            fw.op(POOL, [], bKR, lambda: nc.gpsimd.memset(KR[64:128, :], 0.0))
            s32 = Rot([(sb(f"s32_{i}", [128, 512], F32, sa), Buf()) for i in range(8)])
            s16 = Rot([(sb(f"s16_{i}", [128, 512], BF16, sa), Buf()) for i in range(4)])
            si32 = sb("si32", [128, 512], I32, sa); bsi32 = Buf()
            tmpb = Rot([(psb[i], pb[i]) for i in (6, 7, 0, 1)])

            def mm_acc(out_ap, pairs, reads, writes):
                n = len(pairs)
                for i, (l, r) in enumerate(pairs):
                    fw.op(PE, reads, writes,
                          lambda: nc.tensor.matmul(out_ap, l, r, start=(i == 0), stop=(i == n - 1)),
                          inc=(i == n - 1))

            def rope(src_ps, bsrc, rows, tc, dst_ap, bdst):
                cols = slice(tc * 512, (tc + 1) * 512)
                import os as _os
                _cut = int(_os.environ.get("ROPE_CUT", "9")) if rows == 128 else 9
                if _cut < 1:
                    return
                hb, bhb = s16.get()
                fw.op(ACT, [bsrc], [bhb], lambda: nc.scalar.copy(hb[0:rows, :], src_ps))
                if _cut < 2:
                    return
                sw, bsw = tmpb.get()
                fw.op(PE, [bhb, bcbf], [bsw], lambda: nc.tensor.matmul(sw[0:rows, :], perm_bf[0:rows, 0:rows], hb[0:rows, :], start=True, stop=True))
                if _cut < 3:
                    return
                t1, bt1 = s32.get()
                fw.op(DVE, [bsrc, btab[tc]], [bt1], lambda: nc.vector.tensor_tensor(t1[0:rows, :], src_ps, cosT[0:rows, cols], op=ALU.mult))
                if _cut < 4:
                    return
                t2, bt2 = s32.get()
                fw.op(DVE, [bsw, btab[tc]], [bt2], lambda: nc.vector.tensor_tensor(t2[0:rows, :], sw[0:rows, :], sinS[0:rows, cols], op=ALU.mult))
                if _cut < 5:
                    return
                fw.op(POOL, [bt1, bt2], [bdst], lambda: nc.gpsimd.tensor_tensor(dst_ap, t1[0:rows, :], t2[0:rows, :], op=ALU.add))

            def rms_scale(ps_list, bps_list, n_feat, eps, gcols, dst_aps, bdst):
                sqs = []
                for ps_ap, bps in zip(ps_list, bps_list):
                    sq, bsq = s32.get()
                    fw.op(ACT, [bps], [bsq], lambda: nc.scalar.activation(sq[:], ps_ap, AF.Square))
                    sqs.append((sq, bsq))
                ss, bss = tmpb.get()
                for i, (sq, bsq) in enumerate(sqs):
                    fw.op(PE, [bsq, bones], [bss],
                          lambda: nc.tensor.matmul(ss[:], ones32[:], sq[:], start=(i == 0), stop=(i == len(sqs) - 1)),
                          inc=(i == len(sqs) - 1))
                rstd, brstd = s32.get()
                fw.op(ACT, [bss, bsmall], [brstd], lambda: nc.scalar.activation(rstd[:], ss[:], AF.Sqrt, bias=eps, scale=1.0 / n_feat))
                fw.op(DVE, [brstd], [brstd], lambda: nc.vector.reciprocal(rstd[:], rstd[:]))
                for ps_ap, bps, gc, dst in zip(ps_list, bps_list, gcols, dst_aps):
                    fw.op(DVE, [bps, brstd, bsmall], [bdst],
                          lambda: nc.vector.scalar_tensor_tensor(dst, in0=ps_ap, scalar=gc, in1=rstd[:], op0=ALU.mult, op1=ALU.mult))

            def attention(nsub, s_emit, s_reads, Vt, bV, scale, finalize):
                S_B = [(psb[0], pb[0]), (psb[1], pb[1])]
                O_B = [(psb[2], pb[2]), (psb[3], pb[3])]
                L_B = [(psb[4], pb[4]), (psb[5], pb[5])]
                pending = [None]
                for g in range(4):
                    units = []
                    nkb = 4 * g + 4
                    for half in (0, 1):
                        for kl in range(nkb):
                            i = kl - 4 * g
                            col0 = 0 if i < 0 else i * 128
                            mt = None if i < 0 else half
                            for c in range(nsub):
                                units.append((c, half * 16 + kl, col0, mt))
                    nun = len(units)
                    first = [True] * nsub
                    last_idx = {}
                    for ui, u in enumerate(units):
                        last_idx[u[0]] = ui
                    pts = {}

                    def emit_s(ui):
                        c, kb, col0, mt = units[ui]
                        sbk, bsbk = S_B[ui % 2]
                        s_emit(c, kb, g, col0, sbk, bsbk)
                        pT, bpT = s16.get()
                        fw.op(ACT, [bsbk], [bpT], lambda: nc.scalar.activation(pT[:, col0:512], sbk[:, col0:512], AF.Exp, scale=scale))
                        if mt is not None:
                            fw.op(POOL, [bpT, bcbf], [bpT], lambda: nc.gpsimd.tensor_tensor(pT[:, col0:col0 + 128], pT[:, col0:col0 + 128], masks_bf[mt], op=ALU.mult))
                        pts[ui] = (pT, bpT)

                    def emit_pv(ui):
                        c, kb, col0, mt = units[ui]
                        pT, bpT = pts.pop(ui)
                        o, bo = O_B[c]
                        l, bl = L_B[c]
                        st = first[c]
                        first[c] = False
                        sp_ = (last_idx[c] == ui)
                        fw.op(PE, [bpT, bV[kb // 4]], [bo], lambda: nc.tensor.matmul(o[:, col0:512], Vt[:, kb, :], pT[:, col0:512], start=st, stop=sp_), inc=False)
                        fw.op(PE, [bpT, bcbf], [bl], lambda: nc.tensor.matmul(l[:, col0:512], ones_bf, pT[:, col0:512], start=st, stop=sp_))

                    LOOK = 2
                    for ui in range(min(LOOK, nun)):
                        emit_s(ui)
                    if pending[0] is not None:
                        pending[0]()
                    for ui in range(nun):
                        emit_pv(ui)
                        if ui + LOOK < nun:
                            emit_s(ui + LOOK)
                    pending[0] = (lambda g=g: finalize(g, O_B, L_B))
                pending[0]()

            with contextlib.ExitStack() as sx:
                xTb = sb("xTb", [128, 8, S], BF16, sx); bxT = [Buf() for _ in range(8)]
                xT_v = xT_d.rearrange("(dc p) t -> p dc t", p=128)
                for tc in range(8):
                    fw.dma(POOL, xTb[:, :, tc * 512:(tc + 1) * 512], xT_v[:, :, tc * 512:(tc + 1) * 512], [], [bxT[tc]])
                w_in_v = w_in_d.rearrange("(dc p) c -> p dc c", p=128)

                for tc in range(8):
                    cols = slice(tc * 512, (tc + 1) * 512)
                    fw.dma(SP, si32[:], pos_d[0:1, cols].partition_broadcast(128), [], [bsi32])
                    ang, bang = s32.get()
                    fw.op(DVE, [bsi32], [bang], lambda: nc.vector.tensor_copy(ang[:], si32[:]))
                    fw.op(DVE, [bang, bcst], [bang], lambda: nc.vector.tensor_scalar(ang[:], ang[:], ropec[:, 0:1], None, op0=ALU.mult))
                    for which in (0, 1):
                        shift = 0.5 if which == 0 else 0.75
                        u, bu = s32.get()
                        fw.op(DVE, [bang], [bu], lambda: nc.vector.tensor_scalar(u[:], ang[:], 1.0 / TWO_PI, shift, op0=ALU.mult, op1=ALU.add))
                        ki, bki = s32.get()
                        kiv = ki[:].bitcast(I32)
                        fw.op(DVE, [bu], [bki], lambda: nc.vector.tensor_copy(kiv, u[:]))
                        kf, bkf = s32.get()
                        fw.op(POOL, [bki], [bkf], lambda: nc.gpsimd.tensor_copy(kf[:], kiv))
                        fw.op(POOL, [bkf, bu], [bu], lambda: nc.gpsimd.tensor_tensor(u[:], u[:], kf[:], op=ALU.subtract))
                        fw.op(DVE, [bu], [bkf], lambda: nc.vector.scalar_tensor_tensor(kf[:], in0=u[:], scalar=0.0, in1=u[:], op0=ALU.is_lt, op1=ALU.add))
                        if which == 0:
                            fw.op(ACT, [bkf, bcst], [btab[tc]], lambda: nc.scalar.activation(sinS[:, cols], kf[:], AF.Sin, bias=ropec[:, 2:3], scale=ropec[:, 1:2]))
                        else:
                            fw.op(ACT, [bkf, bcst], [btab[tc]], lambda: nc.scalar.activation(cosT[:, cols], kf[:], AF.Sin, bias=ropec[:, 3:4], scale=TWO_PI * (1.0 - 1e-6)))

                if stage.startswith("tabtt"):
                    import os as _os
                    r0, r1 = [int(v) for v in _os.environ.get("TT_ROWS", "0,128").split(",")]
                    mode = _os.environ.get("TT_MODE", "psum_cos")
                    t1, bt1 = s32.get()
                    kps, bkps = tmpb.get()
                    fw.op(PE, [bcbf], [bkps], lambda: nc.tensor.matmul(kps[:], perm_bf, cbf[:, 0:512], start=True, stop=True))
                    if mode == "psum_cos":
                        fw.op(DVE, [bkps, btab[0]], [bt1], lambda: nc.vector.tensor_tensor(t1[r0:r1, :], kps[r0:r1, :], cosT[r0:r1, 0:512], op=ALU.mult))
                    elif mode == "sb_cos":
                        t2, bt2 = s32.get()
                        fw.op(DVE, [], [bt2], lambda: nc.vector.memset(t2[:], 1.0))
                        fw.op(DVE, [bt2, btab[0]], [bt1], lambda: nc.vector.tensor_tensor(t1[r0:r1, :], t2[r0:r1, :], cosT[r0:r1, 0:512], op=ALU.mult))
                    elif mode == "psum_sb":
                        t2, bt2 = s32.get()
                        fw.op(DVE, [], [bt2], lambda: nc.vector.memset(t2[:], 1.0))
                        fw.op(DVE, [bkps, bt2], [bt1], lambda: nc.vector.tensor_tensor(t1[r0:r1, :], kps[r0:r1, :], t2[r0:r1, :], op=ALU.mult))
                    fw.dma(SP, out_d[0:128, 0:512], t1[:], [bt1], [])
                    fw.dma(SP, out_d[128:256, 0:512], cosT[:, 0:512], [btab[0]], [])
                    fw.dma(SP, out_d[256:384, 0:512], sinS[:, 0:512], [btab[0]], [])
                    fw.finish(SP)
                    return nc
                if stage == "tab":
                    fw.finish(SP)
                    return nc
                with contextlib.ExitStack() as sm0:
                    WC = sb("WC", [128, 8, 448], BF16, sm0); bWC = Buf()
                    fw.dma(POOL, WC[:], w_in_v[:, :, 1536:1984], [], [bWC])
                    for tc in range(8):
                        cols = slice(tc * 512, (tc + 1) * 512)
                        ckv, bckv = tmpb.get()
                        mm_acc(ckv[:], [(WC[:, dc, 256:384], xTb[:, dc, cols]) for dc in range(8)], [bWC, bxT[tc]], [bckv])
                        rms_scale([ckv[:]], [bckv], 128.0, EPS6, [small[:, 5:6]], [ckvn[:, cols]], bckvn[tc])
                        kr, bkr = tmpb.get()
                        mm_acc(kr[0:64, :], [(WC[:, dc, 384:448], xTb[:, dc, cols]) for dc in range(8)], [bWC, bxT[tc]], [bkr])
                        rope(kr[0:64, :], bkr, 64, tc, KR[0:64, cols], bKR[tc])
                        if tc < 4:
                            cq0, bcq0 = tmpb.get()
                            mm_acc(cq0[:], [(WC[:, dc, 0:128], xTb[:, dc, cols]) for dc in range(8)], [bWC, bxT[tc]], [bcq0])
                            cq1, bcq1 = tmpb.get()
                            mm_acc(cq1[:], [(WC[:, dc, 128:256], xTb[:, dc, cols]) for dc in range(8)], [bWC, bxT[tc]], [bcq1])
                            rms_scale([cq0[:], cq1[:]], [bcq0, bcq1], 256.0, EPS6, [small[:, 3:4], small[:, 4:5]],
                                      [cqn[:, 0, cols], cqn[:, 1, cols]], bcqn[tc])

                if stage == "m0":
                    fw.finish(SP)
                    return nc
                fw.fence()
                with contextlib.ExitStack() as sd:
                    WQ = sb("WQ", [128, 8, 128], BF16, sd); WK = sb("WK", [128, 8, 128], BF16, sd); WV = sb("WV", [128, 8, 128], BF16, sd)
                    bW = Buf()
                    KT = sb("KT", [128, S], BF16, sd); bKT = [Buf() for _ in range(8)]
                    QT = sb("QT", [128, TQ], BF16, sd); bQT = [Buf() for _ in range(4)]
                    Vt = sb("Vt", [128, 32, 128], BF16, sd); bV = [Buf() for _ in range(8)]
                    for h in range(4):
                        fw.dma(POOL, WQ[:], w_in_v[:, :, 128 * h:128 * h + 128], [], [bW])
                        fw.dma(POOL, WK[:], w_in_v[:, :, 512 + 128 * h:512 + 128 * h + 128], [], [bW])
                        fw.dma(POOL, WV[:], w_in_v[:, :, 1024 + 128 * h:1024 + 128 * h + 128], [], [bW])
                        if stage == "dprojD":
                            fw.finish(SP)
                            return nc
                        import os as _os
                        _ntc = int(_os.environ.get("DPROJ_NTC", "8"))
                        _noq = _os.environ.get("DPROJ_NOQ", "0") == "1"
                        for tc in range(_ntc):
                            cols = slice(tc * 512, (tc + 1) * 512)
                            if stage != "dprojV":
                                kps, bkps = tmpb.get()
                                mm_acc(kps[:], [(WK[:, dc, :], xTb[:, dc, cols]) for dc in range(8)], [bW, bxT[tc]], [bkps])
                                rope(kps[:], bkps, 128, tc, KT[:, cols], bKT[tc])
                            if tc < 4 and stage != "dprojV" and not _noq:
                                qps, bqps = tmpb.get()
                                mm_acc(qps[:], [(WQ[:, dc, :], xTb[:, dc, cols]) for dc in range(8)], [bW, bxT[tc]], [bqps])
                                rope(qps[:], bqps, 128, tc, QT[:, cols], bQT[tc])
                            if stage == "dprojK":
                                continue
                            vps, bvps = tmpb.get()
                            for i in range(4):
                                mm_acc(vps[:, i * 128:(i + 1) * 128],
                                       [(xTb[:, dc, tc * 512 + i * 128: tc * 512 + (i + 1) * 128], WV[:, dc, :]) for dc in range(8)],
                                       [bW, bxT[tc]], [bvps])
                            fw.op(ACT, [bvps], [bV[tc]], lambda: nc.scalar.copy(Vt[:, tc * 4:(tc + 1) * 4, :], vps[:].rearrange("p (a b) -> p a b", a=4)))

                        if stage in ("dproj", "dprojK", "dprojV"):
                            fw.finish(SP)
                            return nc
                        def s_emit(c, kb, g, col0, sbk, bsbk):
                            fw.op(PE, [bKT[kb // 4], bQT[g]], [bsbk],
                                  lambda: nc.tensor.matmul(sbk[:, col0:512], KT[64 * c:64 * c + 64, kb * 128:(kb + 1) * 128],
                                                           QT[64 * c:64 * c + 64, g * 512 + col0:(g + 1) * 512], start=True, stop=True))

                        def fin_diff(g, O_B, L_B, h=h):
                            ds = []
                            for c in range(2):
                                rl, brl = s32.get()
                                fw.op(DVE, [L_B[c][1]], [brl], lambda: nc.vector.reciprocal(rl[:], L_B[c][0][:]))
                                fw.op(DVE, [O_B[c][1], brl], [brl], lambda: nc.vector.tensor_tensor(rl[:], O_B[c][0][:], rl[:], op=ALU.mult))
                                ds.append((rl, brl))
                            dd, bdd = s32.get()
                            fw.op(DVE, [ds[0][1], ds[1][1], bsmall], [bdd],
                                  lambda: nc.vector.scalar_tensor_tensor(dd[:], in0=ds[1][0][:], scalar=small[:, 2:3], in1=ds[0][0][:], op0=ALU.mult, op1=ALU.add))
                            sq, bsq = s32.get()
                            fw.op(ACT, [bdd], [bsq], lambda: nc.scalar.activation(sq[:], dd[:], AF.Square))
                            ss, bss = tmpb.get()
                            fw.op(PE, [bsq, bones], [bss], lambda: nc.tensor.matmul(ss[:], ones32[:], sq[:], start=True, stop=True))
                            rstd, brstd = s32.get()
                            fw.op(ACT, [bss, bsmall], [brstd], lambda: nc.scalar.activation(rstd[:], ss[:], AF.Sqrt, bias=EPS5, scale=1.0 / 128.0))
                            fw.op(DVE, [brstd], [brstd], lambda: nc.vector.reciprocal(rstd[:], rstd[:]))
                            wr = [botx[4 * g + i] for i in range(4)]
                            fw.op(DVE, [bdd, brstd, bsmall], wr,
                                  lambda: nc.vector.scalar_tensor_tensor(otx[:, h, g * 512:(g + 1) * 512], in0=dd[:], scalar=small[:, 1:2], in1=rstd[:], op0=ALU.mult, op1=ALU.mult))

                        attention(2, s_emit, None, Vt, bV, 64.0 ** -0.5, fin_diff)
                        if stage == "datt":
                            fw.finish(SP)
                            return nc
            fw.fence()
            with contextlib.ExitStack() as sm:
                wuq = sb("wuq", [128, 2, 768], BF16, sm); bwuq = Buf()
                wukv = sb("wukv", [128, 1024], BF16, sm); bwukv = Buf()
                fw.dma(POOL, wuq[:], w_uq_d.rearrange("(rc p) c -> p rc c", p=128), [], [bwuq])
                fw.dma(POOL, wukv[:], w_ukv_d[:, :], [], [bwukv])
                KTm = sb("KTm", [128, S], BF16, sm); bKTm = [Buf() for _ in range(8)]
                Vm = sb("Vm", [128, 32, 128], BF16, sm); bVm = [Buf() for _ in range(8)]
                QTn = sb("QTn", [128, TQ], BF16, sm); bQTn = [Buf() for _ in range(4)]
                QTr = sb("QTr", [128, TQ], BF16, sm); bQTr = [Buf() for _ in range(4)]
                fw.op(POOL, [], bQTr, lambda: nc.gpsimd.memset(QTr[64:128, :], 0.0))
                for h in range(4):
                    for tc in range(8):
                        cols = slice(tc * 512, (tc + 1) * 512)
                        kn, bkn = tmpb.get()
                        fw.op(PE, [bwukv, bckvn[tc]], [bkn], lambda: nc.tensor.matmul(kn[:], wukv[:, h * 256:h * 256 + 128], ckvn[:, cols], start=True, stop=True))
                        fw.op(ACT, [bkn], [bKTm[tc]], lambda: nc.scalar.copy(KTm[:, cols], kn[:]))
                        vps, bvps = tmpb.get()
                        for i in range(4):
                            fw.op(PE, [bwukv, bckvn[tc]], [bvps],
                                  lambda: nc.tensor.matmul(vps[:, i * 128:(i + 1) * 128], ckvn[:, tc * 512 + i * 128: tc * 512 + (i + 1) * 128],
                                                           wukv[:, h * 256 + 128:h * 256 + 256], start=True, stop=True), inc=(i == 3))
                        fw.op(DVE, [bvps], [bVm[tc]], lambda: nc.vector.tensor_copy(Vm[:, tc * 4:(tc + 1) * 4, :], vps[:].rearrange("p (a b) -> p a b", a=4)))
                        if tc < 4:
                            qn, bqn = tmpb.get()
                            mm_acc(qn[:], [(wuq[:, rc, h * 192:h * 192 + 128], cqn[:, rc, cols]) for rc in range(2)], [bwuq, bcqn[tc]], [bqn])
                            fw.op(ACT, [bqn], [bQTn[tc]], lambda: nc.scalar.copy(QTn[:, cols], qn[:]))
                            qr, bqr = tmpb.get()
                            mm_acc(qr[0:64, :], [(wuq[:, rc, h * 192 + 128:h * 192 + 192], cqn[:, rc, cols]) for rc in range(2)], [bwuq, bcqn[tc]], [bqr])
                            rope(qr[0:64, :], bqr, 64, tc, QTr[0:64, cols], bQTr[tc])

                    if stage == "mproj":
                        fw.finish(SP)
                        return nc
                    def s_emit_m(c, kb, g, col0, sbk, bsbk):
                        fw.op(PE, [bKTm[kb // 4], bQTn[g]], [bsbk],
                              lambda: nc.tensor.matmul(sbk[:, col0:512], KTm[:, kb * 128:(kb + 1) * 128], QTn[:, g * 512 + col0:(g + 1) * 512], start=True, stop=False), inc=False)
                        fw.op(PE, [bKR[kb // 4], bQTr[g]], [bsbk],
                              lambda: nc.tensor.matmul(sbk[:, col0:512], KR[:, kb * 128:(kb + 1) * 128], QTr[:, g * 512 + col0:(g + 1) * 512], start=False, stop=True))

                    def fin_mla(g, O_B, L_B, h=h):
                        rl, brl = s32.get()
                        fw.op(DVE, [L_B[0][1]], [brl], lambda: nc.vector.reciprocal(rl[:], L_B[0][0][:]))
                        wr = [botx[4 * g + i] for i in range(4)]
                        fw.op(DVE, [O_B[0][1], brl], wr, lambda: nc.vector.tensor_tensor(otx[:, 4 + h, g * 512:(g + 1) * 512], O_B[0][0][:], rl[:], op=ALU.mult))

                    attention(1, s_emit_m, None, Vm, bVm, 192.0 ** -0.5, fin_mla)
                    if stage == "matt":
                        fw.finish(SP)
                        return nc

        fw.fence()
        ACC = sb("ACC", [128, 16, D], F32); bACC = [Buf() for _ in range(16)]
        sm2 = sb("sm2", [128, 16, 8], F32); bsm2 = [Buf() for _ in range(16)]

        def layer_norm(z, bz, tb, lnbc, blnbc, dst, bdst, junk, bjunk):
            sc = sm2[:, tb, :]
            bs = bsm2[tb]
            fw.op(DVE, [bz], [bs], lambda: nc.vector.reduce_sum(sc[:, 0:1], z, axis=AX.X))
            fw.op(DVE, [bs], [bs], lambda: nc.vector.tensor_scalar(sc[:, 1:2], sc[:, 0:1], -1.0 / D, None, op0=ALU.mult))
            fw.op(ACT, [bz, bs], [bz], lambda: nc.scalar.activation(z, z, AF.Identity, bias=sc[:, 1:2]))
            fw.op(ACT, [bz], [bjunk], lambda: nc.scalar.activation(junk, z, AF.Square))
            fw.op(DVE, [bjunk], [bs], lambda: nc.vector.reduce_sum(sc[:, 2:3], junk, axis=AX.X))
            fw.op(ACT, [bs, bsmall], [bs], lambda: nc.scalar.activation(sc[:, 3:4], sc[:, 2:3], AF.Sqrt, bias=EPS5, scale=1.0 / D))
            fw.op(DVE, [bs], [bs], lambda: nc.vector.reciprocal(sc[:, 3:4], sc[:, 3:4]))
            fw.op(DVE, [bz, bs, blnbc], [bz], lambda: nc.vector.scalar_tensor_tensor(z, in0=z, scalar=sc[:, 3:4], in1=lnbc[:, 0, :], op0=ALU.mult, op1=ALU.mult))
            fw.op(POOL, [bz, blnbc], [bdst], lambda: nc.gpsimd.tensor_tensor(dst, z, lnbc[:, 1, :], op=ALU.add))

        with contextlib.ExitStack() as so:
            ln1bc = sb("ln1bc", [128, 2, D], F32, so); bln1 = Buf()
            fw.dma(SP, ln1bc[:], lnv_d[0:2, :].partition_broadcast(128), [], [bln1])
            wo = sb("wo", [128, 8, D], BF16, so); bwo = Buf()
            fw.dma(POOL, wo[:], w_o_d.rearrange("(hh p) o -> p hh o", p=128), [], [bwo])
            xqt = Rot([(sb(f"xqt{i}", [128, D], F32, so), Buf()) for i in range(2)])
            zt = Rot([(sb(f"zt{i}", [128, D], F32, so), Buf()) for i in range(2)])
            jk = Rot([(sb(f"jk{i}", [128, D], F32, so), Buf()) for i in range(2)])
            mixb = Rot([((psb[0], pb[0]), (psb[1], pb[1])), ((psb[2], pb[2]), (psb[3], pb[3]))])
            for tb in range(16):
                xt_, bxt_ = xqt.get()
                fw.dma(SP, xt_[:], xq_d[tb * 128:(tb + 1) * 128, :], [], [bxt_])
                banks = mixb.get()
                z, bz = zt.get()
                for half in range(2):
                    mps, bmps = banks[half]
                    for hh in range(8):
                        fw.op(PE, [botx[tb], bwo], [bmps],
                              lambda: nc.tensor.matmul(mps[:], otx[:, hh, tb * 128:(tb + 1) * 128], wo[:, hh, half * 512:(half + 1) * 512], start=(hh == 0), stop=(hh == 7)),
                              inc=(hh == 7))
                    fw.op(DVE, [bmps, bxt_], [bz],
                          lambda: nc.vector.scalar_tensor_tensor(z[:, half * 512:(half + 1) * 512], in0=xt_[:, half * 512:(half + 1) * 512], scalar=DN_ALPHA, in1=mps[:], op0=ALU.mult, op1=ALU.add))
                j_, bj_ = jk.get()
                layer_norm(z[:], bz, tb, ln1bc, bln1, ACC[:, tb, :], bACC[tb], j_[:], bj_)

        fw.fence()
        if stage == "ln1":
            for tb in range(16):
                fw.dma(SP, out_d[tb * 128:(tb + 1) * 128, :], ACC[:, tb, :], [bACC[tb]], [])
            fw.finish(SP)
            return nc

        G = sb("G", [128, 16, NE], F32); bG = [Buf() for _ in range(16)]
        MK = sb("MK", [128, 16, NE], F32); bMK = [Buf() for _ in range(16)]
        posm = sb("posm", [128, 16, NE], F32); bposm = [Buf() for _ in range(16)]
        posmT = sb("posmT", [NE, TQ], F32); bposmT = [Buf() for _ in range(4)]
        X1B = otx[:].rearrange("p a b -> p (a b)").rearrange("p (t d) -> p t d", t=16)
        bguT = sb("bguT", [128, NE * 16], F32); bbgu = Buf()
        fw.dma(SP, bguT[:], bgu_d[:, :], [], [bbgu])
        with contextlib.ExitStack() as sr:
            wr32 = sb("wr32", [128, 8, NE], F32, sr); bwr = Buf()
            fw.dma(SP, wr32[:], w_r_d.rearrange("(dc p) e -> p dc e", p=128), [], [bwr])
            brbc = sb("brbc", [128, NE], F32, sr); bbr = Buf()
            fw.dma(SP, brbc[:], b_r_d.partition_broadcast(128), [], [bbr])
            bd32 = sb("bd32", [NE, D], F32, sr); bbd = Buf()
            fw.dma(SP, bd32[:], bd_d[:, :], [], [bbd])
            GT = sb("GT", [NE, TQ], F32, sr); bGT = [Buf() for _ in range(16)]
            x1T32 = Rot([(sb(f"x1T32_{i}", [128, 8, 128], F32, sr), Buf()) for i in range(2)])
            rt = Rot([(sb(f"rt{i}", [128, 128], F32, sr), Buf()) for i in range(2)])
            tpb = Rot([((psb[0], pb[0]), (psb[1], pb[1])), ((psb[2], pb[2]), (psb[3], pb[3]))])
            tmp2 = Rot([(psb[i], pb[i]) for i in (4, 5, 6, 7)])
            for tb in range(16):
                fw.op(DVE, [bACC[tb]], [botx[tb]], lambda: nc.vector.tensor_copy(X1B[:, tb, :], ACC[:, tb, :]))
                banks = tpb.get()
                xT32, bxT32 = x1T32.get()
                for hb_ in range(2):
                    tp, btp = banks[hb_]
                    for q in range(4):
                        dc = hb_ * 4 + q
                        fw.op(PE, [bACC[tb], bcst], [btp], lambda: nc.tensor.transpose(tp[:, q * 128:(q + 1) * 128], ACC[:, tb, dc * 128:(dc + 1) * 128], ident), inc=(q == 3))
                    fw.op(ACT, [btp], [bxT32], lambda: nc.scalar.copy(xT32[:, hb_ * 4:(hb_ + 1) * 4, :], tp[:].rearrange("p (a b) -> p a b", a=4)))
                lgp, blgp = tmp2.get()
                for dc in range(8):
                    fw.op(PE, [bxT32, bwr], [blgp], lambda: nc.tensor.matmul(lgp[:, 0:NE], xT32[:, dc, :], wr32[:, dc, :], start=(dc == 0), stop=(dc == 7)), inc=(dc == 7))
                r_, br_ = rt.get()
                lg = r_[:, 0:32]; m8 = r_[:, 32:40]; ex = r_[:, 40:72]; mk = r_[:, 72:104]; misc = r_[:, 104:112]
                fw.op(DVE, [blgp, bbr], [br_], lambda: nc.vector.tensor_tensor(lg, lgp[:, 0:NE], brbc[:], op=ALU.add))
                fw.op(DVE, [br_], [br_], lambda: nc.vector.max(out=m8, in_=lg))
                fw.op(DVE, [br_], [br_], lambda: nc.vector.tensor_scalar(misc[:, 0:1], m8[:, 0:1], -1.0, None, op0=ALU.mult))
                fw.op(ACT, [br_], [br_], lambda: nc.scalar.activation(ex, lg, AF.Exp, bias=misc[:, 0:1]))
                fw.op(DVE, [br_], [bMK[tb]], lambda: nc.vector.tensor_scalar(MK[:, tb, :], lg, m8[:, 3:4], None, op0=ALU.is_ge))
                fw.op(DVE, [br_, bMK[tb]], [br_], lambda: nc.vector.tensor_tensor(ex, ex, MK[:, tb, :], op=ALU.mult))
                fw.op(DVE, [br_], [br_], lambda: nc.vector.reduce_sum(misc[:, 1:2], ex, axis=AX.X))
                fw.op(DVE, [br_], [br_], lambda: nc.vector.reciprocal(misc[:, 2:3], misc[:, 1:2]))
                fw.op(DVE, [br_], [bG[tb]], lambda: nc.vector.tensor_scalar(G[:, tb, :], ex, misc[:, 2:3], None, op0=ALU.mult))
                gtp, bgtp = tmp2.get()
                fw.op(PE, [bG[tb], bcst], [bgtp], lambda: nc.tensor.transpose(gtp[0:NE, 0:128], G[:, tb, :], ident))
                fw.op(ACT, [bgtp], [bGT[tb]], lambda: nc.scalar.copy(GT[:, tb * 128:(tb + 1) * 128], gtp[0:NE, 0:128]))
                for half in range(2):
                    bdp, bbdp = tmp2.get()
                    fw.op(PE, [bGT[tb], bbd], [bbdp], lambda: nc.tensor.matmul(bdp[:], GT[:, tb * 128:(tb + 1) * 128], bd32[:, half * 512:(half + 1) * 512], start=True, stop=True))
                    fw.op(DVE, [bbdp, bACC[tb]], [bACC[tb]],
                          lambda: nc.vector.scalar_tensor_tensor(ACC[:, tb, half * 512:(half + 1) * 512], in0=ACC[:, tb, half * 512:(half + 1) * 512], scalar=DN_ALPHA, in1=bdp[:], op0=ALU.mult, op1=ALU.add))

            triu = cst[:, 1024:1152]
            for tb in range(16):
                pp, bpp = tmp2.get()
                fw.op(PE, [bMK[tb], bcst], [bpp], lambda: nc.tensor.matmul(pp[:, 0:NE], triu, MK[:, tb, :], start=True, stop=(tb == 0)), inc=(tb == 0))
                for t2_ in range(tb):
                    fw.op(PE, [bMK[t2_], bones], [bpp], lambda: nc.tensor.matmul(pp[:, 0:NE], ones32[:], MK[:, t2_, :], start=False, stop=(t2_ == tb - 1)), inc=(t2_ == tb - 1))
                fw.op(DVE, [bpp, bMK[tb]], [bposm[tb]], lambda: nc.vector.scalar_tensor_tensor(posm[:, tb, :], in0=pp[:, 0:NE], scalar=1.0, in1=MK[:, tb, :], op0=ALU.add, op1=ALU.mult))
                fw.op(DVE, [bposm[tb]], [bposm[tb]], lambda: nc.vector.tensor_scalar(posm[:, tb, :], posm[:, tb, :], -1.0, None, op0=ALU.add))
                ptp, bptp = tmp2.get()
                fw.op(PE, [bposm[tb], bcst], [bptp], lambda: nc.tensor.transpose(ptp[0:NE, 0:128], posm[:, tb, :], ident))
                fw.op(ACT, [bptp], [bposmT[tb // 4]], lambda: nc.scalar.copy(posmT[:, tb * 128:(tb + 1) * 128], ptp[0:NE, 0:128]))
        fw.fence()
        with contextlib.ExitStack() as se:
            NSB = CAP // 128
            stg = Rot([(sb(f"stg{i}", [128, 2048], F32, se), Buf()) for i in range(2)])
            wgr = Rot([(sb(f"wg{i}", [128, 8, 2, 128], BF16, se), Buf()) for i in range(3)])
            Wd = sb("Wd", [128, 8, D], BF16, se); bWd = [Buf() for _ in range(4)]
            xgT = sb("xgT", [128, 8, CAP], BF16, se); bxg = [Buf() for _ in range(8)]
            ACTT = sb("ACTT", [128, 8, CAP], BF16, se); bACTT = [Buf() for _ in range(8)]
            Sel = sb("Sel", [128, 16, CAP], BF16, se); bSel = [Buf() for _ in range(16)]
            SelT = Rot([(sb(f"SelT{i}", [128, NSB, 512], BF16, se), Buf()) for i in range(2)])
            yb = sb("yb", [128, NSB, D], BF16, se); byb = [Buf() for _ in range(NSB)]
            gc_ = sb("gc", [128, CAP], F32, se); bgc_ = Buf()
            sg_ = sb("sg", [128, CAP], F32, se); bsg_ = Buf()
            uc_ = sb("uc", [128, CAP], F32, se); buc_ = Buf()
            le = sb("le", [NE, 128], F32, se); ble = Buf()
            busT = sb("busT", [128, NE * 8], F32, se); bbus = Buf()
            fw.op(DVE, [bbgu], [bbus], lambda: nc.vector.tensor_scalar(busT[:].rearrange("p (e c) -> p e c", c=8), bguT[:].rearrange("p (e c) -> p e c", c=16)[:, :, 8:16], 1.0 / 1.702, None, op0=ALU.mult))
            gab = Rot([(psb[i], pb[i]) for i in (0, 1)])
            gub = Rot([((psb[2], pb[2]), (psb[3], pb[3])), ((psb[4], pb[4]), (psb[5], pb[5]))])
            yb_b = Rot([(psb[i], pb[i]) for i in (6, 7)])
            wd_v = wd_d.rearrange("e (fc p) o -> e p fc o", p=128)
            iotaC = cst[:, 640:640 + CAP]
            kiota = cst[0:NE, 1152:1280]

            def load_wg(e, fc):
                st_, bst_ = stg.get()
                fw.dma(SP, st_[:], wgu_d[e, fc], [], [bst_])
                wg_, bwg_ = wgr.get()
                fw.op(ACT, [bst_], [bwg_], lambda: nc.scalar.copy(wg_[:].rearrange("p a b c -> p (a b c)"), st_[:]))
                return wg_, bwg_

            def load_wd(e, pc):
                st_, bst_ = stg.get()
                fw.dma(SP, st_[:].rearrange("p (a b) -> p a b", a=2), wd_v[e, :, 2 * pc:2 * pc + 2, :], [], [bst_])
                fw.op(ACT, [bst_], [bWd[pc]], lambda: nc.scalar.copy(Wd[:, 2 * pc:2 * pc + 2, :], st_[:].rearrange("p (a b) -> p a b", a=2)))

            def build_sel(e):
                for tb in range(16):
                    fw.op(DVE, [bposm[tb], bcst], [bSel[tb]], lambda: nc.vector.tensor_scalar(Sel[:, tb, :], iotaC, posm[:, tb, e:e + 1], None, op0=ALU.is_equal))

            def gather(e):
                for dc in range(8):
                    gp, bgp = gab.get()
                    for tb in range(16):
                        fw.op(PE, [botx[tb], bSel[tb]], [bgp], lambda: nc.tensor.matmul(gp[:, 0:CAP], X1B[:, tb, dc * 128:(dc + 1) * 128], Sel[:, tb, :], start=(tb == 0), stop=(tb == 15)), inc=(tb == 15))
                    fw.op(ACT, [bgp], [bxg[dc]], lambda: nc.scalar.copy(xgT[:, dc, :], gp[:, 0:CAP]))

            slices = [(e, fc) for e in range(NE) for fc in range(8)]
            PRE = 2
            WD_SCHED = {1: 0, 3: 1, 5: 2, 6: 3}
            loaded = {}
            for k in range(min(PRE, len(slices))):
                loaded[k] = load_wg(*slices[k])
            for k, (e, fc) in enumerate(slices):
                if k == 0:
                    build_sel(0)
                    gather(0)
                    build_sel(1)
                if k + PRE < len(slices):
                    loaded[k + PRE] = load_wg(*slices[k + PRE])
                wg_, bwg_ = loaded.pop(k)
                if fc in WD_SCHED:
                    load_wd(e, WD_SCHED[fc])
                (gps, bgps), (ups, bups) = gub.get()
                rd = [bwg_] + bxg
                for dc in range(8):
                    fw.op(PE, rd, [bgps], lambda: nc.tensor.matmul(gps[:, 0:CAP], wg_[:, dc, 0, :], xgT[:, dc, :], start=(dc == 0), stop=(dc == 7)), inc=(dc == 7))
                for dc in range(8):
                    fw.op(PE, rd, [bups], lambda: nc.tensor.matmul(ups[:, 0:CAP], wg_[:, dc, 1, :], xgT[:, dc, :], start=(dc == 0), stop=(dc == 7)), inc=(dc == 7))
                bg_col = bguT[:, e * 16 + fc:e * 16 + fc + 1]
                bu_col = bguT[:, e * 16 + 8 + fc:e * 16 + 8 + fc + 1]
                bus_col = busT[:, e * 8 + fc:e * 8 + fc + 1]
                fw.op(DVE, [bgps, bbgu], [bgc_], lambda: nc.vector.tensor_scalar(gc_[:], gps[:, 0:CAP], bg_col, 7.0, op0=ALU.add, op1=ALU.min))
                fw.op(ACT, [bups, bbus], [buc_], lambda: nc.scalar.activation(uc_[:], ups[:, 0:CAP], AF.Identity, bias=bus_col, scale=1.0 / 1.702))
                fw.op(ACT, [bgc_], [bsg_], lambda: nc.scalar.activation(sg_[:], gc_[:], AF.Silu, scale=1.702))
                fw.op(DVE, [buc_], [buc_], lambda: nc.vector.tensor_scalar(uc_[:], uc_[:], 7.0 / 1.702, -7.0 / 1.702, op0=ALU.min, op1=ALU.max))
                fw.op(DVE, [bsg_, buc_], [bACTT[fc]],
                      lambda: nc.vector.scalar_tensor_tensor(ACTT[:, fc, :], in0=uc_[:], scalar=1.0 / 1.702, in1=sg_[:], op0=ALU.add, op1=ALU.mult))
                if fc == 7:
                    if e + 1 < NE:
                        gather(e + 1)
                        if e + 2 < NE:
                            build_sel(e + 2)
                    for sbk in range(NSB):
                        for half in range(2):
                            yp, byp = yb_b.get()
                            rd2 = bACTT + bWd
                            for f in range(8):
                                fw.op(PE, rd2, [byp], lambda: nc.tensor.matmul(yp[:], ACTT[:, f, sbk * 128:(sbk + 1) * 128], Wd[:, f, half * 512:(half + 1) * 512], start=(f == 0), stop=(f == 7)), inc=(f == 7))
                            fw.op(ACT, [byp], [byb[sbk]], lambda: nc.scalar.copy(yb[:, sbk, half * 512:(half + 1) * 512], yp[:]))
                    fw.op(DVE, [bcst], [ble], lambda: nc.vector.tensor_scalar(le[:], kiota, float(e), None, op0=ALU.is_equal))
                    bcs = {}

                    def emit_bc(ch):
                        bc, bbc = gab.get()
                        fw.op(PE, [ble, bposmT[ch]], [bbc], lambda: nc.tensor.matmul(bc[:], le[:], posmT[:, ch * 512:(ch + 1) * 512], start=True, stop=True))
                        bcs[ch] = (bc, bbc)

                    emit_bc(0)
                    for ch in range(4):
                        bc, bbc = bcs.pop(ch)
                        if ch + 1 < 4:
                            emit_bc(ch + 1)
                        st_, bst2 = SelT.get()
                        for sbk in range(NSB):
                            fw.op(DVE, [bbc, bcst], [bst2], lambda: nc.vector.tensor_scalar(st_[:, sbk, :], bc[:], cst[:, 516 + sbk:517 + sbk], None, op0=ALU.is_equal))
                        for tb4 in range(4):
                            tb = ch * 4 + tb4
                            for half in range(2):
                                yp, byp = yb_b.get()
                                for sbk in range(NSB):
                                    fw.op(PE, [bst2] + byb, [byp], lambda: nc.tensor.matmul(yp[:], st_[:, sbk, tb4 * 128:(tb4 + 1) * 128], yb[:, sbk, half * 512:(half + 1) * 512], start=(sbk == 0), stop=(sbk == NSB - 1)), inc=(sbk == NSB - 1))
                                fw.op(DVE, [byp, bG[tb], bACC[tb]], [bACC[tb]],
                                      lambda: nc.vector.scalar_tensor_tensor(ACC[:, tb, half * 512:(half + 1) * 512], in0=yp[:], scalar=G[:, tb, e:e + 1], in1=ACC[:, tb, half * 512:(half + 1) * 512], op0=ALU.mult, op1=ALU.add))

        fw.fence()
        with contextlib.ExitStack() as sf:
            ln2bc = sb("ln2bc", [128, 2, D], F32, sf); bln2 = Buf()
            fw.dma(SP, ln2bc[:], lnv_d[2:4, :].partition_broadcast(128), [], [bln2])
            ot = Rot([(sb(f"ot{i}", [128, D], F32, sf), Buf()) for i in range(2)])
            jk2 = Rot([(sb(f"jk2{i}", [128, D], F32, sf), Buf()) for i in range(2)])
            for tb in range(16):
                o_, bo_ = ot.get()
                j_, bj_ = jk2.get()
                layer_norm(ACC[:, tb, :], bACC[tb], tb, ln2bc, bln2, o_[:], bo_, j_[:], bj_)
                fw.dma(SP, out_d[tb * 128:(tb + 1) * 128, :], o_[:], [bo_], [])
        fw.finish(SP)
    return nc


_PROG = {}


def _consts(r):
    c = np.zeros((128, 1280), np.float32)
    c[:, 0:128] = np.eye(128, dtype=np.float32)
    m = np.arange(128)
    partner = np.where(m % 64 < 32, m + 32, m - 32)
    c[partner, 128 + m] = 1.0
    k = np.arange(128)[:, None]
    q = np.arange(128)[None, :]
    c[:, 256:384] = (k <= q).astype(np.float32)
    c[:, 384:512] = 1.0 if r == 1 else 0.0
    invf = 1.0 / (10000.0 ** ((np.arange(128) % 32) * 2.0 / 64.0))
    first = (np.arange(128) % 64) < 32
    c[:, 512] = invf
    sc = TWO_PI * (1.0 - 1e-6)
    c[:, 513] = np.where(first, -sc, sc)
    c[:, 514] = np.where(first, PI_S, -PI_S)
    c[:, 515] = -PI_S
    p = np.arange(128, dtype=np.float32)
    c[:, 516] = p
    c[:, 517] = p + 128.0
    c[:, 518] = p + 256.0
    c[:, 640:1024] = np.arange(CAP, dtype=np.float32)[None, :]
    c[:, 1024:1152] = (p[:, None] < p[None, :]).astype(np.float32)
    c[:, 1152:1280] = p[:, None]
    return c


def _prep_inputs(inp):
    f32 = lambda a: np.ascontiguousarray(np.asarray(a), dtype=np.float32)
    x = f32(inp["x"])
    positions = np.ascontiguousarray(np.asarray(inp["positions"]), dtype=np.int32)
    wgu = f32(inp["w_gate_up"])[0]
    wgu_t = np.ascontiguousarray(
        wgu.reshape(NE, 8, 128, 2, 8, 128).transpose(0, 4, 2, 1, 3, 5)).reshape(NE, 8, 128, 2048)
    bgu = f32(inp["b_gate_up"])[0]
    bguT = np.ascontiguousarray(bgu.reshape(NE, 16, 128).transpose(2, 0, 1)).reshape(128, NE * 16)
    shared = {
        "w_in": f32(inp["w_in"])[0],
        "lamv": np.ascontiguousarray(np.stack([f32(inp["lambda_q1"])[0], f32(inp["lambda_k1"])[0],
                                               f32(inp["lambda_q2"])[0], f32(inp["lambda_k2"])[0]], 0)),
        "subln_g": f32(inp["subln_g"])[0].reshape(128, 1),
        "gq": np.ascontiguousarray(f32(inp["mla_q_norm_g"])[0].reshape(2, 128).T),
        "gkv": f32(inp["mla_kv_norm_g"])[0].reshape(128, 1),
        "w_uq": f32(inp["w_uq"])[0],
        "w_ukv": f32(inp["w_ukv"])[0],
        "w_o": f32(inp["w_o"])[0],
        "lnv": np.ascontiguousarray(np.stack([f32(inp["ln1_g"])[0], f32(inp["ln1_b"])[0],
                                              f32(inp["ln2_g"])[0], f32(inp["ln2_b"])[0]], 0)),
        "w_router": f32(inp["w_router"])[0],
        "b_router": f32(inp["b_router"])[0].reshape(1, NE),
        "wgu_t": wgu_t,
        "bguT": bguT,
        "w_down": f32(inp["w_down"])[0],
        "b_down": f32(inp["b_down"])[0],
    }
    in_maps = []
    toks = []
    for c in range(NCORES):
        b, r = c // 2, c % 2
        own = [2 * j + r for j in range(16)]
        oth = [2 * j + (1 - r) for j in range(16)]
        tok = np.concatenate([np.arange(g * 128, (g + 1) * 128) for g in own + oth])
        toks.append((b, tok[:TQ]))
        xb = x[b][tok]
        m = dict(shared)
        m["xT"] = np.ascontiguousarray(xb.T)
        m["xq"] = np.ascontiguousarray(xb[:TQ])
        m["pos"] = np.ascontiguousarray(positions[b][tok].reshape(1, S))
        m["cst"] = _consts(r)
        in_maps.append(m)
    return in_maps, toks


def kernel(**inputs):
    stage = inputs.pop("_stage", "full")
    if stage not in _PROG:
        _PROG[stage] = build_program(stage)
    nc = _PROG[stage]
    in_maps, toks = _prep_inputs(inputs)
    if stage != "full":
        moe = ("w_router", "b_router", "wgu_t", "bguT", "w_down", "b_down")
        in_maps = [{k: v for k, v in m.items() if k not in moe} for m in in_maps]
    res = run_bass_kernel_spmd(nc, in_maps, core_ids=list(range(NCORES)))
    out = np.zeros((4, S, D), np.float32)
    for c in range(NCORES):
        b, tok = toks[c]
        out[b, tok] = res.results[c]["out"]
    return out
```

```python
import math
import contextlib
import numpy as np
import concourse.bass as bass
import concourse.mybir as mybir
from concourse.bass_utils import run_bass_kernel_spmd

F32 = mybir.dt.float32
BF16 = mybir.dt.bfloat16
I32 = mybir.dt.int32
ALU = mybir.AluOpType
AF = mybir.ActivationFunctionType
AX = mybir.AxisListType

NCORES = 8
D = 1024
S = 4096
TQ = 2048
NE = 32
LAM_INIT = 0.8 - 0.6 * math.exp(0.0)
DN_ALPHA = 2.0 ** 0.25
TWO_PI = 2.0 * math.pi
PI_S = math.pi * (1.0 - 1e-6)
NDMA_SEM = 8
CAP = 384


_FENCE = {}


class Buf:
    __slots__ = ("w", "r", "excl")

    def __init__(self, excl=False):
        self.w = None
        self.r = dict(_FENCE)
        self.excl = excl


class Eng:
    def __init__(self, name, h, sem, dma_sems):
        self.name = name
        self.h = h
        self.sem = sem
        self.count = 0
        self.seen = {}
        self.dma_sems = dma_sems
        self.dma_val = [0] * len(dma_sems)
        self.rr = 0


class FW:
    def __init__(self, nc, es):
        self.nc = nc
        self.es = es
        mk = lambda n: es.enter_context(nc.semaphore(n))
        self.pe = Eng("pe", nc.tensor, mk("s_pe"), [])
        self.act = Eng("act", nc.scalar, mk("s_act"), [])
        self.dve = Eng("dve", nc.vector, mk("s_dve"), [])
        self.pool = Eng("pool", nc.gpsimd, mk("s_pool"), [mk(f"d_pool{i}") for i in range(NDMA_SEM)])
        self.sp = Eng("sp", nc.sync, mk("s_sp"), [mk(f"d_sp{i}") for i in range(NDMA_SEM)])
        self.nwait = 0

    def _wait(self, E, tok):
        sem, val = tok
        if sem is E.sem and (E is self.pe or val > E.count):
            return
        k = id(sem)
        if E.seen.get(k, 0) >= val:
            return
        E.h.wait_ge(sem, val)
        E.seen[k] = val
        self.nwait += 1

    def _deps(self, E, reads, writes):
        for b in reads:
            if b.w is not None:
                self._wait(E, b.w)
            if b.excl:
                for tok in b.r.values():
                    self._wait(E, tok)
        for b in writes:
            if b.w is not None:
                self._wait(E, b.w)
            for tok in b.r.values():
                self._wait(E, tok)

    def _mark(self, tok, reads, writes):
        for b in reads:
            b.r[id(tok[0])] = tok
        for b in writes:
            b.w = tok
            b.r = {}

    def op(self, E, reads, writes, build, inc=True):
        self._deps(E, reads, writes)
        ins = build()
        tok = (E.sem, E.count + 1)
        if inc:
            ins.then_inc(E.sem, 1)
            E.count += 1
        self._mark(tok, reads, writes)
        return ins

    def dma(self, Q, out, in_, reads, writes, **kw):
        i = Q.rr % len(Q.dma_sems)
        Q.rr += 1
        sem = Q.dma_sems[i]
        if Q.dma_val[i] > 0:
            self._wait(Q, (sem, Q.dma_val[i]))
        self._deps(Q, reads, writes)
        Q.h.dma_start(out=out, in_=in_, **kw).then_inc(sem, 16)
        Q.dma_val[i] += 16
        tok = (sem, Q.dma_val[i])
        self._mark(tok, reads, writes)
        return tok

    def fence(self):
        _FENCE.clear()
        for Q in (self.sp, self.pool):
            for sem, v in zip(Q.dma_sems, Q.dma_val):
                if v > 0:
                    _FENCE[id(sem)] = (sem, v)
        for X in (self.pe, self.act, self.dve, self.pool, self.sp):
            if X.count > 0:
                _FENCE[id(X.sem)] = (X.sem, X.count)

    def finish(self, E):
        for Q in (self.sp, self.pool):
            for sem, v in zip(Q.dma_sems, Q.dma_val):
                if v > 0:
                    self._wait(E, (sem, v))
        for X in (self.pe, self.act, self.dve, self.pool, self.sp):
            if X is not E and X.count > 0:
                self._wait(E, (X.sem, X.count))


class Rot:
    def __init__(self, items):
        self.items = items
        self.i = 0

    def get(self):
        it = self.items[self.i % len(self.items)]
        self.i += 1
        return it


def build_program(stage="full"):
    nc = bass.Bass("TRN2", target_bir_lowering=False)
    dt_in = lambda name, shape, dt=F32: nc.dram_tensor(name, shape, dt, kind="ExternalInput").ap()
    xT_d = dt_in("xT", [D, S])
    xq_d = dt_in("xq", [TQ, D])
    pos_d = dt_in("pos", [1, S], I32)
    cst_d = dt_in("cst", [128, 1280])
    w_in_d = dt_in("w_in", [D, 1984])
    lam_d = dt_in("lamv", [4, 64])
    subg_d = dt_in("subln_g", [128, 1])
    gq_d = dt_in("gq", [128, 2])
    gkv_d = dt_in("gkv", [128, 1])
    w_uq_d = dt_in("w_uq", [256, 768])
    w_ukv_d = dt_in("w_ukv", [128, 1024])
    w_o_d = dt_in("w_o", [D, D])
    lnv_d = dt_in("lnv", [4, D])
    if stage == "full":
        w_r_d = dt_in("w_router", [D, NE])
        b_r_d = dt_in("b_router", [1, NE])
        wgu_d = dt_in("wgu_t", [NE, 8, 128, 2048])
        bgu_d = dt_in("bguT", [128, NE * 16])
        wd_d = dt_in("w_down", [NE, D, D])
        bd_d = dt_in("b_down", [NE, D])
    out_d = nc.dram_tensor("out", [TQ, D], F32, kind="ExternalOutput").ap()

    _FENCE.clear()
    with contextlib.ExitStack() as es:
        fw = FW(nc, es)
        PE, ACT, DVE, POOL, SP = fw.pe, fw.act, fw.dve, fw.pool, fw.sp

        def sb(name, shape, dt, st=es):
            return st.enter_context(nc.sbuf_tensor("sb_" + name, shape, dt))

        psb = [es.enter_context(nc.psum_tensor(f"ps{i}", [128, 512], F32)) for i in range(8)]
        pb = [Buf(excl=True) for _ in range(8)]

        cst = sb("cst", [128, 1280], F32); bcst = Buf()
        fw.dma(SP, cst[:], cst_d[:, :], [], [bcst])
        ident = cst[:, 0:128]
        ropec = cst[:, 512:516]
        cbf = sb("cbf", [128, 512], BF16); bcbf = Buf()
        fw.op(DVE, [bcst], [bcbf], lambda: nc.vector.tensor_copy(cbf[:, 0:384], cst[:, 128:512]))
        fw.op(DVE, [], [bcbf], lambda: nc.vector.memset(cbf[:, 384:512], 1.0))
        perm_bf = cbf[:, 0:128]
        masks_bf = [cbf[:, 128:256], cbf[:, 256:384]]
        ones_bf = cbf[:, 384:512]
        ones32 = sb("ones32", [128, 128], F32); bones = Buf()
        fw.op(POOL, [], [bones], lambda: nc.gpsimd.memset(ones32[:], 1.0))
        small = sb("small", [128, 64], F32); bsmall = Buf()
        fw.dma(SP, small[:, 0:1], subg_d[:, :], [], [bsmall])
        fw.dma(SP, small[:, 3:5], gq_d[:, :], [], [bsmall])
        fw.dma(SP, small[:, 5:6], gkv_d[:, :], [], [bsmall])
        fw.op(DVE, [], [bsmall], lambda: nc.vector.memset(small[:, 8:9], 1e-6))
        fw.op(DVE, [], [bsmall], lambda: nc.vector.memset(small[:, 9:10], 1e-5))
        EPS6 = small[:, 8:9]
        EPS5 = small[:, 9:10]
        lamt = sb("lamt", [128, 256], F32); blam = Buf()
        fw.dma(SP, lamt[:].rearrange("p (a b) -> p a b", a=4), lam_d.partition_broadcast(128), [], [blam])
        fw.op(DVE, [blam], [blam], lambda: nc.vector.tensor_tensor(lamt[:, 0:64], lamt[:, 0:64], lamt[:, 64:128], op=ALU.mult))
        fw.op(DVE, [blam], [blam], lambda: nc.vector.tensor_tensor(lamt[:, 128:192], lamt[:, 128:192], lamt[:, 192:256], op=ALU.mult))
        fw.op(DVE, [blam], [bsmall], lambda: nc.vector.reduce_sum(small[:, 6:7], lamt[:, 0:64], axis=AX.X))
        fw.op(DVE, [blam], [bsmall], lambda: nc.vector.reduce_sum(small[:, 7:8], lamt[:, 128:192], axis=AX.X))
        fw.op(ACT, [bsmall], [bsmall], lambda: nc.scalar.activation(small[:, 6:8], small[:, 6:8], AF.Exp))
        fw.op(DVE, [bsmall], [bsmall], lambda: nc.vector.tensor_tensor(small[:, 2:3], small[:, 7:8], small[:, 6:7], op=ALU.subtract))
        fw.op(DVE, [bsmall], [bsmall], lambda: nc.vector.tensor_scalar(small[:, 2:3], small[:, 2:3], -LAM_INIT, None, op0=ALU.add))
        fw.op(DVE, [bsmall], [bsmall], lambda: nc.vector.tensor_scalar(small[:, 1:2], small[:, 0:1], 1.0 - LAM_INIT, None, op0=ALU.mult))

        otx = sb("otx", [128, 8, TQ], BF16)
        botx = [Buf() for _ in range(16)]

        with contextlib.ExitStack() as sa:
            cosT = sb("cosT", [128, S], F32, sa)
            sinS = sb("sinS", [128, S], F32, sa)
            btab = [Buf() for _ in range(8)]
            ckvn = sb("ckvn", [128, S], BF16, sa); bckvn = [Buf() for _ in range(8)]
            cqn = sb("cqn", [128, 2, TQ], BF16, sa); bcqn = [Buf() for _ in range(4)]
            KR = sb("KR", [128, S], BF16, sa); bKR = [Buf() for _ in range(8)]
            fw.op(POOL, [], bKR, lambda: nc.gpsimd.memset(KR[64:128, :], 0.0))
            s32 = Rot([(sb(f"s32_{i}", [128, 512], F32, sa), Buf()) for i in range(8)])
            s16 = Rot([(sb(f"s16_{i}", [128, 512], BF16, sa), Buf()) for i in range(4)])
            si32 = sb("si32", [128, 512], I32, sa); bsi32 = Buf()
            tmpb = Rot([(psb[i], pb[i]) for i in (6, 7, 0, 1)])

            def mm_acc(out_ap, pairs, reads, writes):
                n = len(pairs)
                for i, (l, r) in enumerate(pairs):
                    fw.op(PE, reads, writes,
                          lambda: nc.tensor.matmul(out_ap, l, r, start=(i == 0), stop=(i == n - 1)),
                          inc=(i == n - 1))

            def rope(src_ps, bsrc, rows, tc, dst_ap, bdst):
                cols = slice(tc * 512, (tc + 1) * 512)
                import os as _os
                _cut = int(_os.environ.get("ROPE_CUT", "9")) if rows == 128 else 9
                if _cut < 1:
                    return
                hb, bhb = s16.get()
                fw.op(ACT, [bsrc], [bhb], lambda: nc.scalar.copy(hb[0:rows, :], src_ps))
                if _cut < 2:
                    return
                sw, bsw = tmpb.get()
                fw.op(PE, [bhb, bcbf], [bsw], lambda: nc.tensor.matmul(sw[0:rows, :], perm_bf[0:rows, 0:rows], hb[0:rows, :], start=True, stop=True))
                if _cut < 3:
                    return
                t1, bt1 = s32.get()
                fw.op(DVE, [bsrc, btab[tc]], [bt1], lambda: nc.vector.tensor_tensor(t1[0:rows, :], src_ps, cosT[0:rows, cols], op=ALU.mult))
                if _cut < 4:
                    return
                t2, bt2 = s32.get()
                fw.op(DVE, [bsw, btab[tc]], [bt2], lambda: nc.vector.tensor_tensor(t2[0:rows, :], sw[0:rows, :], sinS[0:rows, cols], op=ALU.mult))
                if _cut < 5:
                    return
                fw.op(POOL, [bt1, bt2], [bdst], lambda: nc.gpsimd.tensor_tensor(dst_ap, t1[0:rows, :], t2[0:rows, :], op=ALU.add))

            def rms_scale(ps_list, bps_list, n_feat, eps, gcols, dst_aps, bdst):
                sqs = []
                for ps_ap, bps in zip(ps_list, bps_list):
                    sq, bsq = s32.get()
                    fw.op(ACT, [bps], [bsq], lambda: nc.scalar.activation(sq[:], ps_ap, AF.Square))
                    sqs.append((sq, bsq))
                ss, bss = tmpb.get()
                for i, (sq, bsq) in enumerate(sqs):
                    fw.op(PE, [bsq, bones], [bss],
                          lambda: nc.tensor.matmul(ss[:], ones32[:], sq[:], start=(i == 0), stop=(i == len(sqs) - 1)),
                          inc=(i == len(sqs) - 1))
                rstd, brstd = s32.get()
                fw.op(ACT, [bss, bsmall], [brstd], lambda: nc.scalar.activation(rstd[:], ss[:], AF.Sqrt, bias=eps, scale=1.0 / n_feat))
                fw.op(DVE, [brstd], [brstd], lambda: nc.vector.reciprocal(rstd[:], rstd[:]))
                for ps_ap, bps, gc, dst in zip(ps_list, bps_list, gcols, dst_aps):
                    fw.op(DVE, [bps, brstd, bsmall], [bdst],
                          lambda: nc.vector.scalar_tensor_tensor(dst, in0=ps_ap, scalar=gc, in1=rstd[:], op0=ALU.mult, op1=ALU.mult))

            def attention(nsub, s_emit, s_reads, Vt, bV, scale, finalize):
                S_B = [(psb[0], pb[0]), (psb[1], pb[1])]
                O_B = [(psb[2], pb[2]), (psb[3], pb[3])]
                L_B = [(psb[4], pb[4]), (psb[5], pb[5])]
                pending = [None]
                for g in range(4):
                    units = []
                    nkb = 4 * g + 4
                    for half in (0, 1):
                        for kl in range(nkb):
                            i = kl - 4 * g
                            col0 = 0 if i < 0 else i * 128
                            mt = None if i < 0 else half
                            for c in range(nsub):
                                units.append((c, half * 16 + kl, col0, mt))
                    nun = len(units)
                    first = [True] * nsub
                    last_idx = {}
                    for ui, u in enumerate(units):
                        last_idx[u[0]] = ui
                    pts = {}

                    def emit_s(ui):
                        c, kb, col0, mt = units[ui]
                        sbk, bsbk = S_B[ui % 2]
                        s_emit(c, kb, g, col0, sbk, bsbk)
                        pT, bpT = s16.get()
                        fw.op(ACT, [bsbk], [bpT], lambda: nc.scalar.activation(pT[:, col0:512], sbk[:, col0:512], AF.Exp, scale=scale))
                        if mt is not None:
                            fw.op(POOL, [bpT, bcbf], [bpT], lambda: nc.gpsimd.tensor_tensor(pT[:, col0:col0 + 128], pT[:, col0:col0 + 128], masks_bf[mt], op=ALU.mult))
                        pts[ui] = (pT, bpT)

                    def emit_pv(ui):
                        c, kb, col0, mt = units[ui]
                        pT, bpT = pts.pop(ui)
                        o, bo = O_B[c]
                        l, bl = L_B[c]
                        st = first[c]
                        first[c] = False
                        sp_ = (last_idx[c] == ui)
                        fw.op(PE, [bpT, bV[kb // 4]], [bo], lambda: nc.tensor.matmul(o[:, col0:512], Vt[:, kb, :], pT[:, col0:512], start=st, stop=sp_), inc=False)
                        fw.op(PE, [bpT, bcbf], [bl], lambda: nc.tensor.matmul(l[:, col0:512], ones_bf, pT[:, col0:512], start=st, stop=sp_))

                    LOOK = 2
                    for ui in range(min(LOOK, nun)):
                        emit_s(ui)
                    if pending[0] is not None:
                        pending[0]()
                    for ui in range(nun):
                        emit_pv(ui)
                        if ui + LOOK < nun:
                            emit_s(ui + LOOK)
                    pending[0] = (lambda g=g: finalize(g, O_B, L_B))
                pending[0]()

            with contextlib.ExitStack() as sx:
                xTb = sb("xTb", [128, 8, S], BF16, sx); bxT = [Buf() for _ in range(8)]
                xT_v = xT_d.rearrange("(dc p) t -> p dc t", p=128)
                for tc in range(8):
                    fw.dma(POOL, xTb[:, :, tc * 512:(tc + 1) * 512], xT_v[:, :, tc * 512:(tc + 1) * 512], [], [bxT[tc]])
                w_in_v = w_in_d.rearrange("(dc p) c -> p dc c", p=128)

                for tc in range(8):
                    cols = slice(tc * 512, (tc + 1) * 512)
                    fw.dma(SP, si32[:], pos_d[0:1, cols].partition_broadcast(128), [], [bsi32])
                    ang, bang = s32.get()
                    fw.op(DVE, [bsi32], [bang], lambda: nc.vector.tensor_copy(ang[:], si32[:]))
                    fw.op(DVE, [bang, bcst], [bang], lambda: nc.vector.tensor_scalar(ang[:], ang[:], ropec[:, 0:1], None, op0=ALU.mult))
                    for which in (0, 1):
                        shift = 0.5 if which == 0 else 0.75
                        u, bu = s32.get()
                        fw.op(DVE, [bang], [bu], lambda: nc.vector.tensor_scalar(u[:], ang[:], 1.0 / TWO_PI, shift, op0=ALU.mult, op1=ALU.add))
                        ki, bki = s32.get()
                        kiv = ki[:].bitcast(I32)
                        fw.op(DVE, [bu], [bki], lambda: nc.vector.tensor_copy(kiv, u[:]))
                        kf, bkf = s32.get()
                        fw.op(POOL, [bki], [bkf], lambda: nc.gpsimd.tensor_copy(kf[:], kiv))
                        fw.op(POOL, [bkf, bu], [bu], lambda: nc.gpsimd.tensor_tensor(u[:], u[:], kf[:], op=ALU.subtract))
                        fw.op(DVE, [bu], [bkf], lambda: nc.vector.scalar_tensor_tensor(kf[:], in0=u[:], scalar=0.0, in1=u[:], op0=ALU.is_lt, op1=ALU.add))
                        if which == 0:
                            fw.op(ACT, [bkf, bcst], [btab[tc]], lambda: nc.scalar.activation(sinS[:, cols], kf[:], AF.Sin, bias=ropec[:, 2:3], scale=ropec[:, 1:2]))
                        else:
                            fw.op(ACT, [bkf, bcst], [btab[tc]], lambda: nc.scalar.activation(cosT[:, cols], kf[:], AF.Sin, bias=ropec[:, 3:4], scale=TWO_PI * (1.0 - 1e-6)))

                if stage.startswith("tabtt"):
                    import os as _os
                    r0, r1 = [int(v) for v in _os.environ.get("TT_ROWS", "0,128").split(",")]
                    mode = _os.environ.get("TT_MODE", "psum_cos")
                    t1, bt1 = s32.get()
                    kps, bkps = tmpb.get()
                    fw.op(PE, [bcbf], [bkps], lambda: nc.tensor.matmul(kps[:], perm_bf, cbf[:, 0:512], start=True, stop=True))
                    if mode == "psum_cos":
                        fw.op(DVE, [bkps, btab[0]], [bt1], lambda: nc.vector.tensor_tensor(t1[r0:r1, :], kps[r0:r1, :], cosT[r0:r1, 0:512], op=ALU.mult))
                    elif mode == "sb_cos":
                        t2, bt2 = s32.get()
                        fw.op(DVE, [], [bt2], lambda: nc.vector.memset(t2[:], 1.0))
                        fw.op(DVE, [bt2, btab[0]], [bt1], lambda: nc.vector.tensor_tensor(t1[r0:r1, :], t2[r0:r1, :], cosT[r0:r1, 0:512], op=ALU.mult))
                    elif mode == "psum_sb":
                        t2, bt2 = s32.get()
                        fw.op(DVE, [], [bt2], lambda: nc.vector.memset(t2[:], 1.0))
                        fw.op(DVE, [bkps, bt2], [bt1], lambda: nc.vector.tensor_tensor(t1[r0:r1, :], kps[r0:r1, :], t2[r0:r1, :], op=ALU.mult))
                    fw.dma(SP, out_d[0:128, 0:512], t1[:], [bt1], [])
                    fw.dma(SP, out_d[128:256, 0:512], cosT[:, 0:512], [btab[0]], [])
                    fw.dma(SP, out_d[256:384, 0:512], sinS[:, 0:512], [btab[0]], [])
                    fw.finish(SP)
                    return nc
                if stage == "tab":
                    fw.finish(SP)
                    return nc
                with contextlib.ExitStack() as sm0:
                    WC = sb("WC", [128, 8, 448], BF16, sm0); bWC = Buf()
                    fw.dma(POOL, WC[:], w_in_v[:, :, 1536:1984], [], [bWC])
                    for tc in range(8):
                        cols = slice(tc * 512, (tc + 1) * 512)
                        ckv, bckv = tmpb.get()
                        mm_acc(ckv[:], [(WC[:, dc, 256:384], xTb[:, dc, cols]) for dc in range(8)], [bWC, bxT[tc]], [bckv])
                        rms_scale([ckv[:]], [bckv], 128.0, EPS6, [small[:, 5:6]], [ckvn[:, cols]], bckvn[tc])
                        kr, bkr = tmpb.get()
                        mm_acc(kr[0:64, :], [(WC[:, dc, 384:448], xTb[:, dc, cols]) for dc in range(8)], [bWC, bxT[tc]], [bkr])
                        rope(kr[0:64, :], bkr, 64, tc, KR[0:64, cols], bKR[tc])
                        if tc < 4:
                            cq0, bcq0 = tmpb.get()
                            mm_acc(cq0[:], [(WC[:, dc, 0:128], xTb[:, dc, cols]) for dc in range(8)], [bWC, bxT[tc]], [bcq0])
                            cq1, bcq1 = tmpb.get()
                            mm_acc(cq1[:], [(WC[:, dc, 128:256], xTb[:, dc, cols]) for dc in range(8)], [bWC, bxT[tc]], [bcq1])
                            rms_scale([cq0[:], cq1[:]], [bcq0, bcq1], 256.0, EPS6, [small[:, 3:4], small[:, 4:5]],
                                      [cqn[:, 0, cols], cqn[:, 1, cols]], bcqn[tc])

                if stage == "m0":
                    fw.finish(SP)
                    return nc
                fw.fence()
                with contextlib.ExitStack() as sd:
                    WQ = sb("WQ", [128, 8, 128], BF16, sd); WK = sb("WK", [128, 8, 128], BF16, sd); WV = sb("WV", [128, 8, 128], BF16, sd)
                    bW = Buf()
                    KT = sb("KT", [128, S], BF16, sd); bKT = [Buf() for _ in range(8)]
                    QT = sb("QT", [128, TQ], BF16, sd); bQT = [Buf() for _ in range(4)]
                    Vt = sb("Vt", [128, 32, 128], BF16, sd); bV = [Buf() for _ in range(8)]
                    for h in range(4):
                        fw.dma(POOL, WQ[:], w_in_v[:, :, 128 * h:128 * h + 128], [], [bW])
                        fw.dma(POOL, WK[:], w_in_v[:, :, 512 + 128 * h:512 + 128 * h + 128], [], [bW])
                        fw.dma(POOL, WV[:], w_in_v[:, :, 1024 + 128 * h:1024 + 128 * h + 128], [], [bW])
                        if stage == "dprojD":
                            fw.finish(SP)
                            return nc
                        import os as _os
                        _ntc = int(_os.environ.get("DPROJ_NTC", "8"))
                        _noq = _os.environ.get("DPROJ_NOQ", "0") == "1"
                        for tc in range(_ntc):
                            cols = slice(tc * 512, (tc + 1) * 512)
                            if stage != "dprojV":
                                kps, bkps = tmpb.get()
                                mm_acc(kps[:], [(WK[:, dc, :], xTb[:, dc, cols]) for dc in range(8)], [bW, bxT[tc]], [bkps])
                                rope(kps[:], bkps, 128, tc, KT[:, cols], bKT[tc])
                            if tc < 4 and stage != "dprojV" and not _noq:
                                qps, bqps = tmpb.get()
                                mm_acc(qps[:], [(WQ[:, dc, :], xTb[:, dc, cols]) for dc in range(8)], [bW, bxT[tc]], [bqps])
                                rope(qps[:], bqps, 128, tc, QT[:, cols], bQT[tc])
                            if stage == "dprojK":
                                continue
                            vps, bvps = tmpb.get()
                            for i in range(4):
                                mm_acc(vps[:, i * 128:(i + 1) * 128],
                                       [(xTb[:, dc, tc * 512 + i * 128: tc * 512 + (i + 1) * 128], WV[:, dc, :]) for dc in range(8)],
                                       [bW, bxT[tc]], [bvps])
                            fw.op(ACT, [bvps], [bV[tc]], lambda: nc.scalar.copy(Vt[:, tc * 4:(tc + 1) * 4, :], vps[:].rearrange("p (a b) -> p a b", a=4)))

                        if stage in ("dproj", "dprojK", "dprojV"):
                            fw.finish(SP)
                            return nc
                        def s_emit(c, kb, g, col0, sbk, bsbk):
                            fw.op(PE, [bKT[kb // 4], bQT[g]], [bsbk],
                                  lambda: nc.tensor.matmul(sbk[:, col0:512], KT[64 * c:64 * c + 64, kb * 128:(kb + 1) * 128],
                                                           QT[64 * c:64 * c + 64, g * 512 + col0:(g + 1) * 512], start=True, stop=True))

                        def fin_diff(g, O_B, L_B, h=h):
                            ds = []
                            for c in range(2):
                                rl, brl = s32.get()
                                fw.op(DVE, [L_B[c][1]], [brl], lambda: nc.vector.reciprocal(rl[:], L_B[c][0][:]))
                                fw.op(DVE, [O_B[c][1], brl], [brl], lambda: nc.vector.tensor_tensor(rl[:], O_B[c][0][:], rl[:], op=ALU.mult))
                                ds.append((rl, brl))
                            dd, bdd = s32.get()
                            fw.op(DVE, [ds[0][1], ds[1][1], bsmall], [bdd],
                                  lambda: nc.vector.scalar_tensor_tensor(dd[:], in0=ds[1][0][:], scalar=small[:, 2:3], in1=ds[0][0][:], op0=ALU.mult, op1=ALU.add))
                            sq, bsq = s32.get()
                            fw.op(ACT, [bdd], [bsq], lambda: nc.scalar.activation(sq[:], dd[:], AF.Square))
                            ss, bss = tmpb.get()
                            fw.op(PE, [bsq, bones], [bss], lambda: nc.tensor.matmul(ss[:], ones32[:], sq[:], start=True, stop=True))
                            rstd, brstd = s32.get()
                            fw.op(ACT, [bss, bsmall], [brstd], lambda: nc.scalar.activation(rstd[:], ss[:], AF.Sqrt, bias=EPS5, scale=1.0 / 128.0))
                            fw.op(DVE, [brstd], [brstd], lambda: nc.vector.reciprocal(rstd[:], rstd[:]))
                            wr = [botx[4 * g + i] for i in range(4)]
                            fw.op(DVE, [bdd, brstd, bsmall], wr,
                                  lambda: nc.vector.scalar_tensor_tensor(otx[:, h, g * 512:(g + 1) * 512], in0=dd[:], scalar=small[:, 1:2], in1=rstd[:], op0=ALU.mult, op1=ALU.mult))

                        attention(2, s_emit, None, Vt, bV, 64.0 ** -0.5, fin_diff)
                        if stage == "datt":
                            fw.finish(SP)
                            return nc
            fw.fence()
            with contextlib.ExitStack() as sm:
                wuq = sb("wuq", [128, 2, 768], BF16, sm); bwuq = Buf()
                wukv = sb("wukv", [128, 1024], BF16, sm); bwukv = Buf()
                fw.dma(POOL, wuq[:], w_uq_d.rearrange("(rc p) c -> p rc c", p=128), [], [bwuq])
                fw.dma(POOL, wukv[:], w_ukv_d[:, :], [], [bwukv])
                KTm = sb("KTm", [128, S], BF16, sm); bKTm = [Buf() for _ in range(8)]
                Vm = sb("Vm", [128, 32, 128], BF16, sm); bVm = [Buf() for _ in range(8)]
                QTn = sb("QTn", [128, TQ], BF16, sm); bQTn = [Buf() for _ in range(4)]
                QTr = sb("QTr", [128, TQ], BF16, sm); bQTr = [Buf() for _ in range(4)]
                fw.op(POOL, [], bQTr, lambda: nc.gpsimd.memset(QTr[64:128, :], 0.0))
                for h in range(4):
                    for tc in range(8):
                        cols = slice(tc * 512, (tc + 1) * 512)
                        kn, bkn = tmpb.get()
                        fw.op(PE, [bwukv, bckvn[tc]], [bkn], lambda: nc.tensor.matmul(kn[:], wukv[:, h * 256:h * 256 + 128], ckvn[:, cols], start=True, stop=True))
                        fw.op(ACT, [bkn], [bKTm[tc]], lambda: nc.scalar.copy(KTm[:, cols], kn[:]))
                        vps, bvps = tmpb.get()
                        for i in range(4):
                            fw.op(PE, [bwukv, bckvn[tc]], [bvps],
                                  lambda: nc.tensor.matmul(vps[:, i * 128:(i + 1) * 128], ckvn[:, tc * 512 + i * 128: tc * 512 + (i + 1) * 128],
                                                           wukv[:, h * 256 + 128:h * 256 + 256], start=True, stop=True), inc=(i == 3))
                        fw.op(DVE, [bvps], [bVm[tc]], lambda: nc.vector.tensor_copy(Vm[:, tc * 4:(tc + 1) * 4, :], vps[:].rearrange("p (a b) -> p a b", a=4)))
                        if tc < 4:
                            qn, bqn = tmpb.get()
                            mm_acc(qn[:], [(wuq[:, rc, h * 192:h * 192 + 128], cqn[:, rc, cols]) for rc in range(2)], [bwuq, bcqn[tc]], [bqn])
                            fw.op(ACT, [bqn], [bQTn[tc]], lambda: nc.scalar.copy(QTn[:, cols], qn[:]))
                            qr, bqr = tmpb.get()
                            mm_acc(qr[0:64, :], [(wuq[:, rc, h * 192 + 128:h * 192 + 192], cqn[:, rc, cols]) for rc in range(2)], [bwuq, bcqn[tc]], [bqr])
                            rope(qr[0:64, :], bqr, 64, tc, QTr[0:64, cols], bQTr[tc])

                    if stage == "mproj":
                        fw.finish(SP)
                        return nc
                    def s_emit_m(c, kb, g, col0, sbk, bsbk):
                        fw.op(PE, [bKTm[kb // 4], bQTn[g]], [bsbk],
                              lambda: nc.tensor.matmul(sbk[:, col0:512], KTm[:, kb * 128:(kb + 1) * 128], QTn[:, g * 512 + col0:(g + 1) * 512], start=True, stop=False), inc=False)
                        fw.op(PE, [bKR[kb // 4], bQTr[g]], [bsbk],
                              lambda: nc.tensor.matmul(sbk[:, col0:512], KR[:, kb * 128:(kb + 1) * 128], QTr[:, g * 512 + col0:(g + 1) * 512], start=False, stop=True))

                    def fin_mla(g, O_B, L_B, h=h):
                        rl, brl = s32.get()
                        fw.op(DVE, [L_B[0][1]], [brl], lambda: nc.vector.reciprocal(rl[:], L_B[0][0][:]))
                        wr = [botx[4 * g + i] for i in range(4)]
                        fw.op(DVE, [O_B[0][1], brl], wr, lambda: nc.vector.tensor_tensor(otx[:, 4 + h, g * 512:(g + 1) * 512], O_B[0][0][:], rl[:], op=ALU.mult))

                    attention(1, s_emit_m, None, Vm, bVm, 192.0 ** -0.5, fin_mla)
                    if stage == "matt":
                        fw.finish(SP)
                        return nc

        fw.fence()
        ACC = sb("ACC", [128, 16, D], F32); bACC = [Buf() for _ in range(16)]
        sm2 = sb("sm2", [128, 16, 8], F32); bsm2 = [Buf() for _ in range(16)]

        def layer_norm(z, bz, tb, lnbc, blnbc, dst, bdst, junk, bjunk):
            sc = sm2[:, tb, :]
            bs = bsm2[tb]
            fw.op(DVE, [bz], [bs], lambda: nc.vector.reduce_sum(sc[:, 0:1], z, axis=AX.X))
            fw.op(DVE, [bs], [bs], lambda: nc.vector.tensor_scalar(sc[:, 1:2], sc[:, 0:1], -1.0 / D, None, op0=ALU.mult))
            fw.op(ACT, [bz, bs], [bz], lambda: nc.scalar.activation(z, z, AF.Identity, bias=sc[:, 1:2]))
            fw.op(ACT, [bz], [bjunk], lambda: nc.scalar.activation(junk, z, AF.Square))
            fw.op(DVE, [bjunk], [bs], lambda: nc.vector.reduce_sum(sc[:, 2:3], junk, axis=AX.X))
            fw.op(ACT, [bs, bsmall], [bs], lambda: nc.scalar.activation(sc[:, 3:4], sc[:, 2:3], AF.Sqrt, bias=EPS5, scale=1.0 / D))
            fw.op(DVE, [bs], [bs], lambda: nc.vector.reciprocal(sc[:, 3:4], sc[:, 3:4]))
            fw.op(DVE, [bz, bs, blnbc], [bz], lambda: nc.vector.scalar_tensor_tensor(z, in0=z, scalar=sc[:, 3:4], in1=lnbc[:, 0, :], op0=ALU.mult, op1=ALU.mult))
            fw.op(POOL, [bz, blnbc], [bdst], lambda: nc.gpsimd.tensor_tensor(dst, z, lnbc[:, 1, :], op=ALU.add))

        with contextlib.ExitStack() as so:
            ln1bc = sb("ln1bc", [128, 2, D], F32, so); bln1 = Buf()
            fw.dma(SP, ln1bc[:], lnv_d[0:2, :].partition_broadcast(128), [], [bln1])
            wo = sb("wo", [128, 8, D], BF16, so); bwo = Buf()
            fw.dma(POOL, wo[:], w_o_d.rearrange("(hh p) o -> p hh o", p=128), [], [bwo])
            xqt = Rot([(sb(f"xqt{i}", [128, D], F32, so), Buf()) for i in range(2)])
            zt = Rot([(sb(f"zt{i}", [128, D], F32, so), Buf()) for i in range(2)])
            jk = Rot([(sb(f"jk{i}", [128, D], F32, so), Buf()) for i in range(2)])
            mixb = Rot([((psb[0], pb[0]), (psb[1], pb[1])), ((psb[2], pb[2]), (psb[3], pb[3]))])
            for tb in range(16):
                xt_, bxt_ = xqt.get()
                fw.dma(SP, xt_[:], xq_d[tb * 128:(tb + 1) * 128, :], [], [bxt_])
                banks = mixb.get()
                z, bz = zt.get()
                for half in range(2):
                    mps, bmps = banks[half]
                    for hh in range(8):
                        fw.op(PE, [botx[tb], bwo], [bmps],
                              lambda: nc.tensor.matmul(mps[:], otx[:, hh, tb * 128:(tb + 1) * 128], wo[:, hh, half * 512:(half + 1) * 512], start=(hh == 0), stop=(hh == 7)),
                              inc=(hh == 7))
                    fw.op(DVE, [bmps, bxt_], [bz],
                          lambda: nc.vector.scalar_tensor_tensor(z[:, half * 512:(half + 1) * 512], in0=xt_[:, half * 512:(half + 1) * 512], scalar=DN_ALPHA, in1=mps[:], op0=ALU.mult, op1=ALU.add))
                j_, bj_ = jk.get()
                layer_norm(z[:], bz, tb, ln1bc, bln1, ACC[:, tb, :], bACC[tb], j_[:], bj_)

        fw.fence()
        if stage == "ln1":
            for tb in range(16):
                fw.dma(SP, out_d[tb * 128:(tb + 1) * 128, :], ACC[:, tb, :], [bACC[tb]], [])
            fw.finish(SP)
            return nc

        G = sb("G", [128, 16, NE], F32); bG = [Buf() for _ in range(16)]
        MK = sb("MK", [128, 16, NE], F32); bMK = [Buf() for _ in range(16)]
        posm = sb("posm", [128, 16, NE], F32); bposm = [Buf() for _ in range(16)]
        posmT = sb("posmT", [NE, TQ], F32); bposmT = [Buf() for _ in range(4)]
        X1B = otx[:].rearrange("p a b -> p (a b)").rearrange("p (t d) -> p t d", t=16)
        bguT = sb("bguT", [128, NE * 16], F32); bbgu = Buf()
        fw.dma(SP, bguT[:], bgu_d[:, :], [], [bbgu])
        with contextlib.ExitStack() as sr:
            wr32 = sb("wr32", [128, 8, NE], F32, sr); bwr = Buf()
            fw.dma(SP, wr32[:], w_r_d.rearrange("(dc p) e -> p dc e", p=128), [], [bwr])
            brbc = sb("brbc", [128, NE], F32, sr); bbr = Buf()
            fw.dma(SP, brbc[:], b_r_d.partition_broadcast(128), [], [bbr])
            bd32 = sb("bd32", [NE, D], F32, sr); bbd = Buf()
            fw.dma(SP, bd32[:], bd_d[:, :], [], [bbd])
            GT = sb("GT", [NE, TQ], F32, sr); bGT = [Buf() for _ in range(16)]
            x1T32 = Rot([(sb(f"x1T32_{i}", [128, 8, 128], F32, sr), Buf()) for i in range(2)])
            rt = Rot([(sb(f"rt{i}", [128, 128], F32, sr), Buf()) for i in range(2)])
            tpb = Rot([((psb[0], pb[0]), (psb[1], pb[1])), ((psb[2], pb[2]), (psb[3], pb[3]))])
            tmp2 = Rot([(psb[i], pb[i]) for i in (4, 5, 6, 7)])
            for tb in range(16):
                fw.op(DVE, [bACC[tb]], [botx[tb]], lambda: nc.vector.tensor_copy(X1B[:, tb, :], ACC[:, tb, :]))
                banks = tpb.get()
                xT32, bxT32 = x1T32.get()
                for hb_ in range(2):
                    tp, btp = banks[hb_]
                    for q in range(4):
                        dc = hb_ * 4 + q
                        fw.op(PE, [bACC[tb], bcst], [btp], lambda: nc.tensor.transpose(tp[:, q * 128:(q + 1) * 128], ACC[:, tb, dc * 128:(dc + 1) * 128], ident), inc=(q == 3))
                    fw.op(ACT, [btp], [bxT32], lambda: nc.scalar.copy(xT32[:, hb_ * 4:(hb_ + 1) * 4, :], tp[:].rearrange("p (a b) -> p a b", a=4)))
                lgp, blgp = tmp2.get()
                for dc in range(8):
                    fw.op(PE, [bxT32, bwr], [blgp], lambda: nc.tensor.matmul(lgp[:, 0:NE], xT32[:, dc, :], wr32[:, dc, :], start=(dc == 0), stop=(dc == 7)), inc=(dc == 7))
                r_, br_ = rt.get()
                lg = r_[:, 0:32]; m8 = r_[:, 32:40]; ex = r_[:, 40:72]; mk = r_[:, 72:104]; misc = r_[:, 104:112]
                fw.op(DVE, [blgp, bbr], [br_], lambda: nc.vector.tensor_tensor(lg, lgp[:, 0:NE], brbc[:], op=ALU.add))
                fw.op(DVE, [br_], [br_], lambda: nc.vector.max(out=m8, in_=lg))
                fw.op(DVE, [br_], [br_], lambda: nc.vector.tensor_scalar(misc[:, 0:1], m8[:, 0:1], -1.0, None, op0=ALU.mult))
                fw.op(ACT, [br_], [br_], lambda: nc.scalar.activation(ex, lg, AF.Exp, bias=misc[:, 0:1]))
                fw.op(DVE, [br_], [bMK[tb]], lambda: nc.vector.tensor_scalar(MK[:, tb, :], lg, m8[:, 3:4], None, op0=ALU.is_ge))
                fw.op(DVE, [br_, bMK[tb]], [br_], lambda: nc.vector.tensor_tensor(ex, ex, MK[:, tb, :], op=ALU.mult))
                fw.op(DVE, [br_], [br_], lambda: nc.vector.reduce_sum(misc[:, 1:2], ex, axis=AX.X))
                fw.op(DVE, [br_], [br_], lambda: nc.vector.reciprocal(misc[:, 2:3], misc[:, 1:2]))
                fw.op(DVE, [br_], [bG[tb]], lambda: nc.vector.tensor_scalar(G[:, tb, :], ex, misc[:, 2:3], None, op0=ALU.mult))
                gtp, bgtp = tmp2.get()
                fw.op(PE, [bG[tb], bcst], [bgtp], lambda: nc.tensor.transpose(gtp[0:NE, 0:128], G[:, tb, :], ident))
                fw.op(ACT, [bgtp], [bGT[tb]], lambda: nc.scalar.copy(GT[:, tb * 128:(tb + 1) * 128], gtp[0:NE, 0:128]))
                for half in range(2):
                    bdp, bbdp = tmp2.get()
                    fw.op(PE, [bGT[tb], bbd], [bbdp], lambda: nc.tensor.matmul(bdp[:], GT[:, tb * 128:(tb + 1) * 128], bd32[:, half * 512:(half + 1) * 512], start=True, stop=True))
                    fw.op(DVE, [bbdp, bACC[tb]], [bACC[tb]],
                          lambda: nc.vector.scalar_tensor_tensor(ACC[:, tb, half * 512:(half + 1) * 512], in0=ACC[:, tb, half * 512:(half + 1) * 512], scalar=DN_ALPHA, in1=bdp[:], op0=ALU.mult, op1=ALU.add))

            triu = cst[:, 1024:1152]
            for tb in range(16):
                pp, bpp = tmp2.get()
                fw.op(PE, [bMK[tb], bcst], [bpp], lambda: nc.tensor.matmul(pp[:, 0:NE], triu, MK[:, tb, :], start=True, stop=(tb == 0)), inc=(tb == 0))
                for t2_ in range(tb):
                    fw.op(PE, [bMK[t2_], bones], [bpp], lambda: nc.tensor.matmul(pp[:, 0:NE], ones32[:], MK[:, t2_, :], start=False, stop=(t2_ == tb - 1)), inc=(t2_ == tb - 1))
                fw.op(DVE, [bpp, bMK[tb]], [bposm[tb]], lambda: nc.vector.scalar_tensor_tensor(posm[:, tb, :], in0=pp[:, 0:NE], scalar=1.0, in1=MK[:, tb, :], op0=ALU.add, op1=ALU.mult))
                fw.op(DVE, [bposm[tb]], [bposm[tb]], lambda: nc.vector.tensor_scalar(posm[:, tb, :], posm[:, tb, :], -1.0, None, op0=ALU.add))
                ptp, bptp = tmp2.get()
                fw.op(PE, [bposm[tb], bcst], [bptp], lambda: nc.tensor.transpose(ptp[0:NE, 0:128], posm[:, tb, :], ident))
                fw.op(ACT, [bptp], [bposmT[tb // 4]], lambda: nc.scalar.copy(posmT[:, tb * 128:(tb + 1) * 128], ptp[0:NE, 0:128]))
        fw.fence()
        with contextlib.ExitStack() as se:
            NSB = CAP // 128
            wgr = Rot([(sb(f"wg{i}", [128, 8, 2, 128], BF16, se), Buf()) for i in range(7)])
            Wd = sb("Wd", [128, 8, D], BF16, se); bWd = [Buf() for _ in range(4)]
            xgT = sb("xgT", [128, 8, CAP], BF16, se); bxg = [Buf() for _ in range(8)]
            ACTT = sb("ACTT", [128, 8, CAP], BF16, se); bACTT = [Buf() for _ in range(8)]
            Sel = sb("Sel", [128, 16, CAP], BF16, se); bSel = [Buf() for _ in range(16)]
            SelT = Rot([(sb(f"SelT{i}", [128, NSB, 512], BF16, se), Buf()) for i in range(2)])
            yb = sb("yb", [128, NSB, D], BF16, se); byb = [Buf() for _ in range(NSB)]
            gc_ = sb("gc", [128, CAP], F32, se); bgc_ = Buf()
            sg_ = sb("sg", [128, CAP], F32, se); bsg_ = Buf()
            uc_ = sb("uc", [128, CAP], F32, se); buc_ = Buf()
            le = sb("le", [NE, 128], F32, se); ble = Buf()
            busT = sb("busT", [128, NE * 8], F32, se); bbus = Buf()
            fw.op(DVE, [bbgu], [bbus], lambda: nc.vector.tensor_scalar(busT[:].rearrange("p (e c) -> p e c", c=8), bguT[:].rearrange("p (e c) -> p e c", c=16)[:, :, 8:16], 1.0 / 1.702, None, op0=ALU.mult))
            gab = Rot([(psb[i], pb[i]) for i in (0, 1)])
            gub = Rot([((psb[2], pb[2]), (psb[3], pb[3])), ((psb[4], pb[4]), (psb[5], pb[5]))])
            yb_b = Rot([(psb[i], pb[i]) for i in (6, 7)])
            wd_v = wd_d.rearrange("e (fc p) o -> e p fc o", p=128)
            iotaC = cst[:, 640:640 + CAP]
            kiota = cst[0:NE, 1152:1280]

            def load_wg(e, fc):
                wg_, bwg_ = wgr.get()
                fw.dma(POOL, wg_[:].rearrange("p a b c -> p (a b c)"), wgu_d[e, fc], [], [bwg_])
                return wg_, bwg_

            def load_wd(e, pc):
                fw.dma(POOL, Wd[:, 2 * pc:2 * pc + 2, :], wd_v[e, :, 2 * pc:2 * pc + 2, :], [], [bWd[pc]])

            def build_sel(e):
                for tb in range(16):
                    fw.op(DVE, [bposm[tb], bcst], [bSel[tb]], lambda: nc.vector.tensor_scalar(Sel[:, tb, :], iotaC, posm[:, tb, e:e + 1], None, op0=ALU.is_equal))

            def gather(e):
                for dc in range(8):
                    gp, bgp = gab.get()
                    for tb in range(16):
                        fw.op(PE, [botx[tb], bSel[tb]], [bgp], lambda: nc.tensor.matmul(gp[:, 0:CAP], X1B[:, tb, dc * 128:(dc + 1) * 128], Sel[:, tb, :], start=(tb == 0), stop=(tb == 15)), inc=(tb == 15))
                    fw.op(ACT, [bgp], [bxg[dc]], lambda: nc.scalar.copy(xgT[:, dc, :], gp[:, 0:CAP]))

            slices = [(e, fc) for e in range(NE) for fc in range(8)]
            PRE = 6
            WD_SCHED = {0: 0, 1: 1, 2: 2, 3: 3}
            loaded = {}
            for k in range(min(PRE, len(slices))):
                loaded[k] = load_wg(*slices[k])
            for k, (e, fc) in enumerate(slices):
                if k == 0:
                    build_sel(0)
                    gather(0)
                    build_sel(1)
                if k + PRE < len(slices):
                    loaded[k + PRE] = load_wg(*slices[k + PRE])
                wg_, bwg_ = loaded.pop(k)
                if fc in WD_SCHED:
                    load_wd(e, WD_SCHED[fc])
                (gps, bgps), (ups, bups) = gub.get()
                rd = [bwg_] + bxg
                for dc in range(8):
                    fw.op(PE, rd, [bgps], lambda: nc.tensor.matmul(gps[:, 0:CAP], wg_[:, dc, 0, :], xgT[:, dc, :], start=(dc == 0), stop=(dc == 7)), inc=(dc == 7))
                for dc in range(8):
                    fw.op(PE, rd, [bups], lambda: nc.tensor.matmul(ups[:, 0:CAP], wg_[:, dc, 1, :], xgT[:, dc, :], start=(dc == 0), stop=(dc == 7)), inc=(dc == 7))
                bg_col = bguT[:, e * 16 + fc:e * 16 + fc + 1]
                bu_col = bguT[:, e * 16 + 8 + fc:e * 16 + 8 + fc + 1]
                bus_col = busT[:, e * 8 + fc:e * 8 + fc + 1]
                fw.op(DVE, [bgps, bbgu], [bgc_], lambda: nc.vector.tensor_scalar(gc_[:], gps[:, 0:CAP], bg_col, 7.0, op0=ALU.add, op1=ALU.min))
                fw.op(ACT, [bups, bbus], [buc_], lambda: nc.scalar.activation(uc_[:], ups[:, 0:CAP], AF.Identity, bias=bus_col, scale=1.0 / 1.702))
                fw.op(ACT, [bgc_], [bsg_], lambda: nc.scalar.activation(sg_[:], gc_[:], AF.Silu, scale=1.702))
                fw.op(DVE, [buc_], [buc_], lambda: nc.vector.tensor_scalar(uc_[:], uc_[:], 7.0 / 1.702, -7.0 / 1.702, op0=ALU.min, op1=ALU.max))
                fw.op(DVE, [bsg_, buc_], [bACTT[fc]],
                      lambda: nc.vector.scalar_tensor_tensor(ACTT[:, fc, :], in0=uc_[:], scalar=1.0 / 1.702, in1=sg_[:], op0=ALU.add, op1=ALU.mult))
                if fc == 7:
                    if e + 1 < NE:
                        gather(e + 1)
                        if e + 2 < NE:
                            build_sel(e + 2)
                    for sbk in range(NSB):
                        for half in range(2):
                            yp, byp = yb_b.get()
                            rd2 = bACTT + bWd
                            for f in range(8):
                                fw.op(PE, rd2, [byp], lambda: nc.tensor.matmul(yp[:], ACTT[:, f, sbk * 128:(sbk + 1) * 128], Wd[:, f, half * 512:(half + 1) * 512], start=(f == 0), stop=(f == 7)), inc=(f == 7))
                            fw.op(ACT, [byp], [byb[sbk]], lambda: nc.scalar.copy(yb[:, sbk, half * 512:(half + 1) * 512], yp[:]))
                    fw.op(DVE, [bcst], [ble], lambda: nc.vector.tensor_scalar(le[:], kiota, float(e), None, op0=ALU.is_equal))
                    bcs = {}

                    def emit_bc(ch):
                        bc, bbc = gab.get()
                        fw.op(PE, [ble, bposmT[ch]], [bbc], lambda: nc.tensor.matmul(bc[:], le[:], posmT[:, ch * 512:(ch + 1) * 512], start=True, stop=True))
                        bcs[ch] = (bc, bbc)

                    emit_bc(0)
                    for ch in range(4):
                        bc, bbc = bcs.pop(ch)
                        if ch + 1 < 4:
                            emit_bc(ch + 1)
                        st_, bst2 = SelT.get()
                        for sbk in range(NSB):
                            fw.op(DVE, [bbc, bcst], [bst2], lambda: nc.vector.tensor_scalar(st_[:, sbk, :], bc[:], cst[:, 516 + sbk:517 + sbk], None, op0=ALU.is_equal))
                        for tb4 in range(4):
                            tb = ch * 4 + tb4
                            for half in range(2):
                                yp, byp = yb_b.get()
                                for sbk in range(NSB):
                                    fw.op(PE, [bst2] + byb, [byp], lambda: nc.tensor.matmul(yp[:], st_[:, sbk, tb4 * 128:(tb4 + 1) * 128], yb[:, sbk, half * 512:(half + 1) * 512], start=(sbk == 0), stop=(sbk == NSB - 1)), inc=(sbk == NSB - 1))
                                fw.op(DVE, [byp, bG[tb], bACC[tb]], [bACC[tb]],
                                      lambda: nc.vector.scalar_tensor_tensor(ACC[:, tb, half * 512:(half + 1) * 512], in0=yp[:], scalar=G[:, tb, e:e + 1], in1=ACC[:, tb, half * 512:(half + 1) * 512], op0=ALU.mult, op1=ALU.add))

        fw.fence()
        with contextlib.ExitStack() as sf:
            ln2bc = sb("ln2bc", [128, 2, D], F32, sf); bln2 = Buf()
            fw.dma(SP, ln2bc[:], lnv_d[2:4, :].partition_broadcast(128), [], [bln2])
            ot = Rot([(sb(f"ot{i}", [128, D], F32, sf), Buf()) for i in range(2)])
            jk2 = Rot([(sb(f"jk2{i}", [128, D], F32, sf), Buf()) for i in range(2)])
            for tb in range(16):
                o_, bo_ = ot.get()
                j_, bj_ = jk2.get()
                layer_norm(ACC[:, tb, :], bACC[tb], tb, ln2bc, bln2, o_[:], bo_, j_[:], bj_)
                fw.dma(SP, out_d[tb * 128:(tb + 1) * 128, :], o_[:], [bo_], [])
        fw.finish(SP)
    return nc


_PROG = {}


def _consts(r):
    c = np.zeros((128, 1280), np.float32)
    c[:, 0:128] = np.eye(128, dtype=np.float32)
    m = np.arange(128)
    partner = np.where(m % 64 < 32, m + 32, m - 32)
    c[partner, 128 + m] = 1.0
    k = np.arange(128)[:, None]
    q = np.arange(128)[None, :]
    c[:, 256:384] = (k <= q).astype(np.float32)
    c[:, 384:512] = 1.0 if r == 1 else 0.0
    invf = 1.0 / (10000.0 ** ((np.arange(128) % 32) * 2.0 / 64.0))
    first = (np.arange(128) % 64) < 32
    c[:, 512] = invf
    sc = TWO_PI * (1.0 - 1e-6)
    c[:, 513] = np.where(first, -sc, sc)
    c[:, 514] = np.where(first, PI_S, -PI_S)
    c[:, 515] = -PI_S
    p = np.arange(128, dtype=np.float32)
    c[:, 516] = p
    c[:, 517] = p + 128.0
    c[:, 518] = p + 256.0
    c[:, 640:1024] = np.arange(CAP, dtype=np.float32)[None, :]
    c[:, 1024:1152] = (p[:, None] < p[None, :]).astype(np.float32)
    c[:, 1152:1280] = p[:, None]
    return c


def _prep_inputs(inp):
    f32 = lambda a: np.ascontiguousarray(np.asarray(a), dtype=np.float32)
    x = f32(inp["x"])
    positions = np.ascontiguousarray(np.asarray(inp["positions"]), dtype=np.int32)
    wgu = f32(inp["w_gate_up"])[0]
    wgu_t = np.ascontiguousarray(
        wgu.reshape(NE, 8, 128, 2, 8, 128).transpose(0, 4, 2, 1, 3, 5)).reshape(NE, 8, 128, 2048)
    bgu = f32(inp["b_gate_up"])[0]
    bguT = np.ascontiguousarray(bgu.reshape(NE, 16, 128).transpose(2, 0, 1)).reshape(128, NE * 16)
    shared = {
        "w_in": f32(inp["w_in"])[0],
        "lamv": np.ascontiguousarray(np.stack([f32(inp["lambda_q1"])[0], f32(inp["lambda_k1"])[0],
                                               f32(inp["lambda_q2"])[0], f32(inp["lambda_k2"])[0]], 0)),
        "subln_g": f32(inp["subln_g"])[0].reshape(128, 1),
        "gq": np.ascontiguousarray(f32(inp["mla_q_norm_g"])[0].reshape(2, 128).T),
        "gkv": f32(inp["mla_kv_norm_g"])[0].reshape(128, 1),
        "w_uq": f32(inp["w_uq"])[0],
        "w_ukv": f32(inp["w_ukv"])[0],
        "w_o": f32(inp["w_o"])[0],
        "lnv": np.ascontiguousarray(np.stack([f32(inp["ln1_g"])[0], f32(inp["ln1_b"])[0],
                                              f32(inp["ln2_g"])[0], f32(inp["ln2_b"])[0]], 0)),
        "w_router": f32(inp["w_router"])[0],
        "b_router": f32(inp["b_router"])[0].reshape(1, NE),
        "wgu_t": wgu_t,
        "bguT": bguT,
        "w_down": f32(inp["w_down"])[0],
        "b_down": f32(inp["b_down"])[0],
    }
    in_maps = []
    toks = []
    for c in range(NCORES):
        b, r = c // 2, c % 2
        own = [2 * j + r for j in range(16)]
        oth = [2 * j + (1 - r) for j in range(16)]
        tok = np.concatenate([np.arange(g * 128, (g + 1) * 128) for g in own + oth])
        toks.append((b, tok[:TQ]))
        xb = x[b][tok]
        m = dict(shared)
        m["xT"] = np.ascontiguousarray(xb.T)
        m["xq"] = np.ascontiguousarray(xb[:TQ])
        m["pos"] = np.ascontiguousarray(positions[b][tok].reshape(1, S))
        m["cst"] = _consts(r)
        in_maps.append(m)
    return in_maps, toks


def kernel(**inputs):
    stage = inputs.pop("_stage", "full")
    if stage not in _PROG:
        _PROG[stage] = build_program(stage)
    nc = _PROG[stage]
    in_maps, toks = _prep_inputs(inputs)
    if stage != "full":
        moe = ("w_router", "b_router", "wgu_t", "bguT", "w_down", "b_down")
        in_maps = [{k: v for k, v in m.items() if k not in moe} for m in in_maps]
    res = run_bass_kernel_spmd(nc, in_maps, core_ids=list(range(NCORES)))
    out = np.zeros((4, S, D), np.float32)
    for c in range(NCORES):
        b, tok = toks[c]
        out[b, tok] = res.results[c]["out"]
    return out
```

```python
import math
import contextlib
import numpy as np
import concourse.bass as bass
import concourse.mybir as mybir
from concourse.bass_utils import run_bass_kernel_spmd

F32 = mybir.dt.float32
BF16 = mybir.dt.bfloat16
I32 = mybir.dt.int32
ALU = mybir.AluOpType
AF = mybir.ActivationFunctionType
AX = mybir.AxisListType

NCORES = 8
D = 1024
S = 4096
TQ = 2048
NE = 32
LAM_INIT = 0.8 - 0.6 * math.exp(0.0)
DN_ALPHA = 2.0 ** 0.25
TWO_PI = 2.0 * math.pi
PI_S = math.pi * (1.0 - 1e-6)
NDMA_SEM = 8
CAP = 384


_FENCE = {}


class Buf:
    __slots__ = ("w", "r", "excl")

    def __init__(self, excl=False):
        self.w = None
        self.r = dict(_FENCE)
        self.excl = excl


class Eng:
    def __init__(self, name, h, sem, dma_sems):
        self.name = name
        self.h = h
        self.sem = sem
        self.count = 0
        self.seen = {}
        self.dma_sems = dma_sems
        self.dma_val = [0] * len(dma_sems)
        self.rr = 0


class FW:
    def __init__(self, nc, es):
        self.nc = nc
        self.es = es
        mk = lambda n: es.enter_context(nc.semaphore(n))
        self.pe = Eng("pe", nc.tensor, mk("s_pe"), [])
        self.act = Eng("act", nc.scalar, mk("s_act"), [])
        self.dve = Eng("dve", nc.vector, mk("s_dve"), [])
        self.pool = Eng("pool", nc.gpsimd, mk("s_pool"), [mk(f"d_pool{i}") for i in range(NDMA_SEM)])
        self.sp = Eng("sp", nc.sync, mk("s_sp"), [mk(f"d_sp{i}") for i in range(NDMA_SEM)])
        self.nwait = 0

    def _wait(self, E, tok):
        sem, val = tok
        if sem is E.sem and (E is self.pe or val > E.count):
            return
        k = id(sem)
        if E.seen.get(k, 0) >= val:
            return
        E.h.wait_ge(sem, val)
        E.seen[k] = val
        self.nwait += 1

    def _deps(self, E, reads, writes):
        for b in reads:
            if b.w is not None:
                self._wait(E, b.w)
            if b.excl:
                for tok in b.r.values():
                    self._wait(E, tok)
        for b in writes:
            if b.w is not None:
                self._wait(E, b.w)
            for tok in b.r.values():
                self._wait(E, tok)

    def _mark(self, tok, reads, writes):
        for b in reads:
            b.r[id(tok[0])] = tok
        for b in writes:
            b.w = tok
            b.r = {}

    def op(self, E, reads, writes, build, inc=True):
        self._deps(E, reads, writes)
        ins = build()
        tok = (E.sem, E.count + 1)
        if inc:
            ins.then_inc(E.sem, 1)
            E.count += 1
        self._mark(tok, reads, writes)
        return ins

    def dma(self, Q, out, in_, reads, writes, **kw):
        i = Q.rr % len(Q.dma_sems)
        Q.rr += 1
        sem = Q.dma_sems[i]
        if Q.dma_val[i] > 0:
            self._wait(Q, (sem, Q.dma_val[i]))
        self._deps(Q, reads, writes)
        Q.h.dma_start(out=out, in_=in_, **kw).then_inc(sem, 16)
        Q.dma_val[i] += 16
        tok = (sem, Q.dma_val[i])
        self._mark(tok, reads, writes)
        return tok

    def fence(self):
        _FENCE.clear()
        for Q in (self.sp, self.pool):
            for sem, v in zip(Q.dma_sems, Q.dma_val):
                if v > 0:
                    _FENCE[id(sem)] = (sem, v)
        for X in (self.pe, self.act, self.dve, self.pool, self.sp):
            if X.count > 0:
                _FENCE[id(X.sem)] = (X.sem, X.count)

    def finish(self, E):
        for Q in (self.sp, self.pool):
            for sem, v in zip(Q.dma_sems, Q.dma_val):
                if v > 0:
                    self._wait(E, (sem, v))
        for X in (self.pe, self.act, self.dve, self.pool, self.sp):
            if X is not E and X.count > 0:
                self._wait(E, (X.sem, X.count))


class Rot:
    def __init__(self, items):
        self.items = items
        self.i = 0

    def get(self):
        it = self.items[self.i % len(self.items)]
        self.i += 1
        return it


def build_program(stage="full"):
    nc = bass.Bass("TRN2", target_bir_lowering=False)
    dt_in = lambda name, shape, dt=F32: nc.dram_tensor(name, shape, dt, kind="ExternalInput").ap()
    xT_d = dt_in("xT", [D, S])
    xq_d = dt_in("xq", [TQ, D])
    pos_d = dt_in("pos", [1, S], I32)
    cst_d = dt_in("cst", [128, 1280])
    w_in_d = dt_in("w_in", [D, 1984])
    lam_d = dt_in("lamv", [4, 64])
    subg_d = dt_in("subln_g", [128, 1])
    gq_d = dt_in("gq", [128, 2])
    gkv_d = dt_in("gkv", [128, 1])
    w_uq_d = dt_in("w_uq", [256, 768])
    w_ukv_d = dt_in("w_ukv", [128, 1024])
    w_o_d = dt_in("w_o", [D, D])
    lnv_d = dt_in("lnv", [4, D])
    if stage == "full":
        w_r_d = dt_in("w_router", [D, NE])
        b_r_d = dt_in("b_router", [1, NE])
        wgu_d = dt_in("wgu_t", [NE, 8, 128, 2048])
        bgu_d = dt_in("bguT", [128, NE * 16])
        wd_d = dt_in("w_down", [NE, D, D])
        bd_d = dt_in("b_down", [NE, D])
    out_d = nc.dram_tensor("out", [TQ, D], F32, kind="ExternalOutput").ap()

    _FENCE.clear()
    with contextlib.ExitStack() as es:
        fw = FW(nc, es)
        PE, ACT, DVE, POOL, SP = fw.pe, fw.act, fw.dve, fw.pool, fw.sp

        def sb(name, shape, dt, st=es):
            return st.enter_context(nc.sbuf_tensor("sb_" + name, shape, dt))

        psb = [es.enter_context(nc.psum_tensor(f"ps{i}", [128, 512], F32)) for i in range(8)]
        pb = [Buf(excl=True) for _ in range(8)]

        cst = sb("cst", [128, 1280], F32); bcst = Buf()
        fw.dma(SP, cst[:], cst_d[:, :], [], [bcst])
        ident = cst[:, 0:128]
        ropec = cst[:, 512:516]
        cbf = sb("cbf", [128, 512], BF16); bcbf = Buf()
        fw.op(DVE, [bcst], [bcbf], lambda: nc.vector.tensor_copy(cbf[:, 0:384], cst[:, 128:512]))
        fw.op(DVE, [], [bcbf], lambda: nc.vector.memset(cbf[:, 384:512], 1.0))
        perm_bf = cbf[:, 0:128]
        masks_bf = [cbf[:, 128:256], cbf[:, 256:384]]
        ones_bf = cbf[:, 384:512]
        ones32 = sb("ones32", [128, 128], F32); bones = Buf()
        fw.op(POOL, [], [bones], lambda: nc.gpsimd.memset(ones32[:], 1.0))
        small = sb("small", [128, 64], F32); bsmall = Buf()
        fw.dma(SP, small[:, 0:1], subg_d[:, :], [], [bsmall])
        fw.dma(SP, small[:, 3:5], gq_d[:, :], [], [bsmall])
        fw.dma(SP, small[:, 5:6], gkv_d[:, :], [], [bsmall])
        fw.op(DVE, [], [bsmall], lambda: nc.vector.memset(small[:, 8:9], 1e-6))
        fw.op(DVE, [], [bsmall], lambda: nc.vector.memset(small[:, 9:10], 1e-5))
        EPS6 = small[:, 8:9]
        EPS5 = small[:, 9:10]
        lamt = sb("lamt", [128, 256], F32); blam = Buf()
        fw.dma(SP, lamt[:].rearrange("p (a b) -> p a b", a=4), lam_d.partition_broadcast(128), [], [blam])
        fw.op(DVE, [blam], [blam], lambda: nc.vector.tensor_tensor(lamt[:, 0:64], lamt[:, 0:64], lamt[:, 64:128], op=ALU.mult))
        fw.op(DVE, [blam], [blam], lambda: nc.vector.tensor_tensor(lamt[:, 128:192], lamt[:, 128:192], lamt[:, 192:256], op=ALU.mult))
        fw.op(DVE, [blam], [bsmall], lambda: nc.vector.reduce_sum(small[:, 6:7], lamt[:, 0:64], axis=AX.X))
        fw.op(DVE, [blam], [bsmall], lambda: nc.vector.reduce_sum(small[:, 7:8], lamt[:, 128:192], axis=AX.X))
        fw.op(ACT, [bsmall], [bsmall], lambda: nc.scalar.activation(small[:, 6:8], small[:, 6:8], AF.Exp))
        fw.op(DVE, [bsmall], [bsmall], lambda: nc.vector.tensor_tensor(small[:, 2:3], small[:, 7:8], small[:, 6:7], op=ALU.subtract))
        fw.op(DVE, [bsmall], [bsmall], lambda: nc.vector.tensor_scalar(small[:, 2:3], small[:, 2:3], -LAM_INIT, None, op0=ALU.add))
        fw.op(DVE, [bsmall], [bsmall], lambda: nc.vector.tensor_scalar(small[:, 1:2], small[:, 0:1], 1.0 - LAM_INIT, None, op0=ALU.mult))

        otx = sb("otx", [128, 8, TQ], BF16)
        botx = [Buf() for _ in range(16)]

        with contextlib.ExitStack() as sa:
            cosT = sb("cosT", [128, S], F32, sa)
            sinS = sb("sinS", [128, S], F32, sa)
            btab = [Buf() for _ in range(8)]
            ckvn = sb("ckvn", [128, S], BF16, sa); bckvn = [Buf() for _ in range(8)]
            cqn = sb("cqn", [128, 2, TQ], BF16, sa); bcqn = [Buf() for _ in range(4)]
            KR = sb("KR", [128, S], BF16, sa); bKR = [Buf() for _ in range(8)]
            fw.op(POOL, [], bKR, lambda: nc.gpsimd.memset(KR[64:128, :], 0.0))
            s32 = Rot([(sb(f"s32_{i}", [128, 512], F32, sa), Buf()) for i in range(8)])
            s16 = Rot([(sb(f"s16_{i}", [128, 512], BF16, sa), Buf()) for i in range(4)])
            si32 = sb("si32", [128, 512], I32, sa); bsi32 = Buf()
            tmpb = Rot([(psb[i], pb[i]) for i in (6, 7, 0, 1)])

            def mm_acc(out_ap, pairs, reads, writes):
                n = len(pairs)
                for i, (l, r) in enumerate(pairs):
                    fw.op(PE, reads, writes,
                          lambda: nc.tensor.matmul(out_ap, l, r, start=(i == 0), stop=(i == n - 1)),
                          inc=(i == n - 1))

            def rope(src_ps, bsrc, rows, tc, dst_ap, bdst):
                cols = slice(tc * 512, (tc + 1) * 512)
                import os as _os
                _cut = int(_os.environ.get("ROPE_CUT", "9")) if rows == 128 else 9
                if _cut < 1:
                    return
                hb, bhb = s16.get()
                fw.op(ACT, [bsrc], [bhb], lambda: nc.scalar.copy(hb[0:rows, :], src_ps))
                if _cut < 2:
                    return
                sw, bsw = tmpb.get()
                fw.op(PE, [bhb, bcbf], [bsw], lambda: nc.tensor.matmul(sw[0:rows, :], perm_bf[0:rows, 0:rows], hb[0:rows, :], start=True, stop=True))
                if _cut < 3:
                    return
                t1, bt1 = s32.get()
                fw.op(DVE, [bsrc, btab[tc]], [bt1], lambda: nc.vector.tensor_tensor(t1[0:rows, :], src_ps, cosT[0:rows, cols], op=ALU.mult))
                if _cut < 4:
                    return
                t2, bt2 = s32.get()
                fw.op(DVE, [bsw, btab[tc]], [bt2], lambda: nc.vector.tensor_tensor(t2[0:rows, :], sw[0:rows, :], sinS[0:rows, cols], op=ALU.mult))
                if _cut < 5:
                    return
                fw.op(DVE, [bt1, bt2], [bdst], lambda: nc.vector.tensor_tensor(dst_ap, t1[0:rows, :], t2[0:rows, :], op=ALU.add))

            def rms_scale(ps_list, bps_list, n_feat, eps, gcols, dst_aps, bdst):
                sqs = []
                for ps_ap, bps in zip(ps_list, bps_list):
                    sq, bsq = s32.get()
                    fw.op(ACT, [bps], [bsq], lambda: nc.scalar.activation(sq[:], ps_ap, AF.Square))
                    sqs.append((sq, bsq))
                ss, bss = tmpb.get()
                for i, (sq, bsq) in enumerate(sqs):
                    fw.op(PE, [bsq, bones], [bss],
                          lambda: nc.tensor.matmul(ss[:], ones32[:], sq[:], start=(i == 0), stop=(i == len(sqs) - 1)),
                          inc=(i == len(sqs) - 1))
                rstd, brstd = s32.get()
                fw.op(ACT, [bss, bsmall], [brstd], lambda: nc.scalar.activation(rstd[:], ss[:], AF.Sqrt, bias=eps, scale=1.0 / n_feat))
                fw.op(DVE, [brstd], [brstd], lambda: nc.vector.reciprocal(rstd[:], rstd[:]))
                for ps_ap, bps, gc, dst in zip(ps_list, bps_list, gcols, dst_aps):
                    fw.op(DVE, [bps, brstd, bsmall], [bdst],
                          lambda: nc.vector.scalar_tensor_tensor(dst, in0=ps_ap, scalar=gc, in1=rstd[:], op0=ALU.mult, op1=ALU.mult))

            def attention(nsub, s_emit, s_reads, Vt, bV, scale, finalize):
                S_B = [(psb[0], pb[0]), (psb[1], pb[1])]
                O_B = [(psb[2], pb[2]), (psb[3], pb[3])]
                L_B = [(psb[4], pb[4]), (psb[5], pb[5])]
                pending = [None]
                for g in range(4):
                    units = []
                    nkb = 4 * g + 4
                    for half in (0, 1):
                        for kl in range(nkb):
                            i = kl - 4 * g
                            col0 = 0 if i < 0 else i * 128
                            mt = None if i < 0 else half
                            for c in range(nsub):
                                units.append((c, half * 16 + kl, col0, mt))
                    nun = len(units)
                    first = [True] * nsub
                    last_idx = {}
                    for ui, u in enumerate(units):
                        last_idx[u[0]] = ui
                    pts = {}

                    def emit_s(ui):
                        c, kb, col0, mt = units[ui]
                        sbk, bsbk = S_B[ui % 2]
                        s_emit(c, kb, g, col0, sbk, bsbk)
                        pT, bpT = s16.get()
                        fw.op(ACT, [bsbk], [bpT], lambda: nc.scalar.activation(pT[:, col0:512], sbk[:, col0:512], AF.Exp, scale=scale))
                        if mt is not None:
                            fw.op(POOL, [bpT, bcbf], [bpT], lambda: nc.gpsimd.tensor_tensor(pT[:, col0:col0 + 128], pT[:, col0:col0 + 128], masks_bf[mt], op=ALU.mult))
                        pts[ui] = (pT, bpT)

                    def emit_pv(ui):
                        c, kb, col0, mt = units[ui]
                        pT, bpT = pts.pop(ui)
                        o, bo = O_B[c]
                        l, bl = L_B[c]
                        st = first[c]
                        first[c] = False
                        sp_ = (last_idx[c] == ui)
                        fw.op(PE, [bpT, bV[kb // 4]], [bo], lambda: nc.tensor.matmul(o[:, col0:512], Vt[:, kb, :], pT[:, col0:512], start=st, stop=sp_), inc=False)
                        fw.op(PE, [bpT, bcbf], [bl], lambda: nc.tensor.matmul(l[:, col0:512], ones_bf, pT[:, col0:512], start=st, stop=sp_))

                    LOOK = 2
                    for ui in range(min(LOOK, nun)):
                        emit_s(ui)
                    if pending[0] is not None:
                        pending[0]()
                    for ui in range(nun):
                        emit_pv(ui)
                        if ui + LOOK < nun:
                            emit_s(ui + LOOK)
                    pending[0] = (lambda g=g: finalize(g, O_B, L_B))
                pending[0]()

            with contextlib.ExitStack() as sx:
                xTb = sb("xTb", [128, 8, S], BF16, sx); bxT = [Buf() for _ in range(8)]
                xT_v = xT_d.rearrange("(dc p) t -> p dc t", p=128)
                for tc in range(8):
                    fw.dma(POOL, xTb[:, :, tc * 512:(tc + 1) * 512], xT_v[:, :, tc * 512:(tc + 1) * 512], [], [bxT[tc]])
                w_in_v = w_in_d.rearrange("(dc p) c -> p dc c", p=128)

                for tc in range(8):
                    cols = slice(tc * 512, (tc + 1) * 512)
                    fw.dma(SP, si32[:], pos_d[0:1, cols].partition_broadcast(128), [], [bsi32])
                    ang, bang = s32.get()
                    fw.op(DVE, [bsi32], [bang], lambda: nc.vector.tensor_copy(ang[:], si32[:]))
                    fw.op(DVE, [bang, bcst], [bang], lambda: nc.vector.tensor_scalar(ang[:], ang[:], ropec[:, 0:1], None, op0=ALU.mult))
                    for which in (0, 1):
                        shift = 0.5 if which == 0 else 0.75
                        u, bu = s32.get()
                        fw.op(DVE, [bang], [bu], lambda: nc.vector.tensor_scalar(u[:], ang[:], 1.0 / TWO_PI, shift, op0=ALU.mult, op1=ALU.add))
                        ki, bki = s32.get()
                        kiv = ki[:].bitcast(I32)
                        fw.op(DVE, [bu], [bki], lambda: nc.vector.tensor_copy(kiv, u[:]))
                        kf, bkf = s32.get()
                        fw.op(DVE, [bki], [bkf], lambda: nc.vector.tensor_copy(kf[:], kiv))
                        fw.op(DVE, [bkf, bu], [bu], lambda: nc.vector.tensor_tensor(u[:], u[:], kf[:], op=ALU.subtract))
                        fw.op(DVE, [bu], [bkf], lambda: nc.vector.scalar_tensor_tensor(kf[:], in0=u[:], scalar=0.0, in1=u[:], op0=ALU.is_lt, op1=ALU.add))
                        if which == 0:
                            fw.op(ACT, [bkf, bcst], [btab[tc]], lambda: nc.scalar.activation(sinS[:, cols], kf[:], AF.Sin, bias=ropec[:, 2:3], scale=ropec[:, 1:2]))
                        else:
                            fw.op(ACT, [bkf, bcst], [btab[tc]], lambda: nc.scalar.activation(cosT[:, cols], kf[:], AF.Sin, bias=ropec[:, 3:4], scale=TWO_PI * (1.0 - 1e-6)))

                if stage.startswith("tabtt"):
                    import os as _os
                    r0, r1 = [int(v) for v in _os.environ.get("TT_ROWS", "0,128").split(",")]
                    mode = _os.environ.get("TT_MODE", "psum_cos")
                    t1, bt1 = s32.get()
                    kps, bkps = tmpb.get()
                    fw.op(PE, [bcbf], [bkps], lambda: nc.tensor.matmul(kps[:], perm_bf, cbf[:, 0:512], start=True, stop=True))
                    if mode == "psum_cos":
                        fw.op(DVE, [bkps, btab[0]], [bt1], lambda: nc.vector.tensor_tensor(t1[r0:r1, :], kps[r0:r1, :], cosT[r0:r1, 0:512], op=ALU.mult))
                    elif mode == "sb_cos":
                        t2, bt2 = s32.get()
                        fw.op(DVE, [], [bt2], lambda: nc.vector.memset(t2[:], 1.0))
                        fw.op(DVE, [bt2, btab[0]], [bt1], lambda: nc.vector.tensor_tensor(t1[r0:r1, :], t2[r0:r1, :], cosT[r0:r1, 0:512], op=ALU.mult))
                    elif mode == "psum_sb":
                        t2, bt2 = s32.get()
                        fw.op(DVE, [], [bt2], lambda: nc.vector.memset(t2[:], 1.0))
                        fw.op(DVE, [bkps, bt2], [bt1], lambda: nc.vector.tensor_tensor(t1[r0:r1, :], kps[r0:r1, :], t2[r0:r1, :], op=ALU.mult))
                    fw.dma(SP, out_d[0:128, 0:512], t1[:], [bt1], [])
                    fw.dma(SP, out_d[128:256, 0:512], cosT[:, 0:512], [btab[0]], [])
                    fw.dma(SP, out_d[256:384, 0:512], sinS[:, 0:512], [btab[0]], [])
                    fw.finish(SP)
                    return nc
                if stage == "tab":
                    fw.finish(SP)
                    return nc
                with contextlib.ExitStack() as sm0:
                    WC = sb("WC", [128, 8, 448], BF16, sm0); bWC = Buf()
                    fw.dma(POOL, WC[:], w_in_v[:, :, 1536:1984], [], [bWC])
                    for tc in range(8):
                        cols = slice(tc * 512, (tc + 1) * 512)
                        ckv, bckv = tmpb.get()
                        mm_acc(ckv[:], [(WC[:, dc, 256:384], xTb[:, dc, cols]) for dc in range(8)], [bWC, bxT[tc]], [bckv])
                        rms_scale([ckv[:]], [bckv], 128.0, EPS6, [small[:, 5:6]], [ckvn[:, cols]], bckvn[tc])
                        kr, bkr = tmpb.get()
                        mm_acc(kr[0:64, :], [(WC[:, dc, 384:448], xTb[:, dc, cols]) for dc in range(8)], [bWC, bxT[tc]], [bkr])
                        rope(kr[0:64, :], bkr, 64, tc, KR[0:64, cols], bKR[tc])
                        if tc < 4:
                            cq0, bcq0 = tmpb.get()
                            mm_acc(cq0[:], [(WC[:, dc, 0:128], xTb[:, dc, cols]) for dc in range(8)], [bWC, bxT[tc]], [bcq0])
                            cq1, bcq1 = tmpb.get()
                            mm_acc(cq1[:], [(WC[:, dc, 128:256], xTb[:, dc, cols]) for dc in range(8)], [bWC, bxT[tc]], [bcq1])
                            rms_scale([cq0[:], cq1[:]], [bcq0, bcq1], 256.0, EPS6, [small[:, 3:4], small[:, 4:5]],
                                      [cqn[:, 0, cols], cqn[:, 1, cols]], bcqn[tc])

                if stage == "m0":
                    fw.finish(SP)
                    return nc
                fw.fence()
                with contextlib.ExitStack() as sd:
                    WQ = sb("WQ", [128, 8, 128], BF16, sd); WK = sb("WK", [128, 8, 128], BF16, sd); WV = sb("WV", [128, 8, 128], BF16, sd)
                    bW = Buf()
                    KT = sb("KT", [128, S], BF16, sd); bKT = [Buf() for _ in range(8)]
                    QT = sb("QT", [128, TQ], BF16, sd); bQT = [Buf() for _ in range(4)]
                    Vt = sb("Vt", [128, 32, 128], BF16, sd); bV = [Buf() for _ in range(8)]
                    for h in range(4):
                        fw.dma(POOL, WQ[:], w_in_v[:, :, 128 * h:128 * h + 128], [], [bW])
                        fw.dma(POOL, WK[:], w_in_v[:, :, 512 + 128 * h:512 + 128 * h + 128], [], [bW])
                        fw.dma(POOL, WV[:], w_in_v[:, :, 1024 + 128 * h:1024 + 128 * h + 128], [], [bW])
                        if stage == "dprojD":
                            fw.finish(SP)
                            return nc
                        import os as _os
                        _ntc = int(_os.environ.get("DPROJ_NTC", "8"))
                        _noq = _os.environ.get("DPROJ_NOQ", "0") == "1"
                        for tc in range(_ntc):
                            cols = slice(tc * 512, (tc + 1) * 512)
                            if stage != "dprojV":
                                kps, bkps = tmpb.get()
                                mm_acc(kps[:], [(WK[:, dc, :], xTb[:, dc, cols]) for dc in range(8)], [bW, bxT[tc]], [bkps])
                                rope(kps[:], bkps, 128, tc, KT[:, cols], bKT[tc])
                            if tc < 4 and stage != "dprojV" and not _noq:
                                qps, bqps = tmpb.get()
                                mm_acc(qps[:], [(WQ[:, dc, :], xTb[:, dc, cols]) for dc in range(8)], [bW, bxT[tc]], [bqps])
                                rope(qps[:], bqps, 128, tc, QT[:, cols], bQT[tc])
                            if stage == "dprojK":
                                continue
                            vps, bvps = tmpb.get()
                            for i in range(4):
                                mm_acc(vps[:, i * 128:(i + 1) * 128],
                                       [(xTb[:, dc, tc * 512 + i * 128: tc * 512 + (i + 1) * 128], WV[:, dc, :]) for dc in range(8)],
                                       [bW, bxT[tc]], [bvps])
                            fw.op(ACT, [bvps], [bV[tc]], lambda: nc.scalar.copy(Vt[:, tc * 4:(tc + 1) * 4, :], vps[:].rearrange("p (a b) -> p a b", a=4)))

                        if stage in ("dproj", "dprojK", "dprojV"):
                            fw.finish(SP)
                            return nc
                        def s_emit(c, kb, g, col0, sbk, bsbk):
                            fw.op(PE, [bKT[kb // 4], bQT[g]], [bsbk],
                                  lambda: nc.tensor.matmul(sbk[:, col0:512], KT[64 * c:64 * c + 64, kb * 128:(kb + 1) * 128],
                                                           QT[64 * c:64 * c + 64, g * 512 + col0:(g + 1) * 512], start=True, stop=True))

                        def fin_diff(g, O_B, L_B, h=h):
                            ds = []
                            for c in range(2):
                                rl, brl = s32.get()
                                fw.op(DVE, [L_B[c][1]], [brl], lambda: nc.vector.reciprocal(rl[:], L_B[c][0][:]))
                                fw.op(DVE, [O_B[c][1], brl], [brl], lambda: nc.vector.tensor_tensor(rl[:], O_B[c][0][:], rl[:], op=ALU.mult))
                                ds.append((rl, brl))
                            dd, bdd = s32.get()
                            fw.op(DVE, [ds[0][1], ds[1][1], bsmall], [bdd],
                                  lambda: nc.vector.scalar_tensor_tensor(dd[:], in0=ds[1][0][:], scalar=small[:, 2:3], in1=ds[0][0][:], op0=ALU.mult, op1=ALU.add))
                            sq, bsq = s32.get()
                            fw.op(ACT, [bdd], [bsq], lambda: nc.scalar.activation(sq[:], dd[:], AF.Square))
                            ss, bss = tmpb.get()
                            fw.op(PE, [bsq, bones], [bss], lambda: nc.tensor.matmul(ss[:], ones32[:], sq[:], start=True, stop=True))
                            rstd, brstd = s32.get()
                            fw.op(ACT, [bss, bsmall], [brstd], lambda: nc.scalar.activation(rstd[:], ss[:], AF.Sqrt, bias=EPS5, scale=1.0 / 128.0))
                            fw.op(DVE, [brstd], [brstd], lambda: nc.vector.reciprocal(rstd[:], rstd[:]))
                            wr = [botx[4 * g + i] for i in range(4)]
                            fw.op(DVE, [bdd, brstd, bsmall], wr,
                                  lambda: nc.vector.scalar_tensor_tensor(otx[:, h, g * 512:(g + 1) * 512], in0=dd[:], scalar=small[:, 1:2], in1=rstd[:], op0=ALU.mult, op1=ALU.mult))

                        attention(2, s_emit, None, Vt, bV, 64.0 ** -0.5, fin_diff)
                        if stage == "datt":
                            fw.finish(SP)
                            return nc
            fw.fence()
            with contextlib.ExitStack() as sm:
                wuq = sb("wuq", [128, 2, 768], BF16, sm); bwuq = Buf()
                wukv = sb("wukv", [128, 1024], BF16, sm); bwukv = Buf()
                fw.dma(POOL, wuq[:], w_uq_d.rearrange("(rc p) c -> p rc c", p=128), [], [bwuq])
                fw.dma(POOL, wukv[:], w_ukv_d[:, :], [], [bwukv])
                KTm = sb("KTm", [128, S], BF16, sm); bKTm = [Buf() for _ in range(8)]
                Vm = sb("Vm", [128, 32, 128], BF16, sm); bVm = [Buf() for _ in range(8)]
                QTn = sb("QTn", [128, TQ], BF16, sm); bQTn = [Buf() for _ in range(4)]
                QTr = sb("QTr", [128, TQ], BF16, sm); bQTr = [Buf() for _ in range(4)]
                fw.op(POOL, [], bQTr, lambda: nc.gpsimd.memset(QTr[64:128, :], 0.0))
                for h in range(4):
                    for tc in range(8):
                        cols = slice(tc * 512, (tc + 1) * 512)
                        kn, bkn = tmpb.get()
                        fw.op(PE, [bwukv, bckvn[tc]], [bkn], lambda: nc.tensor.matmul(kn[:], wukv[:, h * 256:h * 256 + 128], ckvn[:, cols], start=True, stop=True))
                        fw.op(ACT, [bkn], [bKTm[tc]], lambda: nc.scalar.copy(KTm[:, cols], kn[:]))
                        vps, bvps = tmpb.get()
                        for i in range(4):
                            fw.op(PE, [bwukv, bckvn[tc]], [bvps],
                                  lambda: nc.tensor.matmul(vps[:, i * 128:(i + 1) * 128], ckvn[:, tc * 512 + i * 128: tc * 512 + (i + 1) * 128],
                                                           wukv[:, h * 256 + 128:h * 256 + 256], start=True, stop=True), inc=(i == 3))
                        fw.op(DVE, [bvps], [bVm[tc]], lambda: nc.vector.tensor_copy(Vm[:, tc * 4:(tc + 1) * 4, :], vps[:].rearrange("p (a b) -> p a b", a=4)))
                        if tc < 4:
                            qn, bqn = tmpb.get()
                            mm_acc(qn[:], [(wuq[:, rc, h * 192:h * 192 + 128], cqn[:, rc, cols]) for rc in range(2)], [bwuq, bcqn[tc]], [bqn])
                            fw.op(ACT, [bqn], [bQTn[tc]], lambda: nc.scalar.copy(QTn[:, cols], qn[:]))
                            qr, bqr = tmpb.get()
                            mm_acc(qr[0:64, :], [(wuq[:, rc, h * 192 + 128:h * 192 + 192], cqn[:, rc, cols]) for rc in range(2)], [bwuq, bcqn[tc]], [bqr])
                            rope(qr[0:64, :], bqr, 64, tc, QTr[0:64, cols], bQTr[tc])

                    if stage == "mproj":
                        fw.finish(SP)
                        return nc
                    def s_emit_m(c, kb, g, col0, sbk, bsbk):
                        fw.op(PE, [bKTm[kb // 4], bQTn[g]], [bsbk],
                              lambda: nc.tensor.matmul(sbk[:, col0:512], KTm[:, kb * 128:(kb + 1) * 128], QTn[:, g * 512 + col0:(g + 1) * 512], start=True, stop=False), inc=False)
                        fw.op(PE, [bKR[kb // 4], bQTr[g]], [bsbk],
                              lambda: nc.tensor.matmul(sbk[:, col0:512], KR[:, kb * 128:(kb + 1) * 128], QTr[:, g * 512 + col0:(g + 1) * 512], start=False, stop=True))

                    def fin_mla(g, O_B, L_B, h=h):
                        rl, brl = s32.get()
                        fw.op(DVE, [L_B[0][1]], [brl], lambda: nc.vector.reciprocal(rl[:], L_B[0][0][:]))
                        wr = [botx[4 * g + i] for i in range(4)]
                        fw.op(DVE, [O_B[0][1], brl], wr, lambda: nc.vector.tensor_tensor(otx[:, 4 + h, g * 512:(g + 1) * 512], O_B[0][0][:], rl[:], op=ALU.mult))

                    attention(1, s_emit_m, None, Vm, bVm, 192.0 ** -0.5, fin_mla)
                    if stage == "matt":
                        fw.finish(SP)
                        return nc

        fw.fence()
        ACC = sb("ACC", [128, 16, D], F32); bACC = [Buf() for _ in range(16)]
        sm2 = sb("sm2", [128, 16, 8], F32); bsm2 = [Buf() for _ in range(16)]

        def layer_norm(z, bz, tb, lnbc, blnbc, dst, bdst, junk, bjunk):
            sc = sm2[:, tb, :]
            bs = bsm2[tb]
            fw.op(DVE, [bz], [bs], lambda: nc.vector.reduce_sum(sc[:, 0:1], z, axis=AX.X))
            fw.op(DVE, [bs], [bs], lambda: nc.vector.tensor_scalar(sc[:, 1:2], sc[:, 0:1], -1.0 / D, None, op0=ALU.mult))
            fw.op(ACT, [bz, bs], [bz], lambda: nc.scalar.activation(z, z, AF.Identity, bias=sc[:, 1:2]))
            fw.op(ACT, [bz], [bjunk], lambda: nc.scalar.activation(junk, z, AF.Square))
            fw.op(DVE, [bjunk], [bs], lambda: nc.vector.reduce_sum(sc[:, 2:3], junk, axis=AX.X))
            fw.op(ACT, [bs, bsmall], [bs], lambda: nc.scalar.activation(sc[:, 3:4], sc[:, 2:3], AF.Sqrt, bias=EPS5, scale=1.0 / D))
            fw.op(DVE, [bs], [bs], lambda: nc.vector.reciprocal(sc[:, 3:4], sc[:, 3:4]))
            fw.op(DVE, [bz, bs, blnbc], [bz], lambda: nc.vector.scalar_tensor_tensor(z, in0=z, scalar=sc[:, 3:4], in1=lnbc[:, 0, :], op0=ALU.mult, op1=ALU.mult))
            fw.op(POOL, [bz, blnbc], [bdst], lambda: nc.gpsimd.tensor_tensor(dst, z, lnbc[:, 1, :], op=ALU.add))

        with contextlib.ExitStack() as so:
            ln1bc = sb("ln1bc", [128, 2, D], F32, so); bln1 = Buf()
            fw.dma(SP, ln1bc[:], lnv_d[0:2, :].partition_broadcast(128), [], [bln1])
            wo = sb("wo", [128, 8, D], BF16, so); bwo = Buf()
            fw.dma(POOL, wo[:], w_o_d.rearrange("(hh p) o -> p hh o", p=128), [], [bwo])
            xqt = Rot([(sb(f"xqt{i}", [128, D], F32, so), Buf()) for i in range(2)])
            zt = Rot([(sb(f"zt{i}", [128, D], F32, so), Buf()) for i in range(2)])
            jk = Rot([(sb(f"jk{i}", [128, D], F32, so), Buf()) for i in range(2)])
            mixb = Rot([((psb[0], pb[0]), (psb[1], pb[1])), ((psb[2], pb[2]), (psb[3], pb[3]))])
            for tb in range(16):
                xt_, bxt_ = xqt.get()
                fw.dma(SP, xt_[:], xq_d[tb * 128:(tb + 1) * 128, :], [], [bxt_])
                banks = mixb.get()
                z, bz = zt.get()
                for half in range(2):
                    mps, bmps = banks[half]
                    for hh in range(8):
                        fw.op(PE, [botx[tb], bwo], [bmps],
                              lambda: nc.tensor.matmul(mps[:], otx[:, hh, tb * 128:(tb + 1) * 128], wo[:, hh, half * 512:(half + 1) * 512], start=(hh == 0), stop=(hh == 7)),
                              inc=(hh == 7))
                    fw.op(DVE, [bmps, bxt_], [bz],
                          lambda: nc.vector.scalar_tensor_tensor(z[:, half * 512:(half + 1) * 512], in0=xt_[:, half * 512:(half + 1) * 512], scalar=DN_ALPHA, in1=mps[:], op0=ALU.mult, op1=ALU.add))
                j_, bj_ = jk.get()
                layer_norm(z[:], bz, tb, ln1bc, bln1, ACC[:, tb, :], bACC[tb], j_[:], bj_)

        fw.fence()
        if stage == "ln1":
            for tb in range(16):
                fw.dma(SP, out_d[tb * 128:(tb + 1) * 128, :], ACC[:, tb, :], [bACC[tb]], [])
            fw.finish(SP)
            return nc

        G = sb("G", [128, 16, NE], F32); bG = [Buf() for _ in range(16)]
        MK = sb("MK", [128, 16, NE], F32); bMK = [Buf() for _ in range(16)]
        posm = sb("posm", [128, 16, NE], F32); bposm = [Buf() for _ in range(16)]
        posmT = sb("posmT", [NE, TQ], F32); bposmT = [Buf() for _ in range(4)]
        X1B = otx[:].rearrange("p a b -> p (a b)").rearrange("p (t d) -> p t d", t=16)
        bguT = sb("bguT", [128, NE * 16], F32); bbgu = Buf()
        fw.dma(SP, bguT[:], bgu_d[:, :], [], [bbgu])
        with contextlib.ExitStack() as sr:
            wr32 = sb("wr32", [128, 8, NE], F32, sr); bwr = Buf()
            fw.dma(SP, wr32[:], w_r_d.rearrange("(dc p) e -> p dc e", p=128), [], [bwr])
            brbc = sb("brbc", [128, NE], F32, sr); bbr = Buf()
            fw.dma(SP, brbc[:], b_r_d.partition_broadcast(128), [], [bbr])
            bd32 = sb("bd32", [NE, D], F32, sr); bbd = Buf()
            fw.dma(SP, bd32[:], bd_d[:, :], [], [bbd])
            GT = sb("GT", [NE, TQ], F32, sr); bGT = [Buf() for _ in range(16)]
            x1T32 = Rot([(sb(f"x1T32_{i}", [128, 8, 128], F32, sr), Buf()) for i in range(2)])
            rt = Rot([(sb(f"rt{i}", [128, 128], F32, sr), Buf()) for i in range(2)])
            tpb = Rot([((psb[0], pb[0]), (psb[1], pb[1])), ((psb[2], pb[2]), (psb[3], pb[3]))])
            tmp2 = Rot([(psb[i], pb[i]) for i in (4, 5, 6, 7)])
            for tb in range(16):
                fw.op(DVE, [bACC[tb]], [botx[tb]], lambda: nc.vector.tensor_copy(X1B[:, tb, :], ACC[:, tb, :]))
                banks = tpb.get()
                xT32, bxT32 = x1T32.get()
                for hb_ in range(2):
                    tp, btp = banks[hb_]
                    for q in range(4):
                        dc = hb_ * 4 + q
                        fw.op(PE, [bACC[tb], bcst], [btp], lambda: nc.tensor.transpose(tp[:, q * 128:(q + 1) * 128], ACC[:, tb, dc * 128:(dc + 1) * 128], ident), inc=(q == 3))
                    fw.op(ACT, [btp], [bxT32], lambda: nc.scalar.copy(xT32[:, hb_ * 4:(hb_ + 1) * 4, :], tp[:].rearrange("p (a b) -> p a b", a=4)))
                lgp, blgp = tmp2.get()
                for dc in range(8):
                    fw.op(PE, [bxT32, bwr], [blgp], lambda: nc.tensor.matmul(lgp[:, 0:NE], xT32[:, dc, :], wr32[:, dc, :], start=(dc == 0), stop=(dc == 7)), inc=(dc == 7))
                r_, br_ = rt.get()
                lg = r_[:, 0:32]; m8 = r_[:, 32:40]; ex = r_[:, 40:72]; mk = r_[:, 72:104]; misc = r_[:, 104:112]
                fw.op(DVE, [blgp, bbr], [br_], lambda: nc.vector.tensor_tensor(lg, lgp[:, 0:NE], brbc[:], op=ALU.add))
                fw.op(DVE, [br_], [br_], lambda: nc.vector.max(out=m8, in_=lg))
                fw.op(DVE, [br_], [br_], lambda: nc.vector.tensor_scalar(misc[:, 0:1], m8[:, 0:1], -1.0, None, op0=ALU.mult))
                fw.op(ACT, [br_], [br_], lambda: nc.scalar.activation(ex, lg, AF.Exp, bias=misc[:, 0:1]))
                fw.op(DVE, [br_], [bMK[tb]], lambda: nc.vector.tensor_scalar(MK[:, tb, :], lg, m8[:, 3:4], None, op0=ALU.is_ge))
                fw.op(DVE, [br_, bMK[tb]], [br_], lambda: nc.vector.tensor_tensor(ex, ex, MK[:, tb, :], op=ALU.mult))
                fw.op(DVE, [br_], [br_], lambda: nc.vector.reduce_sum(misc[:, 1:2], ex, axis=AX.X))
                fw.op(DVE, [br_], [br_], lambda: nc.vector.reciprocal(misc[:, 2:3], misc[:, 1:2]))
                fw.op(DVE, [br_], [bG[tb]], lambda: nc.vector.tensor_scalar(G[:, tb, :], ex, misc[:, 2:3], None, op0=ALU.mult))
                gtp, bgtp = tmp2.get()
                fw.op(PE, [bG[tb], bcst], [bgtp], lambda: nc.tensor.transpose(gtp[0:NE, 0:128], G[:, tb, :], ident))
                fw.op(ACT, [bgtp], [bGT[tb]], lambda: nc.scalar.copy(GT[:, tb * 128:(tb + 1) * 128], gtp[0:NE, 0:128]))
                for half in range(2):
                    bdp, bbdp = tmp2.get()
                    fw.op(PE, [bGT[tb], bbd], [bbdp], lambda: nc.tensor.matmul(bdp[:], GT[:, tb * 128:(tb + 1) * 128], bd32[:, half * 512:(half + 1) * 512], start=True, stop=True))
                    fw.op(DVE, [bbdp, bACC[tb]], [bACC[tb]],
                          lambda: nc.vector.scalar_tensor_tensor(ACC[:, tb, half * 512:(half + 1) * 512], in0=ACC[:, tb, half * 512:(half + 1) * 512], scalar=DN_ALPHA, in1=bdp[:], op0=ALU.mult, op1=ALU.add))

            triu = cst[:, 1024:1152]
            for tb in range(16):
                pp, bpp = tmp2.get()
                fw.op(PE, [bMK[tb], bcst], [bpp], lambda: nc.tensor.matmul(pp[:, 0:NE], triu, MK[:, tb, :], start=True, stop=(tb == 0)), inc=(tb == 0))
                for t2_ in range(tb):
                    fw.op(PE, [bMK[t2_], bones], [bpp], lambda: nc.tensor.matmul(pp[:, 0:NE], ones32[:], MK[:, t2_, :], start=False, stop=(t2_ == tb - 1)), inc=(t2_ == tb - 1))
                fw.op(DVE, [bpp, bMK[tb]], [bposm[tb]], lambda: nc.vector.scalar_tensor_tensor(posm[:, tb, :], in0=pp[:, 0:NE], scalar=1.0, in1=MK[:, tb, :], op0=ALU.add, op1=ALU.mult))
                fw.op(DVE, [bposm[tb]], [bposm[tb]], lambda: nc.vector.tensor_scalar(posm[:, tb, :], posm[:, tb, :], -1.0, None, op0=ALU.add))
                ptp, bptp = tmp2.get()
                fw.op(PE, [bposm[tb], bcst], [bptp], lambda: nc.tensor.transpose(ptp[0:NE, 0:128], posm[:, tb, :], ident))
                fw.op(ACT, [bptp], [bposmT[tb // 4]], lambda: nc.scalar.copy(posmT[:, tb * 128:(tb + 1) * 128], ptp[0:NE, 0:128]))
        fw.fence()
        with contextlib.ExitStack() as se:
            NSB = CAP // 128
            wgr = Rot([(sb(f"wg{i}", [128, 8, 2, 128], BF16, se), Buf()) for i in range(7)])
            Wd = sb("Wd", [128, 8, D], BF16, se); bWd = [Buf() for _ in range(4)]
            xgT = sb("xgT", [128, 8, CAP], BF16, se); bxg = [Buf() for _ in range(8)]
            ACTT = sb("ACTT", [128, 8, CAP], BF16, se); bACTT = [Buf() for _ in range(8)]
            Sel = sb("Sel", [128, 16, CAP], BF16, se); bSel = [Buf() for _ in range(16)]
            SelT = Rot([(sb(f"SelT{i}", [128, NSB, 512], BF16, se), Buf()) for i in range(2)])
            yb = sb("yb", [128, NSB, D], BF16, se); byb = [Buf() for _ in range(NSB)]
            gc_ = sb("gc", [128, CAP], F32, se); bgc_ = Buf()
            sg_ = sb("sg", [128, CAP], F32, se); bsg_ = Buf()
            uc_ = sb("uc", [128, CAP], F32, se); buc_ = Buf()
            le = sb("le", [NE, 128], F32, se); ble = Buf()
            busT = sb("busT", [128, NE * 8], F32, se); bbus = Buf()
            fw.op(DVE, [bbgu], [bbus], lambda: nc.vector.tensor_scalar(busT[:].rearrange("p (e c) -> p e c", c=8), bguT[:].rearrange("p (e c) -> p e c", c=16)[:, :, 8:16], 1.0 / 1.702, None, op0=ALU.mult))
            gab = Rot([(psb[i], pb[i]) for i in (0, 1)])
            gub = Rot([((psb[2], pb[2]), (psb[3], pb[3])), ((psb[4], pb[4]), (psb[5], pb[5]))])
            yb_b = Rot([(psb[i], pb[i]) for i in (6, 7)])
            wd_v = wd_d.rearrange("e (fc p) o -> e p fc o", p=128)
            iotaC = cst[:, 640:640 + CAP]
            kiota = cst[0:NE, 1152:1280]

            def load_wg(e, fc):
                wg_, bwg_ = wgr.get()
                fw.dma(POOL, wg_[:].rearrange("p a b c -> p (a b c)"), wgu_d[e, fc], [], [bwg_])
                return wg_, bwg_

            def load_wd(e, pc):
                fw.dma(POOL, Wd[:, 2 * pc:2 * pc + 2, :], wd_v[e, :, 2 * pc:2 * pc + 2, :], [], [bWd[pc]])

            def build_sel(e):
                for tb in range(16):
                    fw.op(DVE, [bposm[tb], bcst], [bSel[tb]], lambda: nc.vector.tensor_scalar(Sel[:, tb, :], iotaC, posm[:, tb, e:e + 1], None, op0=ALU.is_equal))

            def gather(e):
                for dc in range(8):
                    gp, bgp = gab.get()
                    for tb in range(16):
                        fw.op(PE, [botx[tb], bSel[tb]], [bgp], lambda: nc.tensor.matmul(gp[:, 0:CAP], X1B[:, tb, dc * 128:(dc + 1) * 128], Sel[:, tb, :], start=(tb == 0), stop=(tb == 15)), inc=(tb == 15))
                    fw.op(ACT, [bgp], [bxg[dc]], lambda: nc.scalar.copy(xgT[:, dc, :], gp[:, 0:CAP]))

            slices = [(e, fc) for e in range(NE) for fc in range(8)]
            PRE = 6
            WD_SCHED = {0: 0, 1: 1, 2: 2, 3: 3}
            loaded = {}
            for k in range(min(PRE, len(slices))):
                loaded[k] = load_wg(*slices[k])
            for k, (e, fc) in enumerate(slices):
                if k == 0:
                    build_sel(0)
                    gather(0)
                    build_sel(1)
                if k + PRE < len(slices):
                    loaded[k + PRE] = load_wg(*slices[k + PRE])
                wg_, bwg_ = loaded.pop(k)
                if fc in WD_SCHED:
                    load_wd(e, WD_SCHED[fc])
                (gps, bgps), (ups, bups) = gub.get()
                rd = [bwg_] + bxg
                for dc in range(8):
                    fw.op(PE, rd, [bgps], lambda: nc.tensor.matmul(gps[:, 0:CAP], wg_[:, dc, 0, :], xgT[:, dc, :], start=(dc == 0), stop=(dc == 7)), inc=(dc == 7))
                for dc in range(8):
                    fw.op(PE, rd, [bups], lambda: nc.tensor.matmul(ups[:, 0:CAP], wg_[:, dc, 1, :], xgT[:, dc, :], start=(dc == 0), stop=(dc == 7)), inc=(dc == 7))
                bg_col = bguT[:, e * 16 + fc:e * 16 + fc + 1]
                bu_col = bguT[:, e * 16 + 8 + fc:e * 16 + 8 + fc + 1]
                bus_col = busT[:, e * 8 + fc:e * 8 + fc + 1]
                fw.op(DVE, [bgps, bbgu], [bgc_], lambda: nc.vector.tensor_scalar(gc_[:], gps[:, 0:CAP], bg_col, 7.0, op0=ALU.add, op1=ALU.min))
                fw.op(ACT, [bups, bbus], [buc_], lambda: nc.scalar.activation(uc_[:], ups[:, 0:CAP], AF.Identity, bias=bus_col, scale=1.0 / 1.702))
                fw.op(ACT, [bgc_], [bsg_], lambda: nc.scalar.activation(sg_[:], gc_[:], AF.Silu, scale=1.702))
                fw.op(DVE, [buc_], [buc_], lambda: nc.vector.tensor_scalar(uc_[:], uc_[:], 7.0 / 1.702, -7.0 / 1.702, op0=ALU.min, op1=ALU.max))
                fw.op(DVE, [bsg_, buc_], [bACTT[fc]],
                      lambda: nc.vector.scalar_tensor_tensor(ACTT[:, fc, :], in0=uc_[:], scalar=1.0 / 1.702, in1=sg_[:], op0=ALU.add, op1=ALU.mult))
                if fc == 7:
                    if e + 1 < NE:
                        gather(e + 1)
                        if e + 2 < NE:
                            build_sel(e + 2)
                    for sbk in range(NSB):
                        for half in range(2):
                            yp, byp = yb_b.get()
                            rd2 = bACTT + bWd
                            for f in range(8):
                                fw.op(PE, rd2, [byp], lambda: nc.tensor.matmul(yp[:], ACTT[:, f, sbk * 128:(sbk + 1) * 128], Wd[:, f, half * 512:(half + 1) * 512], start=(f == 0), stop=(f == 7)), inc=(f == 7))
                            fw.op(ACT, [byp], [byb[sbk]], lambda: nc.scalar.copy(yb[:, sbk, half * 512:(half + 1) * 512], yp[:]))
                    fw.op(DVE, [bcst], [ble], lambda: nc.vector.tensor_scalar(le[:], kiota, float(e), None, op0=ALU.is_equal))
                    bcs = {}

                    def emit_bc(ch):
                        bc, bbc = gab.get()
                        fw.op(PE, [ble, bposmT[ch]], [bbc], lambda: nc.tensor.matmul(bc[:], le[:], posmT[:, ch * 512:(ch + 1) * 512], start=True, stop=True))
                        bcs[ch] = (bc, bbc)

                    emit_bc(0)
                    for ch in range(4):
                        bc, bbc = bcs.pop(ch)
                        if ch + 1 < 4:
                            emit_bc(ch + 1)
                        st_, bst2 = SelT.get()
                        for sbk in range(NSB):
                            fw.op(DVE, [bbc, bcst], [bst2], lambda: nc.vector.tensor_scalar(st_[:, sbk, :], bc[:], cst[:, 516 + sbk:517 + sbk], None, op0=ALU.is_equal))
                        for tb4 in range(4):
                            tb = ch * 4 + tb4
                            for half in range(2):
                                yp, byp = yb_b.get()
                                for sbk in range(NSB):
                                    fw.op(PE, [bst2] + byb, [byp], lambda: nc.tensor.matmul(yp[:], st_[:, sbk, tb4 * 128:(tb4 + 1) * 128], yb[:, sbk, half * 512:(half + 1) * 512], start=(sbk == 0), stop=(sbk == NSB - 1)), inc=(sbk == NSB - 1))
                                fw.op(DVE, [byp, bG[tb], bACC[tb]], [bACC[tb]],
                                      lambda: nc.vector.scalar_tensor_tensor(ACC[:, tb, half * 512:(half + 1) * 512], in0=yp[:], scalar=G[:, tb, e:e + 1], in1=ACC[:, tb, half * 512:(half + 1) * 512], op0=ALU.mult, op1=ALU.add))

        fw.fence()
        with contextlib.ExitStack() as sf:
            ln2bc = sb("ln2bc", [128, 2, D], F32, sf); bln2 = Buf()
            fw.dma(SP, ln2bc[:], lnv_d[2:4, :].partition_broadcast(128), [], [bln2])
            ot = Rot([(sb(f"ot{i}", [128, D], F32, sf), Buf()) for i in range(2)])
            jk2 = Rot([(sb(f"jk2{i}", [128, D], F32, sf), Buf()) for i in range(2)])
            for tb in range(16):
                o_, bo_ = ot.get()
                j_, bj_ = jk2.get()
                layer_norm(ACC[:, tb, :], bACC[tb], tb, ln2bc, bln2, o_[:], bo_, j_[:], bj_)
                fw.dma(SP, out_d[tb * 128:(tb + 1) * 128, :], o_[:], [bo_], [])
        fw.finish(SP)
    return nc


_PROG = {}


def _consts(r):
    c = np.zeros((128, 1280), np.float32)
    c[:, 0:128] = np.eye(128, dtype=np.float32)
    m = np.arange(128)
    partner = np.where(m % 64 < 32, m + 32, m - 32)
    c[partner, 128 + m] = 1.0
    k = np.arange(128)[:, None]
    q = np.arange(128)[None, :]
    c[:, 256:384] = (k <= q).astype(np.float32)
    c[:, 384:512] = 1.0 if r == 1 else 0.0
    invf = 1.0 / (10000.0 ** ((np.arange(128) % 32) * 2.0 / 64.0))
    first = (np.arange(128) % 64) < 32
    c[:, 512] = invf
    sc = TWO_PI * (1.0 - 1e-6)
    c[:, 513] = np.where(first, -sc, sc)
    c[:, 514] = np.where(first, PI_S, -PI_S)
    c[:, 515] = -PI_S
    p = np.arange(128, dtype=np.float32)
    c[:, 516] = p
    c[:, 517] = p + 128.0
    c[:, 518] = p + 256.0
    c[:, 640:1024] = np.arange(CAP, dtype=np.float32)[None, :]
    c[:, 1024:1152] = (p[:, None] < p[None, :]).astype(np.float32)
    c[:, 1152:1280] = p[:, None]
    return c


def _prep_inputs(inp):
    f32 = lambda a: np.ascontiguousarray(np.asarray(a), dtype=np.float32)
    x = f32(inp["x"])
    positions = np.ascontiguousarray(np.asarray(inp["positions"]), dtype=np.int32)
    wgu = f32(inp["w_gate_up"])[0]
    wgu_t = np.ascontiguousarray(
        wgu.reshape(NE, 8, 128, 2, 8, 128).transpose(0, 4, 2, 1, 3, 5)).reshape(NE, 8, 128, 2048)
    bgu = f32(inp["b_gate_up"])[0]
    bguT = np.ascontiguousarray(bgu.reshape(NE, 16, 128).transpose(2, 0, 1)).reshape(128, NE * 16)
    shared = {
        "w_in": f32(inp["w_in"])[0],
        "lamv": np.ascontiguousarray(np.stack([f32(inp["lambda_q1"])[0], f32(inp["lambda_k1"])[0],
                                               f32(inp["lambda_q2"])[0], f32(inp["lambda_k2"])[0]], 0)),
        "subln_g": f32(inp["subln_g"])[0].reshape(128, 1),
        "gq": np.ascontiguousarray(f32(inp["mla_q_norm_g"])[0].reshape(2, 128).T),
        "gkv": f32(inp["mla_kv_norm_g"])[0].reshape(128, 1),
        "w_uq": f32(inp["w_uq"])[0],
        "w_ukv": f32(inp["w_ukv"])[0],
        "w_o": f32(inp["w_o"])[0],
        "lnv": np.ascontiguousarray(np.stack([f32(inp["ln1_g"])[0], f32(inp["ln1_b"])[0],
                                              f32(inp["ln2_g"])[0], f32(inp["ln2_b"])[0]], 0)),
        "w_router": f32(inp["w_router"])[0],
        "b_router": f32(inp["b_router"])[0].reshape(1, NE),
        "wgu_t": wgu_t,
        "bguT": bguT,
        "w_down": f32(inp["w_down"])[0],
        "b_down": f32(inp["b_down"])[0],
    }
    in_maps = []
    toks = []
    for c in range(NCORES):
        b, r = c // 2, c % 2
        own = [2 * j + r for j in range(16)]
        oth = [2 * j + (1 - r) for j in range(16)]
        tok = np.concatenate([np.arange(g * 128, (g + 1) * 128) for g in own + oth])
        toks.append((b, tok[:TQ]))
        xb = x[b][tok]
        m = dict(shared)
        m["xT"] = np.ascontiguousarray(xb.T)
        m["xq"] = np.ascontiguousarray(xb[:TQ])
        m["pos"] = np.ascontiguousarray(positions[b][tok].reshape(1, S))
        m["cst"] = _consts(r)
        in_maps.append(m)
    return in_maps, toks


def kernel(**inputs):
    stage = inputs.pop("_stage", "full")
    if stage not in _PROG:
        _PROG[stage] = build_program(stage)
    nc = _PROG[stage]
    in_maps, toks = _prep_inputs(inputs)
    if stage != "full":
        moe = ("w_router", "b_router", "wgu_t", "bguT", "w_down", "b_down")
        in_maps = [{k: v for k, v in m.items() if k not in moe} for m in in_maps]
    res = run_bass_kernel_spmd(nc, in_maps, core_ids=list(range(NCORES)))
    out = np.zeros((4, S, D), np.float32)
    for c in range(NCORES):
        b, tok = toks[c]
        out[b, tok] = res.results[c]["out"]
    return out
```

```python
import math
import contextlib
import numpy as np
import concourse.bass as bass
import concourse.mybir as mybir
from concourse.bass_utils import run_bass_kernel_spmd

F32 = mybir.dt.float32
BF16 = mybir.dt.bfloat16
I32 = mybir.dt.int32
ALU = mybir.AluOpType
AF = mybir.ActivationFunctionType
AX = mybir.AxisListType

NCORES = 8
D = 1024
S = 4096
TQ = 2048
NE = 32
LAM_INIT = 0.8 - 0.6 * math.exp(0.0)
DN_ALPHA = 2.0 ** 0.25
TWO_PI = 2.0 * math.pi
PI_S = math.pi * (1.0 - 1e-6)
NDMA_SEM = 8
CAP = 384


_FENCE = {}


class Buf:
    __slots__ = ("w", "r", "excl")

    def __init__(self, excl=False):
        self.w = None
        self.r = dict(_FENCE)
        self.excl = excl


class Eng:
    def __init__(self, name, h, sem, dma_sems):
        self.name = name
        self.h = h
        self.sem = sem
        self.count = 0
        self.seen = {}
        self.dma_sems = dma_sems
        self.dma_val = [0] * len(dma_sems)
        self.rr = 0


class FW:
    def __init__(self, nc, es):
        self.nc = nc
        self.es = es
        mk = lambda n: es.enter_context(nc.semaphore(n))
        self.pe = Eng("pe", nc.tensor, mk("s_pe"), [])
        self.act = Eng("act", nc.scalar, mk("s_act"), [])
        self.dve = Eng("dve", nc.vector, mk("s_dve"), [])
        self.pool = Eng("pool", nc.gpsimd, mk("s_pool"), [mk(f"d_pool{i}") for i in range(NDMA_SEM)])
        self.sp = Eng("sp", nc.sync, mk("s_sp"), [mk(f"d_sp{i}") for i in range(NDMA_SEM)])
        self.nwait = 0

    def _wait(self, E, tok):
        sem, val = tok
        if sem is E.sem and (E is self.pe or val > E.count):
            return
        k = id(sem)
        if E.seen.get(k, 0) >= val:
            return
        E.h.wait_ge(sem, val)
        E.seen[k] = val
        self.nwait += 1

    def _deps(self, E, reads, writes):
        for b in reads:
            if b.w is not None:
                self._wait(E, b.w)
            if b.excl:
                for tok in b.r.values():
                    self._wait(E, tok)
        for b in writes:
            if b.w is not None:
                self._wait(E, b.w)
            for tok in b.r.values():
                self._wait(E, tok)

    def _mark(self, tok, reads, writes):
        for b in reads:
            b.r[id(tok[0])] = tok
        for b in writes:
            b.w = tok
            b.r = {}

    def op(self, E, reads, writes, build, inc=True):
        self._deps(E, reads, writes)
        ins = build()
        tok = (E.sem, E.count + 1)
        if inc:
            ins.then_inc(E.sem, 1)
            E.count += 1
        self._mark(tok, reads, writes)
        return ins

    def dma(self, Q, out, in_, reads, writes, **kw):
        i = Q.rr % len(Q.dma_sems)
        Q.rr += 1
        sem = Q.dma_sems[i]
        if Q.dma_val[i] > 0:
            self._wait(Q, (sem, Q.dma_val[i]))
        self._deps(Q, reads, writes)
        Q.h.dma_start(out=out, in_=in_, **kw).then_inc(sem, 16)
        Q.dma_val[i] += 16
        tok = (sem, Q.dma_val[i])
        self._mark(tok, reads, writes)
        return tok

    def fence(self):
        _FENCE.clear()
        for Q in (self.sp, self.pool):
            for sem, v in zip(Q.dma_sems, Q.dma_val):
                if v > 0:
                    _FENCE[id(sem)] = (sem, v)
        for X in (self.pe, self.act, self.dve, self.pool, self.sp):
            if X.count > 0:
                _FENCE[id(X.sem)] = (X.sem, X.count)

    def finish(self, E):
        for Q in (self.sp, self.pool):
            for sem, v in zip(Q.dma_sems, Q.dma_val):
                if v > 0:
                    self._wait(E, (sem, v))
        for X in (self.pe, self.act, self.dve, self.pool, self.sp):
            if X is not E and X.count > 0:
                self._wait(E, (X.sem, X.count))


class Rot:
    def __init__(self, items):
        self.items = items
        self.i = 0

    def get(self):
        it = self.items[self.i % len(self.items)]
        self.i += 1
        return it


def build_program(stage="full"):
    nc = bass.Bass("TRN2", target_bir_lowering=False)
    dt_in = lambda name, shape, dt=F32: nc.dram_tensor(name, shape, dt, kind="ExternalInput").ap()
    xT_d = dt_in("xT", [D, S])
    xq_d = dt_in("xq", [TQ, D])
    pos_d = dt_in("pos", [1, S], I32)
    cst_d = dt_in("cst", [128, 1280])
    w_in_d = dt_in("w_in", [D, 1984])
    lam_d = dt_in("lamv", [4, 64])
    subg_d = dt_in("subln_g", [128, 1])
    gq_d = dt_in("gq", [128, 2])
    gkv_d = dt_in("gkv", [128, 1])
    w_uq_d = dt_in("w_uq", [256, 768])
    w_ukv_d = dt_in("w_ukv", [128, 1024])
    w_o_d = dt_in("w_o", [D, D])
    lnv_d = dt_in("lnv", [4, D])
    if stage == "full":
        w_r_d = dt_in("w_router", [D, NE])
        b_r_d = dt_in("b_router", [1, NE])
        wgu_d = dt_in("wgu_t", [NE, 8, 128, 2048])
        bgu_d = dt_in("bguT", [128, NE * 16])
        wd_d = dt_in("w_down", [NE, D, D])
        bd_d = dt_in("b_down", [NE, D])
    out_d = nc.dram_tensor("out", [TQ, D], F32, kind="ExternalOutput").ap()

    _FENCE.clear()
    with contextlib.ExitStack() as es:
        fw = FW(nc, es)
        PE, ACT, DVE, POOL, SP = fw.pe, fw.act, fw.dve, fw.pool, fw.sp

        def sb(name, shape, dt, st=es):
            return st.enter_context(nc.sbuf_tensor("sb_" + name, shape, dt))

        psb = [es.enter_context(nc.psum_tensor(f"ps{i}", [128, 512], F32)) for i in range(8)]
        pb = [Buf(excl=True) for _ in range(8)]

        cst = sb("cst", [128, 1280], F32); bcst = Buf()
        fw.dma(SP, cst[:], cst_d[:, :], [], [bcst])
        ident = cst[:, 0:128]
        ropec = cst[:, 512:516]
        cbf = sb("cbf", [128, 512], BF16); bcbf = Buf()
        fw.op(DVE, [bcst], [bcbf], lambda: nc.vector.tensor_copy(cbf[:, 0:384], cst[:, 128:512]))
        fw.op(DVE, [], [bcbf], lambda: nc.vector.memset(cbf[:, 384:512], 1.0))
        perm_bf = cbf[:, 0:128]
        masks_bf = [cbf[:, 128:256], cbf[:, 256:384]]
        ones_bf = cbf[:, 384:512]
        ones32 = sb("ones32", [128, 128], F32); bones = Buf()
        fw.op(POOL, [], [bones], lambda: nc.gpsimd.memset(ones32[:], 1.0))
        small = sb("small", [128, 64], F32); bsmall = Buf()
        fw.dma(SP, small[:, 0:1], subg_d[:, :], [], [bsmall])
        fw.dma(SP, small[:, 3:5], gq_d[:, :], [], [bsmall])
        fw.dma(SP, small[:, 5:6], gkv_d[:, :], [], [bsmall])
        fw.op(DVE, [], [bsmall], lambda: nc.vector.memset(small[:, 8:9], 1e-6))
        fw.op(DVE, [], [bsmall], lambda: nc.vector.memset(small[:, 9:10], 1e-5))
        EPS6 = small[:, 8:9]
        EPS5 = small[:, 9:10]
        lamt = sb("lamt", [128, 256], F32); blam = Buf()
        fw.dma(SP, lamt[:].rearrange("p (a b) -> p a b", a=4), lam_d.partition_broadcast(128), [], [blam])
        fw.op(DVE, [blam], [blam], lambda: nc.vector.tensor_tensor(lamt[:, 0:64], lamt[:, 0:64], lamt[:, 64:128], op=ALU.mult))
        fw.op(DVE, [blam], [blam], lambda: nc.vector.tensor_tensor(lamt[:, 128:192], lamt[:, 128:192], lamt[:, 192:256], op=ALU.mult))
        fw.op(DVE, [blam], [bsmall], lambda: nc.vector.reduce_sum(small[:, 6:7], lamt[:, 0:64], axis=AX.X))
        fw.op(DVE, [blam], [bsmall], lambda: nc.vector.reduce_sum(small[:, 7:8], lamt[:, 128:192], axis=AX.X))
        fw.op(ACT, [bsmall], [bsmall], lambda: nc.scalar.activation(small[:, 6:8], small[:, 6:8], AF.Exp))
        fw.op(DVE, [bsmall], [bsmall], lambda: nc.vector.tensor_tensor(small[:, 2:3], small[:, 7:8], small[:, 6:7], op=ALU.subtract))
        fw.op(DVE, [bsmall], [bsmall], lambda: nc.vector.tensor_scalar(small[:, 2:3], small[:, 2:3], -LAM_INIT, None, op0=ALU.add))
        fw.op(DVE, [bsmall], [bsmall], lambda: nc.vector.tensor_scalar(small[:, 1:2], small[:, 0:1], 1.0 - LAM_INIT, None, op0=ALU.mult))

        otx = sb("otx", [128, 8, TQ], BF16)
        botx = [Buf() for _ in range(16)]

        with contextlib.ExitStack() as sa:
            cosT = sb("cosT", [128, S], F32, sa)
            sinS = sb("sinS", [128, S], F32, sa)
            btab = [Buf() for _ in range(8)]
            ckvn = sb("ckvn", [128, S], BF16, sa); bckvn = [Buf() for _ in range(8)]
            cqn = sb("cqn", [128, 2, TQ], BF16, sa); bcqn = [Buf() for _ in range(4)]
            KR = sb("KR", [128, S], BF16, sa); bKR = [Buf() for _ in range(8)]
            fw.op(POOL, [], bKR, lambda: nc.gpsimd.memset(KR[64:128, :], 0.0))
            s32 = Rot([(sb(f"s32_{i}", [128, 512], F32, sa), Buf()) for i in range(8)])
            s16 = Rot([(sb(f"s16_{i}", [128, 512], BF16, sa), Buf()) for i in range(4)])
            si32 = sb("si32", [128, 512], I32, sa); bsi32 = Buf()
            tmpb = Rot([(psb[i], pb[i]) for i in (6, 7, 0, 1)])

            def mm_acc(out_ap, pairs, reads, writes):
                n = len(pairs)
                for i, (l, r) in enumerate(pairs):
                    fw.op(PE, reads, writes,
                          lambda: nc.tensor.matmul(out_ap, l, r, start=(i == 0), stop=(i == n - 1)),
                          inc=(i == n - 1))

            def rope(src_ps, bsrc, rows, tc, dst_ap, bdst):
                cols = slice(tc * 512, (tc + 1) * 512)
                import os as _os
                _cut = int(_os.environ.get("ROPE_CUT", "9")) if rows == 128 else 9
                if _cut < 1:
                    return
                hb, bhb = s16.get()
                fw.op(ACT, [bsrc], [bhb], lambda: nc.scalar.copy(hb[0:rows, :], src_ps))
                if _cut < 2:
                    return
                sw, bsw = tmpb.get()
                fw.op(PE, [bhb, bcbf], [bsw], lambda: nc.tensor.matmul(sw[0:rows, :], perm_bf[0:rows, 0:rows], hb[0:rows, :], start=True, stop=True))
                if _cut < 3:
                    return
                t1, bt1 = s32.get()
                fw.op(DVE, [bsrc, btab[tc]], [bt1], lambda: nc.vector.tensor_tensor(t1[0:rows, :], src_ps, cosT[0:rows, cols], op=ALU.mult))
                if _cut < 4:
                    return
                t2, bt2 = s32.get()
                fw.op(DVE, [bsw, btab[tc]], [bt2], lambda: nc.vector.tensor_tensor(t2[0:rows, :], sw[0:rows, :], sinS[0:rows, cols], op=ALU.mult))
                if _cut < 5:
                    return
                fw.op(DVE, [bt1, bt2], [bdst], lambda: nc.vector.tensor_tensor(dst_ap, t1[0:rows, :], t2[0:rows, :], op=ALU.add))

            def rms_scale(ps_list, bps_list, n_feat, eps, gcols, dst_aps, bdst):
                sqs = []
                for ps_ap, bps in zip(ps_list, bps_list):
                    sq, bsq = s32.get()
                    fw.op(ACT, [bps], [bsq], lambda: nc.scalar.activation(sq[:], ps_ap, AF.Square))
                    sqs.append((sq, bsq))
                ss, bss = tmpb.get()
                for i, (sq, bsq) in enumerate(sqs):
                    fw.op(PE, [bsq, bones], [bss],
                          lambda: nc.tensor.matmul(ss[:], ones32[:], sq[:], start=(i == 0), stop=(i == len(sqs) - 1)),
                          inc=(i == len(sqs) - 1))
                rstd, brstd = s32.get()
                fw.op(ACT, [bss, bsmall], [brstd], lambda: nc.scalar.activation(rstd[:], ss[:], AF.Sqrt, bias=eps, scale=1.0 / n_feat))
                fw.op(DVE, [brstd], [brstd], lambda: nc.vector.reciprocal(rstd[:], rstd[:]))
                for ps_ap, bps, gc, dst in zip(ps_list, bps_list, gcols, dst_aps):
                    fw.op(DVE, [bps, brstd, bsmall], [bdst],
                          lambda: nc.vector.scalar_tensor_tensor(dst, in0=ps_ap, scalar=gc, in1=rstd[:], op0=ALU.mult, op1=ALU.mult))

            def attention(nsub, s_emit, s_reads, Vt, bV, scale, finalize):
                S_B = [(psb[0], pb[0]), (psb[1], pb[1])]
                O_B = [(psb[2], pb[2]), (psb[3], pb[3])]
                L_B = [(psb[4], pb[4]), (psb[5], pb[5])]
                pending = [None]
                for g in range(4):
                    units = []
                    nkb = 4 * g + 4
                    for half in (0, 1):
                        for kl in range(nkb):
                            i = kl - 4 * g
                            col0 = 0 if i < 0 else i * 128
                            mt = None if i < 0 else half
                            for c in range(nsub):
                                units.append((c, half * 16 + kl, col0, mt))
                    nun = len(units)
                    first = [True] * nsub
                    last_idx = {}
                    for ui, u in enumerate(units):
                        last_idx[u[0]] = ui
                    pts = {}

                    def emit_s(ui):
                        c, kb, col0, mt = units[ui]
                        sbk, bsbk = S_B[ui % 2]
                        s_emit(c, kb, g, col0, sbk, bsbk)
                        pT, bpT = s16.get()
                        fw.op(ACT, [bsbk], [bpT], lambda: nc.scalar.activation(pT[:, col0:512], sbk[:, col0:512], AF.Exp, scale=scale))
                        if mt is not None:
                            fw.op(DVE, [bpT, bcbf], [bpT], lambda: nc.vector.tensor_tensor(pT[:, col0:col0 + 128], pT[:, col0:col0 + 128], masks_bf[mt], op=ALU.mult))
                        pts[ui] = (pT, bpT)

                    def emit_pv(ui):
                        c, kb, col0, mt = units[ui]
                        pT, bpT = pts.pop(ui)
                        o, bo = O_B[c]
                        l, bl = L_B[c]
                        st = first[c]
                        first[c] = False
                        sp_ = (last_idx[c] == ui)
                        fw.op(PE, [bpT, bV[kb // 4]], [bo], lambda: nc.tensor.matmul(o[:, col0:512], Vt[:, kb, :], pT[:, col0:512], start=st, stop=sp_), inc=False)
                        fw.op(PE, [bpT, bcbf], [bl], lambda: nc.tensor.matmul(l[:, col0:512], ones_bf, pT[:, col0:512], start=st, stop=sp_))

                    LOOK = 2
                    for ui in range(min(LOOK, nun)):
                        emit_s(ui)
                    if pending[0] is not None:
                        pending[0]()
                    for ui in range(nun):
                        emit_pv(ui)
                        if ui + LOOK < nun:
                            emit_s(ui + LOOK)
                    pending[0] = (lambda g=g: finalize(g, O_B, L_B))
                pending[0]()

            with contextlib.ExitStack() as sx:
                xTb = sb("xTb", [128, 8, S], BF16, sx); bxT = [Buf() for _ in range(8)]
                xT_v = xT_d.rearrange("(dc p) t -> p dc t", p=128)
                for tc in range(8):
                    fw.dma(POOL, xTb[:, :, tc * 512:(tc + 1) * 512], xT_v[:, :, tc * 512:(tc + 1) * 512], [], [bxT[tc]])
                w_in_v = w_in_d.rearrange("(dc p) c -> p dc c", p=128)

                for tc in range(8):
                    cols = slice(tc * 512, (tc + 1) * 512)
                    fw.dma(SP, si32[:], pos_d[0:1, cols].partition_broadcast(128), [], [bsi32])
                    ang, bang = s32.get()
                    fw.op(DVE, [bsi32], [bang], lambda: nc.vector.tensor_copy(ang[:], si32[:]))
                    fw.op(DVE, [bang, bcst], [bang], lambda: nc.vector.tensor_scalar(ang[:], ang[:], ropec[:, 0:1], None, op0=ALU.mult))
                    for which in (0, 1):
                        shift = 0.5 if which == 0 else 0.75
                        u, bu = s32.get()
                        fw.op(DVE, [bang], [bu], lambda: nc.vector.tensor_scalar(u[:], ang[:], 1.0 / TWO_PI, shift, op0=ALU.mult, op1=ALU.add))
                        ki, bki = s32.get()
                        kiv = ki[:].bitcast(I32)
                        fw.op(DVE, [bu], [bki], lambda: nc.vector.tensor_copy(kiv, u[:]))
                        kf, bkf = s32.get()
                        fw.op(DVE, [bki], [bkf], lambda: nc.vector.tensor_copy(kf[:], kiv))
                        fw.op(DVE, [bkf, bu], [bu], lambda: nc.vector.tensor_tensor(u[:], u[:], kf[:], op=ALU.subtract))
                        fw.op(DVE, [bu], [bkf], lambda: nc.vector.scalar_tensor_tensor(kf[:], in0=u[:], scalar=0.0, in1=u[:], op0=ALU.is_lt, op1=ALU.add))
                        if which == 0:
                            fw.op(ACT, [bkf, bcst], [btab[tc]], lambda: nc.scalar.activation(sinS[:, cols], kf[:], AF.Sin, bias=ropec[:, 2:3], scale=ropec[:, 1:2]))
                        else:
                            fw.op(ACT, [bkf, bcst], [btab[tc]], lambda: nc.scalar.activation(cosT[:, cols], kf[:], AF.Sin, bias=ropec[:, 3:4], scale=TWO_PI * (1.0 - 1e-6)))

                if stage.startswith("tabtt"):
                    import os as _os
                    r0, r1 = [int(v) for v in _os.environ.get("TT_ROWS", "0,128").split(",")]
                    mode = _os.environ.get("TT_MODE", "psum_cos")
                    t1, bt1 = s32.get()
                    kps, bkps = tmpb.get()
                    fw.op(PE, [bcbf], [bkps], lambda: nc.tensor.matmul(kps[:], perm_bf, cbf[:, 0:512], start=True, stop=True))
                    if mode == "psum_cos":
                        fw.op(DVE, [bkps, btab[0]], [bt1], lambda: nc.vector.tensor_tensor(t1[r0:r1, :], kps[r0:r1, :], cosT[r0:r1, 0:512], op=ALU.mult))
                    elif mode == "sb_cos":
                        t2, bt2 = s32.get()
                        fw.op(DVE, [], [bt2], lambda: nc.vector.memset(t2[:], 1.0))
                        fw.op(DVE, [bt2, btab[0]], [bt1], lambda: nc.vector.tensor_tensor(t1[r0:r1, :], t2[r0:r1, :], cosT[r0:r1, 0:512], op=ALU.mult))
                    elif mode == "psum_sb":
                        t2, bt2 = s32.get()
                        fw.op(DVE, [], [bt2], lambda: nc.vector.memset(t2[:], 1.0))
                        fw.op(DVE, [bkps, bt2], [bt1], lambda: nc.vector.tensor_tensor(t1[r0:r1, :], kps[r0:r1, :], t2[r0:r1, :], op=ALU.mult))
                    fw.dma(SP, out_d[0:128, 0:512], t1[:], [bt1], [])
                    fw.dma(SP, out_d[128:256, 0:512], cosT[:, 0:512], [btab[0]], [])
                    fw.dma(SP, out_d[256:384, 0:512], sinS[:, 0:512], [btab[0]], [])
                    fw.finish(SP)
                    return nc
                if stage == "tab":
                    fw.finish(SP)
                    return nc
                with contextlib.ExitStack() as sm0:
                    WC = sb("WC", [128, 8, 448], BF16, sm0); bWC = Buf()
                    fw.dma(POOL, WC[:], w_in_v[:, :, 1536:1984], [], [bWC])
                    for tc in range(8):
                        cols = slice(tc * 512, (tc + 1) * 512)
                        ckv, bckv = tmpb.get()
                        mm_acc(ckv[:], [(WC[:, dc, 256:384], xTb[:, dc, cols]) for dc in range(8)], [bWC, bxT[tc]], [bckv])
                        rms_scale([ckv[:]], [bckv], 128.0, EPS6, [small[:, 5:6]], [ckvn[:, cols]], bckvn[tc])
                        kr, bkr = tmpb.get()
                        mm_acc(kr[0:64, :], [(WC[:, dc, 384:448], xTb[:, dc, cols]) for dc in range(8)], [bWC, bxT[tc]], [bkr])
                        rope(kr[0:64, :], bkr, 64, tc, KR[0:64, cols], bKR[tc])
                        if tc < 4:
                            cq0, bcq0 = tmpb.get()
                            mm_acc(cq0[:], [(WC[:, dc, 0:128], xTb[:, dc, cols]) for dc in range(8)], [bWC, bxT[tc]], [bcq0])
                            cq1, bcq1 = tmpb.get()
                            mm_acc(cq1[:], [(WC[:, dc, 128:256], xTb[:, dc, cols]) for dc in range(8)], [bWC, bxT[tc]], [bcq1])
                            rms_scale([cq0[:], cq1[:]], [bcq0, bcq1], 256.0, EPS6, [small[:, 3:4], small[:, 4:5]],
                                      [cqn[:, 0, cols], cqn[:, 1, cols]], bcqn[tc])

                if stage == "m0":
                    fw.finish(SP)
                    return nc
                fw.fence()
                with contextlib.ExitStack() as sd:
                    WQ = sb("WQ", [128, 8, 128], BF16, sd); WK = sb("WK", [128, 8, 128], BF16, sd); WV = sb("WV", [128, 8, 128], BF16, sd)
                    bW = Buf()
                    KT = sb("KT", [128, S], BF16, sd); bKT = [Buf() for _ in range(8)]
                    QT = sb("QT", [128, TQ], BF16, sd); bQT = [Buf() for _ in range(4)]
                    Vt = sb("Vt", [128, 32, 128], BF16, sd); bV = [Buf() for _ in range(8)]
                    for h in range(4):
                        fw.dma(POOL, WQ[:], w_in_v[:, :, 128 * h:128 * h + 128], [], [bW])
                        fw.dma(POOL, WK[:], w_in_v[:, :, 512 + 128 * h:512 + 128 * h + 128], [], [bW])
                        fw.dma(POOL, WV[:], w_in_v[:, :, 1024 + 128 * h:1024 + 128 * h + 128], [], [bW])
                        if stage == "dprojD":
                            fw.finish(SP)
                            return nc
                        import os as _os
                        _ntc = int(_os.environ.get("DPROJ_NTC", "8"))
                        _noq = _os.environ.get("DPROJ_NOQ", "0") == "1"
                        for tc in range(_ntc):
                            cols = slice(tc * 512, (tc + 1) * 512)
                            if stage != "dprojV":
                                kps, bkps = tmpb.get()
                                mm_acc(kps[:], [(WK[:, dc, :], xTb[:, dc, cols]) for dc in range(8)], [bW, bxT[tc]], [bkps])
                                rope(kps[:], bkps, 128, tc, KT[:, cols], bKT[tc])
                            if tc < 4 and stage != "dprojV" and not _noq:
                                qps, bqps = tmpb.get()
                                mm_acc(qps[:], [(WQ[:, dc, :], xTb[:, dc, cols]) for dc in range(8)], [bW, bxT[tc]], [bqps])
                                rope(qps[:], bqps, 128, tc, QT[:, cols], bQT[tc])
                            if stage == "dprojK":
                                continue
                            vps, bvps = tmpb.get()
                            for i in range(4):
                                mm_acc(vps[:, i * 128:(i + 1) * 128],
                                       [(xTb[:, dc, tc * 512 + i * 128: tc * 512 + (i + 1) * 128], WV[:, dc, :]) for dc in range(8)],
                                       [bW, bxT[tc]], [bvps])
                            fw.op(ACT, [bvps], [bV[tc]], lambda: nc.scalar.copy(Vt[:, tc * 4:(tc + 1) * 4, :], vps[:].rearrange("p (a b) -> p a b", a=4)))

                        if stage in ("dproj", "dprojK", "dprojV"):
                            fw.finish(SP)
                            return nc
                        def s_emit(c, kb, g, col0, sbk, bsbk):
                            fw.op(PE, [bKT[kb // 4], bQT[g]], [bsbk],
                                  lambda: nc.tensor.matmul(sbk[:, col0:512], KT[64 * c:64 * c + 64, kb * 128:(kb + 1) * 128],
                                                           QT[64 * c:64 * c + 64, g * 512 + col0:(g + 1) * 512], start=True, stop=True))

                        def fin_diff(g, O_B, L_B, h=h):
                            ds = []
                            for c in range(2):
                                rl, brl = s32.get()
                                fw.op(DVE, [L_B[c][1]], [brl], lambda: nc.vector.reciprocal(rl[:], L_B[c][0][:]))
                                fw.op(DVE, [O_B[c][1], brl], [brl], lambda: nc.vector.tensor_tensor(rl[:], O_B[c][0][:], rl[:], op=ALU.mult))
                                ds.append((rl, brl))
                            dd, bdd = s32.get()
                            fw.op(DVE, [ds[0][1], ds[1][1], bsmall], [bdd],
                                  lambda: nc.vector.scalar_tensor_tensor(dd[:], in0=ds[1][0][:], scalar=small[:, 2:3], in1=ds[0][0][:], op0=ALU.mult, op1=ALU.add))
                            sq, bsq = s32.get()
                            fw.op(ACT, [bdd], [bsq], lambda: nc.scalar.activation(sq[:], dd[:], AF.Square))
                            ss, bss = tmpb.get()
                            fw.op(PE, [bsq, bones], [bss], lambda: nc.tensor.matmul(ss[:], ones32[:], sq[:], start=True, stop=True))
                            rstd, brstd = s32.get()
                            fw.op(ACT, [bss, bsmall], [brstd], lambda: nc.scalar.activation(rstd[:], ss[:], AF.Sqrt, bias=EPS5, scale=1.0 / 128.0))
                            fw.op(DVE, [brstd], [brstd], lambda: nc.vector.reciprocal(rstd[:], rstd[:]))
                            wr = [botx[4 * g + i] for i in range(4)]
                            fw.op(DVE, [bdd, brstd, bsmall], wr,
                                  lambda: nc.vector.scalar_tensor_tensor(otx[:, h, g * 512:(g + 1) * 512], in0=dd[:], scalar=small[:, 1:2], in1=rstd[:], op0=ALU.mult, op1=ALU.mult))

                        attention(2, s_emit, None, Vt, bV, 64.0 ** -0.5, fin_diff)
                        if stage == "datt":
                            fw.finish(SP)
                            return nc
            fw.fence()
            with contextlib.ExitStack() as sm:
                wuq = sb("wuq", [128, 2, 768], BF16, sm); bwuq = Buf()
                wukv = sb("wukv", [128, 1024], BF16, sm); bwukv = Buf()
                fw.dma(POOL, wuq[:], w_uq_d.rearrange("(rc p) c -> p rc c", p=128), [], [bwuq])
                fw.dma(POOL, wukv[:], w_ukv_d[:, :], [], [bwukv])
                KTm = sb("KTm", [128, S], BF16, sm); bKTm = [Buf() for _ in range(8)]
                Vm = sb("Vm", [128, 32, 128], BF16, sm); bVm = [Buf() for _ in range(8)]
                QTn = sb("QTn", [128, TQ], BF16, sm); bQTn = [Buf() for _ in range(4)]
                QTr = sb("QTr", [128, TQ], BF16, sm); bQTr = [Buf() for _ in range(4)]
                fw.op(POOL, [], bQTr, lambda: nc.gpsimd.memset(QTr[64:128, :], 0.0))
                for h in range(4):
                    for tc in range(8):
                        cols = slice(tc * 512, (tc + 1) * 512)
                        kn, bkn = tmpb.get()
                        fw.op(PE, [bwukv, bckvn[tc]], [bkn], lambda: nc.tensor.matmul(kn[:], wukv[:, h * 256:h * 256 + 128], ckvn[:, cols], start=True, stop=True))
                        fw.op(ACT, [bkn], [bKTm[tc]], lambda: nc.scalar.copy(KTm[:, cols], kn[:]))
                        vps, bvps = tmpb.get()
                        for i in range(4):
                            fw.op(PE, [bwukv, bckvn[tc]], [bvps],
                                  lambda: nc.tensor.matmul(vps[:, i * 128:(i + 1) * 128], ckvn[:, tc * 512 + i * 128: tc * 512 + (i + 1) * 128],
                                                           wukv[:, h * 256 + 128:h * 256 + 256], start=True, stop=True), inc=(i == 3))
                        fw.op(DVE, [bvps], [bVm[tc]], lambda: nc.vector.tensor_copy(Vm[:, tc * 4:(tc + 1) * 4, :], vps[:].rearrange("p (a b) -> p a b", a=4)))
                        if tc < 4:
                            qn, bqn = tmpb.get()
                            mm_acc(qn[:], [(wuq[:, rc, h * 192:h * 192 + 128], cqn[:, rc, cols]) for rc in range(2)], [bwuq, bcqn[tc]], [bqn])
                            fw.op(ACT, [bqn], [bQTn[tc]], lambda: nc.scalar.copy(QTn[:, cols], qn[:]))
                            qr, bqr = tmpb.get()
                            mm_acc(qr[0:64, :], [(wuq[:, rc, h * 192 + 128:h * 192 + 192], cqn[:, rc, cols]) for rc in range(2)], [bwuq, bcqn[tc]], [bqr])
                            rope(qr[0:64, :], bqr, 64, tc, QTr[0:64, cols], bQTr[tc])

                    if stage == "mproj":
                        fw.finish(SP)
                        return nc
                    def s_emit_m(c, kb, g, col0, sbk, bsbk):
                        fw.op(PE, [bKTm[kb // 4], bQTn[g]], [bsbk],
                              lambda: nc.tensor.matmul(sbk[:, col0:512], KTm[:, kb * 128:(kb + 1) * 128], QTn[:, g * 512 + col0:(g + 1) * 512], start=True, stop=False), inc=False)
                        fw.op(PE, [bKR[kb // 4], bQTr[g]], [bsbk],
                              lambda: nc.tensor.matmul(sbk[:, col0:512], KR[:, kb * 128:(kb + 1) * 128], QTr[:, g * 512 + col0:(g + 1) * 512], start=False, stop=True))

                    def fin_mla(g, O_B, L_B, h=h):
                        rl, brl = s32.get()
                        fw.op(DVE, [L_B[0][1]], [brl], lambda: nc.vector.reciprocal(rl[:], L_B[0][0][:]))
                        wr = [botx[4 * g + i] for i in range(4)]
                        fw.op(DVE, [O_B[0][1], brl], wr, lambda: nc.vector.tensor_tensor(otx[:, 4 + h, g * 512:(g + 1) * 512], O_B[0][0][:], rl[:], op=ALU.mult))

                    attention(1, s_emit_m, None, Vm, bVm, 192.0 ** -0.5, fin_mla)
                    if stage == "matt":
                        fw.finish(SP)
                        return nc

        fw.fence()
        ACC = sb("ACC", [128, 16, D], F32); bACC = [Buf() for _ in range(16)]
        sm2 = sb("sm2", [128, 16, 8], F32); bsm2 = [Buf() for _ in range(16)]

        def layer_norm(z, bz, tb, lnbc, blnbc, dst, bdst, junk, bjunk):
            sc = sm2[:, tb, :]
            bs = bsm2[tb]
            fw.op(DVE, [bz], [bs], lambda: nc.vector.reduce_sum(sc[:, 0:1], z, axis=AX.X))
            fw.op(DVE, [bs], [bs], lambda: nc.vector.tensor_scalar(sc[:, 1:2], sc[:, 0:1], -1.0 / D, None, op0=ALU.mult))
            fw.op(ACT, [bz, bs], [bz], lambda: nc.scalar.activation(z, z, AF.Identity, bias=sc[:, 1:2]))
            fw.op(ACT, [bz], [bjunk], lambda: nc.scalar.activation(junk, z, AF.Square))
            fw.op(DVE, [bjunk], [bs], lambda: nc.vector.reduce_sum(sc[:, 2:3], junk, axis=AX.X))
            fw.op(ACT, [bs, bsmall], [bs], lambda: nc.scalar.activation(sc[:, 3:4], sc[:, 2:3], AF.Sqrt, bias=EPS5, scale=1.0 / D))
            fw.op(DVE, [bs], [bs], lambda: nc.vector.reciprocal(sc[:, 3:4], sc[:, 3:4]))
            fw.op(DVE, [bz, bs, blnbc], [bz], lambda: nc.vector.scalar_tensor_tensor(z, in0=z, scalar=sc[:, 3:4], in1=lnbc[:, 0, :], op0=ALU.mult, op1=ALU.mult))
            fw.op(POOL, [bz, blnbc], [bdst], lambda: nc.gpsimd.tensor_tensor(dst, z, lnbc[:, 1, :], op=ALU.add))

        with contextlib.ExitStack() as so:
            ln1bc = sb("ln1bc", [128, 2, D], F32, so); bln1 = Buf()
            fw.dma(SP, ln1bc[:], lnv_d[0:2, :].partition_broadcast(128), [], [bln1])
            wo = sb("wo", [128, 8, D], BF16, so); bwo = Buf()
            fw.dma(POOL, wo[:], w_o_d.rearrange("(hh p) o -> p hh o", p=128), [], [bwo])
            xqt = Rot([(sb(f"xqt{i}", [128, D], F32, so), Buf()) for i in range(2)])
            zt = Rot([(sb(f"zt{i}", [128, D], F32, so), Buf()) for i in range(2)])
            jk = Rot([(sb(f"jk{i}", [128, D], F32, so), Buf()) for i in range(2)])
            mixb = Rot([((psb[0], pb[0]), (psb[1], pb[1])), ((psb[2], pb[2]), (psb[3], pb[3]))])
            for tb in range(16):
                xt_, bxt_ = xqt.get()
                fw.dma(SP, xt_[:], xq_d[tb * 128:(tb + 1) * 128, :], [], [bxt_])
                banks = mixb.get()
                z, bz = zt.get()
                for half in range(2):
                    mps, bmps = banks[half]
                    for hh in range(8):
                        fw.op(PE, [botx[tb], bwo], [bmps],
                              lambda: nc.tensor.matmul(mps[:], otx[:, hh, tb * 128:(tb + 1) * 128], wo[:, hh, half * 512:(half + 1) * 512], start=(hh == 0), stop=(hh == 7)),
                              inc=(hh == 7))
                    fw.op(DVE, [bmps, bxt_], [bz],
                          lambda: nc.vector.scalar_tensor_tensor(z[:, half * 512:(half + 1) * 512], in0=xt_[:, half * 512:(half + 1) * 512], scalar=DN_ALPHA, in1=mps[:], op0=ALU.mult, op1=ALU.add))
                j_, bj_ = jk.get()
                layer_norm(z[:], bz, tb, ln1bc, bln1, ACC[:, tb, :], bACC[tb], j_[:], bj_)

        fw.fence()
        if stage == "ln1":
            for tb in range(16):
                fw.dma(SP, out_d[tb * 128:(tb + 1) * 128, :], ACC[:, tb, :], [bACC[tb]], [])
            fw.finish(SP)
            return nc

        G = sb("G", [128, 16, NE], F32); bG = [Buf() for _ in range(16)]
        MK = sb("MK", [128, 16, NE], F32); bMK = [Buf() for _ in range(16)]
        posm = sb("posm", [128, 16, NE], F32); bposm = [Buf() for _ in range(16)]
        posmT = sb("posmT", [NE, TQ], F32); bposmT = [Buf() for _ in range(4)]
        X1B = otx[:].rearrange("p a b -> p (a b)").rearrange("p (t d) -> p t d", t=16)
        bguT = sb("bguT", [128, NE * 16], F32); bbgu = Buf()
        fw.dma(SP, bguT[:], bgu_d[:, :], [], [bbgu])
        with contextlib.ExitStack() as sr:
            wr32 = sb("wr32", [128, 8, NE], F32, sr); bwr = Buf()
            fw.dma(SP, wr32[:], w_r_d.rearrange("(dc p) e -> p dc e", p=128), [], [bwr])
            brbc = sb("brbc", [128, NE], F32, sr); bbr = Buf()
            fw.dma(SP, brbc[:], b_r_d.partition_broadcast(128), [], [bbr])
            bd32 = sb("bd32", [NE, D], F32, sr); bbd = Buf()
            fw.dma(SP, bd32[:], bd_d[:, :], [], [bbd])
            GT = sb("GT", [NE, TQ], F32, sr); bGT = [Buf() for _ in range(16)]
            x1T32 = Rot([(sb(f"x1T32_{i}", [128, 8, 128], F32, sr), Buf()) for i in range(2)])
            rt = Rot([(sb(f"rt{i}", [128, 128], F32, sr), Buf()) for i in range(2)])
            tpb = Rot([((psb[0], pb[0]), (psb[1], pb[1])), ((psb[2], pb[2]), (psb[3], pb[3]))])
            tmp2 = Rot([(psb[i], pb[i]) for i in (4, 5, 6, 7)])
            for tb in range(16):
                fw.op(DVE, [bACC[tb]], [botx[tb]], lambda: nc.vector.tensor_copy(X1B[:, tb, :], ACC[:, tb, :]))
                banks = tpb.get()
                xT32, bxT32 = x1T32.get()
                for hb_ in range(2):
                    tp, btp = banks[hb_]
                    for q in range(4):
                        dc = hb_ * 4 + q
                        fw.op(PE, [bACC[tb], bcst], [btp], lambda: nc.tensor.transpose(tp[:, q * 128:(q + 1) * 128], ACC[:, tb, dc * 128:(dc + 1) * 128], ident), inc=(q == 3))
                    fw.op(ACT, [btp], [bxT32], lambda: nc.scalar.copy(xT32[:, hb_ * 4:(hb_ + 1) * 4, :], tp[:].rearrange("p (a b) -> p a b", a=4)))
                lgp, blgp = tmp2.get()
                for dc in range(8):
                    fw.op(PE, [bxT32, bwr], [blgp], lambda: nc.tensor.matmul(lgp[:, 0:NE], xT32[:, dc, :], wr32[:, dc, :], start=(dc == 0), stop=(dc == 7)), inc=(dc == 7))
                r_, br_ = rt.get()
                lg = r_[:, 0:32]; m8 = r_[:, 32:40]; ex = r_[:, 40:72]; mk = r_[:, 72:104]; misc = r_[:, 104:112]
                fw.op(DVE, [blgp, bbr], [br_], lambda: nc.vector.tensor_tensor(lg, lgp[:, 0:NE], brbc[:], op=ALU.add))
                fw.op(DVE, [br_], [br_], lambda: nc.vector.max(out=m8, in_=lg))
                fw.op(DVE, [br_], [br_], lambda: nc.vector.tensor_scalar(misc[:, 0:1], m8[:, 0:1], -1.0, None, op0=ALU.mult))
                fw.op(ACT, [br_], [br_], lambda: nc.scalar.activation(ex, lg, AF.Exp, bias=misc[:, 0:1]))
                fw.op(DVE, [br_], [bMK[tb]], lambda: nc.vector.tensor_scalar(MK[:, tb, :], lg, m8[:, 3:4], None, op0=ALU.is_ge))
                fw.op(DVE, [br_, bMK[tb]], [br_], lambda: nc.vector.tensor_tensor(ex, ex, MK[:, tb, :], op=ALU.mult))
                fw.op(DVE, [br_], [br_], lambda: nc.vector.reduce_sum(misc[:, 1:2], ex, axis=AX.X))
                fw.op(DVE, [br_], [br_], lambda: nc.vector.reciprocal(misc[:, 2:3], misc[:, 1:2]))
                fw.op(DVE, [br_], [bG[tb]], lambda: nc.vector.tensor_scalar(G[:, tb, :], ex, misc[:, 2:3], None, op0=ALU.mult))
                gtp, bgtp = tmp2.get()
                fw.op(PE, [bG[tb], bcst], [bgtp], lambda: nc.tensor.transpose(gtp[0:NE, 0:128], G[:, tb, :], ident))
                fw.op(ACT, [bgtp], [bGT[tb]], lambda: nc.scalar.copy(GT[:, tb * 128:(tb + 1) * 128], gtp[0:NE, 0:128]))
                for half in range(2):
                    bdp, bbdp = tmp2.get()
                    fw.op(PE, [bGT[tb], bbd], [bbdp], lambda: nc.tensor.matmul(bdp[:], GT[:, tb * 128:(tb + 1) * 128], bd32[:, half * 512:(half + 1) * 512], start=True, stop=True))
                    fw.op(DVE, [bbdp, bACC[tb]], [bACC[tb]],
                          lambda: nc.vector.scalar_tensor_tensor(ACC[:, tb, half * 512:(half + 1) * 512], in0=ACC[:, tb, half * 512:(half + 1) * 512], scalar=DN_ALPHA, in1=bdp[:], op0=ALU.mult, op1=ALU.add))

            triu = cst[:, 1024:1152]
            for tb in range(16):
                pp, bpp = tmp2.get()
                fw.op(PE, [bMK[tb], bcst], [bpp], lambda: nc.tensor.matmul(pp[:, 0:NE], triu, MK[:, tb, :], start=True, stop=(tb == 0)), inc=(tb == 0))
                for t2_ in range(tb):
                    fw.op(PE, [bMK[t2_], bones], [bpp], lambda: nc.tensor.matmul(pp[:, 0:NE], ones32[:], MK[:, t2_, :], start=False, stop=(t2_ == tb - 1)), inc=(t2_ == tb - 1))
                fw.op(DVE, [bpp, bMK[tb]], [bposm[tb]], lambda: nc.vector.scalar_tensor_tensor(posm[:, tb, :], in0=pp[:, 0:NE], scalar=1.0, in1=MK[:, tb, :], op0=ALU.add, op1=ALU.mult))
                fw.op(DVE, [bposm[tb]], [bposm[tb]], lambda: nc.vector.tensor_scalar(posm[:, tb, :], posm[:, tb, :], -1.0, None, op0=ALU.add))
                ptp, bptp = tmp2.get()
                fw.op(PE, [bposm[tb], bcst], [bptp], lambda: nc.tensor.transpose(ptp[0:NE, 0:128], posm[:, tb, :], ident))
                fw.op(ACT, [bptp], [bposmT[tb // 4]], lambda: nc.scalar.copy(posmT[:, tb * 128:(tb + 1) * 128], ptp[0:NE, 0:128]))
        fw.fence()
        with contextlib.ExitStack() as se:
            NSB = CAP // 128
            wgr = Rot([(sb(f"wg{i}", [128, 8, 2, 128], BF16, se), Buf()) for i in range(7)])
            Wd = sb("Wd", [128, 8, D], BF16, se); bWd = [Buf() for _ in range(4)]
            xgT = sb("xgT", [128, 8, CAP], BF16, se); bxg = [Buf() for _ in range(8)]
            ACTT = sb("ACTT", [128, 8, CAP], BF16, se); bACTT = [Buf() for _ in range(8)]
            Sel = sb("Sel", [128, 16, CAP], BF16, se); bSel = [Buf() for _ in range(16)]
            SelT = Rot([(sb(f"SelT{i}", [128, NSB, 512], BF16, se), Buf()) for i in range(2)])
            yb = sb("yb", [128, NSB, D], BF16, se); byb = [Buf() for _ in range(NSB)]
            gc_ = sb("gc", [128, CAP], F32, se); bgc_ = Buf()
            sg_ = sb("sg", [128, CAP], F32, se); bsg_ = Buf()
            uc_ = sb("uc", [128, CAP], F32, se); buc_ = Buf()
            le = sb("le", [NE, 128], F32, se); ble = Buf()
            busT = sb("busT", [128, NE * 8], F32, se); bbus = Buf()
            fw.op(DVE, [bbgu], [bbus], lambda: nc.vector.tensor_scalar(busT[:].rearrange("p (e c) -> p e c", c=8), bguT[:].rearrange("p (e c) -> p e c", c=16)[:, :, 8:16], 1.0 / 1.702, None, op0=ALU.mult))
            gab = Rot([(psb[i], pb[i]) for i in (0, 1)])
            gub = Rot([((psb[2], pb[2]), (psb[3], pb[3])), ((psb[4], pb[4]), (psb[5], pb[5]))])
            yb_b = Rot([(psb[i], pb[i]) for i in (6, 7)])
            wd_v = wd_d.rearrange("e (fc p) o -> e p fc o", p=128)
            iotaC = cst[:, 640:640 + CAP]
            kiota = cst[0:NE, 1152:1280]

            def load_wg(e, fc):
                wg_, bwg_ = wgr.get()
                fw.dma(POOL, wg_[:].rearrange("p a b c -> p (a b c)"), wgu_d[e, fc], [], [bwg_])
                return wg_, bwg_

            def load_wd(e, pc):
                fw.dma(POOL, Wd[:, 2 * pc:2 * pc + 2, :], wd_v[e, :, 2 * pc:2 * pc + 2, :], [], [bWd[pc]])

            def build_sel(e):
                for tb in range(16):
                    fw.op(DVE, [bposm[tb], bcst], [bSel[tb]], lambda: nc.vector.tensor_scalar(Sel[:, tb, :], iotaC, posm[:, tb, e:e + 1], None, op0=ALU.is_equal))

            def gather(e):
                for dc in range(8):
                    gp, bgp = gab.get()
                    for tb in range(16):
                        fw.op(PE, [botx[tb], bSel[tb]], [bgp], lambda: nc.tensor.matmul(gp[:, 0:CAP], X1B[:, tb, dc * 128:(dc + 1) * 128], Sel[:, tb, :], start=(tb == 0), stop=(tb == 15)), inc=(tb == 15))
                    fw.op(ACT, [bgp], [bxg[dc]], lambda: nc.scalar.copy(xgT[:, dc, :], gp[:, 0:CAP]))

            slices = [(e, fc) for e in range(NE) for fc in range(8)]
            PRE = 6
            WD_SCHED = {0: 0, 1: 1, 2: 2, 3: 3}
            loaded = {}
            for k in range(min(PRE, len(slices))):
                loaded[k] = load_wg(*slices[k])
            for k, (e, fc) in enumerate(slices):
                if k == 0:
                    build_sel(0)
                    gather(0)
                    build_sel(1)
                if k + PRE < len(slices):
                    loaded[k + PRE] = load_wg(*slices[k + PRE])
                wg_, bwg_ = loaded.pop(k)
                if fc in WD_SCHED:
                    load_wd(e, WD_SCHED[fc])
                (gps, bgps), (ups, bups) = gub.get()
                rd = [bwg_] + bxg
                for dc in range(8):
                    fw.op(PE, rd, [bgps], lambda: nc.tensor.matmul(gps[:, 0:CAP], wg_[:, dc, 0, :], xgT[:, dc, :], start=(dc == 0), stop=(dc == 7)), inc=(dc == 7))
                for dc in range(8):
                    fw.op(PE, rd, [bups], lambda: nc.tensor.matmul(ups[:, 0:CAP], wg_[:, dc, 1, :], xgT[:, dc, :], start=(dc == 0), stop=(dc == 7)), inc=(dc == 7))
                bg_col = bguT[:, e * 16 + fc:e * 16 + fc + 1]
                bu_col = bguT[:, e * 16 + 8 + fc:e * 16 + 8 + fc + 1]
                bus_col = busT[:, e * 8 + fc:e * 8 + fc + 1]
                fw.op(DVE, [bgps, bbgu], [bgc_], lambda: nc.vector.tensor_scalar(gc_[:], gps[:, 0:CAP], bg_col, 7.0, op0=ALU.add, op1=ALU.min))
                fw.op(ACT, [bups, bbus], [buc_], lambda: nc.scalar.activation(uc_[:], ups[:, 0:CAP], AF.Identity, bias=bus_col, scale=1.0 / 1.702))
                fw.op(ACT, [bgc_], [bsg_], lambda: nc.scalar.activation(sg_[:], gc_[:], AF.Silu, scale=1.702))
                fw.op(DVE, [buc_], [buc_], lambda: nc.vector.tensor_scalar(uc_[:], uc_[:], 7.0 / 1.702, -7.0 / 1.702, op0=ALU.min, op1=ALU.max))
                fw.op(DVE, [bsg_, buc_], [bACTT[fc]],
                      lambda: nc.vector.scalar_tensor_tensor(ACTT[:, fc, :], in0=uc_[:], scalar=1.0 / 1.702, in1=sg_[:], op0=ALU.add, op1=ALU.mult))
                if fc == 7:
                    if e + 1 < NE:
                        gather(e + 1)
                        if e + 2 < NE:
                            build_sel(e + 2)
                    for sbk in range(NSB):
                        for half in range(2):
                            yp, byp = yb_b.get()
                            rd2 = bACTT + bWd
                            for f in range(8):
                                fw.op(PE, rd2, [byp], lambda: nc.tensor.matmul(yp[:], ACTT[:, f, sbk * 128:(sbk + 1) * 128], Wd[:, f, half * 512:(half + 1) * 512], start=(f == 0), stop=(f == 7)), inc=(f == 7))
                            fw.op(ACT, [byp], [byb[sbk]], lambda: nc.scalar.copy(yb[:, sbk, half * 512:(half + 1) * 512], yp[:]))
                    fw.op(DVE, [bcst], [ble], lambda: nc.vector.tensor_scalar(le[:], kiota, float(e), None, op0=ALU.is_equal))
                    bcs = {}

                    def emit_bc(ch):
                        bc, bbc = gab.get()
                        fw.op(PE, [ble, bposmT[ch]], [bbc], lambda: nc.tensor.matmul(bc[:], le[:], posmT[:, ch * 512:(ch + 1) * 512], start=True, stop=True))
                        bcs[ch] = (bc, bbc)

                    emit_bc(0)
                    for ch in range(4):
                        bc, bbc = bcs.pop(ch)
                        if ch + 1 < 4:
                            emit_bc(ch + 1)
                        st_, bst2 = SelT.get()
                        for sbk in range(NSB):
                            fw.op(DVE, [bbc, bcst], [bst2], lambda: nc.vector.tensor_scalar(st_[:, sbk, :], bc[:], cst[:, 516 + sbk:517 + sbk], None, op0=ALU.is_equal))
                        for tb4 in range(4):
                            tb = ch * 4 + tb4
                            for half in range(2):
                                yp, byp = yb_b.get()
                                for sbk in range(NSB):
                                    fw.op(PE, [bst2] + byb, [byp], lambda: nc.tensor.matmul(yp[:], st_[:, sbk, tb4 * 128:(tb4 + 1) * 128], yb[:, sbk, half * 512:(half + 1) * 512], start=(sbk == 0), stop=(sbk == NSB - 1)), inc=(sbk == NSB - 1))
                                fw.op(DVE, [byp, bG[tb], bACC[tb]], [bACC[tb]],
                                      lambda: nc.vector.scalar_tensor_tensor(ACC[:, tb, half * 512:(half + 1) * 512], in0=yp[:], scalar=G[:, tb, e:e + 1], in1=ACC[:, tb, half * 512:(half + 1) * 512], op0=ALU.mult, op1=ALU.add))

        fw.fence()
        with contextlib.ExitStack() as sf:
            ln2bc = sb("ln2bc", [128, 2, D], F32, sf); bln2 = Buf()
            fw.dma(SP, ln2bc[:], lnv_d[2:4, :].partition_broadcast(128), [], [bln2])
            ot = Rot([(sb(f"ot{i}", [128, D], F32, sf), Buf()) for i in range(2)])
            jk2 = Rot([(sb(f"jk2{i}", [128, D], F32, sf), Buf()) for i in range(2)])
            for tb in range(16):
                o_, bo_ = ot.get()
                j_, bj_ = jk2.get()
                layer_norm(ACC[:, tb, :], bACC[tb], tb, ln2bc, bln2, o_[:], bo_, j_[:], bj_)
                fw.dma(SP, out_d[tb * 128:(tb + 1) * 128, :], o_[:], [bo_], [])
        fw.finish(SP)
    return nc


_PROG = {}


def _consts(r):
    c = np.zeros((128, 1280), np.float32)
    c[:, 0:128] = np.eye(128, dtype=np.float32)
    m = np.arange(128)
    partner = np.where(m % 64 < 32, m + 32, m - 32)
    c[partner, 128 + m] = 1.0
    k = np.arange(128)[:, None]
    q = np.arange(128)[None, :]
    c[:, 256:384] = (k <= q).astype(np.float32)
    c[:, 384:512] = 1.0 if r == 1 else 0.0
    invf = 1.0 / (10000.0 ** ((np.arange(128) % 32) * 2.0 / 64.0))
    first = (np.arange(128) % 64) < 32
    c[:, 512] = invf
    sc = TWO_PI * (1.0 - 1e-6)
    c[:, 513] = np.where(first, -sc, sc)
    c[:, 514] = np.where(first, PI_S, -PI_S)
    c[:, 515] = -PI_S
    p = np.arange(128, dtype=np.float32)
    c[:, 516] = p
    c[:, 517] = p + 128.0
    c[:, 518] = p + 256.0
    c[:, 640:1024] = np.arange(CAP, dtype=np.float32)[None, :]
    c[:, 1024:1152] = (p[:, None] < p[None, :]).astype(np.float32)
    c[:, 1152:1280] = p[:, None]
    return c


def _prep_inputs(inp):
    f32 = lambda a: np.ascontiguousarray(np.asarray(a), dtype=np.float32)
    x = f32(inp["x"])
    positions = np.ascontiguousarray(np.asarray(inp["positions"]), dtype=np.int32)
    wgu = f32(inp["w_gate_up"])[0]
    wgu_t = np.ascontiguousarray(
        wgu.reshape(NE, 8, 128, 2, 8, 128).transpose(0, 4, 2, 1, 3, 5)).reshape(NE, 8, 128, 2048)
    bgu = f32(inp["b_gate_up"])[0]
    bguT = np.ascontiguousarray(bgu.reshape(NE, 16, 128).transpose(2, 0, 1)).reshape(128, NE * 16)
    shared = {
        "w_in": f32(inp["w_in"])[0],
        "lamv": np.ascontiguousarray(np.stack([f32(inp["lambda_q1"])[0], f32(inp["lambda_k1"])[0],
                                               f32(inp["lambda_q2"])[0], f32(inp["lambda_k2"])[0]], 0)),
        "subln_g": f32(inp["subln_g"])[0].reshape(128, 1),
        "gq": np.ascontiguousarray(f32(inp["mla_q_norm_g"])[0].reshape(2, 128).T),
        "gkv": f32(inp["mla_kv_norm_g"])[0].reshape(128, 1),
        "w_uq": f32(inp["w_uq"])[0],
        "w_ukv": f32(inp["w_ukv"])[0],
        "w_o": f32(inp["w_o"])[0],
        "lnv": np.ascontiguousarray(np.stack([f32(inp["ln1_g"])[0], f32(inp["ln1_b"])[0],
                                              f32(inp["ln2_g"])[0], f32(inp["ln2_b"])[0]], 0)),
        "w_router": f32(inp["w_router"])[0],
        "b_router": f32(inp["b_router"])[0].reshape(1, NE),
        "wgu_t": wgu_t,
        "bguT": bguT,
        "w_down": f32(inp["w_down"])[0],
        "b_down": f32(inp["b_down"])[0],
    }
    in_maps = []
    toks = []
    for c in range(NCORES):
        b, r = c // 2, c % 2
        own = [2 * j + r for j in range(16)]
        oth = [2 * j + (1 - r) for j in range(16)]
        tok = np.concatenate([np.arange(g * 128, (g + 1) * 128) for g in own + oth])
        toks.append((b, tok[:TQ]))
        xb = x[b][tok]
        m = dict(shared)
        m["xT"] = np.ascontiguousarray(xb.T)
        m["xq"] = np.ascontiguousarray(xb[:TQ])
        m["pos"] = np.ascontiguousarray(positions[b][tok].reshape(1, S))
        m["cst"] = _consts(r)
        in_maps.append(m)
    return in_maps, toks


def kernel(**inputs):
    stage = inputs.pop("_stage", "full")
    if stage not in _PROG:
        _PROG[stage] = build_program(stage)
    nc = _PROG[stage]
    in_maps, toks = _prep_inputs(inputs)
    if stage != "full":
        moe = ("w_router", "b_router", "wgu_t", "bguT", "w_down", "b_down")
        in_maps = [{k: v for k, v in m.items() if k not in moe} for m in in_maps]
    res = run_bass_kernel_spmd(nc, in_maps, core_ids=list(range(NCORES)))
    out = np.zeros((4, S, D), np.float32)
    for c in range(NCORES):
        b, tok = toks[c]
        out[b, tok] = res.results[c]["out"]
    return out
```

```python
import math
import contextlib
import numpy as np
import concourse.bass as bass
import concourse.mybir as mybir
from concourse.bass_utils import run_bass_kernel_spmd

F32 = mybir.dt.float32
BF16 = mybir.dt.bfloat16
I32 = mybir.dt.int32
ALU = mybir.AluOpType
AF = mybir.ActivationFunctionType
AX = mybir.AxisListType

NCORES = 8
D = 1024
S = 4096
TQ = 2048
NE = 32
LAM_INIT = 0.8 - 0.6 * math.exp(0.0)
DN_ALPHA = 2.0 ** 0.25
TWO_PI = 2.0 * math.pi
PI_S = math.pi * (1.0 - 1e-6)
NDMA_SEM = 8
CAP = 384


_FENCE = {}


class Buf:
    __slots__ = ("w", "r", "excl")

    def __init__(self, excl=False):
        self.w = None
        self.r = dict(_FENCE)
        self.excl = excl


class Eng:
    def __init__(self, name, h, sem, dma_sems):
        self.name = name
        self.h = h
        self.sem = sem
        self.count = 0
        self.seen = {}
        self.dma_sems = dma_sems
        self.dma_val = [0] * len(dma_sems)
        self.rr = 0


class FW:
    def __init__(self, nc, es):
        self.nc = nc
        self.es = es
        mk = lambda n: es.enter_context(nc.semaphore(n))
        self.pe = Eng("pe", nc.tensor, mk("s_pe"), [])
        self.act = Eng("act", nc.scalar, mk("s_act"), [])
        self.dve = Eng("dve", nc.vector, mk("s_dve"), [])
        self.pool = Eng("pool", nc.gpsimd, mk("s_pool"), [mk(f"d_pool{i}") for i in range(NDMA_SEM)])
        self.sp = Eng("sp", nc.sync, mk("s_sp"), [mk(f"d_sp{i}") for i in range(NDMA_SEM)])
        self.nwait = 0

    def _wait(self, E, tok):
        sem, val = tok
        if sem is E.sem and (E is self.pe or val > E.count):
            return
        k = id(sem)
        if E.seen.get(k, 0) >= val:
            return
        E.h.wait_ge(sem, val)
        E.seen[k] = val
        self.nwait += 1

    def _deps(self, E, reads, writes):
        for b in reads:
            if b.w is not None:
                self._wait(E, b.w)
            if b.excl:
                for tok in b.r.values():
                    self._wait(E, tok)
        for b in writes:
            if b.w is not None:
                self._wait(E, b.w)
            for tok in b.r.values():
                self._wait(E, tok)

    def _mark(self, tok, reads, writes):
        for b in reads:
            b.r[id(tok[0])] = tok
        for b in writes:
            b.w = tok
            b.r = {}

    def op(self, E, reads, writes, build, inc=True):
        self._deps(E, reads, writes)
        ins = build()
        tok = (E.sem, E.count + 1)
        if inc:
            ins.then_inc(E.sem, 1)
            E.count += 1
        self._mark(tok, reads, writes)
        return ins

    def dma(self, Q, out, in_, reads, writes, **kw):
        i = Q.rr % len(Q.dma_sems)
        Q.rr += 1
        sem = Q.dma_sems[i]
        if Q.dma_val[i] > 0:
            self._wait(Q, (sem, Q.dma_val[i]))
        self._deps(Q, reads, writes)
        Q.h.dma_start(out=out, in_=in_, **kw).then_inc(sem, 16)
        Q.dma_val[i] += 16
        tok = (sem, Q.dma_val[i])
        self._mark(tok, reads, writes)
        return tok

    def fence(self):
        _FENCE.clear()
        for Q in (self.sp, self.pool):
            for sem, v in zip(Q.dma_sems, Q.dma_val):
                if v > 0:
                    _FENCE[id(sem)] = (sem, v)
        for X in (self.pe, self.act, self.dve, self.pool, self.sp):
            if X.count > 0:
                _FENCE[id(X.sem)] = (X.sem, X.count)

    def finish(self, E):
        for Q in (self.sp, self.pool):
            for sem, v in zip(Q.dma_sems, Q.dma_val):
                if v > 0:
                    self._wait(E, (sem, v))
        for X in (self.pe, self.act, self.dve, self.pool, self.sp):
            if X is not E and X.count > 0:
                self._wait(E, (X.sem, X.count))


class Rot:
    def __init__(self, items):
        self.items = items
        self.i = 0

    def get(self):
        it = self.items[self.i % len(self.items)]
        self.i += 1
        return it


def build_program(stage="full"):
    nc = bass.Bass("TRN2", target_bir_lowering=False)
    dt_in = lambda name, shape, dt=F32: nc.dram_tensor(name, shape, dt, kind="ExternalInput").ap()
    xT_d = dt_in("xT", [D, S])
    xq_d = dt_in("xq", [TQ, D])
    pos_d = dt_in("pos", [1, S], I32)
    cst_d = dt_in("cst", [128, 1280])
    w_in_d = dt_in("w_in", [D, 1984])
    lam_d = dt_in("lamv", [4, 64])
    subg_d = dt_in("subln_g", [128, 1])
    gq_d = dt_in("gq", [128, 2])
    gkv_d = dt_in("gkv", [128, 1])
    w_uq_d = dt_in("w_uq", [256, 768])
    w_ukv_d = dt_in("w_ukv", [128, 1024])
    w_o_d = dt_in("w_o", [D, D])
    lnv_d = dt_in("lnv", [4, D])
    if stage == "full":
        w_r_d = dt_in("w_router", [D, NE])
        b_r_d = dt_in("b_router", [1, NE])
        wgu_d = dt_in("wgu_t", [NE, 8, 128, 2048])
        bgu_d = dt_in("bguT", [128, NE * 16])
        wd_d = dt_in("w_down", [NE, D, D])
        bd_d = dt_in("b_down", [NE, D])
    out_d = nc.dram_tensor("out", [TQ, D], F32, kind="ExternalOutput").ap()

    _FENCE.clear()
    with contextlib.ExitStack() as es:
        fw = FW(nc, es)
        PE, ACT, DVE, POOL, SP = fw.pe, fw.act, fw.dve, fw.pool, fw.sp

        def sb(name, shape, dt, st=es):
            return st.enter_context(nc.sbuf_tensor("sb_" + name, shape, dt))

        psb = [es.enter_context(nc.psum_tensor(f"ps{i}", [128, 512], F32)) for i in range(8)]
        pb = [Buf(excl=True) for _ in range(8)]

        cst = sb("cst", [128, 1280], F32); bcst = Buf()
        fw.dma(SP, cst[:], cst_d[:, :], [], [bcst])
        ident = cst[:, 0:128]
        ropec = cst[:, 512:516]
        cbf = sb("cbf", [128, 512], BF16); bcbf = Buf()
        fw.op(DVE, [bcst], [bcbf], lambda: nc.vector.tensor_copy(cbf[:, 0:384], cst[:, 128:512]))
        fw.op(DVE, [], [bcbf], lambda: nc.vector.memset(cbf[:, 384:512], 1.0))
        perm_bf = cbf[:, 0:128]
        masks_bf = [cbf[:, 128:256], cbf[:, 256:384]]
        ones_bf = cbf[:, 384:512]
        ones32 = sb("ones32", [128, 128], F32); bones = Buf()
        fw.op(POOL, [], [bones], lambda: nc.gpsimd.memset(ones32[:], 1.0))
        small = sb("small", [128, 64], F32); bsmall = Buf()
        fw.dma(SP, small[:, 0:1], subg_d[:, :], [], [bsmall])
        fw.dma(SP, small[:, 3:5], gq_d[:, :], [], [bsmall])
        fw.dma(SP, small[:, 5:6], gkv_d[:, :], [], [bsmall])
        fw.op(DVE, [], [bsmall], lambda: nc.vector.memset(small[:, 8:9], 1e-6))
        fw.op(DVE, [], [bsmall], lambda: nc.vector.memset(small[:, 9:10], 1e-5))
        EPS6 = small[:, 8:9]
        EPS5 = small[:, 9:10]
        lamt = sb("lamt", [128, 256], F32); blam = Buf()
        fw.dma(SP, lamt[:].rearrange("p (a b) -> p a b", a=4), lam_d.partition_broadcast(128), [], [blam])
        fw.op(DVE, [blam], [blam], lambda: nc.vector.tensor_tensor(lamt[:, 0:64], lamt[:, 0:64], lamt[:, 64:128], op=ALU.mult))
        fw.op(DVE, [blam], [blam], lambda: nc.vector.tensor_tensor(lamt[:, 128:192], lamt[:, 128:192], lamt[:, 192:256], op=ALU.mult))
        fw.op(DVE, [blam], [bsmall], lambda: nc.vector.reduce_sum(small[:, 6:7], lamt[:, 0:64], axis=AX.X))
        fw.op(DVE, [blam], [bsmall], lambda: nc.vector.reduce_sum(small[:, 7:8], lamt[:, 128:192], axis=AX.X))
        fw.op(ACT, [bsmall], [bsmall], lambda: nc.scalar.activation(small[:, 6:8], small[:, 6:8], AF.Exp))
        fw.op(DVE, [bsmall], [bsmall], lambda: nc.vector.tensor_tensor(small[:, 2:3], small[:, 7:8], small[:, 6:7], op=ALU.subtract))
        fw.op(DVE, [bsmall], [bsmall], lambda: nc.vector.tensor_scalar(small[:, 2:3], small[:, 2:3], -LAM_INIT, None, op0=ALU.add))
        fw.op(DVE, [bsmall], [bsmall], lambda: nc.vector.tensor_scalar(small[:, 1:2], small[:, 0:1], 1.0 - LAM_INIT, None, op0=ALU.mult))

        otx = sb("otx", [128, 8, TQ], BF16)
        botx = [Buf() for _ in range(16)]

        with contextlib.ExitStack() as sa:
            cosT = sb("cosT", [128, S], F32, sa)
            sinS = sb("sinS", [128, S], F32, sa)
            btab = [Buf() for _ in range(8)]
            ckvn = sb("ckvn", [128, S], BF16, sa); bckvn = [Buf() for _ in range(8)]
            cqn = sb("cqn", [128, 2, TQ], BF16, sa); bcqn = [Buf() for _ in range(4)]
            KR = sb("KR", [128, S], BF16, sa); bKR = [Buf() for _ in range(8)]
            fw.op(POOL, [], bKR, lambda: nc.gpsimd.memset(KR[64:128, :], 0.0))
            s32 = Rot([(sb(f"s32_{i}", [128, 512], F32, sa), Buf()) for i in range(8)])
            s16 = Rot([(sb(f"s16_{i}", [128, 512], BF16, sa), Buf()) for i in range(4)])
            si32 = sb("si32", [128, 512], I32, sa); bsi32 = Buf()
            tmpb = Rot([(psb[i], pb[i]) for i in (6, 7, 0, 1)])

            def mm_acc(out_ap, pairs, reads, writes):
                n = len(pairs)
                for i, (l, r) in enumerate(pairs):
                    fw.op(PE, reads, writes,
                          lambda: nc.tensor.matmul(out_ap, l, r, start=(i == 0), stop=(i == n - 1)),
                          inc=(i == n - 1))

            def rope(src_ps, bsrc, rows, tc, dst_ap, bdst):
                cols = slice(tc * 512, (tc + 1) * 512)
                import os as _os
                _cut = int(_os.environ.get("ROPE_CUT", "9")) if rows == 128 else 9
                if _cut < 1:
                    return
                hb, bhb = s16.get()
                fw.op(ACT, [bsrc], [bhb], lambda: nc.scalar.copy(hb[0:rows, :], src_ps))
                if _cut < 2:
                    return
                sw, bsw = tmpb.get()
                fw.op(PE, [bhb, bcbf], [bsw], lambda: nc.tensor.matmul(sw[0:rows, :], perm_bf[0:rows, 0:rows], hb[0:rows, :], start=True, stop=True))
                if _cut < 3:
                    return
                t1, bt1 = s32.get()
                fw.op(DVE, [bsrc, btab[tc]], [bt1], lambda: nc.vector.tensor_tensor(t1[0:rows, :], src_ps, cosT[0:rows, cols], op=ALU.mult))
                if _cut < 4:
                    return
                t2, bt2 = s32.get()
                fw.op(DVE, [bsw, btab[tc]], [bt2], lambda: nc.vector.tensor_tensor(t2[0:rows, :], sw[0:rows, :], sinS[0:rows, cols], op=ALU.mult))
                if _cut < 5:
                    return
                fw.op(DVE, [bt1, bt2], [bdst], lambda: nc.vector.tensor_tensor(dst_ap, t1[0:rows, :], t2[0:rows, :], op=ALU.add))

            def rms_scale(ps_list, bps_list, n_feat, eps, gcols, dst_aps, bdst):
                sqs = []
                for ps_ap, bps in zip(ps_list, bps_list):
                    sq, bsq = s32.get()
                    fw.op(ACT, [bps], [bsq], lambda: nc.scalar.activation(sq[:], ps_ap, AF.Square))
                    sqs.append((sq, bsq))
                ss, bss = tmpb.get()
                for i, (sq, bsq) in enumerate(sqs):
                    fw.op(PE, [bsq, bones], [bss],
                          lambda: nc.tensor.matmul(ss[:], ones32[:], sq[:], start=(i == 0), stop=(i == len(sqs) - 1)),
                          inc=(i == len(sqs) - 1))
                rstd, brstd = s32.get()
                fw.op(ACT, [bss, bsmall], [brstd], lambda: nc.scalar.activation(rstd[:], ss[:], AF.Sqrt, bias=eps, scale=1.0 / n_feat))
                fw.op(DVE, [brstd], [brstd], lambda: nc.vector.reciprocal(rstd[:], rstd[:]))
                for ps_ap, bps, gc, dst in zip(ps_list, bps_list, gcols, dst_aps):
                    fw.op(DVE, [bps, brstd, bsmall], [bdst],
                          lambda: nc.vector.scalar_tensor_tensor(dst, in0=ps_ap, scalar=gc, in1=rstd[:], op0=ALU.mult, op1=ALU.mult))

            def attention(nsub, s_emit, s_reads, Vt, bV, scale, finalize):
                S_B = [(psb[0], pb[0]), (psb[1], pb[1]), (psb[6], pb[6]), (psb[7], pb[7])]
                O_B = [(psb[2], pb[2]), (psb[3], pb[3])]
                L_B = [(psb[4], pb[4]), (psb[5], pb[5])]
                pending = [None]
                for g in range(4):
                    units = []
                    nkb = 4 * g + 4
                    for half in (0, 1):
                        for kl in range(nkb):
                            i = kl - 4 * g
                            col0 = 0 if i < 0 else i * 128
                            mt = None if i < 0 else half
                            for c in range(nsub):
                                units.append((c, half * 16 + kl, col0, mt))
                    nun = len(units)
                    first = [True] * nsub
                    last_idx = {}
                    for ui, u in enumerate(units):
                        last_idx[u[0]] = ui
                    pts = {}

                    def emit_s(ui):
                        c, kb, col0, mt = units[ui]
                        sbk, bsbk = S_B[ui % 4]
                        s_emit(c, kb, g, col0, sbk, bsbk)
                        pT, bpT = s16.get()
                        fw.op(ACT, [bsbk], [bpT], lambda: nc.scalar.activation(pT[:, col0:512], sbk[:, col0:512], AF.Exp, scale=scale))
                        if mt is not None:
                            fw.op(DVE, [bpT, bcbf], [bpT], lambda: nc.vector.tensor_tensor(pT[:, col0:col0 + 128], pT[:, col0:col0 + 128], masks_bf[mt], op=ALU.mult))
                        pts[ui] = (pT, bpT)

                    def emit_pv(ui):
                        c, kb, col0, mt = units[ui]
                        pT, bpT = pts.pop(ui)
                        o, bo = O_B[c]
                        l, bl = L_B[c]
                        st = first[c]
                        first[c] = False
                        sp_ = (last_idx[c] == ui)
                        fw.op(PE, [bpT, bV[kb // 4]], [bo], lambda: nc.tensor.matmul(o[:, col0:512], Vt[:, kb, :], pT[:, col0:512], start=st, stop=sp_), inc=False)
                        fw.op(PE, [bpT, bcbf], [bl], lambda: nc.tensor.matmul(l[:, col0:512], ones_bf, pT[:, col0:512], start=st, stop=sp_))

                    LOOK = 3
                    for ui in range(min(LOOK, nun)):
                        emit_s(ui)
                    if pending[0] is not None:
                        pending[0]()
                    for ui in range(nun):
                        emit_pv(ui)
                        if ui + LOOK < nun:
                            emit_s(ui + LOOK)
                    pending[0] = (lambda g=g: finalize(g, O_B, L_B))
                pending[0]()

            with contextlib.ExitStack() as sx:
                xTb = sb("xTb", [128, 8, S], BF16, sx); bxT = [Buf() for _ in range(8)]
                xT_v = xT_d.rearrange("(dc p) t -> p dc t", p=128)
                for tc in range(8):
                    fw.dma(POOL, xTb[:, :, tc * 512:(tc + 1) * 512], xT_v[:, :, tc * 512:(tc + 1) * 512], [], [bxT[tc]])
                w_in_v = w_in_d.rearrange("(dc p) c -> p dc c", p=128)

                for tc in range(8):
                    cols = slice(tc * 512, (tc + 1) * 512)
                    fw.dma(SP, si32[:], pos_d[0:1, cols].partition_broadcast(128), [], [bsi32])
                    ang, bang = s32.get()
                    fw.op(DVE, [bsi32], [bang], lambda: nc.vector.tensor_copy(ang[:], si32[:]))
                    fw.op(DVE, [bang, bcst], [bang], lambda: nc.vector.tensor_scalar(ang[:], ang[:], ropec[:, 0:1], None, op0=ALU.mult))
                    for which in (0, 1):
                        shift = 0.5 if which == 0 else 0.75
                        u, bu = s32.get()
                        fw.op(DVE, [bang], [bu], lambda: nc.vector.tensor_scalar(u[:], ang[:], 1.0 / TWO_PI, shift, op0=ALU.mult, op1=ALU.add))
                        ki, bki = s32.get()
                        kiv = ki[:].bitcast(I32)
                        fw.op(DVE, [bu], [bki], lambda: nc.vector.tensor_copy(kiv, u[:]))
                        kf, bkf = s32.get()
                        fw.op(DVE, [bki], [bkf], lambda: nc.vector.tensor_copy(kf[:], kiv))
                        fw.op(DVE, [bkf, bu], [bu], lambda: nc.vector.tensor_tensor(u[:], u[:], kf[:], op=ALU.subtract))
                        fw.op(DVE, [bu], [bkf], lambda: nc.vector.scalar_tensor_tensor(kf[:], in0=u[:], scalar=0.0, in1=u[:], op0=ALU.is_lt, op1=ALU.add))
                        if which == 0:
                            fw.op(ACT, [bkf, bcst], [btab[tc]], lambda: nc.scalar.activation(sinS[:, cols], kf[:], AF.Sin, bias=ropec[:, 2:3], scale=ropec[:, 1:2]))
                        else:
                            fw.op(ACT, [bkf, bcst], [btab[tc]], lambda: nc.scalar.activation(cosT[:, cols], kf[:], AF.Sin, bias=ropec[:, 3:4], scale=TWO_PI * (1.0 - 1e-6)))

                if stage.startswith("tabtt"):
                    import os as _os
                    r0, r1 = [int(v) for v in _os.environ.get("TT_ROWS", "0,128").split(",")]
                    mode = _os.environ.get("TT_MODE", "psum_cos")
                    t1, bt1 = s32.get()
                    kps, bkps = tmpb.get()
                    fw.op(PE, [bcbf], [bkps], lambda: nc.tensor.matmul(kps[:], perm_bf, cbf[:, 0:512], start=True, stop=True))
                    if mode == "psum_cos":
                        fw.op(DVE, [bkps, btab[0]], [bt1], lambda: nc.vector.tensor_tensor(t1[r0:r1, :], kps[r0:r1, :], cosT[r0:r1, 0:512], op=ALU.mult))
                    elif mode == "sb_cos":
                        t2, bt2 = s32.get()
                        fw.op(DVE, [], [bt2], lambda: nc.vector.memset(t2[:], 1.0))
                        fw.op(DVE, [bt2, btab[0]], [bt1], lambda: nc.vector.tensor_tensor(t1[r0:r1, :], t2[r0:r1, :], cosT[r0:r1, 0:512], op=ALU.mult))
                    elif mode == "psum_sb":
                        t2, bt2 = s32.get()
                        fw.op(DVE, [], [bt2], lambda: nc.vector.memset(t2[:], 1.0))
                        fw.op(DVE, [bkps, bt2], [bt1], lambda: nc.vector.tensor_tensor(t1[r0:r1, :], kps[r0:r1, :], t2[r0:r1, :], op=ALU.mult))
                    fw.dma(SP, out_d[0:128, 0:512], t1[:], [bt1], [])
                    fw.dma(SP, out_d[128:256, 0:512], cosT[:, 0:512], [btab[0]], [])
                    fw.dma(SP, out_d[256:384, 0:512], sinS[:, 0:512], [btab[0]], [])
                    fw.finish(SP)
                    return nc
                if stage == "tab":
                    fw.finish(SP)
                    return nc
                with contextlib.ExitStack() as sm0:
                    WC = sb("WC", [128, 8, 448], BF16, sm0); bWC = Buf()
                    fw.dma(POOL, WC[:], w_in_v[:, :, 1536:1984], [], [bWC])
                    for tc in range(8):
                        cols = slice(tc * 512, (tc + 1) * 512)
                        ckv, bckv = tmpb.get()
                        mm_acc(ckv[:], [(WC[:, dc, 256:384], xTb[:, dc, cols]) for dc in range(8)], [bWC, bxT[tc]], [bckv])
                        rms_scale([ckv[:]], [bckv], 128.0, EPS6, [small[:, 5:6]], [ckvn[:, cols]], bckvn[tc])
                        kr, bkr = tmpb.get()
                        mm_acc(kr[0:64, :], [(WC[:, dc, 384:448], xTb[:, dc, cols]) for dc in range(8)], [bWC, bxT[tc]], [bkr])
                        rope(kr[0:64, :], bkr, 64, tc, KR[0:64, cols], bKR[tc])
                        if tc < 4:
                            cq0, bcq0 = tmpb.get()
                            mm_acc(cq0[:], [(WC[:, dc, 0:128], xTb[:, dc, cols]) for dc in range(8)], [bWC, bxT[tc]], [bcq0])
                            cq1, bcq1 = tmpb.get()
                            mm_acc(cq1[:], [(WC[:, dc, 128:256], xTb[:, dc, cols]) for dc in range(8)], [bWC, bxT[tc]], [bcq1])
                            rms_scale([cq0[:], cq1[:]], [bcq0, bcq1], 256.0, EPS6, [small[:, 3:4], small[:, 4:5]],
                                      [cqn[:, 0, cols], cqn[:, 1, cols]], bcqn[tc])

                if stage == "m0":
                    fw.finish(SP)
                    return nc
                fw.fence()
                with contextlib.ExitStack() as sd:
                    WQ = sb("WQ", [128, 8, 128], BF16, sd); WK = sb("WK", [128, 8, 128], BF16, sd); WV = sb("WV", [128, 8, 128], BF16, sd)
                    bW = Buf()
                    KT = sb("KT", [128, S], BF16, sd); bKT = [Buf() for _ in range(8)]
                    QT = sb("QT", [128, TQ], BF16, sd); bQT = [Buf() for _ in range(4)]
                    Vt = sb("Vt", [128, 32, 128], BF16, sd); bV = [Buf() for _ in range(8)]
                    for h in range(4):
                        fw.dma(POOL, WQ[:], w_in_v[:, :, 128 * h:128 * h + 128], [], [bW])
                        fw.dma(POOL, WK[:], w_in_v[:, :, 512 + 128 * h:512 + 128 * h + 128], [], [bW])
                        fw.dma(POOL, WV[:], w_in_v[:, :, 1024 + 128 * h:1024 + 128 * h + 128], [], [bW])
                        if stage == "dprojD":
                            fw.finish(SP)
                            return nc
                        import os as _os
                        _ntc = int(_os.environ.get("DPROJ_NTC", "8"))
                        _noq = _os.environ.get("DPROJ_NOQ", "0") == "1"
                        for tc in range(_ntc):
                            cols = slice(tc * 512, (tc + 1) * 512)
                            if stage != "dprojV":
                                kps, bkps = tmpb.get()
                                mm_acc(kps[:], [(WK[:, dc, :], xTb[:, dc, cols]) for dc in range(8)], [bW, bxT[tc]], [bkps])
                                rope(kps[:], bkps, 128, tc, KT[:, cols], bKT[tc])
                            if tc < 4 and stage != "dprojV" and not _noq:
                                qps, bqps = tmpb.get()
                                mm_acc(qps[:], [(WQ[:, dc, :], xTb[:, dc, cols]) for dc in range(8)], [bW, bxT[tc]], [bqps])
                                rope(qps[:], bqps, 128, tc, QT[:, cols], bQT[tc])
                            if stage == "dprojK":
                                continue
                            vps, bvps = tmpb.get()
                            for i in range(4):
                                mm_acc(vps[:, i * 128:(i + 1) * 128],
                                       [(xTb[:, dc, tc * 512 + i * 128: tc * 512 + (i + 1) * 128], WV[:, dc, :]) for dc in range(8)],
                                       [bW, bxT[tc]], [bvps])
                            fw.op(ACT, [bvps], [bV[tc]], lambda: nc.scalar.copy(Vt[:, tc * 4:(tc + 1) * 4, :], vps[:].rearrange("p (a b) -> p a b", a=4)))

                        if stage in ("dproj", "dprojK", "dprojV"):
                            fw.finish(SP)
                            return nc
                        def s_emit(c, kb, g, col0, sbk, bsbk):
                            fw.op(PE, [bKT[kb // 4], bQT[g]], [bsbk],
                                  lambda: nc.tensor.matmul(sbk[:, col0:512], KT[64 * c:64 * c + 64, kb * 128:(kb + 1) * 128],
                                                           QT[64 * c:64 * c + 64, g * 512 + col0:(g + 1) * 512], start=True, stop=True))

                        def fin_diff(g, O_B, L_B, h=h):
                            ds = []
                            for c in range(2):
                                rl, brl = s32.get()
                                fw.op(DVE, [L_B[c][1]], [brl], lambda: nc.vector.reciprocal(rl[:], L_B[c][0][:]))
                                fw.op(DVE, [O_B[c][1], brl], [brl], lambda: nc.vector.tensor_tensor(rl[:], O_B[c][0][:], rl[:], op=ALU.mult))
                                ds.append((rl, brl))
                            dd, bdd = s32.get()
                            fw.op(DVE, [ds[0][1], ds[1][1], bsmall], [bdd],
                                  lambda: nc.vector.scalar_tensor_tensor(dd[:], in0=ds[1][0][:], scalar=small[:, 2:3], in1=ds[0][0][:], op0=ALU.mult, op1=ALU.add))
                            sq, bsq = s32.get()
                            fw.op(ACT, [bdd], [bsq], lambda: nc.scalar.activation(sq[:], dd[:], AF.Square))
                            ss, bss = tmpb.get()
                            fw.op(PE, [bsq, bones], [bss], lambda: nc.tensor.matmul(ss[:], ones32[:], sq[:], start=True, stop=True))
                            rstd, brstd = s32.get()
                            fw.op(ACT, [bss, bsmall], [brstd], lambda: nc.scalar.activation(rstd[:], ss[:], AF.Sqrt, bias=EPS5, scale=1.0 / 128.0))
                            fw.op(DVE, [brstd], [brstd], lambda: nc.vector.reciprocal(rstd[:], rstd[:]))
                            wr = [botx[4 * g + i] for i in range(4)]
                            fw.op(DVE, [bdd, brstd, bsmall], wr,
                                  lambda: nc.vector.scalar_tensor_tensor(otx[:, h, g * 512:(g + 1) * 512], in0=dd[:], scalar=small[:, 1:2], in1=rstd[:], op0=ALU.mult, op1=ALU.mult))

                        attention(2, s_emit, None, Vt, bV, 64.0 ** -0.5, fin_diff)
                        if stage == "datt":
                            fw.finish(SP)
                            return nc
            fw.fence()
            with contextlib.ExitStack() as sm:
                wuq = sb("wuq", [128, 2, 768], BF16, sm); bwuq = Buf()
                wukv = sb("wukv", [128, 1024], BF16, sm); bwukv = Buf()
                fw.dma(POOL, wuq[:], w_uq_d.rearrange("(rc p) c -> p rc c", p=128), [], [bwuq])
                fw.dma(POOL, wukv[:], w_ukv_d[:, :], [], [bwukv])
                KTm = sb("KTm", [128, S], BF16, sm); bKTm = [Buf() for _ in range(8)]
                Vm = sb("Vm", [128, 32, 128], BF16, sm); bVm = [Buf() for _ in range(8)]
                QTn = sb("QTn", [128, TQ], BF16, sm); bQTn = [Buf() for _ in range(4)]
                QTr = sb("QTr", [128, TQ], BF16, sm); bQTr = [Buf() for _ in range(4)]
                fw.op(POOL, [], bQTr, lambda: nc.gpsimd.memset(QTr[64:128, :], 0.0))
                for h in range(4):
                    for tc in range(8):
                        cols = slice(tc * 512, (tc + 1) * 512)
                        kn, bkn = tmpb.get()
                        fw.op(PE, [bwukv, bckvn[tc]], [bkn], lambda: nc.tensor.matmul(kn[:], wukv[:, h * 256:h * 256 + 128], ckvn[:, cols], start=True, stop=True))
                        fw.op(ACT, [bkn], [bKTm[tc]], lambda: nc.scalar.copy(KTm[:, cols], kn[:]))
                        vps, bvps = tmpb.get()
                        for i in range(4):
                            fw.op(PE, [bwukv, bckvn[tc]], [bvps],
                                  lambda: nc.tensor.matmul(vps[:, i * 128:(i + 1) * 128], ckvn[:, tc * 512 + i * 128: tc * 512 + (i + 1) * 128],
                                                           wukv[:, h * 256 + 128:h * 256 + 256], start=True, stop=True), inc=(i == 3))
                        fw.op(DVE, [bvps], [bVm[tc]], lambda: nc.vector.tensor_copy(Vm[:, tc * 4:(tc + 1) * 4, :], vps[:].rearrange("p (a b) -> p a b", a=4)))
                        if tc < 4:
                            qn, bqn = tmpb.get()
                            mm_acc(qn[:], [(wuq[:, rc, h * 192:h * 192 + 128], cqn[:, rc, cols]) for rc in range(2)], [bwuq, bcqn[tc]], [bqn])
                            fw.op(ACT, [bqn], [bQTn[tc]], lambda: nc.scalar.copy(QTn[:, cols], qn[:]))
                            qr, bqr = tmpb.get()
                            mm_acc(qr[0:64, :], [(wuq[:, rc, h * 192 + 128:h * 192 + 192], cqn[:, rc, cols]) for rc in range(2)], [bwuq, bcqn[tc]], [bqr])
                            rope(qr[0:64, :], bqr, 64, tc, QTr[0:64, cols], bQTr[tc])

                    if stage == "mproj":
                        fw.finish(SP)
                        return nc
                    def s_emit_m(c, kb, g, col0, sbk, bsbk):
                        fw.op(PE, [bKTm[kb // 4], bQTn[g]], [bsbk],
                              lambda: nc.tensor.matmul(sbk[:, col0:512], KTm[:, kb * 128:(kb + 1) * 128], QTn[:, g * 512 + col0:(g + 1) * 512], start=True, stop=False), inc=False)
                        fw.op(PE, [bKR[kb // 4], bQTr[g]], [bsbk],
                              lambda: nc.tensor.matmul(sbk[:, col0:512], KR[:, kb * 128:(kb + 1) * 128], QTr[:, g * 512 + col0:(g + 1) * 512], start=False, stop=True))

                    def fin_mla(g, O_B, L_B, h=h):
                        rl, brl = s32.get()
                        fw.op(DVE, [L_B[0][1]], [brl], lambda: nc.vector.reciprocal(rl[:], L_B[0][0][:]))
                        wr = [botx[4 * g + i] for i in range(4)]
                        fw.op(DVE, [O_B[0][1], brl], wr, lambda: nc.vector.tensor_tensor(otx[:, 4 + h, g * 512:(g + 1) * 512], O_B[0][0][:], rl[:], op=ALU.mult))

                    attention(1, s_emit_m, None, Vm, bVm, 192.0 ** -0.5, fin_mla)
                    if stage == "matt":
                        fw.finish(SP)
                        return nc

        fw.fence()
        ACC = sb("ACC", [128, 16, D], F32); bACC = [Buf() for _ in range(16)]
        sm2 = sb("sm2", [128, 16, 8], F32); bsm2 = [Buf() for _ in range(16)]

        def layer_norm(z, bz, tb, lnbc, blnbc, dst, bdst, junk, bjunk):
            sc = sm2[:, tb, :]
            bs = bsm2[tb]
            fw.op(DVE, [bz], [bs], lambda: nc.vector.reduce_sum(sc[:, 0:1], z, axis=AX.X))
            fw.op(DVE, [bs], [bs], lambda: nc.vector.tensor_scalar(sc[:, 1:2], sc[:, 0:1], -1.0 / D, None, op0=ALU.mult))
            fw.op(ACT, [bz, bs], [bz], lambda: nc.scalar.activation(z, z, AF.Identity, bias=sc[:, 1:2]))
            fw.op(ACT, [bz], [bjunk], lambda: nc.scalar.activation(junk, z, AF.Square))
            fw.op(DVE, [bjunk], [bs], lambda: nc.vector.reduce_sum(sc[:, 2:3], junk, axis=AX.X))
            fw.op(ACT, [bs, bsmall], [bs], lambda: nc.scalar.activation(sc[:, 3:4], sc[:, 2:3], AF.Sqrt, bias=EPS5, scale=1.0 / D))
            fw.op(DVE, [bs], [bs], lambda: nc.vector.reciprocal(sc[:, 3:4], sc[:, 3:4]))
            fw.op(DVE, [bz, bs, blnbc], [bz], lambda: nc.vector.scalar_tensor_tensor(z, in0=z, scalar=sc[:, 3:4], in1=lnbc[:, 0, :], op0=ALU.mult, op1=ALU.mult))
            fw.op(POOL, [bz, blnbc], [bdst], lambda: nc.gpsimd.tensor_tensor(dst, z, lnbc[:, 1, :], op=ALU.add))

        with contextlib.ExitStack() as so:
            ln1bc = sb("ln1bc", [128, 2, D], F32, so); bln1 = Buf()
            fw.dma(SP, ln1bc[:], lnv_d[0:2, :].partition_broadcast(128), [], [bln1])
            wo = sb("wo", [128, 8, D], BF16, so); bwo = Buf()
            fw.dma(POOL, wo[:], w_o_d.rearrange("(hh p) o -> p hh o", p=128), [], [bwo])
            xqt = Rot([(sb(f"xqt{i}", [128, D], F32, so), Buf()) for i in range(2)])
            zt = Rot([(sb(f"zt{i}", [128, D], F32, so), Buf()) for i in range(2)])
            jk = Rot([(sb(f"jk{i}", [128, D], F32, so), Buf()) for i in range(2)])
            mixb = Rot([((psb[0], pb[0]), (psb[1], pb[1])), ((psb[2], pb[2]), (psb[3], pb[3]))])
            for tb in range(16):
                xt_, bxt_ = xqt.get()
                fw.dma(SP, xt_[:], xq_d[tb * 128:(tb + 1) * 128, :], [], [bxt_])
                banks = mixb.get()
                z, bz = zt.get()
                for half in range(2):
                    mps, bmps = banks[half]
                    for hh in range(8):
                        fw.op(PE, [botx[tb], bwo], [bmps],
                              lambda: nc.tensor.matmul(mps[:], otx[:, hh, tb * 128:(tb + 1) * 128], wo[:, hh, half * 512:(half + 1) * 512], start=(hh == 0), stop=(hh == 7)),
                              inc=(hh == 7))
                    fw.op(DVE, [bmps, bxt_], [bz],
                          lambda: nc.vector.scalar_tensor_tensor(z[:, half * 512:(half + 1) * 512], in0=xt_[:, half * 512:(half + 1) * 512], scalar=DN_ALPHA, in1=mps[:], op0=ALU.mult, op1=ALU.add))
                j_, bj_ = jk.get()
                layer_norm(z[:], bz, tb, ln1bc, bln1, ACC[:, tb, :], bACC[tb], j_[:], bj_)

        fw.fence()
        if stage == "ln1":
            for tb in range(16):
                fw.dma(SP, out_d[tb * 128:(tb + 1) * 128, :], ACC[:, tb, :], [bACC[tb]], [])
            fw.finish(SP)
            return nc

        G = sb("G", [128, 16, NE], F32); bG = [Buf() for _ in range(16)]
        MK = sb("MK", [128, 16, NE], F32); bMK = [Buf() for _ in range(16)]
        posm = sb("posm", [128, 16, NE], F32); bposm = [Buf() for _ in range(16)]
        posmT = sb("posmT", [NE, TQ], F32); bposmT = [Buf() for _ in range(4)]
        X1B = otx[:].rearrange("p a b -> p (a b)").rearrange("p (t d) -> p t d", t=16)
        bguT = sb("bguT", [128, NE * 16], F32); bbgu = Buf()
        fw.dma(SP, bguT[:], bgu_d[:, :], [], [bbgu])
        with contextlib.ExitStack() as sr:
            wr32 = sb("wr32", [128, 8, NE], F32, sr); bwr = Buf()
            fw.dma(SP, wr32[:], w_r_d.rearrange("(dc p) e -> p dc e", p=128), [], [bwr])
            brbc = sb("brbc", [128, NE], F32, sr); bbr = Buf()
            fw.dma(SP, brbc[:], b_r_d.partition_broadcast(128), [], [bbr])
            bd32 = sb("bd32", [NE, D], F32, sr); bbd = Buf()
            fw.dma(SP, bd32[:], bd_d[:, :], [], [bbd])
            GT = sb("GT", [NE, TQ], F32, sr); bGT = [Buf() for _ in range(16)]
            x1T32 = Rot([(sb(f"x1T32_{i}", [128, 8, 128], F32, sr), Buf()) for i in range(2)])
            rt = Rot([(sb(f"rt{i}", [128, 128], F32, sr), Buf()) for i in range(2)])
            tpb = Rot([((psb[0], pb[0]), (psb[1], pb[1])), ((psb[2], pb[2]), (psb[3], pb[3]))])
            tmp2 = Rot([(psb[i], pb[i]) for i in (4, 5, 6, 7)])
            for tb in range(16):
                fw.op(DVE, [bACC[tb]], [botx[tb]], lambda: nc.vector.tensor_copy(X1B[:, tb, :], ACC[:, tb, :]))
                banks = tpb.get()
                xT32, bxT32 = x1T32.get()
                for hb_ in range(2):
                    tp, btp = banks[hb_]
                    for q in range(4):
                        dc = hb_ * 4 + q
                        fw.op(PE, [bACC[tb], bcst], [btp], lambda: nc.tensor.transpose(tp[:, q * 128:(q + 1) * 128], ACC[:, tb, dc * 128:(dc + 1) * 128], ident), inc=(q == 3))
                    fw.op(ACT, [btp], [bxT32], lambda: nc.scalar.copy(xT32[:, hb_ * 4:(hb_ + 1) * 4, :], tp[:].rearrange("p (a b) -> p a b", a=4)))
                lgp, blgp = tmp2.get()
                for dc in range(8):
                    fw.op(PE, [bxT32, bwr], [blgp], lambda: nc.tensor.matmul(lgp[:, 0:NE], xT32[:, dc, :], wr32[:, dc, :], start=(dc == 0), stop=(dc == 7)), inc=(dc == 7))
                r_, br_ = rt.get()
                lg = r_[:, 0:32]; m8 = r_[:, 32:40]; ex = r_[:, 40:72]; mk = r_[:, 72:104]; misc = r_[:, 104:112]
                fw.op(DVE, [blgp, bbr], [br_], lambda: nc.vector.tensor_tensor(lg, lgp[:, 0:NE], brbc[:], op=ALU.add))
                fw.op(DVE, [br_], [br_], lambda: nc.vector.max(out=m8, in_=lg))
                fw.op(DVE, [br_], [br_], lambda: nc.vector.tensor_scalar(misc[:, 0:1], m8[:, 0:1], -1.0, None, op0=ALU.mult))
                fw.op(ACT, [br_], [br_], lambda: nc.scalar.activation(ex, lg, AF.Exp, bias=misc[:, 0:1]))
                fw.op(DVE, [br_], [bMK[tb]], lambda: nc.vector.tensor_scalar(MK[:, tb, :], lg, m8[:, 3:4], None, op0=ALU.is_ge))
                fw.op(DVE, [br_, bMK[tb]], [br_], lambda: nc.vector.tensor_tensor(ex, ex, MK[:, tb, :], op=ALU.mult))
                fw.op(DVE, [br_], [br_], lambda: nc.vector.reduce_sum(misc[:, 1:2], ex, axis=AX.X))
                fw.op(DVE, [br_], [br_], lambda: nc.vector.reciprocal(misc[:, 2:3], misc[:, 1:2]))
                fw.op(DVE, [br_], [bG[tb]], lambda: nc.vector.tensor_scalar(G[:, tb, :], ex, misc[:, 2:3], None, op0=ALU.mult))
                gtp, bgtp = tmp2.get()
                fw.op(PE, [bG[tb], bcst], [bgtp], lambda: nc.tensor.transpose(gtp[0:NE, 0:128], G[:, tb, :], ident))
                fw.op(ACT, [bgtp], [bGT[tb]], lambda: nc.scalar.copy(GT[:, tb * 128:(tb + 1) * 128], gtp[0:NE, 0:128]))
                for half in range(2):
                    bdp, bbdp = tmp2.get()
                    fw.op(PE, [bGT[tb], bbd], [bbdp], lambda: nc.tensor.matmul(bdp[:], GT[:, tb * 128:(tb + 1) * 128], bd32[:, half * 512:(half + 1) * 512], start=True, stop=True))
                    fw.op(DVE, [bbdp, bACC[tb]], [bACC[tb]],
                          lambda: nc.vector.scalar_tensor_tensor(ACC[:, tb, half * 512:(half + 1) * 512], in0=ACC[:, tb, half * 512:(half + 1) * 512], scalar=DN_ALPHA, in1=bdp[:], op0=ALU.mult, op1=ALU.add))

            triu = cst[:, 1024:1152]
            for tb in range(16):
                pp, bpp = tmp2.get()
                fw.op(PE, [bMK[tb], bcst], [bpp], lambda: nc.tensor.matmul(pp[:, 0:NE], triu, MK[:, tb, :], start=True, stop=(tb == 0)), inc=(tb == 0))
                for t2_ in range(tb):
                    fw.op(PE, [bMK[t2_], bones], [bpp], lambda: nc.tensor.matmul(pp[:, 0:NE], ones32[:], MK[:, t2_, :], start=False, stop=(t2_ == tb - 1)), inc=(t2_ == tb - 1))
                fw.op(DVE, [bpp, bMK[tb]], [bposm[tb]], lambda: nc.vector.scalar_tensor_tensor(posm[:, tb, :], in0=pp[:, 0:NE], scalar=1.0, in1=MK[:, tb, :], op0=ALU.add, op1=ALU.mult))
                fw.op(DVE, [bposm[tb]], [bposm[tb]], lambda: nc.vector.tensor_scalar(posm[:, tb, :], posm[:, tb, :], -1.0, None, op0=ALU.add))
                ptp, bptp = tmp2.get()
                fw.op(PE, [bposm[tb], bcst], [bptp], lambda: nc.tensor.transpose(ptp[0:NE, 0:128], posm[:, tb, :], ident))
                fw.op(ACT, [bptp], [bposmT[tb // 4]], lambda: nc.scalar.copy(posmT[:, tb * 128:(tb + 1) * 128], ptp[0:NE, 0:128]))
        fw.fence()
        with contextlib.ExitStack() as se:
            NSB = CAP // 128
            wgr = Rot([(sb(f"wg{i}", [128, 8, 2, 128], BF16, se), Buf()) for i in range(7)])
            Wd = sb("Wd", [128, 8, D], BF16, se); bWd = [Buf() for _ in range(4)]
            xgT = sb("xgT", [128, 8, CAP], BF16, se); bxg = [Buf() for _ in range(8)]
            ACTT = sb("ACTT", [128, 8, CAP], BF16, se); bACTT = [Buf() for _ in range(8)]
            Sel = sb("Sel", [128, 16, CAP], BF16, se); bSel = [Buf() for _ in range(16)]
            SelT = Rot([(sb(f"SelT{i}", [128, NSB, 512], BF16, se), Buf()) for i in range(2)])
            yb = sb("yb", [128, NSB, D], BF16, se); byb = [Buf() for _ in range(NSB)]
            gc_ = sb("gc", [128, CAP], F32, se); bgc_ = Buf()
            sg_ = sb("sg", [128, CAP], F32, se); bsg_ = Buf()
            uc_ = sb("uc", [128, CAP], F32, se); buc_ = Buf()
            le = sb("le", [NE, 128], F32, se); ble = Buf()
            busT = sb("busT", [128, NE * 8], F32, se); bbus = Buf()
            fw.op(DVE, [bbgu], [bbus], lambda: nc.vector.tensor_scalar(busT[:].rearrange("p (e c) -> p e c", c=8), bguT[:].rearrange("p (e c) -> p e c", c=16)[:, :, 8:16], 1.0 / 1.702, None, op0=ALU.mult))
            gab = Rot([(psb[i], pb[i]) for i in (0, 1)])
            gub = Rot([((psb[2], pb[2]), (psb[3], pb[3])), ((psb[4], pb[4]), (psb[5], pb[5]))])
            yb_b = Rot([(psb[i], pb[i]) for i in (6, 7)])
            wd_v = wd_d.rearrange("e (fc p) o -> e p fc o", p=128)
            iotaC = cst[:, 640:640 + CAP]
            kiota = cst[0:NE, 1152:1280]

            def load_wg(e, fc):
                wg_, bwg_ = wgr.get()
                fw.dma(POOL, wg_[:].rearrange("p a b c -> p (a b c)"), wgu_d[e, fc], [], [bwg_])
                return wg_, bwg_

            def load_wd(e, pc):
                fw.dma(POOL, Wd[:, 2 * pc:2 * pc + 2, :], wd_v[e, :, 2 * pc:2 * pc + 2, :], [], [bWd[pc]])

            def build_sel(e):
                for tb in range(16):
                    fw.op(DVE, [bposm[tb], bcst], [bSel[tb]], lambda: nc.vector.tensor_scalar(Sel[:, tb, :], iotaC, posm[:, tb, e:e + 1], None, op0=ALU.is_equal))

            def gather(e):
                for dc in range(8):
                    gp, bgp = gab.get()
                    for tb in range(16):
                        fw.op(PE, [botx[tb], bSel[tb]], [bgp], lambda: nc.tensor.matmul(gp[:, 0:CAP], X1B[:, tb, dc * 128:(dc + 1) * 128], Sel[:, tb, :], start=(tb == 0), stop=(tb == 15)), inc=(tb == 15))
                    fw.op(ACT, [bgp], [bxg[dc]], lambda: nc.scalar.copy(xgT[:, dc, :], gp[:, 0:CAP]))

            slices = [(e, fc) for e in range(NE) for fc in range(8)]
            PRE = 6
            WD_SCHED = {0: 0, 1: 1, 2: 2, 3: 3}
            loaded = {}
            for k in range(min(PRE, len(slices))):
                loaded[k] = load_wg(*slices[k])
            for k, (e, fc) in enumerate(slices):
                if k == 0:
                    build_sel(0)
                    gather(0)
                    build_sel(1)
                if k + PRE < len(slices):
                    loaded[k + PRE] = load_wg(*slices[k + PRE])
                wg_, bwg_ = loaded.pop(k)
                if fc in WD_SCHED:
                    load_wd(e, WD_SCHED[fc])
                (gps, bgps), (ups, bups) = gub.get()
                rd = [bwg_] + bxg
                for dc in range(8):
                    fw.op(PE, rd, [bgps], lambda: nc.tensor.matmul(gps[:, 0:CAP], wg_[:, dc, 0, :], xgT[:, dc, :], start=(dc == 0), stop=(dc == 7)), inc=(dc == 7))
                for dc in range(8):
                    fw.op(PE, rd, [bups], lambda: nc.tensor.matmul(ups[:, 0:CAP], wg_[:, dc, 1, :], xgT[:, dc, :], start=(dc == 0), stop=(dc == 7)), inc=(dc == 7))
                bg_col = bguT[:, e * 16 + fc:e * 16 + fc + 1]
                bu_col = bguT[:, e * 16 + 8 + fc:e * 16 + 8 + fc + 1]
                bus_col = busT[:, e * 8 + fc:e * 8 + fc + 1]
                fw.op(DVE, [bgps, bbgu], [bgc_], lambda: nc.vector.tensor_scalar(gc_[:], gps[:, 0:CAP], bg_col, 7.0, op0=ALU.add, op1=ALU.min))
                fw.op(ACT, [bups, bbus], [buc_], lambda: nc.scalar.activation(uc_[:], ups[:, 0:CAP], AF.Identity, bias=bus_col, scale=1.0 / 1.702))
                fw.op(ACT, [bgc_], [bsg_], lambda: nc.scalar.activation(sg_[:], gc_[:], AF.Silu, scale=1.702))
                fw.op(DVE, [buc_], [buc_], lambda: nc.vector.tensor_scalar(uc_[:], uc_[:], 7.0 / 1.702, -7.0 / 1.702, op0=ALU.min, op1=ALU.max))
                fw.op(DVE, [bsg_, buc_], [bACTT[fc]],
                      lambda: nc.vector.scalar_tensor_tensor(ACTT[:, fc, :], in0=uc_[:], scalar=1.0 / 1.702, in1=sg_[:], op0=ALU.add, op1=ALU.mult))
                if fc == 7:
                    if e + 1 < NE:
                        gather(e + 1)
                        if e + 2 < NE:
                            build_sel(e + 2)
                    for sbk in range(NSB):
                        for half in range(2):
                            yp, byp = yb_b.get()
                            rd2 = bACTT + bWd
                            for f in range(8):
                                fw.op(PE, rd2, [byp], lambda: nc.tensor.matmul(yp[:], ACTT[:, f, sbk * 128:(sbk + 1) * 128], Wd[:, f, half * 512:(half + 1) * 512], start=(f == 0), stop=(f == 7)), inc=(f == 7))
                            fw.op(ACT, [byp], [byb[sbk]], lambda: nc.scalar.copy(yb[:, sbk, half * 512:(half + 1) * 512], yp[:]))
                    fw.op(DVE, [bcst], [ble], lambda: nc.vector.tensor_scalar(le[:], kiota, float(e), None, op0=ALU.is_equal))
                    bcs = {}

                    def emit_bc(ch):
                        bc, bbc = gab.get()
                        fw.op(PE, [ble, bposmT[ch]], [bbc], lambda: nc.tensor.matmul(bc[:], le[:], posmT[:, ch * 512:(ch + 1) * 512], start=True, stop=True))
                        bcs[ch] = (bc, bbc)

                    emit_bc(0)
                    for ch in range(4):
                        bc, bbc = bcs.pop(ch)
                        if ch + 1 < 4:
                            emit_bc(ch + 1)
                        st_, bst2 = SelT.get()
                        for sbk in range(NSB):
                            fw.op(DVE, [bbc, bcst], [bst2], lambda: nc.vector.tensor_scalar(st_[:, sbk, :], bc[:], cst[:, 516 + sbk:517 + sbk], None, op0=ALU.is_equal))
                        for tb4 in range(4):
                            tb = ch * 4 + tb4
                            for half in range(2):
                                yp, byp = yb_b.get()
                                for sbk in range(NSB):
                                    fw.op(PE, [bst2] + byb, [byp], lambda: nc.tensor.matmul(yp[:], st_[:, sbk, tb4 * 128:(tb4 + 1) * 128], yb[:, sbk, half * 512:(half + 1) * 512], start=(sbk == 0), stop=(sbk == NSB - 1)), inc=(sbk == NSB - 1))
                                fw.op(DVE, [byp, bG[tb], bACC[tb]], [bACC[tb]],
                                      lambda: nc.vector.scalar_tensor_tensor(ACC[:, tb, half * 512:(half + 1) * 512], in0=yp[:], scalar=G[:, tb, e:e + 1], in1=ACC[:, tb, half * 512:(half + 1) * 512], op0=ALU.mult, op1=ALU.add))

        fw.fence()
        with contextlib.ExitStack() as sf:
            ln2bc = sb("ln2bc", [128, 2, D], F32, sf); bln2 = Buf()
            fw.dma(SP, ln2bc[:], lnv_d[2:4, :].partition_broadcast(128), [], [bln2])
            ot = Rot([(sb(f"ot{i}", [128, D], F32, sf), Buf()) for i in range(2)])
            jk2 = Rot([(sb(f"jk2{i}", [128, D], F32, sf), Buf()) for i in range(2)])
            for tb in range(16):
                o_, bo_ = ot.get()
                j_, bj_ = jk2.get()
                layer_norm(ACC[:, tb, :], bACC[tb], tb, ln2bc, bln2, o_[:], bo_, j_[:], bj_)
                fw.dma(SP, out_d[tb * 128:(tb + 1) * 128, :], o_[:], [bo_], [])
        fw.finish(SP)
    return nc


_PROG = {}


def _consts(r):
    c = np.zeros((128, 1280), np.float32)
    c[:, 0:128] = np.eye(128, dtype=np.float32)
    m = np.arange(128)
    partner = np.where(m % 64 < 32, m + 32, m - 32)
    c[partner, 128 + m] = 1.0
    k = np.arange(128)[:, None]
    q = np.arange(128)[None, :]
    c[:, 256:384] = (k <= q).astype(np.float32)
    c[:, 384:512] = 1.0 if r == 1 else 0.0
    invf = 1.0 / (10000.0 ** ((np.arange(128) % 32) * 2.0 / 64.0))
    first = (np.arange(128) % 64) < 32
    c[:, 512] = invf
    sc = TWO_PI * (1.0 - 1e-6)
    c[:, 513] = np.where(first, -sc, sc)
    c[:, 514] = np.where(first, PI_S, -PI_S)
    c[:, 515] = -PI_S
    p = np.arange(128, dtype=np.float32)
    c[:, 516] = p
    c[:, 517] = p + 128.0
    c[:, 518] = p + 256.0
    c[:, 640:1024] = np.arange(CAP, dtype=np.float32)[None, :]
    c[:, 1024:1152] = (p[:, None] < p[None, :]).astype(np.float32)
    c[:, 1152:1280] = p[:, None]
    return c


def _prep_inputs(inp):
    f32 = lambda a: np.ascontiguousarray(np.asarray(a), dtype=np.float32)
    x = f32(inp["x"])
    positions = np.ascontiguousarray(np.asarray(inp["positions"]), dtype=np.int32)
    wgu = f32(inp["w_gate_up"])[0]
    wgu_t = np.ascontiguousarray(
        wgu.reshape(NE, 8, 128, 2, 8, 128).transpose(0, 4, 2, 1, 3, 5)).reshape(NE, 8, 128, 2048)
    bgu = f32(inp["b_gate_up"])[0]
    bguT = np.ascontiguousarray(bgu.reshape(NE, 16, 128).transpose(2, 0, 1)).reshape(128, NE * 16)
    shared = {
        "w_in": f32(inp["w_in"])[0],
        "lamv": np.ascontiguousarray(np.stack([f32(inp["lambda_q1"])[0], f32(inp["lambda_k1"])[0],
                                               f32(inp["lambda_q2"])[0], f32(inp["lambda_k2"])[0]], 0)),
        "subln_g": f32(inp["subln_g"])[0].reshape(128, 1),
        "gq": np.ascontiguousarray(f32(inp["mla_q_norm_g"])[0].reshape(2, 128).T),
        "gkv": f32(inp["mla_kv_norm_g"])[0].reshape(128, 1),
        "w_uq": f32(inp["w_uq"])[0],
        "w_ukv": f32(inp["w_ukv"])[0],
        "w_o": f32(inp["w_o"])[0],
        "lnv": np.ascontiguousarray(np.stack([f32(inp["ln1_g"])[0], f32(inp["ln1_b"])[0],
                                              f32(inp["ln2_g"])[0], f32(inp["ln2_b"])[0]], 0)),
        "w_router": f32(inp["w_router"])[0],
        "b_router": f32(inp["b_router"])[0].reshape(1, NE),
        "wgu_t": wgu_t,
        "bguT": bguT,
        "w_down": f32(inp["w_down"])[0],
        "b_down": f32(inp["b_down"])[0],
    }
    in_maps = []
    toks = []
    for c in range(NCORES):
        b, r = c // 2, c % 2
        own = [2 * j + r for j in range(16)]
        oth = [2 * j + (1 - r) for j in range(16)]
        tok = np.concatenate([np.arange(g * 128, (g + 1) * 128) for g in own + oth])
        toks.append((b, tok[:TQ]))
        xb = x[b][tok]
        m = dict(shared)
        m["xT"] = np.ascontiguousarray(xb.T)
        m["xq"] = np.ascontiguousarray(xb[:TQ])
        m["pos"] = np.ascontiguousarray(positions[b][tok].reshape(1, S))
        m["cst"] = _consts(r)
        in_maps.append(m)
    return in_maps, toks


def kernel(**inputs):
    stage = inputs.pop("_stage", "full")
    if stage not in _PROG:
        _PROG[stage] = build_program(stage)
    nc = _PROG[stage]
    in_maps, toks = _prep_inputs(inputs)
    if stage != "full":
        moe = ("w_router", "b_router", "wgu_t", "bguT", "w_down", "b_down")
        in_maps = [{k: v for k, v in m.items() if k not in moe} for m in in_maps]
    res = run_bass_kernel_spmd(nc, in_maps, core_ids=list(range(NCORES)))
    out = np.zeros((4, S, D), np.float32)
    for c in range(NCORES):
        b, tok = toks[c]
        out[b, tok] = res.results[c]["out"]
    return out
```
